# Optimizing a Trainium2 kernel written in Bass

```python
import jax
import jax.numpy as jnp
from jax import lax
import numpy as np

D_MODEL = 1024
BATCH = 2
SEQ = 8192
DEPTH = 1

MOBA_HEADS = 8
MOBA_HEAD_DIM = 128
MOBA_WIDTH = MOBA_HEADS * MOBA_HEAD_DIM
MOBA_BLOCK = 256
MOBA_TOPK = 3
MOBA_QCHUNK = 32
ROPE_DIM = MOBA_HEAD_DIM // 4
ROPE_THETA = 500000.0

GLA_HEADS = 4
GLA_KEY_DIM = D_MODEL // 2
GLA_VAL_DIM = D_MODEL
GLA_DK = GLA_KEY_DIM // GLA_HEADS
GLA_DV = GLA_VAL_DIM // GLA_HEADS
GLA_GATE_RANK = 16
GLA_GATE_NORMALIZER = 16.0
GLA_CHUNK = 64

N_EXPERTS = 32
TOP_K = 4
D_EXPERT = D_MODEL
SWIGLU_ALPHA = 1.702
SWIGLU_LIMIT = 7.0
EXPERT_ROW_BLOCK = 128

RMS_EPS = 1e-5
N_BRANCHES = 2
IN_SPLITS = (MOBA_WIDTH, MOBA_WIDTH, MOBA_WIDTH, GLA_KEY_DIM, GLA_KEY_DIM, GLA_VAL_DIM, GLA_VAL_DIM, GLA_GATE_RANK, D_MODEL, D_MODEL)
N_IN = 3 * MOBA_WIDTH + 2 * GLA_KEY_DIM + 2 * GLA_VAL_DIM + GLA_GATE_RANK + N_BRANCHES * D_MODEL

kernel_name = "hybrid_moba_gla_moe_adaln_block"


def _rmsnorm(x, w):
    xf = x.astype(jnp.float32)
    y = xf * lax.rsqrt(jnp.mean(xf * xf, axis=-1, keepdims=True) + RMS_EPS)
    return (y * w.astype(jnp.float32)).astype(x.dtype)


def _partial_rope(x):
    S = x.shape[1]
    half = ROPE_DIM // 2
    inv_freq = jnp.float32(ROPE_THETA) ** (-jnp.arange(half, dtype=jnp.float32) * 2.0 / ROPE_DIM)
    ang = jnp.arange(S, dtype=jnp.float32)[:, None] * inv_freq[None, :]
    cos = jnp.cos(ang)[None, :, None, :]
    sin = jnp.sin(ang)[None, :, None, :]
    xr = x[..., :ROPE_DIM].astype(jnp.float32)
    x1, x2 = xr[..., :half], xr[..., half:]
    rot = jnp.concatenate([x1 * cos - x2 * sin, x2 * cos + x1 * sin], axis=-1).astype(x.dtype)
    return jnp.concatenate([rot, x[..., ROPE_DIM:]], axis=-1)


def _moba_attention(q, k, v):
    B, S, H, Dh = q.shape
    n_blocks = -(-S // MOBA_BLOCK)
    s_pad = n_blocks * MOBA_BLOCK
    topk = min(MOBA_TOPK, n_blocks)
    scale = Dh ** -0.5

    def prep(t):
        t = jnp.pad(t, ((0, 0), (0, s_pad - S), (0, 0), (0, 0)))
        return t.transpose(0, 2, 1, 3)

    qh, kh, vh = prep(q), prep(k), prep(v)
    kb = kh.reshape(B, H, n_blocks, MOBA_BLOCK, Dh)
    vb = vh.reshape(B, H, n_blocks, MOBA_BLOCK, Dh)
    k_mean = jnp.mean(kb.astype(jnp.float32), axis=3)
    b_idx = jnp.arange(B)[:, None, None, None]
    h_idx = jnp.arange(H)[None, :, None, None]
    block_ids = jnp.arange(n_blocks)

    def query_chunk(ci):
        start = ci * MOBA_QCHUNK
        blk = start // MOBA_BLOCK
        qc = lax.dynamic_slice_in_dim(qh, start, MOBA_QCHUNK, axis=2)
        gate = jnp.einsum('bhqd,bhnd->bhqn', qc.astype(jnp.float32), k_mean)
        gate = jnp.where(block_ids < blk, gate, -jnp.inf)
        _, sel = lax.top_k(gate, topk)
        valid = sel < blk
        k_sel = kb[b_idx, h_idx, sel]
        v_sel = vb[b_idx, h_idx, sel]
        s_sel = jnp.einsum('bhqd,bhqnkd->bhqnk', qc, k_sel).astype(jnp.float32) * scale
        s_sel = jnp.where(valid[..., None], s_sel, -jnp.inf).reshape(B, H, MOBA_QCHUNK, topk * MOBA_BLOCK)
        k_own = lax.dynamic_index_in_dim(kb, blk, axis=2, keepdims=False)
        v_own = lax.dynamic_index_in_dim(vb, blk, axis=2, keepdims=False)
        s_own = jnp.einsum('bhqd,bhkd->bhqk', qc, k_own).astype(jnp.float32) * scale
        q_pos = start + jnp.arange(MOBA_QCHUNK)
        k_pos = blk * MOBA_BLOCK + jnp.arange(MOBA_BLOCK)
        s_own = jnp.where(k_pos[None, :] <= q_pos[:, None], s_own, -jnp.inf)
        p = jax.nn.softmax(jnp.concatenate([s_sel, s_own], axis=-1), axis=-1).astype(vh.dtype)
        p_sel = p[..., :topk * MOBA_BLOCK].reshape(B, H, MOBA_QCHUNK, topk, MOBA_BLOCK)
        p_own = p[..., topk * MOBA_BLOCK:]
        return (jnp.einsum('bhqnk,bhqnkd->bhqd', p_sel, v_sel)
                + jnp.einsum('bhqk,bhkd->bhqd', p_own, v_own))

    out = lax.map(query_chunk, jnp.arange(s_pad // MOBA_QCHUNK))
    out = out.transpose(1, 0, 3, 2, 4).reshape(B, s_pad, H, Dh)
    return out[:, :S]


def _gla_chunked(q, k, v, log_g):
    B, S, H, dk = q.shape
    dv = v.shape[-1]
    n_chunks = S // GLA_CHUNK

    def to_chunks(t):
        t = t.astype(jnp.float32).reshape(B, n_chunks, GLA_CHUNK, H, t.shape[-1])
        return t.transpose(1, 0, 3, 2, 4)

    qc = to_chunks(q) * (dk ** -0.5)
    kc, vc, gc = to_chunks(k), to_chunks(v), to_chunks(log_g)
    causal = jnp.tril(jnp.ones((GLA_CHUNK, GLA_CHUNK), dtype=bool))[:, :, None]

    def step(state, inp):
        qi, ki, vi, gi = inp
        b = jnp.cumsum(gi, axis=2)
        b_last = b[:, :, -1:, :]
        o_inter = jnp.einsum('bhcd,bhdv->bhcv', qi * jnp.exp(b), state)
        diff = b[:, :, :, None, :] - b[:, :, None, :, :]
        decay = jnp.exp(jnp.where(causal, diff, -jnp.inf))
        scores = jnp.einsum('bhid,bhjd,bhijd->bhij', qi, ki, decay)
        o = o_inter + jnp.einsum('bhij,bhjv->bhiv', scores, vi)
        state = (state * jnp.exp(b_last[:, :, 0, :, None])
                 + jnp.einsum('bhcd,bhcv->bhdv', ki * jnp.exp(b_last - b), vi))
        return state, o

    state0 = jnp.zeros((B, H, dk, dv), jnp.float32)
    _, o = lax.scan(step, state0, (qc, kc, vc, gc))
    return o.transpose(1, 0, 3, 2, 4).reshape(B, S, H, dv)


def _mixer(h, w_in, gla_gate_w, gla_gate_b, gla_norm_w, w_o_moba, w_o_gla, b_merge, w_out):
    B, S, _ = h.shape
    proj = h @ w_in
    idx = []
    acc = 0
    for size in IN_SPLITS[:-1]:
        acc += size
        idx.append(acc)
    mq, mk, mv, gq, gk, gv, gr, g_low, gate_a, gate_b = jnp.split(proj, idx, axis=-1)

    def heads(t, n):
        return t.reshape(B, S, n, -1)

    mq = _partial_rope(heads(mq, MOBA_HEADS))
    mk = _partial_rope(heads(mk, MOBA_HEADS))
    o_a = _moba_attention(mq, mk, heads(mv, MOBA_HEADS)).reshape(B, S, MOBA_WIDTH)
    y_a = o_a @ w_o_moba

    log_g = jax.nn.log_sigmoid((g_low @ gla_gate_w + gla_gate_b).astype(jnp.float32)) / GLA_GATE_NORMALIZER
    o_b = _gla_chunked(heads(gq, GLA_HEADS), heads(gk, GLA_HEADS), heads(gv, GLA_HEADS), heads(log_g, GLA_HEADS))
    o_b = _rmsnorm(o_b, gla_norm_w) * jax.nn.silu(heads(gr, GLA_HEADS).astype(jnp.float32))
    y_b = o_b.reshape(B, S, GLA_VAL_DIM).astype(h.dtype) @ w_o_gla

    g_a = jax.nn.sigmoid(gate_a + b_merge[:D_MODEL])
    g_b = jax.nn.sigmoid(gate_b + b_merge[D_MODEL:])
    return (g_a * y_a + g_b * y_b) @ w_out


def _clamped_swiglu(h):
    x_glu = jnp.minimum(h[..., ::2], SWIGLU_LIMIT)
    x_lin = jnp.clip(h[..., 1::2], -SWIGLU_LIMIT, SWIGLU_LIMIT)
    return x_glu * jax.nn.sigmoid(SWIGLU_ALPHA * x_glu) * (x_lin + 1.0)


def _moe(h, w_router, b_router, w_exp_in, b_exp_in, w_exp_out, b_exp_out):
    B, S, D = h.shape
    T = B * S
    R = EXPERT_ROW_BLOCK
    tokens = h.reshape(T, D)
    logits = (tokens @ w_router + b_router).astype(jnp.float32)
    top_logits, top_idx = lax.top_k(logits, TOP_K)
    top_w = jax.nn.softmax(top_logits, axis=-1)
    flat_expert = top_idx.reshape(-1)
    flat_token = jnp.arange(T * TOP_K, dtype=jnp.int32) // TOP_K
    flat_w = top_w.reshape(-1)
    order = jnp.argsort(flat_expert)
    sorted_expert = flat_expert[order]
    counts = jax.ops.segment_sum(jnp.ones_like(flat_expert), flat_expert, num_segments=N_EXPERTS)
    group_start = jnp.cumsum(counts) - counts
    padded_counts = (counts + R - 1) // R * R
    padded_end = jnp.cumsum(padded_counts)
    padded_start = padded_end - padded_counts
    rank = jnp.arange(T * TOP_K, dtype=jnp.int32) - group_start[sorted_expert]
    dest = padded_start[sorted_expert] + rank
    n_row_blocks = -(-(T * TOP_K + N_EXPERTS * (R - 1)) // R)
    P = n_row_blocks * R
    row_token = jnp.zeros((P,), jnp.int32).at[dest].set(flat_token[order])
    row_weight = jnp.zeros((P,), jnp.float32).at[dest].set(flat_w[order])
    block_expert = jnp.minimum(
        jnp.searchsorted(padded_end, jnp.arange(n_row_blocks) * R, side='right'), N_EXPERTS - 1)

    def expert_block(j):
        rows = lax.dynamic_slice_in_dim(row_token, j * R, R)
        wts = lax.dynamic_slice_in_dim(row_weight, j * R, R)
        e = block_expert[j]
        hid = tokens[rows] @ w_exp_in[e] + b_exp_in[e]
        out = _clamped_swiglu(hid) @ w_exp_out[e] + b_exp_out[e]
        return out * wts[:, None].astype(out.dtype)

    out = lax.map(expert_block, jnp.arange(n_row_blocks)).reshape(P, D)
    y = jnp.zeros((T, D), out.dtype).at[row_token].add(out)
    return y.reshape(B, S, D)


def setup_inputs(seed: int = 0) -> dict:
    key = jax.random.key(seed)
    ks = jax.random.split(key, 21)
    L = DEPTH

    def nrm(k, shape, scale):
        return jax.random.normal(k, shape, jnp.float32) * scale

    return {
        'x': nrm(ks[0], (BATCH, SEQ, D_MODEL), 1.0),
        'c': nrm(ks[1], (BATCH, D_MODEL), 1.0),
        'w_ada': nrm(ks[2], (L, D_MODEL, 6 * D_MODEL), 0.5 * D_MODEL ** -0.5),
        'b_ada': nrm(ks[3], (L, 6 * D_MODEL), 0.01),
        'norm1_w': 1.0 + nrm(ks[4], (L, D_MODEL), 0.01),
        'w_in': nrm(ks[5], (L, D_MODEL, N_IN), D_MODEL ** -0.5),
        'gla_gate_w': nrm(ks[6], (L, GLA_GATE_RANK, GLA_KEY_DIM), GLA_GATE_RANK ** -0.5),
        'gla_gate_b': nrm(ks[7], (L, GLA_KEY_DIM), 0.01),
        'gla_norm_w': 1.0 + nrm(ks[8], (L, GLA_DV), 0.01),
        'w_o_moba': nrm(ks[9], (L, MOBA_WIDTH, D_MODEL), MOBA_WIDTH ** -0.5),
        'w_o_gla': nrm(ks[10], (L, GLA_VAL_DIM, D_MODEL), GLA_VAL_DIM ** -0.5),
        'b_merge': nrm(ks[11], (L, N_BRANCHES * D_MODEL), 0.01),
        'w_out': nrm(ks[12], (L, D_MODEL, D_MODEL), D_MODEL ** -0.5),
        'norm2_w': 1.0 + nrm(ks[13], (L, D_MODEL), 0.01),
        'w_router': nrm(ks[14], (L, D_MODEL, N_EXPERTS), D_MODEL ** -0.5),
        'b_router': nrm(ks[15], (L, N_EXPERTS), 0.01),
        'w_exp_in': nrm(ks[16], (L, N_EXPERTS, D_MODEL, 2 * D_EXPERT), D_MODEL ** -0.5),
        'b_exp_in': nrm(ks[17], (L, N_EXPERTS, 2 * D_EXPERT), 0.01),
        'w_exp_out': nrm(ks[18], (L, N_EXPERTS, D_EXPERT, D_MODEL), D_EXPERT ** -0.5),
        'b_exp_out': nrm(ks[19], (L, N_EXPERTS, D_MODEL), 0.01),
        'final_norm_w': 1.0 + nrm(ks[20], (D_MODEL,), 0.01),
    }


def reference(x, c, w_ada, b_ada, norm1_w, w_in, gla_gate_w, gla_gate_b, gla_norm_w, w_o_moba, w_o_gla, b_merge, w_out, norm2_w, w_router, b_router, w_exp_in, b_exp_in, w_exp_out, b_exp_out, final_norm_w):
    c_act = jax.nn.silu(c)
    for l in range(DEPTH):
        mod = (c_act @ w_ada[l] + b_ada[l])[:, None, :]
        shift1, scale1, gate1, shift2, scale2, gate2 = jnp.split(mod, 6, axis=-1)
        h = _rmsnorm(x, norm1_w[l]) * (1.0 + scale1) + shift1
        x = x + gate1 * _mixer(h, w_in[l], gla_gate_w[l], gla_gate_b[l], gla_norm_w[l],
                               w_o_moba[l], w_o_gla[l], b_merge[l], w_out[l])
        h = _rmsnorm(x, norm2_w[l]) * (1.0 + scale2) + shift2
        x = x + gate2 * _moe(h, w_router[l], b_router[l], w_exp_in[l], b_exp_in[l],
                             w_exp_out[l], b_exp_out[l])
    return _rmsnorm(x, final_norm_w)
```

```python
import numpy as np
from contextlib import ExitStack
import concourse.bass as bass
import concourse.mybir as mybir
from concourse.bass_utils import run_bass_kernel_spmd

F32 = mybir.dt.float32
BF16 = mybir.dt.bfloat16
I32 = mybir.dt.int32
AF = mybir.ActivationFunctionType
ALU = mybir.AluOpType
AX = mybir.AxisListType

D = 1024
SEQ = 8192
NCORE = 8
OWN = 2048
GS = 256
TPG = 2
NG = 32
OWNG0 = 24
NVB = 32
NE = 32
CAP = 1024
BIG = 30000.0
NEGINF = -1.0e30
EPS = 1e-5
STAGE = 99
DEBUG = False


class Prog:
    def __init__(self, nc, same_engine_sync=True):
        self.nc = nc
        self.engs = {"pe": nc.tensor, "act": nc.scalar, "dve": nc.vector,
                     "pool": nc.gpsimd, "sp": nc.sync}
        self.sem = {k: nc.alloc_semaphore("prog_" + k) for k in self.engs}
        self.cnt = {k: 0 for k in self.engs}
        self.seen = {k: {} for k in self.engs}
        self.bufs = {}
        self.pending = {k: ([], []) for k in self.engs}
        self.dsem = {}
        self.dcnt = {}
        self.same = same_engine_sync
        self.nwait = 0
        self.nins = 0
        self.free_sems = {"sw": [], "hw": []}
        self.dkind = {}
        self.nalloc = 0

    def _deps(self, reads, writes):
        deps = []
        for k in reads:
            b = self.bufs.get(k)
            if b is not None and b[0] is not None:
                deps.append(b[0])
        for k in writes:
            b = self.bufs.get(k)
            if b is not None:
                if b[0] is not None:
                    deps.append(b[0])
                deps.extend(b[1])
        return deps

    def _wait(self, eng, deps, own_ok=True):
        need = {}
        for (s, v) in deps:
            if own_ok and s is self.sem[eng]:
                if eng == "pe" or not self.same:
                    continue
            key = id(s)
            if self.seen[eng].get(key, 0) >= v:
                continue
            if key not in need or need[key][1] < v:
                need[key] = (s, v)
        for key, (s, v) in need.items():
            self.engs[eng].wait_ge(s, v)
            self.seen[eng][key] = v
            self.nwait += 1

    def _record(self, ev, reads, writes):
        for k in reads:
            b = self.bufs.get(k)
            if b is None:
                self.bufs[k] = [None, [ev]]
            else:
                b[1].append(ev)
                if len(b[1]) > 24:
                    b[1] = b[1][-24:] if False else b[1]
        for k in writes:
            self.bufs[k] = [ev, []]

    def op(self, eng, emit, reads=(), writes=(), inc=True):
        reads = list(reads)
        writes = list(writes)
        self._wait(eng, self._deps(reads, writes))
        ins = emit(self.engs[eng])
        self.nins += 1
        if not inc:
            self.pending[eng][0].extend(reads)
            self.pending[eng][1].extend(writes)
            return None
        self.cnt[eng] += 1
        ev = (self.sem[eng], self.cnt[eng])
        ins.then_inc(self.sem[eng], 1)
        pr, pw = self.pending[eng]
        self._record(ev, pr + reads, pw + writes)
        self.pending[eng] = ([], [])
        return ev

    def dma(self, queue, out, in_, reads=(), writes=(), sem=None, emit=None, **kw):
        reads = list(reads)
        writes = list(writes)
        if sem is None:
            sem = ("auto",) + tuple(writes) + tuple(reads)
        kind = "sw" if queue == "pool" else "hw"
        if sem in self.dsem:
            assert self.dkind[sem] == kind, (sem, kind)
        if sem not in self.dsem:
            self.dkind[sem] = kind
            if self.free_sems[kind]:
                self.dsem[sem], self.dcnt[sem] = self.free_sems[kind].pop()
            else:
                self.dsem[sem] = self.nc.alloc_semaphore("dma_%d" % self.nalloc)
                self.nalloc += 1
                self.dcnt[sem] = 0
        self._wait(queue, self._deps(reads, writes))
        if emit is not None:
            ins = emit(self.engs[queue])
        else:
            ins = self.engs[queue].dma_start(out=out, in_=in_, **kw)
        self.nins += 1
        self.dcnt[sem] += 16
        ins.then_inc(self.dsem[sem], 16)
        ev = (self.dsem[sem], self.dcnt[sem])
        self._record(ev, reads, writes)
        return ev

    def wait_all(self, eng, keys=None):
        deps = []
        for k, b in self.bufs.items():
            if keys is not None and k not in keys:
                continue
            if b[0] is not None:
                deps.append(b[0])
            deps.extend(b[1])
        self._wait(eng, deps, own_ok=False)

    def barrier(self):
        assert all(len(p[0]) == 0 and len(p[1]) == 0 for p in self.pending.values())
        for eng in self.engs:
            deps = [(self.sem[o], self.cnt[o]) for o in self.engs if o != eng and self.cnt[o] > 0]
            deps += [(self.dsem[k], self.dcnt[k]) for k in self.dsem if self.dcnt[k] > 0]
            self._wait(eng, deps, own_ok=False)
        self.bufs = {}
        for k in list(self.dsem):
            self.free_sems[self.dkind.pop(k)].append((self.dsem.pop(k), self.dcnt.pop(k)))


def build_nc():
    nc = bass.Bass("TRN2", target_bir_lowering=False)
    P = Prog(nc)
    dt_in = lambda n, s, d=F32: nc.dram_tensor(n, list(s), d, kind="ExternalInput").ap()
    dt_sc = lambda n, s, d: nc.dram_tensor(n, list(s), d).ap()

    xTv = dt_in("xTv", [D, SEQ])
    xtok = dt_in("xtok", [OWN, D])
    cT = dt_in("cT", [128, 8])
    w_ada = dt_in("w_ada", [D, 6 * D])
    b_ada_bc = dt_in("b_ada_bc", [128, 6 * D])
    nw = dt_in("nw", [128, 16])
    fnw_bc = dt_in("fnw_bc", [128, D])
    wA = dt_in("wA", [D, 3600])
    wO = dt_in("wO", [D, 2560])
    wG = dt_in("wG", [D, 2048])
    w_oa = dt_in("w_oa", [D, D])
    w_ob = dt_in("w_ob", [D, D])
    w_out = dt_in("w_out", [D, D])
    nbm = dt_in("nbm", [128, 16])
    ropec = dt_in("ropec", [32, SEQ])
    ropes = dt_in("ropes", [32, SEQ])
    vflag = dt_in("vflag", [128, 64])
    gmask = dt_in("gmask", [128, 8 * 32])
    gvalid = dt_in("gvalid", [128, 8 * 32])
    gw_aug = dt_in("gw_aug", [32, 512])
    gnw = dt_in("gnw", [128, 2])
    w_r = dt_in("w_r", [D, NE])
    b_r_bc = dt_in("b_r_bc", [128, NE])
    w1d = dt_in("w1d", [NE, D, 2048]) if STAGE >= 4 else None
    w2 = dt_in("w2", [NE, D, D]) if STAGE >= 4 else None
    b1T = dt_in("b1T", [128, NE * 16])
    b2 = dt_in("b2", [NE, D])
    consts = dt_in("consts", [128, 10, 128])
    esel_in = dt_in("esel_in", [32, 32 * 128])
    ecap = dt_in("ecap", [128, NE])
    out = nc.dram_tensor("out", [OWN, D], F32, kind="ExternalOutput").ap()

    KT_d = dt_sc("KT_d", [8, 128, SEQ], BF16)
    V_d = dt_sc("V_d", [SEQ, D], BF16)
    QT_d = dt_sc("QT_d", [8, 128, OWN], BF16)
    OB_d = dt_sc("OB_d", [D, OWN], BF16)
    HT_d = dt_sc("HT_d", [D, OWN], BF16)
    X1_d = dt_sc("X1_d", [OWN, D], F32)
    XG_d = dt_sc("XG_d", [NE * CAP, D], BF16)
    OUT_d = dt_sc("OUT_d", [NE * CAP, D], F32)

    dbg = {}
    if DEBUG:
        dbg["mod"] = nc.dram_tensor("dbg_mod", [128, 48], F32, kind="ExternalOutput").ap()
        dbg["kT"] = nc.dram_tensor("dbg_kT", [8, 128, SEQ], BF16, kind="ExternalOutput").ap()
        dbg["qT"] = nc.dram_tensor("dbg_qT", [8, 128, OWN], BF16, kind="ExternalOutput").ap()
        dbg["ob"] = nc.dram_tensor("dbg_ob", [D, OWN], BF16, kind="ExternalOutput").ap()
        dbg["oa"] = nc.dram_tensor("dbg_oa", [128, 8, OWN], BF16, kind="ExternalOutput").ap()
        dbg["x1"] = nc.dram_tensor("dbg_x1", [OWN, D], F32, kind="ExternalOutput").ap()
        dbg["lg"] = nc.dram_tensor("dbg_lg", [128, 16, 32], F32, kind="ExternalOutput").ap()

    es_all = ExitStack()
    with es_all:
        sb = lambda es, n, s, d: es.enter_context(nc.sbuf_tensor(n, list(s), d))
        ps = lambda es, n, s, d: es.enter_context(nc.psum_tensor(n, list(s), d))
        cst = sb(es_all, "cst", [128, 10, 128], F32)
        ident_f = cst[:, 0, :]
        ident_b = sb(es_all, "ident_b", [128, 128], BF16)
        ones_rms = sb(es_all, "ones_rms", [128, 128], BF16)
        ones_256 = sb(es_all, "ones_256", [128, 128], BF16)
        ones_1 = sb(es_all, "ones_1", [128, 128], BF16)
        onesf = sb(es_all, "onesf", [128, 128], F32)
        modT = sb(es_all, "modT", [128, 48], F32)
        a1 = sb(es_all, "a1", [128, 8], F32)
        a2 = sb(es_all, "a2", [128, 8], F32)
        nw_sb = sb(es_all, "nw_sb", [128, 16], F32)
        gate1_bc = sb(es_all, "gate1_bc", [128, D], F32)
        gate2_bc = sb(es_all, "gate2_bc", [128, D], F32)
        kmb = sb(es_all, "kmb", [128, 8, 32], BF16)
        bc_reg = nc.gpsimd.to_reg(NE * CAP - 1)
        dest_i = sb(es_all, "dest_i", [128, 64], I32)
        w4 = sb(es_all, "w4", [128, 16, 4], F32)
        RWT = sb(es_all, "RWT", [32, 16, 128], F32)
        s1 = modT[:, 0:8]
        g1 = modT[:, 16:24]
        s2 = modT[:, 24:32]

        P.dma("sp", cst[:], consts, writes=["cst"])
        P.dma("sp", nw_sb[:], nw, writes=["nw"])
        P.dma("pool", ident_b[:], consts[:, 0, :], writes=["ident_b"])
        P.op("dve", lambda e: e.memset(ones_rms[:], 1.0 / 1024.0), writes=["ones_rms"])
        P.op("dve", lambda e: e.memset(ones_256[:], 1.0 / 256.0), writes=["ones_256"])
        P.op("dve", lambda e: e.memset(ones_1[:], 1.0), writes=["ones_1"])
        P.op("dve", lambda e: e.memset(onesf[:], 1.0), writes=["onesf"])

        with ExitStack() as es:
            c_sb = sb(es, "c_sb", [128, 8], F32)
            c_e = sb(es, "c_e", [128, 8], F32)
            cact_b = sb(es, "cact_b", [128, 8, 128], F32)
            wst = sb(es, "wst", [128, 2, 8, 512], F32)
            modb = sb(es, "modb", [128, 6 * D], F32)
            bab = sb(es, "bab", [128, 6 * D], F32)
            p_mod = ps(es, "p_mod", [128, 2, 512], F32)
            p_col = ps(es, "p_col", [128, 48], F32)
            P.dma("sp", c_sb[:], cT, writes=["c_sb"])
            P.dma("sp", bab[:], b_ada_bc, writes=["bab"])
            P.op("act", lambda e: e.activation(c_e[:], c_sb[:], AF.Exp, scale=-1.0), reads=["c_sb"], writes=["c_e"])
            P.op("dve", lambda e: e.tensor_scalar(c_e[:], c_e[:], 1.0, None, ALU.add), reads=["c_e"], writes=["c_e"])
            P.op("dve", lambda e: e.reciprocal(c_e[:], c_e[:]), reads=["c_e"], writes=["c_e"])
            P.op("dve", lambda e: e.tensor_tensor(c_e[:], c_e[:], c_sb[:], ALU.mult), reads=["c_e", "c_sb"], writes=["c_e"])
            for k in range(8):
                P.op("dve", lambda e: e.tensor_scalar(cact_b[:, k, :], onesf[:], c_e[:, k:k + 1], None, ALU.mult),
                     reads=["c_e", "onesf"], writes=[("cactb", k)])
            for j in range(12):
                s = j % 2
                P.dma("sp", wst[:, s], w_ada[:, j * 512:(j + 1) * 512].rearrange("(k p) n -> p k n", p=128),
                      writes=[("wst", s)], sem=("wst", s))
                for k in range(8):
                    P.op("pe", lambda e: e.matmul(p_mod[:, s, :], lhsT=cact_b[:, k, :], rhs=wst[:, s, k, :],
                                                  start=(k == 0), stop=(k == 7)),
                         reads=[("wst", s), ("cactb", k)], writes=[("p_mod", s)], inc=(k == 7))
                P.op("dve", lambda e: e.tensor_tensor(modb[:, j * 512:(j + 1) * 512], p_mod[:, s, :],
                                                      bab[:, j * 512:(j + 1) * 512], ALU.add),
                     reads=[("p_mod", s), "bab"], writes=[("modb", j)])
            for j in range(48):
                P.op("pe", lambda e: e.matmul(p_col[:, j:j + 1], lhsT=modb[0:1, j * 128:(j + 1) * 128],
                                              rhs=onesf[0:1, 0:1], start=True, stop=True),
                     reads=[("modb", j // 4), "onesf"], writes=["p_col"], inc=(j == 47))
            P.op("dve", lambda e: e.tensor_copy(modT[:], p_col[:]), reads=["p_col"], writes=["modT"])
            P.op("dve", lambda e: e.scalar_tensor_tensor(a1[:], modT[:, 8:16], 1.0, nw_sb[:, 0:8], ALU.add, ALU.mult),
                 reads=["modT", "nw"], writes=["a1"])
            P.op("dve", lambda e: e.scalar_tensor_tensor(a2[:], modT[:, 32:40], 1.0, nw_sb[:, 8:16], ALU.add, ALU.mult),
                 reads=["modT", "nw"], writes=["a2"])
            P.op("act", lambda e: e.copy(gate1_bc[:], modb[:, 2048:3072]), reads=[("modb", 4), ("modb", 5)], writes=["gate1_bc"])
            P.op("act", lambda e: e.copy(gate2_bc[:], modb[:, 5120:6144]), reads=[("modb", 10), ("modb", 11)], writes=["gate2_bc"])
            if DEBUG:
                P.dma("sp", dbg["mod"], modT[:], reads=["modT"])
            P.barrier()

        G_ = dict(locals())
        if STAGE >= 1:
            phase_a1(nc, P, G_)
        with ExitStack() as es_ab:
            oaT = sb(es_ab, "oaT", [128, 8, OWN], BF16)
            G_["oaT"] = oaT
            if STAGE >= 2:
                phase_a2(nc, P, G_)
            if STAGE >= 3:
                phase_b(nc, P, G_)
        if STAGE >= 4:
            phase_moe(nc, P, G_)
        P.wait_all("sp")
    return nc, P


def rmsnorm_fm(P, nc, xs_ap, sq_ap, p_ss, rstd_ap, ones_rms, keyx, tag, pkey):
    P.op("act", lambda e: e.activation(sq_ap, xs_ap, AF.Square), reads=list(keyx), writes=[tag + "sq"])
    for k in range(8):
        P.op("pe", lambda e: e.matmul(p_ss, lhsT=ones_rms[:], rhs=sq_ap[:, k, :], start=(k == 0), stop=(k == 7)),
             reads=[tag + "sq", "ones_rms"], writes=[pkey], inc=(k == 7))
    rsqrt_act(P, rstd_ap, p_ss, [pkey], tag + "rstd", 1.0)


def rsqrt_act(P, out_ap, in_ap, rkeys, wkey, mul):
    P.op("act", lambda e: e.activation(out_ap, in_ap, AF.Ln, bias=EPS, scale=mul), reads=list(rkeys), writes=[wkey])
    P.op("act", lambda e: e.activation(out_ap, out_ap, AF.Exp, scale=-0.5), reads=[wkey], writes=[wkey])


def phase_a1(nc, P, L):
    g = L
    sb, ps = g["sb"], g["ps"]
    xTv, wA, wO, ropec, ropes = g["xTv"], g["wA"], g["wO"], g["ropec"], g["ropes"]
    cst, ident_b = g["cst"], g["ident_b"]
    ones_rms, ones_256 = g["ones_rms"], g["ones_256"]
    a1, s1, kmb = g["a1"], g["s1"], g["kmb"]
    KT_d, V_d, QT_d, OB_d, HT_d = g["KT_d"], g["V_d"], g["QT_d"], g["OB_d"], g["HT_d"]
    dbg = g["dbg"]
    with ExitStack() as es:
        wA_b = sb(es, "wA_b", [128, 8, 3600], BF16)
        wO_b = sb(es, "wO_b", [128, 8, 2560], BF16)
        xs = sb(es, "xs", [128, 2, 8, GS], F32)
        sq = sb(es, "sq", [128, 8, GS], BF16)
        rstd = sb(es, "rstd", [128, GS], F32)
        hT = sb(es, "hT", [128, 8, GS], BF16)
        rc = sb(es, "rc", [32, 2, GS], F32)
        rs = sb(es, "rs", [32, 2, GS], F32)
        kf = sb(es, "kf", [128, 2, GS], F32)
        rt = sb(es, "rt", [32, 2, GS], F32)
        kb = sb(es, "kb", [128, 2, GS], BF16)
        kmean = sb(es, "kmean", [128, 8, 32], F32)
        gkf = sb(es, "gkf", [128, 4, GS], F32)
        gqf = sb(es, "gqf", [128, 4, GS], F32)
        sgr = sb(es, "sgr", [128, 8, GS], F32)
        sgt = sb(es, "sgt", [128, GS], F32)
        glow = sb(es, "glow", [32, GS], F32)
        gw_sb = sb(es, "gw_sb", [32, 512], F32)
        gnw_sb = sb(es, "gnw_sb", [128, 2], F32)
        vfl = sb(es, "vfl", [128, 64], F32)
        Ltok = sb(es, "Ltok", [128, TPG, 512], F32)
        lex = sb(es, "lex", [128, 512], F32)
        vst = sb(es, "vst", [128, TPG, 1024], BF16)
        gvst = sb(es, "gvst", [128, TPG, 1024], BF16)
        EbT = sb(es, "EbT", [128, 2, 128], F32)
        EnbT = sb(es, "EnbT", [128, 2, 128], F32)
        qtl = sb(es, "qtl", [128, 2, 128], BF16)
        ktl = sb(es, "ktl", [128, 2, 128], BF16)
        ktok = sb(es, "ktok", [128, 2, 128], BF16)
        atm = sb(es, "atm", [128, 2, 128], BF16)
        Sst = sb(es, "Sst", [128, 4, 256], F32)
        Ssc = sb(es, "Ssc", [128, 2, 256], F32)
        Sbf = sb(es, "Sbf", [128, 4, 256], BF16)
        osq = sb(es, "osq", [128, 2, 128], BF16)
        orst = sb(es, "orst", [128, 128], F32)
        otmp = sb(es, "otmp", [128, 128], F32)
        obst = sb(es, "obst", [128, 8, GS], BF16)
        p_pj = ps(es, "p_pj", [128, 2, 512], F32)
        p_sx = ps(es, "p_sx", [128, 512], F32)
        p_ss = p_sx[:, 0:GS]
        p_xs = p_sx[0:32, 256:256 + GS]
        p_g = ps(es, "p_g", [128, 2, 4, 128], F32)
        p_km = ps(es, "p_km", [128, 512], F32)
        p_tr = ps(es, "p_tr", [128, 2, 128], BF16)
        permf = cst[:, 1, 0:32]
        triN = cst[:, 2, :]
        utm = cst[:, 3, :]

        for k in range(8):
            P.dma("pool", wA_b[:, k, :], wA[k * 128:(k + 1) * 128, :], writes=[("wA", k)], sem=("wA", k))
        for k in range(8):
            P.dma("pool", wO_b[:, k, :], wO[k * 128:(k + 1) * 128, :], writes=[("wO", k)], sem=("wO", k))
        P.dma("sp", gw_sb[:], g["gw_aug"], writes=["gw_sb"])
        P.dma("sp", gnw_sb[:], g["gnw"], writes=["gnw_sb"])
        P.dma("sp", vfl[:], g["vflag"], writes=["vfl"])
        P.op("pool", lambda e: e.memset(glow[:], 1.0), writes=["glow"])
        P.op("pool", lambda e: e.memset(Sst[:], 0.0), writes=[("S", h) for h in range(4)])
        P.op("pool", lambda e: e.memset(Sbf[:], 0.0), writes=[("Sbf", h) for h in range(4)])
        wAk = [("wA", k) for k in range(8)]
        wOk = [("wO", k) for k in range(8)]
        pj_n = [0]
        kf_n = [0]
        uniq = [0]

        def ukey(n):
            uniq[0] += 1
            return (n, uniq[0])

        def proj_fm(wt, col0, ncol, wkeys, evac):
            s_ = pj_n[0] % 2
            pj_n[0] += 1
            for k in range(8):
                P.op("pe", lambda e: e.matmul(p_pj[0:ncol, s_, 0:GS], lhsT=wt[:, k, col0:col0 + ncol], rhs=hT[:, k, :],
                                              start=(k == 0), stop=(k == 7)),
                     reads=wkeys + ["hT"], writes=[("p_pj", s_)], inc=(k == 7))
            evac(p_pj[0:ncol, s_, 0:GS], ("p_pj", s_))

        for gi in range(NG):
            own = gi >= OWNG0
            s = gi % 2
            t0 = gi * GS
            o0 = (gi - OWNG0) * GS
            xk = [("xs", s, k) for k in range(8)]
            P.dma("sp", xs[:, s], xTv[:, t0:t0 + GS].rearrange("(k p) n -> p k n", p=128), writes=xk, sem=("xs", s))
            P.dma("sp", rc[:, s, :], ropec[:, t0:t0 + GS], writes=[("rc", s)], sem=("rc", s))
            P.dma("sp", rs[:, s, :], ropes[:, t0:t0 + GS], writes=[("rs", s)], sem=("rs", s))
            rmsnorm_fm(P, nc, xs[:, s], sq[:], p_ss, rstd[:], ones_rms, xk, "a1", "p_sx")
            for k in range(8):
                P.op("dve", lambda e: e.scalar_tensor_tensor(xs[:, s, k, :], xs[:, s, k, :], a1[:, k:k + 1], rstd[:],
                                                             ALU.mult, ALU.mult),
                     reads=[("xs", s, k), "a1rstd", "a1"], writes=[("xs", s, k)])
                P.op("act", lambda e: e.activation(hT[:, k, :], xs[:, s, k, :], AF.Identity, bias=s1[:, k:k + 1], scale=1.0),
                     reads=[("xs", s, k), "modT"], writes=["hT"])
            if own:
                P.dma("sp", HT_d[:, o0:o0 + GS].rearrange("(k p) n -> p k n", p=128), hT[:], reads=["hT"], writes=[ukey("HT_d")],
                      sem="HT_o")

            def qk_evac(h, is_k):
                def ev(pt, pkey):
                    u = kf_n[0] % 2
                    kf_n[0] += 1
                    P.op("act", lambda e: e.copy(kf[:, u, :], pt), reads=[pkey], writes=[("kf", u)])
                    P.op("pe", lambda e: e.matmul(p_xs, lhsT=permf, rhs=kf[:, u, :], start=True, stop=True),
                         reads=[("kf", u), "cst"], writes=["p_sx"])
                    P.op("dve", lambda e: e.tensor_tensor(rt[:, 0, :], kf[0:32, u, :], rc[:, s, :], ALU.mult),
                         reads=[("kf", u), ("rc", s)], writes=["rt0"])
                    P.op("dve", lambda e: e.tensor_tensor(rt[:, 1, :], p_xs, rs[:, s, :], ALU.mult),
                         reads=["p_sx", ("rs", s)], writes=["rt1"])
                    P.op("dve", lambda e: e.tensor_tensor(kf[0:32, u, :], rt[:, 0, :], rt[:, 1, :], ALU.add),
                         reads=["rt0", "rt1", ("kf", u)], writes=[("kf", u)])
                    if is_k:
                        P.op("dve", lambda e: e.tensor_reduce(kmean[:, h, gi:gi + 1], kf[:, u, :], AX.X, ALU.add),
                             reads=[("kf", u)], writes=[("kmean", h)])
                    P.op("pool", lambda e: e.tensor_copy(kb[:, u, :], kf[:, u, :]), reads=[("kf", u)], writes=[("kb", u)])
                    if is_k:
                        P.dma("sp", KT_d[h, :, t0:t0 + GS], kb[:, u, :], reads=[("kb", u)], writes=[ukey("KT_d")], sem=("kbo", u))
                    else:
                        P.dma("sp", QT_d[h, :, o0:o0 + GS], kb[:, u, :], reads=[("kb", u)], writes=[ukey("QT_d")], sem=("kbo", u))
                return ev

            for h in range(8):
                proj_fm(wA_b, h * 128, 128, wAk, qk_evac(h, True))
            if own:
                for h in range(8):
                    proj_fm(wO_b, h * 128, 128, wOk, qk_evac(h, False))

            for t in range(TPG):
                for half in range(2):
                    sl = pj_n[0] % 2
                    pj_n[0] += 1
                    for k in range(8):
                        P.op("pe", lambda e: e.matmul(p_pj[:, sl, :], lhsT=hT[:, k, t * 128:(t + 1) * 128],
                                                      rhs=wA_b[:, k, 1024 + half * 512:1024 + (half + 1) * 512],
                                                      start=(k == 0), stop=(k == 7)),
                             reads=wAk + ["hT"], writes=[("p_pj", sl)], inc=(k == 7))
                    P.op("act", lambda e: e.copy(vst[:, t, half * 512:(half + 1) * 512], p_pj[:, sl, :]),
                         reads=[("p_pj", sl)], writes=["vst"])
                for half in range(2):
                    sl = pj_n[0] % 2
                    pj_n[0] += 1
                    for k in range(8):
                        P.op("pe", lambda e: e.matmul(p_pj[:, sl, :], lhsT=hT[:, k, t * 128:(t + 1) * 128],
                                                      rhs=wA_b[:, k, 2560 + half * 512:2560 + (half + 1) * 512],
                                                      start=(k == 0), stop=(k == 7)),
                             reads=wAk + ["hT"], writes=[("p_pj", sl)], inc=(k == 7))
                    P.op("dve", lambda e: e.tensor_scalar(gvst[:, t, half * 512:(half + 1) * 512], p_pj[:, sl, :],
                                                          vfl[:, gi * TPG + t:gi * TPG + t + 1], None, ALU.mult),
                         reads=[("p_pj", sl), "vfl"], writes=[("gvst", t)])
            P.dma("sp", V_d[t0:t0 + GS, :].rearrange("(t p) n -> p t n", p=128), vst[:], reads=["vst"], writes=[ukey("V_d")],
                  sem="V_o")

            for h in range(4):
                proj_fm(wA_b, 2048 + h * 128, 128, wAk,
                        lambda pt, pkey, h=h: P.op("act", lambda e: e.copy(gkf[:, h, :], pt), reads=[pkey], writes=[("gkf", h)]))
            proj_fm(wA_b, 3584, 16, wAk,
                    lambda pt, pkey: P.op("act", lambda e: e.copy(glow[0:16, :], pt), reads=[pkey], writes=["glow"]))
            if own:
                for h in range(4):
                    proj_fm(wO_b, 1024 + h * 128, 128, wOk,
                            lambda pt, pkey, h=h: P.op("act", lambda e: e.copy(gqf[:, h, :], pt), reads=[pkey], writes=[("gqf", h)]))
                for c in range(8):
                    def gr_ev(pt, pkey, c=c):
                        P.op("act", lambda e: e.activation(sgt[:], pt, AF.Exp, scale=-1.0), reads=[pkey], writes=["sgt"])
                        P.op("dve", lambda e: e.tensor_scalar(sgt[:], sgt[:], 1.0, None, ALU.add), reads=["sgt"], writes=["sgt"])
                        P.op("dve", lambda e: e.reciprocal(sgt[:], sgt[:]), reads=["sgt"], writes=["sgt"])
                        P.op("dve", lambda e: e.tensor_tensor(sgr[:, c, :], sgt[:], pt, ALU.mult), reads=["sgt", pkey], writes=[("sgr", c)])
                    proj_fm(wO_b, 1536 + c * 128, 128, wOk, gr_ev)
            for t in range(TPG):
                sl = pj_n[0] % 2
                pj_n[0] += 1
                P.op("pe", lambda e: e.matmul(p_pj[:, sl, :], lhsT=glow[:, t * 128:(t + 1) * 128], rhs=gw_sb[:],
                                              start=True, stop=True),
                     reads=["glow", "gw_sb"], writes=[("p_pj", sl)])
                P.op("act", lambda e: e.activation(lex[:], p_pj[:, sl, :], AF.Exp, scale=-1.0), reads=[("p_pj", sl)], writes=["lex"])
                P.op("act", lambda e: e.activation(Ltok[:, t, :], lex[:], AF.Ln, bias=1.0, scale=1.0), reads=["lex"], writes=[("Ltok", t)])

            for t in range(TPG):
                for h in range(4):
                    u = (t * 4 + h) % 2
                    c0 = t * 128
                    P.op("pe", lambda e: e.matmul(p_g[:, u, 0, :], lhsT=Ltok[:, t, h * 128:(h + 1) * 128], rhs=triN,
                                                  start=True, stop=True),
                         reads=[("Ltok", t), "cst"], writes=[("p_g", u)])
                    P.op("act", lambda e: e.activation(EbT[:, u, :], p_g[:, u, 0, :], AF.Exp), reads=[("p_g", u)], writes=[("EbT", u)])
                    P.op("act", lambda e: e.activation(EnbT[:, u, :], p_g[:, u, 0, :], AF.Exp, scale=-1.0),
                         reads=[("p_g", u)], writes=[("EnbT", u)])
                    P.op("dve", lambda e: e.tensor_tensor(ktl[:, u, :], gkf[:, h, c0:c0 + 128], EnbT[:, u, :], ALU.mult),
                         reads=[("gkf", h), ("EnbT", u)], writes=[("ktl", u)])
                    P.op("pe", lambda e: e.transpose(p_tr[:, u, :], ktl[:, u, :], ident_b[:]),
                         reads=[("ktl", u), "ident_b"], writes=["p_tr"])
                    P.op("act", lambda e: e.copy(ktok[:, u, :], p_tr[:, u, :]), reads=["p_tr"], writes=[("ktok", u)])
                    P.op("pe", lambda e: e.matmul(p_km[:, 0:256], lhsT=ktok[:, u, :], rhs=gvst[:, t, h * 256:(h + 1) * 256],
                                                  start=True, stop=True),
                         reads=[("ktok", u), ("gvst", t)], writes=["p_km"])
                    if own:
                        P.op("dve", lambda e: e.scalar_tensor_tensor(qtl[:, u, :], gqf[:, h, c0:c0 + 128], 128.0 ** -0.5,
                                                                     EbT[:, u, :], ALU.mult, ALU.mult),
                             reads=[("gqf", h), ("EbT", u)], writes=[("qtl", u)])
                        P.op("pe", lambda e: e.matmul(p_g[:, u, 1, :], lhsT=ktl[:, u, :], rhs=qtl[:, u, :], start=True, stop=True),
                             reads=[("ktl", u), ("qtl", u)], writes=[("p_g", u)])
                        P.op("dve", lambda e: e.tensor_tensor(atm[:, u, :], p_g[:, u, 1, :], utm, ALU.mult),
                             reads=[("p_g", u), "cst"], writes=[("atm", u)])
                        for dv in range(2):
                            P.op("pe", lambda e: e.matmul(p_g[:, u, 2 + dv, :],
                                                          lhsT=gvst[:, t, h * 256 + dv * 128:h * 256 + (dv + 1) * 128],
                                                          rhs=atm[:, u, :], start=True, stop=False),
                                 reads=[("gvst", t), ("atm", u)], writes=[("p_g", u)], inc=False)
                            P.op("pe", lambda e: e.matmul(p_g[:, u, 2 + dv, :], lhsT=Sbf[:, h, dv * 128:(dv + 1) * 128],
                                                          rhs=qtl[:, u, :], start=False, stop=True),
                                 reads=[("Sbf", h), ("qtl", u)], writes=[("p_g", u)])
                            P.op("act", lambda e: e.activation(osq[:, dv, :], p_g[:, u, 2 + dv, :], AF.Square),
                                 reads=[("p_g", u)], writes=[("osq", dv)])
                        for dv in range(2):
                            P.op("pe", lambda e: e.matmul(p_km[:, 256 + u * 128:256 + (u + 1) * 128], lhsT=ones_256[:], rhs=osq[:, dv, :],
                                                          start=(dv == 0), stop=(dv == 1)),
                                 reads=[("osq", dv), "ones_256"], writes=["p_km"], inc=(dv == 1))
                        rsqrt_act(P, orst[:], p_km[:, 256 + u * 128:256 + (u + 1) * 128], ["p_km"], "orst", 1.0)
                        for dv in range(2):
                            P.op("dve", lambda e: e.scalar_tensor_tensor(otmp[:], p_g[:, u, 2 + dv, :], gnw_sb[:, dv:dv + 1],
                                                                         orst[:], ALU.mult, ALU.mult),
                                 reads=[("p_g", u), "orst", "gnw_sb"], writes=["otmp"])
                            P.op("dve", lambda e: e.tensor_tensor(obst[:, h * 2 + dv, c0:c0 + 128], otmp[:],
                                                                  sgr[:, h * 2 + dv, c0:c0 + 128], ALU.mult),
                                 reads=["otmp", ("sgr", h * 2 + dv)], writes=["obst"])
                    P.op("pool", lambda e: e.tensor_scalar(Ssc[:, u, :], Sst[:, h, :], EbT[:, u, 127:128], None, ALU.mult),
                         reads=[("S", h), ("EbT", u)], writes=[("Ssc", u)])
                    P.op("dve", lambda e: e.scalar_tensor_tensor(Sst[:, h, :], p_km[:, 0:256], EbT[:, u, 127:128], Ssc[:, u, :],
                                                                 ALU.mult, ALU.add),
                         reads=["p_km", ("EbT", u), ("Ssc", u)], writes=[("S", h)])
                    P.op("act", lambda e: e.copy(Sbf[:, h, :], Sst[:, h, :]), reads=[("S", h)], writes=[("Sbf", h)])
            if own:
                P.dma("sp", OB_d[:, o0:o0 + GS].rearrange("(k p) n -> p k n", p=128), obst[:], reads=["obst"], writes=[ukey("OB_d")],
                      sem="OB_o")
        P.op("dve", lambda e: e.tensor_copy(kmb[:], kmean[:]), reads=[("kmean", h) for h in range(8)], writes=["kmb"])
        if DEBUG:
            P.wait_all("sp")
            P.dma("sp", dbg["kT"], KT_d, reads=[])
            P.dma("sp", dbg["qT"], QT_d, reads=[])
            P.dma("sp", dbg["ob"], OB_d, reads=[])
        P.barrier()


def phase_a2(nc, P, L):
    g = L
    sb, ps = g["sb"], g["ps"]
    KT_d, V_d, QT_d = g["KT_d"], g["V_d"], g["QT_d"]
    cst, ident_b, ident_f, ones_1, oaT, kmb = g["cst"], g["ident_b"], g["ident_f"], g["ones_1"], g["oaT"], g["kmb"]
    scale = 128.0 ** -0.5
    with ExitStack() as es:
        KT = sb(es, "KT", [128, 2, SEQ], BF16)
        Vh = sb(es, "Vh", [128, 2, 64, 128], BF16)
        QT = sb(es, "QT", [128, 2, OWN], BF16)
        esel = sb(es, "esel", [32, 32, 128], BF16)
        cmask = sb(es, "cmask", [128, 2, 256], BF16)
        gm_sb = sb(es, "gm_sb", [128, 8, 32], F32)
        gv_sb = sb(es, "gv_sb", [128, 8, 32], F32)
        gt = sb(es, "gt", [128, 2, 32], F32)
        top8 = sb(es, "top8", [128, 2, 8], F32)
        mbT = sb(es, "mbT", [32, 2, 256], BF16)
        PT = sb(es, "PT", [128, 4, 256], BF16)
        rden = sb(es, "rden", [128, 256], F32)
        p_st = ps(es, "p_st", [128, 4, 512], F32)
        p_od = ps(es, "p_od", [128, 2, 512], F32)
        p_gm = ps(es, "p_gm", [128, 512], F32)
        p_gt = p_gm[:, 0:64].rearrange("p (a b) -> p a b", a=2)
        p_mb = p_gm[0:32, 256:512]
        P.dma("pool", esel[:].rearrange("p a b -> p (a b)"), g["esel_in"], writes=["esel"])
        P.dma("pool", cmask[:], cst_dram_causal(g), writes=["cmask"])
        P.dma("sp", gm_sb[:].rearrange("p a b -> p (a b)"), g["gmask"], writes=["gm_sb"])
        P.dma("sp", gv_sb[:].rearrange("p a b -> p (a b)"), g["gvalid"], writes=["gv_sb"])
        n_st = [0]
        n_blk = [0]
        for h in range(8):
            hs = h % 2
            P.dma("sp", KT[:, hs, :], KT_d[h], writes=[("KT", hs)], sem=("KT", hs))
            P.dma("sp", Vh[:, hs], V_d[:, h * 128:(h + 1) * 128].rearrange("(t p) d -> p t d", p=128),
                  writes=[("Vh", hs)], sem=("Vh", hs))
            P.dma("sp", QT[:, hs, :], QT_d[h], writes=[("QT", hs)], sem=("QT", hs))
            for l in range(8):
                bs = n_blk[0] % 2
                n_blk[0] += 1
                q0 = l * 256
                for qt in range(2):
                    P.op("pe", lambda e: e.matmul(p_gt[:, qt, :], lhsT=QT[:, hs, q0 + qt * 128:q0 + (qt + 1) * 128],
                                                  rhs=kmb[:, h, :], start=True, stop=True),
                         reads=[("QT", hs), "kmb"], writes=["p_gm"])
                    P.op("dve", lambda e: e.tensor_tensor(gt[:, qt, :], p_gt[:, qt, :], gm_sb[:, l, :], ALU.add),
                         reads=["p_gm", "gm_sb"], writes=[("gt", qt)])
                    P.op("dve", lambda e: e.max(top8[:, qt, :], gt[:, qt, :]), reads=[("gt", qt)], writes=[("top8", qt)])
                    P.op("dve", lambda e: e.tensor_scalar(gt[:, qt, :], gt[:, qt, :], top8[:, qt, 2:3], None, ALU.is_ge),
                         reads=[("gt", qt), ("top8", qt)], writes=[("gt", qt)])
                    P.op("dve", lambda e: e.tensor_tensor(gt[:, qt, :], gt[:, qt, :], gv_sb[:, l, :], ALU.mult),
                         reads=[("gt", qt), "gv_sb"], writes=[("gt", qt)])
                    P.op("dve", lambda e: e.tensor_scalar(gt[:, qt, :], gt[:, qt, :], BIG, -BIG, ALU.mult, ALU.add),
                         reads=[("gt", qt)], writes=[("gt", qt)])
                    P.op("pe", lambda e: e.transpose(p_mb[:, qt * 128:(qt + 1) * 128], gt[:, qt, :], ident_f),
                         reads=[("gt", qt), "cst"], writes=["p_gm"])
                P.op("dve", lambda e: e.tensor_copy(mbT[:, bs, :], p_mb), reads=["p_gm"], writes=[("mbT", bs)])
                nkv = 24 + l
                pairs = [(v, half) for v in range(nkv + 1) for half in range(2)]
                for i, (v, half) in enumerate(pairs):
                    st = n_st[0] % 4
                    n_st[0] += 1
                    k0 = v * 256 + half * 128
                    P.op("pe", lambda e: e.matmul(p_st[:, st, 0:256], lhsT=KT[:, hs, k0:k0 + 128], rhs=QT[:, hs, q0:q0 + 256],
                                                  start=True, stop=False),
                         reads=[("KT", hs), ("QT", hs)], writes=[("p_st", st)], inc=False)
                    if v < nkv:
                        P.op("pe", lambda e: e.matmul(p_st[:, st, 0:256], lhsT=esel[:, v, :], rhs=mbT[:, bs, :], start=False, stop=True),
                             reads=["esel", ("mbT", bs)], writes=[("p_st", st)])
                    else:
                        P.op("pe", lambda e: e.matmul(p_st[:, st, 0:256], lhsT=ident_b[:], rhs=cmask[:, half, :], start=False, stop=True),
                             reads=["ident_b", "cmask"], writes=[("p_st", st)])
                    P.op("act", lambda e: e.activation(PT[:, st, :], p_st[:, st, 0:256], AF.Exp, scale=scale),
                         reads=[("p_st", st)], writes=[("PT", st)])
                    last = (i == len(pairs) - 1)
                    P.op("pe", lambda e: e.matmul(p_od[:, 0, 0:256], lhsT=Vh[:, hs, v * 2 + half, :], rhs=PT[:, st, :],
                                                  start=(i == 0), stop=last),
                         reads=[("Vh", hs), ("PT", st)], writes=["p_od"], inc=False)
                    P.op("pe", lambda e: e.matmul(p_od[:, 1, 0:256], lhsT=ones_1[:], rhs=PT[:, st, :],
                                                  start=(i == 0), stop=last),
                         reads=["ones_1", ("PT", st)], writes=["p_od"], inc=True)
                P.op("dve", lambda e: e.reciprocal(rden[:], p_od[:, 1, 0:256]), reads=["p_od"], writes=["rden"])
                P.op("dve", lambda e: e.tensor_tensor(oaT[:, h, q0:q0 + 256], p_od[:, 0, 0:256], rden[:], ALU.mult),
                     reads=["p_od", "rden"], writes=[("oaT", h)])
        if DEBUG:
            P.dma("sp", g["dbg"]["oa"], oaT[:], reads=[("oaT", h) for h in range(8)])
        P.barrier()


def cst_dram_causal(g):
    return g["consts"][:, 5:9, :].rearrange("p (a b) c -> p a (b c)", a=2)


def phase_b(nc, P, L):
    g = L
    sb, ps = g["sb"], g["ps"]
    oaT, ones_rms, ones_1, ident_b, ident_f, cst = g["oaT"], g["ones_rms"], g["ones_1"], g["ident_b"], g["ident_f"], g["cst"]
    a2, s2, g1, gate1_bc = g["a2"], g["s2"], g["g1"], g["gate1_bc"]
    HT_d, OB_d, X1_d, XG_d = g["HT_d"], g["OB_d"], g["X1_d"], g["XG_d"]
    xTv, xtok = g["xTv"], g["xtok"]
    dest_i, w4, RWT = g["dest_i"], g["w4"], g["RWT"]
    with ExitStack() as es:
        wG_b = sb(es, "wG_b", [128, 8, 2048], BF16)
        woa_b = sb(es, "woa_b", [128, 8, D], BF16)
        wob_b = sb(es, "wob_b", [128, 8, D], BF16)
        wout_b = sb(es, "wout_b", [128, 8, D], BF16)
        nbm_sb = sb(es, "nbm_sb", [128, 16], F32)
        wr_sb = sb(es, "wr_sb", [128, 8, NE], F32)
        br_sb = sb(es, "br_sb", [128, NE], F32)
        ecap_sb = sb(es, "ecap_sb", [128, NE], F32)
        hTg = sb(es, "hTg", [128, 8, GS], BF16)
        obg = sb(es, "obg", [128, 8, GS], BF16)
        xo = sb(es, "xo", [128, 8, GS], F32)
        ge = sb(es, "ge", [128, GS], F32)
        mf = sb(es, "mf", [128, GS], F32)
        mt2 = sb(es, "mt2", [128, GS], F32)
        mT = sb(es, "mT", [128, 8, GS], BF16)
        sq = sb(es, "sqb", [128, 8, GS], BF16)
        rstd = sb(es, "rstdb", [128, GS], F32)
        h2f = sb(es, "h2f", [128, 8, GS], F32)
        h2b = sb(es, "h2b", [128, 8, GS], BF16)
        xt = sb(es, "xt", [128, D], F32)
        x1t = sb(es, "x1t", [128, D], F32)
        h2tok = sb(es, "h2tok", [128, D], BF16)
        lg = sb(es, "lg", [128, NE], F32)
        t8 = sb(es, "t8", [128, 8], F32)
        nmax = sb(es, "nmax", [128, 1], F32)
        den = sb(es, "den", [128, 1], F32)
        sel = sb(es, "sel", [128, NE], F32)
        selb = sb(es, "selb", [128, NE], BF16)
        carry = sb(es, "carry", [128, NE], F32)
        slot = sb(es, "slot", [128, NE], F32)
        oh = sb(es, "oh", [128, NE], F32)
        destf = sb(es, "destf", [128, 4], F32)
        RW = sb(es, "RW", [128, NE], F32)
        stri = sb(es, "stri", [128, 128], BF16)
        p_a = ps(es, "pb_a", [128, 2, 512], F32)
        p_b = ps(es, "pb_b", [128, 2, 512], F32)
        p_ss = ps(es, "pb_ss", [128, GS], F32)
        p_lg = ps(es, "pb_lg", [128, 2, NE], F32)
        p_tr = ps(es, "pb_tr", [128, 2, 512], BF16)
        p_rw = ps(es, "pb_rw", [32, 128], F32)
        for k in range(8):
            P.dma("pool", wG_b[:, k, :], g["wG"][k * 128:(k + 1) * 128, :], writes=[("wG", k)], sem=("wG", k))
            P.dma("pool", woa_b[:, k, :], g["w_oa"][k * 128:(k + 1) * 128, :], writes=[("woa", k)], sem=("woa", k))
            P.dma("pool", wob_b[:, k, :], g["w_ob"][k * 128:(k + 1) * 128, :], writes=[("wob", k)], sem=("wob", k))
            P.dma("pool", wout_b[:, k, :], g["w_out"][k * 128:(k + 1) * 128, :], writes=[("wout", k)], sem=("wout", k))
        P.dma("pool", stri[:], g["consts"][:, 4, :], writes=["stri"])
        P.dma("sp", nbm_sb[:], g["nbm"], writes=["nbm"])
        P.dma("sp", wr_sb[:], g["w_r"].rearrange("(k p) n -> p k n", p=128), writes=["wr"])
        P.dma("sp", br_sb[:], g["b_r_bc"], writes=["br"])
        P.dma("sp", ecap_sb[:], g["ecap"], writes=["ecap"])
        P.op("pool", lambda e: e.memset(carry[:], 0.0), writes=["carry"])
        P.op("dve", lambda e: e.tensor_scalar(nbm_sb[:], nbm_sb[:], -1.0, None, ALU.mult), reads=["nbm"], writes=["nbm"])
        kk = lambda n: [(n, k) for k in range(8)]
        for G in range(OWN // GS):
            o0 = G * GS
            P.dma("sp", hTg[:], HT_d[:, o0:o0 + GS].rearrange("(k p) n -> p k n", p=128), writes=["hTg"])
            P.dma("sp", obg[:], OB_d[:, o0:o0 + GS].rearrange("(k p) n -> p k n", p=128), writes=["obg"])
            P.dma("sp", xo[:], xTv[:, 6144 + o0:6144 + o0 + GS].rearrange("(k p) n -> p k n", p=128), writes=["xo"])
            for c in range(8):
                for br in range(2):
                    wy = woa_b if br == 0 else wob_b
                    wyk = kk("woa") if br == 0 else kk("wob")
                    for k in range(8):
                        P.op("pe", lambda e: e.matmul(p_a[:, br, 0:GS], lhsT=wG_b[:, k, br * 1024 + c * 128:br * 1024 + (c + 1) * 128],
                                                      rhs=hTg[:, k, :], start=(k == 0), stop=(k == 7)),
                             reads=kk("wG") + ["hTg"], writes=[("p_a", br)], inc=(k == 7))
                    for k in range(8):
                        rhs = oaT[:, k, o0:o0 + GS] if br == 0 else obg[:, k, :]
                        P.op("pe", lambda e: e.matmul(p_b[:, br, 0:GS], lhsT=wy[:, k, c * 128:(c + 1) * 128], rhs=rhs,
                                                      start=(k == 0), stop=(k == 7)),
                             reads=wyk + (["obg"] if br else []), writes=[("p_b", br)], inc=(k == 7))
                    P.op("act", lambda e: e.activation(ge[:], p_a[:, br, 0:GS], AF.Exp, bias=nbm_sb[:, br * 8 + c:br * 8 + c + 1], scale=-1.0),
                         reads=[("p_a", br), "nbm"], writes=["ge"])
                    P.op("dve", lambda e: e.tensor_scalar(ge[:], ge[:], 1.0, None, ALU.add), reads=["ge"], writes=["ge"])
                    P.op("dve", lambda e: e.reciprocal(ge[:], ge[:]), reads=["ge"], writes=["ge"])
                    if br == 0:
                        P.op("dve", lambda e: e.tensor_tensor(mf[:], ge[:], p_b[:, br, 0:GS], ALU.mult), reads=["ge", ("p_b", br)], writes=["mf"])
                    else:
                        P.op("dve", lambda e: e.tensor_tensor(mt2[:], ge[:], p_b[:, br, 0:GS], ALU.mult), reads=["ge", ("p_b", br)], writes=["mt2"])
                        P.op("dve", lambda e: e.tensor_tensor(mT[:, c, :], mf[:], mt2[:], ALU.add), reads=["mf", "mt2"], writes=["mT"])
            for c in range(8):
                for k in range(8):
                    P.op("pe", lambda e: e.matmul(p_a[:, c % 2, 0:GS], lhsT=wout_b[:, k, c * 128:(c + 1) * 128], rhs=mT[:, k, :],
                                                  start=(k == 0), stop=(k == 7)),
                         reads=kk("wout") + ["mT"], writes=[("p_a", c % 2)], inc=(k == 7))
                P.op("dve", lambda e: e.scalar_tensor_tensor(xo[:, c, :], p_a[:, c % 2, 0:GS], g1[:, c:c + 1], xo[:, c, :], ALU.mult, ALU.add),
                     reads=[("p_a", c % 2), "xo"], writes=["xo"])
            for t in range(TPG):
                T = G * TPG + t
                P.dma("sp", xt[:], xtok[T * 128:(T + 1) * 128, :], writes=["xt"])
                for half in range(2):
                    for k in range(8):
                        P.op("pe", lambda e: e.matmul(p_b[:, half, :], lhsT=mT[:, k, t * 128:(t + 1) * 128],
                                                      rhs=wout_b[:, k, half * 512:(half + 1) * 512], start=(k == 0), stop=(k == 7)),
                             reads=kk("wout") + ["mT"], writes=[("p_b", half)], inc=(k == 7))
                    P.op("dve", lambda e: e.tensor_tensor(x1t[:, half * 512:(half + 1) * 512], p_b[:, half, :],
                                                          gate1_bc[:, half * 512:(half + 1) * 512], ALU.mult),
                         reads=[("p_b", half)], writes=["x1t"])
                P.op("pool", lambda e: e.tensor_tensor(x1t[:], x1t[:], xt[:], ALU.add), reads=["x1t", "xt"], writes=["x1t"])
                P.dma("sp", X1_d[T * 128:(T + 1) * 128, :], x1t[:], reads=["x1t"], writes=[("X1_d", T)], sem="X1_o")
            rmsnorm_fm(P, nc, xo[:], sq[:], p_ss[:], rstd[:], ones_rms, ["xo"], "b", "pb_ss")
            for k in range(8):
                P.op("dve", lambda e: e.scalar_tensor_tensor(xo[:, k, :], xo[:, k, :], a2[:, k:k + 1], rstd[:], ALU.mult, ALU.mult),
                     reads=["xo", "brstd"], writes=["xo"])
                P.op("act", lambda e: e.activation(h2f[:, k, :], xo[:, k, :], AF.Identity, bias=s2[:, k:k + 1], scale=1.0),
                     reads=["xo"], writes=["h2f"])
            P.op("pool", lambda e: e.tensor_copy(h2b[:], h2f[:]), reads=["h2f"], writes=["h2b"])
            for t in range(TPG):
                T = G * TPG + t
                lq = t
                for k in range(8):
                    P.op("pe", lambda e: e.matmul(p_lg[:, lq, :], lhsT=h2f[:, k, t * 128:(t + 1) * 128], rhs=wr_sb[:, k, :],
                                                  start=(k == 0), stop=(k == 7)),
                         reads=["h2f", "wr"], writes=["p_lg"], inc=(k == 7))
                P.op("dve", lambda e: e.tensor_tensor(lg[:], p_lg[:, lq, :], br_sb[:], ALU.add), reads=["p_lg", "br"], writes=["lg"])
                if DEBUG:
                    P.dma("sp", g["dbg"]["lg"][:, T, :], lg[:], reads=["lg"], sem="dbg_lg")
                P.op("dve", lambda e: e.max(t8[:], lg[:]), reads=["lg"], writes=["t8"])
                P.op("dve", lambda e: e.tensor_scalar(nmax[:], t8[:, 0:1], -1.0, None, ALU.mult), reads=["t8"], writes=["nmax"])
                P.op("act", lambda e: e.activation(w4[:, T, :], t8[:, 0:4], AF.Exp, bias=nmax[:], scale=1.0),
                     reads=["t8", "nmax"], writes=[("w4", T)])
                P.op("dve", lambda e: e.tensor_reduce(den[:], w4[:, T, :], AX.X, ALU.add), reads=[("w4", T)], writes=["den"])
                P.op("dve", lambda e: e.reciprocal(den[:], den[:]), reads=["den"], writes=["den"])
                P.op("dve", lambda e: e.tensor_scalar(w4[:, T, :], w4[:, T, :], den[:], None, ALU.mult), reads=[("w4", T), "den"], writes=[("w4", T)])
                P.op("dve", lambda e: e.tensor_scalar(sel[:], lg[:], t8[:, 3:4], None, ALU.is_ge), reads=["lg", "t8"], writes=["sel"])
                P.op("dve", lambda e: e.tensor_copy(selb[:], sel[:]), reads=["sel"], writes=["selb"])
                P.op("pe", lambda e: e.matmul(p_lg[:, lq, :], lhsT=stri[:], rhs=selb[:], start=True, stop=True),
                     reads=["stri", "selb"], writes=["p_lg"])
                P.op("dve", lambda e: e.tensor_tensor(slot[:], p_lg[:, lq, :], carry[:], ALU.add), reads=["p_lg", "carry"], writes=["slot"])
                P.op("pe", lambda e: e.matmul(p_lg[:, lq, :], lhsT=ones_1[:], rhs=selb[:], start=True, stop=True),
                     reads=["selb"], writes=["p_lg"])
                P.op("dve", lambda e: e.tensor_tensor(carry[:], p_lg[:, lq, :], carry[:], ALU.add), reads=["p_lg", "carry"], writes=["carry"])
                P.op("dve", lambda e: e.tensor_tensor(slot[:], slot[:], ecap_sb[:], ALU.add), reads=["slot", "ecap"], writes=["slot"])
                P.op("pool", lambda e: e.memset(RW[:], 0.0), writes=["RW"])
                for k4 in range(4):
                    P.op("dve", lambda e: e.tensor_scalar(oh[:], lg[:], t8[:, k4:k4 + 1], None, ALU.is_equal), reads=["lg", "t8"], writes=["oh"])
                    P.op("dve", lambda e: e.scalar_tensor_tensor(RW[:], oh[:], w4[:, T, k4:k4 + 1], RW[:], ALU.mult, ALU.add),
                         reads=["oh", ("w4", T), "RW"], writes=["RW"])
                    P.op("dve", lambda e: e.tensor_tensor(oh[:], oh[:], slot[:], ALU.mult), reads=["oh", "slot"], writes=["oh"])
                    P.op("dve", lambda e: e.tensor_reduce(destf[:, k4:k4 + 1], oh[:], AX.X, ALU.add), reads=["oh"], writes=["destf"])
                P.op("dve", lambda e: e.tensor_copy(dest_i[:, T * 4:T * 4 + 4], destf[:]), reads=["destf"], writes=[("dest_i", T)])
                P.op("pe", lambda e: e.transpose(p_rw[:], RW[:], ident_f), reads=["RW"], writes=["p_rw"])
                P.op("act", lambda e: e.copy(RWT[:, T, :], p_rw[:]), reads=["p_rw"], writes=[("RWT", T)])
                for hf in range(2):
                    for k in range(4):
                        P.op("pe", lambda e: e.transpose(p_tr[:, hf, k * 128:(k + 1) * 128], h2b[:, hf * 4 + k, t * 128:(t + 1) * 128], ident_b[:]),
                             reads=["h2b"], writes=["p_tr"], inc=(k == 3))
                    P.op("act", lambda e: e.copy(h2tok[:, hf * 512:(hf + 1) * 512], p_tr[:, hf, :]), reads=["p_tr"], writes=["h2tok"])
                for k4 in range(4):
                    P.dma("pool", None, None, reads=["h2tok", ("dest_i", T)], writes=["XG_d"], sem=("xgs", k4),
                          emit=lambda e: e.indirect_dma_start(
                              out=XG_d[:, :], out_offset=bass.IndirectOffsetOnAxis(ap=dest_i[:, T * 4 + k4:T * 4 + k4 + 1], axis=0),
                              in_=h2tok[:, :], in_offset=None, bounds_check=g["bc_reg"], oob_is_err=False))
        if DEBUG:
            P.wait_all("sp")
            P.dma("sp", g["dbg"]["x1"], X1_d, reads=[])
        P.barrier()


def phase_moe(nc, P, L):
    g = L
    sb, ps = g["sb"], g["ps"]
    ident_b, gate2_bc = g["ident_b"], g["gate2_bc"]
    XG_d, OUT_d, X1_d = g["XG_d"], g["OUT_d"], g["X1_d"]
    w1d, w2, out = g["w1d"], g["w2"], g["out"]
    dest_i, w4, RWT = g["dest_i"], g["w4"], g["RWT"]
    NCH = CAP // 512
    with ExitStack() as es:
        wp = sb(es, "wp", [128, 12, 8, 512], BF16)
        b1 = sb(es, "b1", [128, NE, 16], F32)
        xg = sb(es, "xg", [128, 2, 4, D], BF16)
        xgT = sb(es, "xgT", [128, 2, 8, 512], BF16)
        actT = sb(es, "actT", [128, 2, 8, 512], BF16)
        gg = sb(es, "gg", [128, 2, 512], F32)
        ll = sb(es, "ll", [128, 2, 512], F32)
        ee = sb(es, "ee", [128, 2, 512], F32)
        orow = sb(es, "orow", [128, 4, D], F32)
        p_h = ps(es, "pm_h", [128, 4, 512], F32)
        p_o = ps(es, "pm_o", [128, 2, 512], F32)
        p_t = ps(es, "pm_t", [128, 2, 512], BF16)
        P.dma("sp", b1[:].rearrange("p a b -> p (a b)"), g["b1T"], writes=["b1"])

        def load_expert(e):
            base = (e % 2) * 6
            for p in range(4):
                for two in range(2):
                    src = w1d[e][:, two * 1024 + p * 256:two * 1024 + (p + 1) * 256]
                    P.dma("pool", wp[:, base + p, :, two * 256:(two + 1) * 256], src.rearrange("(k p) f -> p k f", p=128),
                          writes=[("wp", base + p, two)], sem=("wp", base + p, two))
            for hf in range(2):
                src = w2[e][:, hf * 512:(hf + 1) * 512]
                P.dma("pool", wp[:, base + 4 + hf], src.rearrange("(k p) n -> p k n", p=128),
                      writes=[("wp", base + 4 + hf, 0)], sem=("wp", base + 4 + hf, 0))

        load_expert(0)
        nch = [0]
        for e in range(NE):
            base = (e % 2) * 6
            if e + 1 < NE:
                load_expert(e + 1)
            for ch in range(NCH):
                u = nch[0] % 2
                nch[0] += 1
                r0 = e * CAP + ch * 512
                P.dma("sp", xg[:, u], XG_d[r0:r0 + 512, :].rearrange("(r p) d -> p r d", p=128),
                      writes=[("xg", u)], sem=("xg", u))
                for r in range(4):
                    for hf in range(2):
                        for k in range(4):
                            P.op("pe", lambda e_: e_.transpose(p_t[:, hf, k * 128:(k + 1) * 128],
                                                               xg[:, u, r, (hf * 4 + k) * 128:(hf * 4 + k + 1) * 128], ident_b[:]),
                                 reads=[("xg", u)], writes=["p_t"], inc=(k == 3))
                        P.op("act", lambda e_: e_.copy(xgT[:, u, hf * 4:(hf + 1) * 4, r * 128:(r + 1) * 128],
                                                       p_t[:, hf, :].rearrange("p (k c) -> p k c", k=4)),
                             reads=["p_t"], writes=[("xgT", u)])
                for j in range(8):
                    sl = base + j // 2
                    hs = j % 2
                    for part in range(2):
                        c0 = part * 256 + (j % 2) * 128
                        for k in range(8):
                            P.op("pe", lambda e_: e_.matmul(p_h[:, hs * 2 + part, :], lhsT=wp[:, sl, k, c0:c0 + 128], rhs=xgT[:, u, k, :],
                                                            start=(k == 0), stop=(k == 7)),
                                 reads=[("wp", sl, part), ("xgT", u)], writes=[("p_h", hs, part)], inc=(k == 7))
                    P.op("dve", lambda e_: e_.tensor_scalar(gg[:, hs, :], p_h[:, hs * 2, :], b1[:, e, j:j + 1], 7.0, ALU.add, ALU.min),
                         reads=[("p_h", hs, 0), "b1"], writes=[("gg", hs)])
                    P.op("dve", lambda e_: e_.tensor_scalar(ll[:, hs, :], p_h[:, hs * 2 + 1, :], b1[:, e, 8 + j:9 + j], 7.0, ALU.add, ALU.min),
                         reads=[("p_h", hs, 1), "b1"], writes=[("ll", hs)])
                    P.op("pool", lambda e_: e_.tensor_scalar(ll[:, hs, :], ll[:, hs, :], -7.0, 1.0, ALU.max, ALU.add),
                         reads=[("ll", hs)], writes=[("ll", hs)])
                    P.op("act", lambda e_: e_.activation(ee[:, hs, :], gg[:, hs, :], AF.Exp, scale=-1.702), reads=[("gg", hs)], writes=[("ee", hs)])
                    P.op("pool", lambda e_: e_.tensor_scalar(ee[:, hs, :], ee[:, hs, :], 1.0, None, ALU.add), reads=[("ee", hs)], writes=[("ee", hs)])
                    P.op("dve", lambda e_: e_.reciprocal(ee[:, hs, :], ee[:, hs, :]), reads=[("ee", hs)], writes=[("ee", hs)])
                    P.op("pool", lambda e_: e_.tensor_tensor(gg[:, hs, :], gg[:, hs, :], ll[:, hs, :], ALU.mult),
                         reads=[("gg", hs), ("ll", hs)], writes=[("gg", hs)])
                    P.op("dve", lambda e_: e_.tensor_tensor(actT[:, u, j, :], gg[:, hs, :], ee[:, hs, :], ALU.mult),
                         reads=[("gg", hs), ("ee", hs)], writes=[("actT", u)])
                for hf in range(2):
                    sl = base + 4 + hf
                    for r in range(4):
                        os_ = (hf * 4 + r) % 2
                        for k in range(8):
                            P.op("pe", lambda e_: e_.matmul(p_o[:, os_, :], lhsT=actT[:, u, k, r * 128:(r + 1) * 128], rhs=wp[:, sl, k, :],
                                                            start=(k == 0), stop=(k == 7)),
                                 reads=[("actT", u), ("wp", sl, 0)], writes=[("p_o", os_)], inc=(k == 7))
                        P.op("act", lambda e_: e_.copy(orow[:, r, hf * 512:(hf + 1) * 512], p_o[:, os_, :]),
                             reads=[("p_o", os_)], writes=["orow"])
                P.dma("sp", OUT_d[r0:r0 + 512, :].rearrange("(r p) d -> p r d", p=128), orow[:],
                      reads=["orow"], writes=[("OUT_d", e, ch)], sem="orow")
        P.barrier()
    with ExitStack() as es:
        yk = sb(es, "yk", [128, 2, 4, D], F32)
        acc = sb(es, "acc", [128, 2, D], F32)
        x1t = sb(es, "x1c", [128, 2, D], F32)
        fsq = sb(es, "fsq", [128, D], F32)
        ssq = sb(es, "ssq", [128, 2], F32)
        b2_sb = sb(es, "b2c", [32, D], F32)
        fnw = sb(es, "fnwc", [128, D], F32)
        p_bias = ps(es, "pc_b", [128, 2, 2, 512], F32)
        P.dma("sp", b2_sb[:], g["b2"], writes=["b2c"])
        P.dma("sp", fnw[:], g["fnw_bc"], writes=["fnwc"])
        for T in range(16):
            u = T % 2
            for k4 in range(4):
                P.dma("pool", None, None, reads=["OUT_d"], writes=[("yk", u, k4)], sem=("yk", u, k4),
                      emit=lambda e: e.indirect_dma_start(
                          out=yk[:, u, k4, :], out_offset=None, in_=OUT_d[:, :],
                          in_offset=bass.IndirectOffsetOnAxis(ap=dest_i[:, T * 4 + k4:T * 4 + k4 + 1], axis=0),
                          bounds_check=g["bc_reg"], oob_is_err=False))
            P.dma("sp", x1t[:, u, :], X1_d[T * 128:(T + 1) * 128, :], reads=["X1_d"], writes=[("x1c", u)], sem=("x1c", u))
            for half in range(2):
                P.op("pe", lambda e: e.matmul(p_bias[:, u, half, :], lhsT=RWT[:, T, :], rhs=b2_sb[:, half * 512:(half + 1) * 512],
                                              start=True, stop=True),
                     reads=["b2c"], writes=[("p_bias", u)], inc=(half == 1))
            P.op("dve", lambda e: e.tensor_scalar(acc[:, u, :], yk[:, u, 0, :], w4[:, T, 0:1], None, ALU.mult),
                 reads=[("yk", u, 0)], writes=[("acc", u)])
            for k4 in range(1, 4):
                P.op("dve", lambda e: e.scalar_tensor_tensor(acc[:, u, :], yk[:, u, k4, :], w4[:, T, k4:k4 + 1], acc[:, u, :], ALU.mult, ALU.add),
                     reads=[("yk", u, k4), ("acc", u)], writes=[("acc", u)])
            for half in range(2):
                P.op("dve", lambda e: e.tensor_tensor(acc[:, u, half * 512:(half + 1) * 512], acc[:, u, half * 512:(half + 1) * 512],
                                                      p_bias[:, u, half, :], ALU.add),
                     reads=[("acc", u), ("p_bias", u)], writes=[("acc", u)])
            P.op("pool", lambda e: e.tensor_tensor(acc[:, u, :], acc[:, u, :], gate2_bc[:], ALU.mult), reads=[("acc", u), "gate2_bc"], writes=[("acc", u)])
            P.op("pool", lambda e: e.tensor_tensor(acc[:, u, :], acc[:, u, :], x1t[:, u, :], ALU.add), reads=[("acc", u), ("x1c", u)], writes=[("acc", u)])
            P.op("pool", lambda e: e.memset(ssq[:, u:u + 1], 0.0), writes=[("ssq", u)])
            P.op("act", lambda e: e.activation(fsq[:], acc[:, u, :], AF.Square, accum_out=ssq[:, u:u + 1]), reads=[("acc", u), ("ssq", u)], writes=["fsq", ("ssq", u)])
            rsqrt_act(P, ssq[:, u:u + 1], ssq[:, u:u + 1], [("ssq", u)], ("ssq", u), 1.0 / D)
            P.op("dve", lambda e: e.scalar_tensor_tensor(acc[:, u, :], acc[:, u, :], ssq[:, u:u + 1], fnw[:], ALU.mult, ALU.mult),
                 reads=[("acc", u), ("ssq", u), "fnwc"], writes=[("acc", u)])
            P.dma("sp", out[T * 128:(T + 1) * 128, :], acc[:, u, :], reads=[("acc", u)], writes=[("out", T)], sem=("out", u))


def _consts():
    c = np.zeros((128, 10, 128), np.float32)
    c[:, 0, :] = np.eye(128, dtype=np.float32)
    perm = np.zeros((128, 128), np.float32)
    for m in range(32):
        perm[(m + 16) % 32, m] = 1.0
    c[:, 1, :] = perm
    i = np.arange(128)
    c[:, 2, :] = np.where(i[:, None] <= i[None, :], -1.0 / 16.0, 0.0)
    c[:, 3, :] = (i[:, None] <= i[None, :]).astype(np.float32)
    c[:, 4, :] = (i[:, None] < i[None, :]).astype(np.float32)
    q = np.arange(256)
    for half in range(2):
        kpos = half * 128 + i
        m = np.where(kpos[:, None] <= q[None, :], 0.0, -BIG).astype(np.float32)
        c[:, 5 + 2 * half, :] = m[:, :128]
        c[:, 6 + 2 * half, :] = m[:, 128:]
    return c


def _rope_tables():
    half = 16
    inv = np.float32(500000.0) ** (-np.arange(half, dtype=np.float32) * np.float32(2.0) / np.float32(32))
    ang = np.arange(SEQ, dtype=np.float32)[:, None] * inv[None, :].astype(np.float32)
    cos = np.cos(ang).astype(np.float32).T
    sin = np.sin(ang).astype(np.float32).T
    return np.concatenate([cos, cos], 0), np.concatenate([-sin, sin], 0)


def prep_inputs(inputs):
    f = lambda k: np.asarray(inputs[k], np.float32)
    x = f("x")
    c = f("c")
    w_in = f("w_in")[0]
    fm = lambda v: np.ascontiguousarray(v.reshape(-1, 128).T)
    bc = lambda v: np.ascontiguousarray(np.broadcast_to(v[None, :], (128, v.shape[0])))
    o = [0]
    for sz in (1024, 1024, 1024, 512, 512, 1024, 1024, 16, 1024, 1024):
        o.append(o[-1] + sz)
    mq, mk, mv, gq, gk, gv, gr, gl, ga, gb = [w_in[:, o[i]:o[i + 1]] for i in range(10)]
    shared = {
        "w_ada": f("w_ada")[0],
        "b_ada_bc": bc(f("b_ada")[0]),
        "nw": np.concatenate([fm(f("norm1_w")[0]), fm(f("norm2_w")[0])], 1),
        "fnw_bc": bc(f("final_norm_w")),
        "wA": np.ascontiguousarray(np.concatenate([mk, mv, gk, gv, gl], 1)),
        "wO": np.ascontiguousarray(np.concatenate([mq, gq, gr], 1)),
        "wG": np.ascontiguousarray(np.concatenate([ga, gb], 1)),
        "w_oa": f("w_o_moba")[0], "w_ob": f("w_o_gla")[0], "w_out": f("w_out")[0],
        "nbm": fm(f("b_merge")[0]),
        "gnw": fm(f("gla_norm_w")[0]),
        "w_r": f("w_router")[0], "b_r_bc": bc(f("b_router")[0]),
        "w2": f("w_exp_out")[0], "b2": f("b_exp_out")[0],
        "consts": _consts(),
        "ecap": bc(np.arange(NE, dtype=np.float32) * CAP),
    }
    gw = np.zeros((32, 512), np.float32)
    gw[0:16] = f("gla_gate_w")[0]
    gw[16] = f("gla_gate_b")[0]
    shared["gw_aug"] = gw
    w1 = f("w_exp_in")[0]
    shared["w1d"] = np.ascontiguousarray(np.concatenate([w1[:, :, 0::2], w1[:, :, 1::2]], 2))
    b1 = f("b_exp_in")[0]
    b1d = np.concatenate([b1[:, 0::2], b1[:, 1::2]], 1)
    shared["b1T"] = np.ascontiguousarray(b1d.reshape(NE, 16, 128).transpose(2, 0, 1).reshape(128, NE * 16))
    es = np.zeros((32, 32, 128), np.float32)
    for j in range(32):
        es[j, j, :] = 1.0
    shared["esel_in"] = es.reshape(32, 32 * 128)
    cosT, sinT = _rope_tables()
    per_core = []
    for core in range(NCORE):
        b, r = core // 4, core % 4
        nnull = (24 - 8 * r) * 256
        nreal = 2048 * (r + 1)
        xv = np.zeros((D, SEQ), np.float32)
        xv[:, nnull:] = x[b, :nreal, :].T
        rc_ = np.zeros((32, SEQ), np.float32)
        rs_ = np.zeros((32, SEQ), np.float32)
        rc_[:, nnull:] = cosT[:, :nreal]
        rs_[:, nnull:] = sinT[:, :nreal]
        vf = np.zeros((SEQ,), np.float32)
        vf[nnull:] = 1.0
        gm = np.full((8, 32), NEGINF, np.float32)
        gvv = np.zeros((8, 32), np.float32)
        for l in range(8):
            gm[l, 24 - 8 * r:24 + l] = 0.0
            gvv[l, 24 - 8 * r:24 + l] = 1.0
        d = dict(shared)
        d.update({
            "xTv": xv,
            "xtok": np.ascontiguousarray(x[b, 2048 * r:2048 * (r + 1), :]),
            "cT": fm(c[b]),
            "ropec": rc_, "ropes": rs_,
            "vflag": np.ascontiguousarray(vf.reshape(64, 128).T),
            "gmask": bc(gm.reshape(-1)), "gvalid": bc(gvv.reshape(-1)),
        })
        per_core.append(d)
    return per_core


_NC_CACHE = {}


def kernel(**inputs):
    in_maps = prep_inputs(inputs)
    if "nc" not in _NC_CACHE:
        _NC_CACHE["nc"] = build_nc()[0]
    nc = _NC_CACHE["nc"]
    res = run_bass_kernel_spmd(nc, in_maps, core_ids=list(range(NCORE)))
    outp = np.zeros((2, SEQ, D), np.float32)
    for core in range(NCORE):
        b, r = core // 4, core % 4
        outp[b, 2048 * r:2048 * (r + 1), :] = res.results[core]["out"]
    return outp
```

```python
import numpy as np
from contextlib import ExitStack
import concourse.bass as bass
import concourse.mybir as mybir
from concourse.bass_utils import run_bass_kernel_spmd

F32 = mybir.dt.float32
BF16 = mybir.dt.bfloat16
I32 = mybir.dt.int32
AF = mybir.ActivationFunctionType
ALU = mybir.AluOpType
AX = mybir.AxisListType

D = 1024
SEQ = 8192
NCORE = 8
OWN = 2048
GS = 256
TPG = 2
NG = 32
OWNG0 = 24
NVB = 32
NE = 32
CAP = 1024
BIG = 30000.0
NEGINF = -1.0e30
EPS = 1e-5
STAGE = 99
DEBUG = False


class Prog:
    def __init__(self, nc, same_engine_sync=True):
        self.nc = nc
        self.engs = {"pe": nc.tensor, "act": nc.scalar, "dve": nc.vector,
                     "pool": nc.gpsimd, "sp": nc.sync}
        self.sem = {k: nc.alloc_semaphore("prog_" + k) for k in self.engs}
        self.cnt = {k: 0 for k in self.engs}
        self.seen = {k: {} for k in self.engs}
        self.bufs = {}
        self.pending = {k: ([], []) for k in self.engs}
        self.dsem = {}
        self.dcnt = {}
        self.same = same_engine_sync
        self.nwait = 0
        self.nins = 0
        self.free_sems = {"sw": [], "hw": []}
        self.dkind = {}
        self.nalloc = 0

    def _deps(self, reads, writes):
        deps = []
        for k in reads:
            b = self.bufs.get(k)
            if b is not None and b[0] is not None:
                deps.append(b[0])
        for k in writes:
            b = self.bufs.get(k)
            if b is not None:
                if b[0] is not None:
                    deps.append(b[0])
                deps.extend(b[1])
        return deps

    def _wait(self, eng, deps, own_ok=True):
        need = {}
        for (s, v) in deps:
            if own_ok and s is self.sem[eng]:
                if eng == "pe" or not self.same:
                    continue
            key = id(s)
            if self.seen[eng].get(key, 0) >= v:
                continue
            if key not in need or need[key][1] < v:
                need[key] = (s, v)
        for key, (s, v) in need.items():
            self.engs[eng].wait_ge(s, v)
            self.seen[eng][key] = v
            self.nwait += 1

    def _record(self, ev, reads, writes):
        for k in reads:
            b = self.bufs.get(k)
            if b is None:
                self.bufs[k] = [None, [ev]]
            else:
                b[1].append(ev)
                if len(b[1]) > 24:
                    b[1] = b[1][-24:] if False else b[1]
        for k in writes:
            self.bufs[k] = [ev, []]

    def op(self, eng, emit, reads=(), writes=(), inc=True):
        reads = list(reads)
        writes = list(writes)
        self._wait(eng, self._deps(reads, writes))
        ins = emit(self.engs[eng])
        self.nins += 1
        if not inc:
            self.pending[eng][0].extend(reads)
            self.pending[eng][1].extend(writes)
            return None
        self.cnt[eng] += 1
        ev = (self.sem[eng], self.cnt[eng])
        ins.then_inc(self.sem[eng], 1)
        pr, pw = self.pending[eng]
        self._record(ev, pr + reads, pw + writes)
        self.pending[eng] = ([], [])
        return ev

    def dma(self, queue, out, in_, reads=(), writes=(), sem=None, emit=None, **kw):
        reads = list(reads)
        writes = list(writes)
        if sem is None:
            sem = ("auto",) + tuple(writes) + tuple(reads)
        kind = "sw" if queue == "pool" else "hw"
        if sem in self.dsem:
            assert self.dkind[sem] == kind, (sem, kind)
        if sem not in self.dsem:
            self.dkind[sem] = kind
            if self.free_sems[kind]:
                self.dsem[sem], self.dcnt[sem] = self.free_sems[kind].pop()
            else:
                self.dsem[sem] = self.nc.alloc_semaphore("dma_%d" % self.nalloc)
                self.nalloc += 1
                self.dcnt[sem] = 0
        self._wait(queue, self._deps(reads, writes))
        if emit is not None:
            ins = emit(self.engs[queue])
        else:
            ins = self.engs[queue].dma_start(out=out, in_=in_, **kw)
        self.nins += 1
        self.dcnt[sem] += 16
        ins.then_inc(self.dsem[sem], 16)
        ev = (self.dsem[sem], self.dcnt[sem])
        self._record(ev, reads, writes)
        return ev

    def wait_all(self, eng, keys=None):
        deps = []
        for k, b in self.bufs.items():
            if keys is not None and k not in keys:
                continue
            if b[0] is not None:
                deps.append(b[0])
            deps.extend(b[1])
        self._wait(eng, deps, own_ok=False)

    def barrier(self):
        assert all(len(p[0]) == 0 and len(p[1]) == 0 for p in self.pending.values())
        for eng in self.engs:
            deps = [(self.sem[o], self.cnt[o]) for o in self.engs if o != eng and self.cnt[o] > 0]
            deps += [(self.dsem[k], self.dcnt[k]) for k in self.dsem if self.dcnt[k] > 0]
            self._wait(eng, deps, own_ok=False)
        self.bufs = {}
        for k in list(self.dsem):
            self.free_sems[self.dkind.pop(k)].append((self.dsem.pop(k), self.dcnt.pop(k)))


def build_nc():
    nc = bass.Bass("TRN2", target_bir_lowering=False)
    P = Prog(nc)
    dt_in = lambda n, s, d=F32: nc.dram_tensor(n, list(s), d, kind="ExternalInput").ap()
    dt_sc = lambda n, s, d: nc.dram_tensor(n, list(s), d).ap()

    xTv = dt_in("xTv", [D, SEQ])
    xtok = dt_in("xtok", [OWN, D])
    cT = dt_in("cT", [128, 8])
    w_ada = dt_in("w_ada", [D, 6 * D])
    b_ada_bc = dt_in("b_ada_bc", [128, 6 * D])
    nw = dt_in("nw", [128, 16])
    fnw_bc = dt_in("fnw_bc", [128, D])
    wA = dt_in("wA", [D, 3600])
    wO = dt_in("wO", [D, 2560])
    wG = dt_in("wG", [D, 2048])
    w_oa = dt_in("w_oa", [D, D])
    w_ob = dt_in("w_ob", [D, D])
    w_out = dt_in("w_out", [D, D])
    nbm = dt_in("nbm", [128, 16])
    ropec = dt_in("ropec", [32, SEQ])
    ropes = dt_in("ropes", [32, SEQ])
    vflag = dt_in("vflag", [128, 64])
    gmask = dt_in("gmask", [128, 8 * 32])
    gvalid = dt_in("gvalid", [128, 8 * 32])
    gw_aug = dt_in("gw_aug", [32, 512])
    gnw = dt_in("gnw", [128, 2])
    w_r = dt_in("w_r", [D, NE])
    b_r_bc = dt_in("b_r_bc", [128, NE])
    w1d = dt_in("w1d", [NE, D, 2048]) if STAGE >= 4 else None
    w2 = dt_in("w2", [NE, D, D]) if STAGE >= 4 else None
    b1T = dt_in("b1T", [128, NE * 16])
    b2 = dt_in("b2", [NE, D])
    consts = dt_in("consts", [128, 10, 128])
    esel_in = dt_in("esel_in", [32, 32 * 128])
    ecap = dt_in("ecap", [128, NE])
    out = nc.dram_tensor("out", [OWN, D], F32, kind="ExternalOutput").ap()

    KT_d = dt_sc("KT_d", [8, 128, SEQ], BF16)
    V_d = dt_sc("V_d", [SEQ, D], BF16)
    QT_d = dt_sc("QT_d", [8, 128, OWN], BF16)
    OB_d = dt_sc("OB_d", [D, OWN], BF16)
    HT_d = dt_sc("HT_d", [D, OWN], BF16)
    X1_d = dt_sc("X1_d", [OWN, D], F32)
    XG_d = dt_sc("XG_d", [NE * CAP, D], BF16)
    OUT_d = dt_sc("OUT_d", [NE * CAP, D], F32)

    dbg = {}
    if DEBUG:
        dbg["mod"] = nc.dram_tensor("dbg_mod", [128, 48], F32, kind="ExternalOutput").ap()
        dbg["kT"] = nc.dram_tensor("dbg_kT", [8, 128, SEQ], BF16, kind="ExternalOutput").ap()
        dbg["qT"] = nc.dram_tensor("dbg_qT", [8, 128, OWN], BF16, kind="ExternalOutput").ap()
        dbg["ob"] = nc.dram_tensor("dbg_ob", [D, OWN], BF16, kind="ExternalOutput").ap()
        dbg["oa"] = nc.dram_tensor("dbg_oa", [128, 8, OWN], BF16, kind="ExternalOutput").ap()
        dbg["x1"] = nc.dram_tensor("dbg_x1", [OWN, D], F32, kind="ExternalOutput").ap()
        dbg["lg"] = nc.dram_tensor("dbg_lg", [128, 16, 32], F32, kind="ExternalOutput").ap()

    es_all = ExitStack()
    with es_all:
        sb = lambda es, n, s, d: es.enter_context(nc.sbuf_tensor(n, list(s), d))
        ps = lambda es, n, s, d: es.enter_context(nc.psum_tensor(n, list(s), d))
        cst = sb(es_all, "cst", [128, 10, 128], F32)
        ident_f = cst[:, 0, :]
        ident_b = sb(es_all, "ident_b", [128, 128], BF16)
        ones_rms = sb(es_all, "ones_rms", [128, 128], BF16)
        ones_256 = sb(es_all, "ones_256", [128, 128], BF16)
        ones_1 = sb(es_all, "ones_1", [128, 128], BF16)
        onesf = sb(es_all, "onesf", [128, 128], F32)
        modT = sb(es_all, "modT", [128, 48], F32)
        a1 = sb(es_all, "a1", [128, 8], F32)
        a2 = sb(es_all, "a2", [128, 8], F32)
        nw_sb = sb(es_all, "nw_sb", [128, 16], F32)
        gate1_bc = sb(es_all, "gate1_bc", [128, D], F32)
        gate2_bc = sb(es_all, "gate2_bc", [128, D], F32)
        kmb = sb(es_all, "kmb", [128, 8, 32], BF16)
        bc_reg = nc.gpsimd.to_reg(NE * CAP - 1)
        dest_i = sb(es_all, "dest_i", [128, 64], I32)
        w4 = sb(es_all, "w4", [128, 16, 4], F32)
        RWT = sb(es_all, "RWT", [32, 16, 128], F32)
        s1 = modT[:, 0:8]
        g1 = modT[:, 16:24]
        s2 = modT[:, 24:32]

        P.dma("sp", cst[:], consts, writes=["cst"])
        P.dma("sp", nw_sb[:], nw, writes=["nw"])
        P.dma("pool", ident_b[:], consts[:, 0, :], writes=["ident_b"])
        P.op("dve", lambda e: e.memset(ones_rms[:], 1.0 / 1024.0), writes=["ones_rms"])
        P.op("dve", lambda e: e.memset(ones_256[:], 1.0 / 256.0), writes=["ones_256"])
        P.op("dve", lambda e: e.memset(ones_1[:], 1.0), writes=["ones_1"])
        P.op("dve", lambda e: e.memset(onesf[:], 1.0), writes=["onesf"])

        with ExitStack() as es:
            c_sb = sb(es, "c_sb", [128, 8], F32)
            c_e = sb(es, "c_e", [128, 8], F32)
            cact_b = sb(es, "cact_b", [128, 8, 128], F32)
            wst = sb(es, "wst", [128, 2, 8, 512], F32)
            modb = sb(es, "modb", [128, 6 * D], F32)
            bab = sb(es, "bab", [128, 6 * D], F32)
            p_mod = ps(es, "p_mod", [128, 2, 512], F32)
            p_col = ps(es, "p_col", [128, 48], F32)
            P.dma("sp", c_sb[:], cT, writes=["c_sb"])
            P.dma("sp", bab[:], b_ada_bc, writes=["bab"])
            P.op("act", lambda e: e.activation(c_e[:], c_sb[:], AF.Exp, scale=-1.0), reads=["c_sb"], writes=["c_e"])
            P.op("dve", lambda e: e.tensor_scalar(c_e[:], c_e[:], 1.0, None, ALU.add), reads=["c_e"], writes=["c_e"])
            P.op("dve", lambda e: e.reciprocal(c_e[:], c_e[:]), reads=["c_e"], writes=["c_e"])
            P.op("dve", lambda e: e.tensor_tensor(c_e[:], c_e[:], c_sb[:], ALU.mult), reads=["c_e", "c_sb"], writes=["c_e"])
            for k in range(8):
                P.op("dve", lambda e: e.tensor_scalar(cact_b[:, k, :], onesf[:], c_e[:, k:k + 1], None, ALU.mult),
                     reads=["c_e", "onesf"], writes=[("cactb", k)])
            for j in range(12):
                s = j % 2
                P.dma("sp", wst[:, s], w_ada[:, j * 512:(j + 1) * 512].rearrange("(k p) n -> p k n", p=128),
                      writes=[("wst", s)], sem=("wst", s))
                for k in range(8):
                    P.op("pe", lambda e: e.matmul(p_mod[:, s, :], lhsT=cact_b[:, k, :], rhs=wst[:, s, k, :],
                                                  start=(k == 0), stop=(k == 7)),
                         reads=[("wst", s), ("cactb", k)], writes=[("p_mod", s)], inc=(k == 7))
                P.op("dve", lambda e: e.tensor_tensor(modb[:, j * 512:(j + 1) * 512], p_mod[:, s, :],
                                                      bab[:, j * 512:(j + 1) * 512], ALU.add),
                     reads=[("p_mod", s), "bab"], writes=[("modb", j)])
            for j in range(48):
                P.op("pe", lambda e: e.matmul(p_col[:, j:j + 1], lhsT=modb[0:1, j * 128:(j + 1) * 128],
                                              rhs=onesf[0:1, 0:1], start=True, stop=True),
                     reads=[("modb", j // 4), "onesf"], writes=["p_col"], inc=(j == 47))
            P.op("dve", lambda e: e.tensor_copy(modT[:], p_col[:]), reads=["p_col"], writes=["modT"])
            P.op("dve", lambda e: e.scalar_tensor_tensor(a1[:], modT[:, 8:16], 1.0, nw_sb[:, 0:8], ALU.add, ALU.mult),
                 reads=["modT", "nw"], writes=["a1"])
            P.op("dve", lambda e: e.scalar_tensor_tensor(a2[:], modT[:, 32:40], 1.0, nw_sb[:, 8:16], ALU.add, ALU.mult),
                 reads=["modT", "nw"], writes=["a2"])
            P.op("act", lambda e: e.copy(gate1_bc[:], modb[:, 2048:3072]), reads=[("modb", 4), ("modb", 5)], writes=["gate1_bc"])
            P.op("act", lambda e: e.copy(gate2_bc[:], modb[:, 5120:6144]), reads=[("modb", 10), ("modb", 11)], writes=["gate2_bc"])
            if DEBUG:
                P.dma("sp", dbg["mod"], modT[:], reads=["modT"])
            P.barrier()

        G_ = dict(locals())
        if STAGE >= 1:
            phase_a1(nc, P, G_)
        with ExitStack() as es_ab:
            oaT = sb(es_ab, "oaT", [128, 8, OWN], BF16)
            G_["oaT"] = oaT
            if STAGE >= 2:
                phase_a2(nc, P, G_)
            if STAGE >= 3:
                phase_b(nc, P, G_)
        if STAGE >= 4:
            phase_moe(nc, P, G_)
        P.wait_all("sp")
    return nc, P


def rmsnorm_fm(P, nc, xs_ap, sq_ap, p_ss, rstd_ap, ones_rms, keyx, tag, pkey):
    P.op("act", lambda e: e.activation(sq_ap, xs_ap, AF.Square), reads=list(keyx), writes=[tag + "sq"])
    for k in range(8):
        P.op("pe", lambda e: e.matmul(p_ss, lhsT=ones_rms[:], rhs=sq_ap[:, k, :], start=(k == 0), stop=(k == 7)),
             reads=[tag + "sq", "ones_rms"], writes=[pkey], inc=(k == 7))
    rsqrt_act(P, rstd_ap, p_ss, [pkey], tag + "rstd", 1.0)


def rsqrt_act(P, out_ap, in_ap, rkeys, wkey, mul):
    P.op("act", lambda e: e.activation(out_ap, in_ap, AF.Ln, bias=EPS, scale=mul), reads=list(rkeys), writes=[wkey])
    P.op("act", lambda e: e.activation(out_ap, out_ap, AF.Exp, scale=-0.5), reads=[wkey], writes=[wkey])


def phase_a1(nc, P, L):
    g = L
    sb, ps = g["sb"], g["ps"]
    xTv, wA, wO, ropec, ropes = g["xTv"], g["wA"], g["wO"], g["ropec"], g["ropes"]
    cst, ident_b = g["cst"], g["ident_b"]
    ones_rms, ones_256 = g["ones_rms"], g["ones_256"]
    a1, s1, kmb = g["a1"], g["s1"], g["kmb"]
    KT_d, V_d, QT_d, OB_d, HT_d = g["KT_d"], g["V_d"], g["QT_d"], g["OB_d"], g["HT_d"]
    dbg = g["dbg"]
    with ExitStack() as es:
        wA_b = sb(es, "wA_b", [128, 8, 3600], BF16)
        wO_b = sb(es, "wO_b", [128, 8, 2560], BF16)
        xs = sb(es, "xs", [128, 2, 8, GS], F32)
        sq = sb(es, "sq", [128, 8, GS], BF16)
        rstd = sb(es, "rstd", [128, GS], F32)
        hT = sb(es, "hT", [128, 8, GS], BF16)
        rc = sb(es, "rc", [32, 1, GS], F32)
        rs = sb(es, "rs", [32, 1, GS], F32)
        kf = sb(es, "kf", [128, 2, GS], F32)
        rt = sb(es, "rt", [32, 2, GS], F32)
        kb = sb(es, "kb", [128, 2, GS], BF16)
        kmean = sb(es, "kmean", [128, 8, 32], F32)
        gkf = sb(es, "gkf", [128, 4, GS], F32)
        gqf = sb(es, "gqf", [128, 4, GS], F32)
        sgr = sb(es, "sgr", [128, 8, GS], F32)
        sgt = sb(es, "sgt", [128, GS], F32)
        glow = sb(es, "glow", [32, GS], F32)
        gw_sb = sb(es, "gw_sb", [32, 512], F32)
        gnw_sb = sb(es, "gnw_sb", [128, 2], F32)
        vfl = sb(es, "vfl", [128, 64], F32)
        Ltok = sb(es, "Ltok", [128, TPG, 512], F32)
        lex = sb(es, "lex", [128, 512], F32)
        vst = sb(es, "vst", [128, TPG, 1024], BF16)
        gvst = sb(es, "gvst", [128, TPG, 1024], BF16)
        EbT = sb(es, "EbT", [128, 2, 128], F32)
        EnbT = sb(es, "EnbT", [128, 2, 128], F32)
        qtl = sb(es, "qtl", [128, 2, 128], BF16)
        ktl = sb(es, "ktl", [128, 2, 128], BF16)
        ktok = sb(es, "ktok", [128, 2, 128], BF16)
        atm = sb(es, "atm", [128, 2, 128], BF16)
        Sst = sb(es, "Sst", [128, 4, 256], F32)
        Ssc = sb(es, "Ssc", [128, 2, 256], F32)
        Sbf = sb(es, "Sbf", [128, 4, 256], BF16)
        osq = sb(es, "osq", [128, 2, 2, 128], BF16)
        orst = sb(es, "orst", [128, 2, 128], F32)
        otmp = sb(es, "otmp", [128, 2, 128], F32)
        obst = sb(es, "obst", [128, 8, GS], BF16)
        p_pj = ps(es, "p_pj", [128, 2, 512], F32)
        p_sx = ps(es, "p_sx", [128, 512], F32)
        p_ss = p_sx[:, 0:GS]
        p_xs = p_sx[0:32, 256:256 + GS]
        p_g = ps(es, "p_g", [128, 2, 4, 128], F32)
        p_km = ps(es, "p_km", [128, 512], F32)
        p_kvb = ps(es, "p_kvb", [128, 2, 256], F32)
        p_tr = ps(es, "p_tr", [128, 2, 128], BF16)
        permf = cst[:, 1, 0:32]
        triN = cst[:, 2, :]
        utm = cst[:, 3, :]

        for k in range(8):
            P.dma("pool", wA_b[:, k, :], wA[k * 128:(k + 1) * 128, :], writes=[("wA", k)], sem=("wA", k))
        for k in range(8):
            P.dma("pool", wO_b[:, k, :], wO[k * 128:(k + 1) * 128, :], writes=[("wO", k)], sem=("wO", k))
        P.dma("sp", gw_sb[:], g["gw_aug"], writes=["gw_sb"])
        P.dma("sp", gnw_sb[:], g["gnw"], writes=["gnw_sb"])
        P.dma("sp", vfl[:], g["vflag"], writes=["vfl"])
        P.op("pool", lambda e: e.memset(glow[:], 1.0), writes=["glow"])
        P.op("pool", lambda e: e.memset(Sst[:], 0.0), writes=[("S", h) for h in range(4)])
        P.op("pool", lambda e: e.memset(Sbf[:], 0.0), writes=[("Sbf", h) for h in range(4)])
        wAk = [("wA", k) for k in range(8)]
        wOk = [("wO", k) for k in range(8)]
        pj_n = [0]
        kf_n = [0]
        uniq = [0]

        def ukey(n):
            uniq[0] += 1
            return (n, uniq[0])

        def proj_fm(wt, col0, ncol, wkeys, evac):
            s_ = pj_n[0] % 2
            pj_n[0] += 1
            for k in range(8):
                P.op("pe", lambda e: e.matmul(p_pj[0:ncol, s_, 0:GS], lhsT=wt[:, k, col0:col0 + ncol], rhs=hT[:, k, :],
                                              start=(k == 0), stop=(k == 7)),
                     reads=wkeys + ["hT"], writes=[("p_pj", s_)], inc=(k == 7))
            flush_evac()
            pend[0] = lambda: evac(p_pj[0:ncol, s_, 0:GS], ("p_pj", s_))

        pend = [None]

        def flush_evac():
            if pend[0] is not None:
                f_ = pend[0]
                pend[0] = None
                f_()

        for gi in range(NG):
            own = gi >= OWNG0
            s = gi % 2
            t0 = gi * GS
            o0 = (gi - OWNG0) * GS
            xk = [("xs", s, k) for k in range(8)]
            P.dma("sp", xs[:, s], xTv[:, t0:t0 + GS].rearrange("(k p) n -> p k n", p=128), writes=xk, sem=("xs", s))
            P.dma("sp", rc[:, 0, :], ropec[:, t0:t0 + GS], writes=[("rc", 0)], sem=("rc", 0))
            P.dma("sp", rs[:, 0, :], ropes[:, t0:t0 + GS], writes=[("rs", 0)], sem=("rs", 0))
            rmsnorm_fm(P, nc, xs[:, s], sq[:], p_ss, rstd[:], ones_rms, xk, "a1", "p_sx")
            for k in range(8):
                P.op("dve", lambda e: e.scalar_tensor_tensor(xs[:, s, k, :], xs[:, s, k, :], a1[:, k:k + 1], rstd[:],
                                                             ALU.mult, ALU.mult),
                     reads=[("xs", s, k), "a1rstd", "a1"], writes=[("xs", s, k)])
                P.op("act", lambda e: e.activation(hT[:, k, :], xs[:, s, k, :], AF.Identity, bias=s1[:, k:k + 1], scale=1.0),
                     reads=[("xs", s, k), "modT"], writes=["hT"])
            if own:
                P.dma("sp", HT_d[:, o0:o0 + GS].rearrange("(k p) n -> p k n", p=128), hT[:], reads=["hT"], writes=[ukey("HT_d")],
                      sem="HT_o")

            def qk_evac(h, is_k):
                def ev(pt, pkey):
                    u = kf_n[0] % 2
                    kf_n[0] += 1
                    P.op("act", lambda e: e.copy(kf[:, u, :], pt), reads=[pkey], writes=[("kf", u)])
                    P.op("pe", lambda e: e.matmul(p_xs, lhsT=permf, rhs=kf[:, u, :], start=True, stop=True),
                         reads=[("kf", u), "cst"], writes=["p_sx"])
                    P.op("dve", lambda e: e.tensor_tensor(rt[:, 0, :], kf[0:32, u, :], rc[:, 0, :], ALU.mult),
                         reads=[("kf", u), ("rc", 0)], writes=["rt0"])
                    P.op("dve", lambda e: e.tensor_tensor(rt[:, 1, :], p_xs, rs[:, 0, :], ALU.mult),
                         reads=["p_sx", ("rs", 0)], writes=["rt1"])
                    P.op("dve", lambda e: e.tensor_tensor(kf[0:32, u, :], rt[:, 0, :], rt[:, 1, :], ALU.add),
                         reads=["rt0", "rt1", ("kf", u)], writes=[("kf", u)])
                    if is_k:
                        P.op("dve", lambda e: e.tensor_reduce(kmean[:, h, gi:gi + 1], kf[:, u, :], AX.X, ALU.add),
                             reads=[("kf", u)], writes=[("kmean", h)])
                    P.op("pool", lambda e: e.tensor_copy(kb[:, u, :], kf[:, u, :]), reads=[("kf", u)], writes=[("kb", u)])
                    if is_k:
                        P.dma("sp", KT_d[h, :, t0:t0 + GS], kb[:, u, :], reads=[("kb", u)], writes=[ukey("KT_d")], sem=("kbo", u))
                    else:
                        P.dma("sp", QT_d[h, :, o0:o0 + GS], kb[:, u, :], reads=[("kb", u)], writes=[ukey("QT_d")], sem=("kbo", u))
                return ev

            for h in range(8):
                proj_fm(wA_b, h * 128, 128, wAk, qk_evac(h, True))
            if own:
                for h in range(8):
                    proj_fm(wO_b, h * 128, 128, wOk, qk_evac(h, False))

            flush_evac()
            for t in range(TPG):
                for half in range(2):
                    sl = pj_n[0] % 2
                    pj_n[0] += 1
                    for k in range(8):
                        P.op("pe", lambda e: e.matmul(p_pj[:, sl, :], lhsT=hT[:, k, t * 128:(t + 1) * 128],
                                                      rhs=wA_b[:, k, 1024 + half * 512:1024 + (half + 1) * 512],
                                                      start=(k == 0), stop=(k == 7)),
                             reads=wAk + ["hT"], writes=[("p_pj", sl)], inc=(k == 7))
                    P.op("act", lambda e: e.copy(vst[:, t, half * 512:(half + 1) * 512], p_pj[:, sl, :]),
                         reads=[("p_pj", sl)], writes=["vst"])
                for half in range(2):
                    sl = pj_n[0] % 2
                    pj_n[0] += 1
                    for k in range(8):
                        P.op("pe", lambda e: e.matmul(p_pj[:, sl, :], lhsT=hT[:, k, t * 128:(t + 1) * 128],
                                                      rhs=wA_b[:, k, 2560 + half * 512:2560 + (half + 1) * 512],
                                                      start=(k == 0), stop=(k == 7)),
                             reads=wAk + ["hT"], writes=[("p_pj", sl)], inc=(k == 7))
                    P.op("dve", lambda e: e.tensor_scalar(gvst[:, t, half * 512:(half + 1) * 512], p_pj[:, sl, :],
                                                          vfl[:, gi * TPG + t:gi * TPG + t + 1], None, ALU.mult),
                         reads=[("p_pj", sl), "vfl"], writes=[("gvst", t)])
            P.dma("sp", V_d[t0:t0 + GS, :].rearrange("(t p) n -> p t n", p=128), vst[:], reads=["vst"], writes=[ukey("V_d")],
                  sem="V_o")

            for h in range(4):
                proj_fm(wA_b, 2048 + h * 128, 128, wAk,
                        lambda pt, pkey, h=h: P.op("act", lambda e: e.copy(gkf[:, h, :], pt), reads=[pkey], writes=[("gkf", h)]))
            proj_fm(wA_b, 3584, 16, wAk,
                    lambda pt, pkey: P.op("act", lambda e: e.copy(glow[0:16, :], pt), reads=[pkey], writes=["glow"]))
            if own:
                for h in range(4):
                    proj_fm(wO_b, 1024 + h * 128, 128, wOk,
                            lambda pt, pkey, h=h: P.op("act", lambda e: e.copy(gqf[:, h, :], pt), reads=[pkey], writes=[("gqf", h)]))
                for c in range(8):
                    def gr_ev(pt, pkey, c=c):
                        P.op("act", lambda e: e.activation(sgt[:], pt, AF.Exp, scale=-1.0), reads=[pkey], writes=["sgt"])
                        P.op("dve", lambda e: e.tensor_scalar(sgt[:], sgt[:], 1.0, None, ALU.add), reads=["sgt"], writes=["sgt"])
                        P.op("dve", lambda e: e.reciprocal(sgt[:], sgt[:]), reads=["sgt"], writes=["sgt"])
                        P.op("dve", lambda e: e.tensor_tensor(sgr[:, c, :], sgt[:], pt, ALU.mult), reads=["sgt", pkey], writes=[("sgr", c)])
                    proj_fm(wO_b, 1536 + c * 128, 128, wOk, gr_ev)
            flush_evac()
            for t in range(TPG):
                sl = pj_n[0] % 2
                pj_n[0] += 1
                P.op("pe", lambda e: e.matmul(p_pj[:, sl, :], lhsT=glow[:, t * 128:(t + 1) * 128], rhs=gw_sb[:],
                                              start=True, stop=True),
                     reads=["glow", "gw_sb"], writes=[("p_pj", sl)])
                P.op("act", lambda e: e.activation(lex[:], p_pj[:, sl, :], AF.Exp, scale=-1.0), reads=[("p_pj", sl)], writes=["lex"])
                P.op("act", lambda e: e.activation(Ltok[:, t, :], lex[:], AF.Ln, bias=1.0, scale=1.0), reads=["lex"], writes=[("Ltok", t)])

            def gla_chain(t, h):
                u = h % 2
                c0 = t * 128
                P.op("pe", lambda e: e.matmul(p_g[:, u, 0, :], lhsT=Ltok[:, t, h * 128:(h + 1) * 128], rhs=triN,
                                              start=True, stop=True),
                     reads=[("Ltok", t), "cst"], writes=[("p_g", u)])
                yield
                P.op("act", lambda e: e.activation(EbT[:, u, :], p_g[:, u, 0, :], AF.Exp), reads=[("p_g", u)], writes=[("EbT", u)])
                P.op("act", lambda e: e.activation(EnbT[:, u, :], p_g[:, u, 0, :], AF.Exp, scale=-1.0),
                     reads=[("p_g", u)], writes=[("EnbT", u)])
                yield
                P.op("dve", lambda e: e.tensor_tensor(ktl[:, u, :], gkf[:, h, c0:c0 + 128], EnbT[:, u, :], ALU.mult),
                     reads=[("gkf", h), ("EnbT", u)], writes=[("ktl", u)])
                yield
                P.op("pe", lambda e: e.transpose(p_tr[:, u, :], ktl[:, u, :], ident_b[:]),
                     reads=[("ktl", u), "ident_b"], writes=["p_tr"])
                yield
                P.op("act", lambda e: e.copy(ktok[:, u, :], p_tr[:, u, :]), reads=["p_tr"], writes=[("ktok", u)])
                P.op("pe", lambda e: e.matmul(p_kvb[:, u, :], lhsT=ktok[:, u, :], rhs=gvst[:, t, h * 256:(h + 1) * 256],
                                              start=True, stop=True),
                     reads=[("ktok", u), ("gvst", t)], writes=["p_kvb"])
                yield
                if own:
                    P.op("dve", lambda e: e.scalar_tensor_tensor(qtl[:, u, :], gqf[:, h, c0:c0 + 128], 128.0 ** -0.5,
                                                                 EbT[:, u, :], ALU.mult, ALU.mult),
                         reads=[("gqf", h), ("EbT", u)], writes=[("qtl", u)])
                    yield
                    P.op("pe", lambda e: e.matmul(p_g[:, u, 1, :], lhsT=ktl[:, u, :], rhs=qtl[:, u, :], start=True, stop=True),
                         reads=[("ktl", u), ("qtl", u)], writes=[("p_g", u)])
                    yield
                    P.op("dve", lambda e: e.tensor_tensor(atm[:, u, :], p_g[:, u, 1, :], utm, ALU.mult),
                         reads=[("p_g", u), "cst"], writes=[("atm", u)])
                    yield
                    for dv in range(2):
                        P.op("pe", lambda e: e.matmul(p_g[:, u, 2 + dv, :],
                                                      lhsT=gvst[:, t, h * 256 + dv * 128:h * 256 + (dv + 1) * 128],
                                                      rhs=atm[:, u, :], start=True, stop=False),
                             reads=[("gvst", t), ("atm", u)], writes=[("p_g", u)], inc=False)
                        yield
                        P.op("pe", lambda e: e.matmul(p_g[:, u, 2 + dv, :], lhsT=Sbf[:, h, dv * 128:(dv + 1) * 128],
                                                      rhs=qtl[:, u, :], start=False, stop=True),
                             reads=[("Sbf", h), ("qtl", u)], writes=[("p_g", u)])
                        yield
                        P.op("act", lambda e: e.activation(osq[:, u, dv, :], p_g[:, u, 2 + dv, :], AF.Square),
                             reads=[("p_g", u)], writes=[("osq", u, dv)])
                        yield
                    for dv in range(2):
                        P.op("pe", lambda e: e.matmul(p_km[:, 256 + u * 128:256 + (u + 1) * 128], lhsT=ones_256[:], rhs=osq[:, u, dv, :],
                                                      start=(dv == 0), stop=(dv == 1)),
                             reads=[("osq", u, dv), "ones_256"], writes=["p_km"], inc=(dv == 1))
                    rsqrt_act(P, orst[:, u, :], p_km[:, 256 + u * 128:256 + (u + 1) * 128], ["p_km"], ("orst", u), 1.0)
                    for dv in range(2):
                        P.op("dve", lambda e: e.scalar_tensor_tensor(otmp[:, u, :], p_g[:, u, 2 + dv, :], gnw_sb[:, dv:dv + 1],
                                                                     orst[:, u, :], ALU.mult, ALU.mult),
                             reads=[("p_g", u), ("orst", u), "gnw_sb"], writes=[("otmp", u)])
                        yield
                        P.op("dve", lambda e: e.tensor_tensor(obst[:, h * 2 + dv, c0:c0 + 128], otmp[:, u, :],
                                                              sgr[:, h * 2 + dv, c0:c0 + 128], ALU.mult),
                             reads=[("otmp", u), ("sgr", h * 2 + dv)], writes=["obst"])
                        yield
                P.op("pool", lambda e: e.tensor_scalar(Ssc[:, u, :], Sst[:, h, :], EbT[:, u, 127:128], None, ALU.mult),
                     reads=[("S", h), ("EbT", u)], writes=[("Ssc", u)])
                yield
                P.op("dve", lambda e: e.scalar_tensor_tensor(Sst[:, h, :], p_kvb[:, u, :], EbT[:, u, 127:128], Ssc[:, u, :],
                                                             ALU.mult, ALU.add),
                     reads=["p_kvb", ("EbT", u), ("Ssc", u)], writes=[("S", h)])
                yield
                P.op("act", lambda e: e.copy(Sbf[:, h, :], Sst[:, h, :]), reads=[("S", h)], writes=[("Sbf", h)])

            for t in range(TPG):
                for hp in (0, 2):
                    gens = [gla_chain(t, hp), gla_chain(t, hp + 1)]
                    while gens:
                        for g_ in list(gens):
                            try:
                                next(g_)
                            except StopIteration:
                                gens.remove(g_)
            if own:
                P.dma("sp", OB_d[:, o0:o0 + GS].rearrange("(k p) n -> p k n", p=128), obst[:], reads=["obst"], writes=[ukey("OB_d")],
                      sem="OB_o")
        P.op("dve", lambda e: e.tensor_copy(kmb[:], kmean[:]), reads=[("kmean", h) for h in range(8)], writes=["kmb"])
        if DEBUG:
            P.wait_all("sp")
            P.dma("sp", dbg["kT"], KT_d, reads=[])
            P.dma("sp", dbg["qT"], QT_d, reads=[])
            P.dma("sp", dbg["ob"], OB_d, reads=[])
        P.barrier()


def phase_a2(nc, P, L):
    g = L
    sb, ps = g["sb"], g["ps"]
    KT_d, V_d, QT_d = g["KT_d"], g["V_d"], g["QT_d"]
    cst, ident_b, ident_f, ones_1, oaT, kmb = g["cst"], g["ident_b"], g["ident_f"], g["ones_1"], g["oaT"], g["kmb"]
    scale = 128.0 ** -0.5
    with ExitStack() as es:
        KT = sb(es, "KT", [128, 2, SEQ], BF16)
        Vh = sb(es, "Vh", [128, 2, 64, 128], BF16)
        QT = sb(es, "QT", [128, 2, OWN], BF16)
        esel = sb(es, "esel", [32, 32, 128], BF16)
        cmask = sb(es, "cmask", [128, 2, 256], BF16)
        gm_sb = sb(es, "gm_sb", [128, 8, 32], F32)
        gv_sb = sb(es, "gv_sb", [128, 8, 32], F32)
        gt = sb(es, "gt", [128, 2, 32], F32)
        top8 = sb(es, "top8", [128, 2, 8], F32)
        mbT = sb(es, "mbT", [32, 2, 256], BF16)
        PT = sb(es, "PT", [128, 4, 256], BF16)
        rden = sb(es, "rden", [128, 256], F32)
        p_st = ps(es, "p_st", [128, 4, 512], F32)
        p_od = ps(es, "p_od", [128, 2, 512], F32)
        p_gm = ps(es, "p_gm", [128, 512], F32)
        p_gt = p_gm[:, 0:64].rearrange("p (a b) -> p a b", a=2)
        p_mb = p_gm[0:32, 256:512]
        P.dma("pool", esel[:].rearrange("p a b -> p (a b)"), g["esel_in"], writes=["esel"])
        P.dma("pool", cmask[:], cst_dram_causal(g), writes=["cmask"])
        P.dma("sp", gm_sb[:].rearrange("p a b -> p (a b)"), g["gmask"], writes=["gm_sb"])
        P.dma("sp", gv_sb[:].rearrange("p a b -> p (a b)"), g["gvalid"], writes=["gv_sb"])
        n_st = [0]
        n_blk = [0]
        for h in range(8):
            hs = h % 2
            P.dma("sp", KT[:, hs, :], KT_d[h], writes=[("KT", hs)], sem=("KT", hs))
            P.dma("sp", Vh[:, hs], V_d[:, h * 128:(h + 1) * 128].rearrange("(t p) d -> p t d", p=128),
                  writes=[("Vh", hs)], sem=("Vh", hs))
            P.dma("sp", QT[:, hs, :], QT_d[h], writes=[("QT", hs)], sem=("QT", hs))
            for l in range(8):
                bs = n_blk[0] % 2
                n_blk[0] += 1
                q0 = l * 256
                for qt in range(2):
                    P.op("pe", lambda e: e.matmul(p_gt[:, qt, :], lhsT=QT[:, hs, q0 + qt * 128:q0 + (qt + 1) * 128],
                                                  rhs=kmb[:, h, :], start=True, stop=True),
                         reads=[("QT", hs), "kmb"], writes=["p_gm"])
                    P.op("dve", lambda e: e.tensor_tensor(gt[:, qt, :], p_gt[:, qt, :], gm_sb[:, l, :], ALU.add),
                         reads=["p_gm", "gm_sb"], writes=[("gt", qt)])
                    P.op("dve", lambda e: e.max(top8[:, qt, :], gt[:, qt, :]), reads=[("gt", qt)], writes=[("top8", qt)])
                    P.op("dve", lambda e: e.tensor_scalar(gt[:, qt, :], gt[:, qt, :], top8[:, qt, 2:3], None, ALU.is_ge),
                         reads=[("gt", qt), ("top8", qt)], writes=[("gt", qt)])
                    P.op("dve", lambda e: e.tensor_tensor(gt[:, qt, :], gt[:, qt, :], gv_sb[:, l, :], ALU.mult),
                         reads=[("gt", qt), "gv_sb"], writes=[("gt", qt)])
                    P.op("dve", lambda e: e.tensor_scalar(gt[:, qt, :], gt[:, qt, :], BIG, -BIG, ALU.mult, ALU.add),
                         reads=[("gt", qt)], writes=[("gt", qt)])
                    P.op("pe", lambda e: e.transpose(p_mb[:, qt * 128:(qt + 1) * 128], gt[:, qt, :], ident_f),
                         reads=[("gt", qt), "cst"], writes=["p_gm"])
                P.op("dve", lambda e: e.tensor_copy(mbT[:, bs, :], p_mb), reads=["p_gm"], writes=[("mbT", bs)])
                nkv = 24 + l
                pairs = [(v, half) for v in range(nkv + 1) for half in range(2)]
                for i, (v, half) in enumerate(pairs):
                    st = n_st[0] % 4
                    n_st[0] += 1
                    k0 = v * 256 + half * 128
                    P.op("pe", lambda e: e.matmul(p_st[:, st, 0:256], lhsT=KT[:, hs, k0:k0 + 128], rhs=QT[:, hs, q0:q0 + 256],
                                                  start=True, stop=False),
                         reads=[("KT", hs), ("QT", hs)], writes=[("p_st", st)], inc=False)
                    if v < nkv:
                        P.op("pe", lambda e: e.matmul(p_st[:, st, 0:256], lhsT=esel[:, v, :], rhs=mbT[:, bs, :], start=False, stop=True),
                             reads=["esel", ("mbT", bs)], writes=[("p_st", st)])
                    else:
                        P.op("pe", lambda e: e.matmul(p_st[:, st, 0:256], lhsT=ident_b[:], rhs=cmask[:, half, :], start=False, stop=True),
                             reads=["ident_b", "cmask"], writes=[("p_st", st)])
                    P.op("act", lambda e: e.activation(PT[:, st, :], p_st[:, st, 0:256], AF.Exp, scale=scale),
                         reads=[("p_st", st)], writes=[("PT", st)])
                    last = (i == len(pairs) - 1)
                    P.op("pe", lambda e: e.matmul(p_od[:, 0, 0:256], lhsT=Vh[:, hs, v * 2 + half, :], rhs=PT[:, st, :],
                                                  start=(i == 0), stop=last),
                         reads=[("Vh", hs), ("PT", st)], writes=["p_od"], inc=False)
                    P.op("pe", lambda e: e.matmul(p_od[:, 1, 0:256], lhsT=ones_1[:], rhs=PT[:, st, :],
                                                  start=(i == 0), stop=last),
                         reads=["ones_1", ("PT", st)], writes=["p_od"], inc=True)
                P.op("dve", lambda e: e.reciprocal(rden[:], p_od[:, 1, 0:256]), reads=["p_od"], writes=["rden"])
                P.op("dve", lambda e: e.tensor_tensor(oaT[:, h, q0:q0 + 256], p_od[:, 0, 0:256], rden[:], ALU.mult),
                     reads=["p_od", "rden"], writes=[("oaT", h)])
        if DEBUG:
            P.dma("sp", g["dbg"]["oa"], oaT[:], reads=[("oaT", h) for h in range(8)])
        P.barrier()


def cst_dram_causal(g):
    return g["consts"][:, 5:9, :].rearrange("p (a b) c -> p a (b c)", a=2)


def phase_b(nc, P, L):
    g = L
    sb, ps = g["sb"], g["ps"]
    oaT, ones_rms, ones_1, ident_b, ident_f, cst = g["oaT"], g["ones_rms"], g["ones_1"], g["ident_b"], g["ident_f"], g["cst"]
    a2, s2, g1, gate1_bc = g["a2"], g["s2"], g["g1"], g["gate1_bc"]
    HT_d, OB_d, X1_d, XG_d = g["HT_d"], g["OB_d"], g["X1_d"], g["XG_d"]
    xTv, xtok = g["xTv"], g["xtok"]
    dest_i, w4, RWT = g["dest_i"], g["w4"], g["RWT"]
    with ExitStack() as es:
        wG_b = sb(es, "wG_b", [128, 8, 2048], BF16)
        woa_b = sb(es, "woa_b", [128, 8, D], BF16)
        wob_b = sb(es, "wob_b", [128, 8, D], BF16)
        wout_b = sb(es, "wout_b", [128, 8, D], BF16)
        nbm_sb = sb(es, "nbm_sb", [128, 16], F32)
        wr_sb = sb(es, "wr_sb", [128, 8, NE], F32)
        br_sb = sb(es, "br_sb", [128, NE], F32)
        ecap_sb = sb(es, "ecap_sb", [128, NE], F32)
        hTg = sb(es, "hTg", [128, 8, GS], BF16)
        obg = sb(es, "obg", [128, 8, GS], BF16)
        xo = sb(es, "xo", [128, 8, GS], F32)
        ge = sb(es, "ge", [128, GS], F32)
        mf = sb(es, "mf", [128, GS], F32)
        mt2 = sb(es, "mt2", [128, GS], F32)
        mT = sb(es, "mT", [128, 8, GS], BF16)
        sq = sb(es, "sqb", [128, 8, GS], BF16)
        rstd = sb(es, "rstdb", [128, GS], F32)
        h2f = sb(es, "h2f", [128, 8, GS], F32)
        h2b = sb(es, "h2b", [128, 8, GS], BF16)
        xt = sb(es, "xt", [128, D], F32)
        x1t = sb(es, "x1t", [128, D], F32)
        h2tok = sb(es, "h2tok", [128, D], BF16)
        lg = sb(es, "lg", [128, NE], F32)
        t8 = sb(es, "t8", [128, 8], F32)
        nmax = sb(es, "nmax", [128, 1], F32)
        den = sb(es, "den", [128, 1], F32)
        sel = sb(es, "sel", [128, NE], F32)
        selb = sb(es, "selb", [128, NE], BF16)
        carry = sb(es, "carry", [128, NE], F32)
        slot = sb(es, "slot", [128, NE], F32)
        oh = sb(es, "oh", [128, NE], F32)
        destf = sb(es, "destf", [128, 4], F32)
        RW = sb(es, "RW", [128, NE], F32)
        stri = sb(es, "stri", [128, 128], BF16)
        p_a = ps(es, "pb_a", [128, 2, 512], F32)
        p_b = ps(es, "pb_b", [128, 2, 512], F32)
        p_ss = ps(es, "pb_ss", [128, GS], F32)
        p_lg = ps(es, "pb_lg", [128, 2, NE], F32)
        p_tr = ps(es, "pb_tr", [128, 2, 512], BF16)
        p_rw = ps(es, "pb_rw", [32, 128], F32)
        for k in range(8):
            P.dma("pool", wG_b[:, k, :], g["wG"][k * 128:(k + 1) * 128, :], writes=[("wG", k)], sem=("wG", k))
            P.dma("pool", woa_b[:, k, :], g["w_oa"][k * 128:(k + 1) * 128, :], writes=[("woa", k)], sem=("woa", k))
            P.dma("pool", wob_b[:, k, :], g["w_ob"][k * 128:(k + 1) * 128, :], writes=[("wob", k)], sem=("wob", k))
            P.dma("pool", wout_b[:, k, :], g["w_out"][k * 128:(k + 1) * 128, :], writes=[("wout", k)], sem=("wout", k))
        P.dma("pool", stri[:], g["consts"][:, 4, :], writes=["stri"])
        P.dma("sp", nbm_sb[:], g["nbm"], writes=["nbm"])
        P.dma("sp", wr_sb[:], g["w_r"].rearrange("(k p) n -> p k n", p=128), writes=["wr"])
        P.dma("sp", br_sb[:], g["b_r_bc"], writes=["br"])
        P.dma("sp", ecap_sb[:], g["ecap"], writes=["ecap"])
        P.op("pool", lambda e: e.memset(carry[:], 0.0), writes=["carry"])
        P.op("dve", lambda e: e.tensor_scalar(nbm_sb[:], nbm_sb[:], -1.0, None, ALU.mult), reads=["nbm"], writes=["nbm"])
        kk = lambda n: [(n, k) for k in range(8)]
        for G in range(OWN // GS):
            o0 = G * GS
            P.dma("sp", hTg[:], HT_d[:, o0:o0 + GS].rearrange("(k p) n -> p k n", p=128), writes=["hTg"])
            P.dma("sp", obg[:], OB_d[:, o0:o0 + GS].rearrange("(k p) n -> p k n", p=128), writes=["obg"])
            P.dma("sp", xo[:], xTv[:, 6144 + o0:6144 + o0 + GS].rearrange("(k p) n -> p k n", p=128), writes=["xo"])
            for c in range(8):
                for br in range(2):
                    wy = woa_b if br == 0 else wob_b
                    wyk = kk("woa") if br == 0 else kk("wob")
                    for k in range(8):
                        P.op("pe", lambda e: e.matmul(p_a[:, br, 0:GS], lhsT=wG_b[:, k, br * 1024 + c * 128:br * 1024 + (c + 1) * 128],
                                                      rhs=hTg[:, k, :], start=(k == 0), stop=(k == 7)),
                             reads=kk("wG") + ["hTg"], writes=[("p_a", br)], inc=(k == 7))
                    for k in range(8):
                        rhs = oaT[:, k, o0:o0 + GS] if br == 0 else obg[:, k, :]
                        P.op("pe", lambda e: e.matmul(p_b[:, br, 0:GS], lhsT=wy[:, k, c * 128:(c + 1) * 128], rhs=rhs,
                                                      start=(k == 0), stop=(k == 7)),
                             reads=wyk + (["obg"] if br else []), writes=[("p_b", br)], inc=(k == 7))
                    P.op("act", lambda e: e.activation(ge[:], p_a[:, br, 0:GS], AF.Exp, bias=nbm_sb[:, br * 8 + c:br * 8 + c + 1], scale=-1.0),
                         reads=[("p_a", br), "nbm"], writes=["ge"])
                    P.op("dve", lambda e: e.tensor_scalar(ge[:], ge[:], 1.0, None, ALU.add), reads=["ge"], writes=["ge"])
                    P.op("dve", lambda e: e.reciprocal(ge[:], ge[:]), reads=["ge"], writes=["ge"])
                    if br == 0:
                        P.op("dve", lambda e: e.tensor_tensor(mf[:], ge[:], p_b[:, br, 0:GS], ALU.mult), reads=["ge", ("p_b", br)], writes=["mf"])
                    else:
                        P.op("dve", lambda e: e.tensor_tensor(mt2[:], ge[:], p_b[:, br, 0:GS], ALU.mult), reads=["ge", ("p_b", br)], writes=["mt2"])
                        P.op("dve", lambda e: e.tensor_tensor(mT[:, c, :], mf[:], mt2[:], ALU.add), reads=["mf", "mt2"], writes=["mT"])
            for c in range(8):
                for k in range(8):
                    P.op("pe", lambda e: e.matmul(p_a[:, c % 2, 0:GS], lhsT=wout_b[:, k, c * 128:(c + 1) * 128], rhs=mT[:, k, :],
                                                  start=(k == 0), stop=(k == 7)),
                         reads=kk("wout") + ["mT"], writes=[("p_a", c % 2)], inc=(k == 7))
                P.op("dve", lambda e: e.scalar_tensor_tensor(xo[:, c, :], p_a[:, c % 2, 0:GS], g1[:, c:c + 1], xo[:, c, :], ALU.mult, ALU.add),
                     reads=[("p_a", c % 2), "xo"], writes=["xo"])
            for t in range(TPG):
                T = G * TPG + t
                P.dma("sp", xt[:], xtok[T * 128:(T + 1) * 128, :], writes=["xt"])
                for half in range(2):
                    for k in range(8):
                        P.op("pe", lambda e: e.matmul(p_b[:, half, :], lhsT=mT[:, k, t * 128:(t + 1) * 128],
                                                      rhs=wout_b[:, k, half * 512:(half + 1) * 512], start=(k == 0), stop=(k == 7)),
                             reads=kk("wout") + ["mT"], writes=[("p_b", half)], inc=(k == 7))
                    P.op("dve", lambda e: e.tensor_tensor(x1t[:, half * 512:(half + 1) * 512], p_b[:, half, :],
                                                          gate1_bc[:, half * 512:(half + 1) * 512], ALU.mult),
                         reads=[("p_b", half)], writes=["x1t"])
                P.op("pool", lambda e: e.tensor_tensor(x1t[:], x1t[:], xt[:], ALU.add), reads=["x1t", "xt"], writes=["x1t"])
                P.dma("sp", X1_d[T * 128:(T + 1) * 128, :], x1t[:], reads=["x1t"], writes=[("X1_d", T)], sem="X1_o")
            rmsnorm_fm(P, nc, xo[:], sq[:], p_ss[:], rstd[:], ones_rms, ["xo"], "b", "pb_ss")
            for k in range(8):
                P.op("dve", lambda e: e.scalar_tensor_tensor(xo[:, k, :], xo[:, k, :], a2[:, k:k + 1], rstd[:], ALU.mult, ALU.mult),
                     reads=["xo", "brstd"], writes=["xo"])
                P.op("act", lambda e: e.activation(h2f[:, k, :], xo[:, k, :], AF.Identity, bias=s2[:, k:k + 1], scale=1.0),
                     reads=["xo"], writes=["h2f"])
            P.op("pool", lambda e: e.tensor_copy(h2b[:], h2f[:]), reads=["h2f"], writes=["h2b"])
            for t in range(TPG):
                T = G * TPG + t
                lq = t
                for k in range(8):
                    P.op("pe", lambda e: e.matmul(p_lg[:, lq, :], lhsT=h2f[:, k, t * 128:(t + 1) * 128], rhs=wr_sb[:, k, :],
                                                  start=(k == 0), stop=(k == 7)),
                         reads=["h2f", "wr"], writes=["p_lg"], inc=(k == 7))
                P.op("dve", lambda e: e.tensor_tensor(lg[:], p_lg[:, lq, :], br_sb[:], ALU.add), reads=["p_lg", "br"], writes=["lg"])
                if DEBUG:
                    P.dma("sp", g["dbg"]["lg"][:, T, :], lg[:], reads=["lg"], sem="dbg_lg")
                P.op("dve", lambda e: e.max(t8[:], lg[:]), reads=["lg"], writes=["t8"])
                P.op("dve", lambda e: e.tensor_scalar(nmax[:], t8[:, 0:1], -1.0, None, ALU.mult), reads=["t8"], writes=["nmax"])
                P.op("act", lambda e: e.activation(w4[:, T, :], t8[:, 0:4], AF.Exp, bias=nmax[:], scale=1.0),
                     reads=["t8", "nmax"], writes=[("w4", T)])
                P.op("dve", lambda e: e.tensor_reduce(den[:], w4[:, T, :], AX.X, ALU.add), reads=[("w4", T)], writes=["den"])
                P.op("dve", lambda e: e.reciprocal(den[:], den[:]), reads=["den"], writes=["den"])
                P.op("dve", lambda e: e.tensor_scalar(w4[:, T, :], w4[:, T, :], den[:], None, ALU.mult), reads=[("w4", T), "den"], writes=[("w4", T)])
                P.op("dve", lambda e: e.tensor_scalar(sel[:], lg[:], t8[:, 3:4], None, ALU.is_ge), reads=["lg", "t8"], writes=["sel"])
                P.op("dve", lambda e: e.tensor_copy(selb[:], sel[:]), reads=["sel"], writes=["selb"])
                P.op("pe", lambda e: e.matmul(p_lg[:, lq, :], lhsT=stri[:], rhs=selb[:], start=True, stop=True),
                     reads=["stri", "selb"], writes=["p_lg"])
                P.op("dve", lambda e: e.tensor_tensor(slot[:], p_lg[:, lq, :], carry[:], ALU.add), reads=["p_lg", "carry"], writes=["slot"])
                P.op("pe", lambda e: e.matmul(p_lg[:, lq, :], lhsT=ones_1[:], rhs=selb[:], start=True, stop=True),
                     reads=["selb"], writes=["p_lg"])
                P.op("dve", lambda e: e.tensor_tensor(carry[:], p_lg[:, lq, :], carry[:], ALU.add), reads=["p_lg", "carry"], writes=["carry"])
                P.op("dve", lambda e: e.tensor_tensor(slot[:], slot[:], ecap_sb[:], ALU.add), reads=["slot", "ecap"], writes=["slot"])
                P.op("pool", lambda e: e.memset(RW[:], 0.0), writes=["RW"])
                for k4 in range(4):
                    P.op("dve", lambda e: e.tensor_scalar(oh[:], lg[:], t8[:, k4:k4 + 1], None, ALU.is_equal), reads=["lg", "t8"], writes=["oh"])
                    P.op("dve", lambda e: e.scalar_tensor_tensor(RW[:], oh[:], w4[:, T, k4:k4 + 1], RW[:], ALU.mult, ALU.add),
                         reads=["oh", ("w4", T), "RW"], writes=["RW"])
                    P.op("dve", lambda e: e.tensor_tensor(oh[:], oh[:], slot[:], ALU.mult), reads=["oh", "slot"], writes=["oh"])
                    P.op("dve", lambda e: e.tensor_reduce(destf[:, k4:k4 + 1], oh[:], AX.X, ALU.add), reads=["oh"], writes=["destf"])
                P.op("dve", lambda e: e.tensor_copy(dest_i[:, T * 4:T * 4 + 4], destf[:]), reads=["destf"], writes=[("dest_i", T)])
                P.op("pe", lambda e: e.transpose(p_rw[:], RW[:], ident_f), reads=["RW"], writes=["p_rw"])
                P.op("act", lambda e: e.copy(RWT[:, T, :], p_rw[:]), reads=["p_rw"], writes=[("RWT", T)])
                for hf in range(2):
                    for k in range(4):
                        P.op("pe", lambda e: e.transpose(p_tr[:, hf, k * 128:(k + 1) * 128], h2b[:, hf * 4 + k, t * 128:(t + 1) * 128], ident_b[:]),
                             reads=["h2b"], writes=["p_tr"], inc=(k == 3))
                    P.op("act", lambda e: e.copy(h2tok[:, hf * 512:(hf + 1) * 512], p_tr[:, hf, :]), reads=["p_tr"], writes=["h2tok"])
                for k4 in range(4):
                    P.dma("pool", None, None, reads=["h2tok", ("dest_i", T)], writes=["XG_d"], sem=("xgs", k4),
                          emit=lambda e: e.indirect_dma_start(
                              out=XG_d[:, :], out_offset=bass.IndirectOffsetOnAxis(ap=dest_i[:, T * 4 + k4:T * 4 + k4 + 1], axis=0),
                              in_=h2tok[:, :], in_offset=None, bounds_check=g["bc_reg"], oob_is_err=False))
        if DEBUG:
            P.wait_all("sp")
            P.dma("sp", g["dbg"]["x1"], X1_d, reads=[])
        P.barrier()


def phase_moe(nc, P, L):
    g = L
    sb, ps = g["sb"], g["ps"]
    ident_b, gate2_bc = g["ident_b"], g["gate2_bc"]
    XG_d, OUT_d, X1_d = g["XG_d"], g["OUT_d"], g["X1_d"]
    w1d, w2, out = g["w1d"], g["w2"], g["out"]
    dest_i, w4, RWT = g["dest_i"], g["w4"], g["RWT"]
    NCH = CAP // 512
    with ExitStack() as es:
        wp = sb(es, "wp", [128, 12, 8, 512], BF16)
        b1 = sb(es, "b1", [128, NE, 16], F32)
        xg = sb(es, "xg", [128, 2, 4, D], BF16)
        xgT = sb(es, "xgT", [128, 2, 8, 512], BF16)
        actT = sb(es, "actT", [128, 2, 8, 512], BF16)
        gg = sb(es, "gg", [128, 2, 512], F32)
        ll = sb(es, "ll", [128, 2, 512], F32)
        ee = sb(es, "ee", [128, 2, 512], F32)
        orow = sb(es, "orow", [128, 4, D], F32)
        p_h = ps(es, "pm_h", [128, 4, 512], F32)
        p_o = ps(es, "pm_o", [128, 2, 512], F32)
        p_t = ps(es, "pm_t", [128, 2, 512], BF16)
        P.dma("sp", b1[:].rearrange("p a b -> p (a b)"), g["b1T"], writes=["b1"])

        def load_expert(e):
            base = (e % 2) * 6
            for p in range(4):
                for two in range(2):
                    src = w1d[e][:, two * 1024 + p * 256:two * 1024 + (p + 1) * 256]
                    P.dma("pool", wp[:, base + p, :, two * 256:(two + 1) * 256], src.rearrange("(k p) f -> p k f", p=128),
                          writes=[("wp", base + p, two)], sem=("wp", base + p, two))
            for hf in range(2):
                src = w2[e][:, hf * 512:(hf + 1) * 512]
                P.dma("pool", wp[:, base + 4 + hf], src.rearrange("(k p) n -> p k n", p=128),
                      writes=[("wp", base + 4 + hf, 0)], sem=("wp", base + 4 + hf, 0))

        load_expert(0)
        nch = [0]
        for e in range(NE):
            base = (e % 2) * 6
            if e + 1 < NE:
                load_expert(e + 1)
            for ch in range(NCH):
                u = nch[0] % 2
                nch[0] += 1
                r0 = e * CAP + ch * 512
                P.dma("sp", xg[:, u], XG_d[r0:r0 + 512, :].rearrange("(r p) d -> p r d", p=128),
                      writes=[("xg", u)], sem=("xg", u))
                for r in range(4):
                    for hf in range(2):
                        for k in range(4):
                            P.op("pe", lambda e_: e_.transpose(p_t[:, hf, k * 128:(k + 1) * 128],
                                                               xg[:, u, r, (hf * 4 + k) * 128:(hf * 4 + k + 1) * 128], ident_b[:]),
                                 reads=[("xg", u)], writes=["p_t"], inc=(k == 3))
                        P.op("act", lambda e_: e_.copy(xgT[:, u, hf * 4:(hf + 1) * 4, r * 128:(r + 1) * 128],
                                                       p_t[:, hf, :].rearrange("p (k c) -> p k c", k=4)),
                             reads=["p_t"], writes=[("xgT", u)])
                for j in range(8):
                    sl = base + j // 2
                    hs = j % 2
                    for part in range(2):
                        c0 = part * 256 + (j % 2) * 128
                        for k in range(8):
                            P.op("pe", lambda e_: e_.matmul(p_h[:, hs * 2 + part, :], lhsT=wp[:, sl, k, c0:c0 + 128], rhs=xgT[:, u, k, :],
                                                            start=(k == 0), stop=(k == 7)),
                                 reads=[("wp", sl, part), ("xgT", u)], writes=[("p_h", hs, part)], inc=(k == 7))
                    P.op("dve", lambda e_: e_.tensor_scalar(gg[:, hs, :], p_h[:, hs * 2, :], b1[:, e, j:j + 1], 7.0, ALU.add, ALU.min),
                         reads=[("p_h", hs, 0), "b1"], writes=[("gg", hs)])
                    P.op("dve", lambda e_: e_.tensor_scalar(ll[:, hs, :], p_h[:, hs * 2 + 1, :], b1[:, e, 8 + j:9 + j], 7.0, ALU.add, ALU.min),
                         reads=[("p_h", hs, 1), "b1"], writes=[("ll", hs)])
                    P.op("pool", lambda e_: e_.tensor_scalar(ll[:, hs, :], ll[:, hs, :], -7.0, 1.0, ALU.max, ALU.add),
                         reads=[("ll", hs)], writes=[("ll", hs)])
                    P.op("act", lambda e_: e_.activation(ee[:, hs, :], gg[:, hs, :], AF.Exp, scale=-1.702), reads=[("gg", hs)], writes=[("ee", hs)])
                    P.op("pool", lambda e_: e_.tensor_scalar(ee[:, hs, :], ee[:, hs, :], 1.0, None, ALU.add), reads=[("ee", hs)], writes=[("ee", hs)])
                    P.op("dve", lambda e_: e_.reciprocal(ee[:, hs, :], ee[:, hs, :]), reads=[("ee", hs)], writes=[("ee", hs)])
                    P.op("pool", lambda e_: e_.tensor_tensor(gg[:, hs, :], gg[:, hs, :], ll[:, hs, :], ALU.mult),
                         reads=[("gg", hs), ("ll", hs)], writes=[("gg", hs)])
                    P.op("dve", lambda e_: e_.tensor_tensor(actT[:, u, j, :], gg[:, hs, :], ee[:, hs, :], ALU.mult),
                         reads=[("gg", hs), ("ee", hs)], writes=[("actT", u)])
                for hf in range(2):
                    sl = base + 4 + hf
                    for r in range(4):
                        os_ = (hf * 4 + r) % 2
                        for k in range(8):
                            P.op("pe", lambda e_: e_.matmul(p_o[:, os_, :], lhsT=actT[:, u, k, r * 128:(r + 1) * 128], rhs=wp[:, sl, k, :],
                                                            start=(k == 0), stop=(k == 7)),
                                 reads=[("actT", u), ("wp", sl, 0)], writes=[("p_o", os_)], inc=(k == 7))
                        P.op("act", lambda e_: e_.copy(orow[:, r, hf * 512:(hf + 1) * 512], p_o[:, os_, :]),
                             reads=[("p_o", os_)], writes=["orow"])
                P.dma("sp", OUT_d[r0:r0 + 512, :].rearrange("(r p) d -> p r d", p=128), orow[:],
                      reads=["orow"], writes=[("OUT_d", e, ch)], sem="orow")
        P.barrier()
    with ExitStack() as es:
        yk = sb(es, "yk", [128, 2, 4, D], F32)
        acc = sb(es, "acc", [128, 2, D], F32)
        x1t = sb(es, "x1c", [128, 2, D], F32)
        fsq = sb(es, "fsq", [128, D], F32)
        ssq = sb(es, "ssq", [128, 2], F32)
        b2_sb = sb(es, "b2c", [32, D], F32)
        fnw = sb(es, "fnwc", [128, D], F32)
        p_bias = ps(es, "pc_b", [128, 2, 2, 512], F32)
        P.dma("sp", b2_sb[:], g["b2"], writes=["b2c"])
        P.dma("sp", fnw[:], g["fnw_bc"], writes=["fnwc"])
        for T in range(16):
            u = T % 2
            for k4 in range(4):
                P.dma("pool", None, None, reads=["OUT_d"], writes=[("yk", u, k4)], sem=("yk", u, k4),
                      emit=lambda e: e.indirect_dma_start(
                          out=yk[:, u, k4, :], out_offset=None, in_=OUT_d[:, :],
                          in_offset=bass.IndirectOffsetOnAxis(ap=dest_i[:, T * 4 + k4:T * 4 + k4 + 1], axis=0),
                          bounds_check=g["bc_reg"], oob_is_err=False))
            P.dma("sp", x1t[:, u, :], X1_d[T * 128:(T + 1) * 128, :], reads=["X1_d"], writes=[("x1c", u)], sem=("x1c", u))
            for half in range(2):
                P.op("pe", lambda e: e.matmul(p_bias[:, u, half, :], lhsT=RWT[:, T, :], rhs=b2_sb[:, half * 512:(half + 1) * 512],
                                              start=True, stop=True),
                     reads=["b2c"], writes=[("p_bias", u)], inc=(half == 1))
            P.op("dve", lambda e: e.tensor_scalar(acc[:, u, :], yk[:, u, 0, :], w4[:, T, 0:1], None, ALU.mult),
                 reads=[("yk", u, 0)], writes=[("acc", u)])
            for k4 in range(1, 4):
                P.op("dve", lambda e: e.scalar_tensor_tensor(acc[:, u, :], yk[:, u, k4, :], w4[:, T, k4:k4 + 1], acc[:, u, :], ALU.mult, ALU.add),
                     reads=[("yk", u, k4), ("acc", u)], writes=[("acc", u)])
            for half in range(2):
                P.op("dve", lambda e: e.tensor_tensor(acc[:, u, half * 512:(half + 1) * 512], acc[:, u, half * 512:(half + 1) * 512],
                                                      p_bias[:, u, half, :], ALU.add),
                     reads=[("acc", u), ("p_bias", u)], writes=[("acc", u)])
            P.op("pool", lambda e: e.tensor_tensor(acc[:, u, :], acc[:, u, :], gate2_bc[:], ALU.mult), reads=[("acc", u), "gate2_bc"], writes=[("acc", u)])
            P.op("pool", lambda e: e.tensor_tensor(acc[:, u, :], acc[:, u, :], x1t[:, u, :], ALU.add), reads=[("acc", u), ("x1c", u)], writes=[("acc", u)])
            P.op("pool", lambda e: e.memset(ssq[:, u:u + 1], 0.0), writes=[("ssq", u)])
            P.op("act", lambda e: e.activation(fsq[:], acc[:, u, :], AF.Square, accum_out=ssq[:, u:u + 1]), reads=[("acc", u), ("ssq", u)], writes=["fsq", ("ssq", u)])
            rsqrt_act(P, ssq[:, u:u + 1], ssq[:, u:u + 1], [("ssq", u)], ("ssq", u), 1.0 / D)
            P.op("dve", lambda e: e.scalar_tensor_tensor(acc[:, u, :], acc[:, u, :], ssq[:, u:u + 1], fnw[:], ALU.mult, ALU.mult),
                 reads=[("acc", u), ("ssq", u), "fnwc"], writes=[("acc", u)])
            P.dma("sp", out[T * 128:(T + 1) * 128, :], acc[:, u, :], reads=[("acc", u)], writes=[("out", T)], sem=("out", u))


def _consts():
    c = np.zeros((128, 10, 128), np.float32)
    c[:, 0, :] = np.eye(128, dtype=np.float32)
    perm = np.zeros((128, 128), np.float32)
    for m in range(32):
        perm[(m + 16) % 32, m] = 1.0
    c[:, 1, :] = perm
    i = np.arange(128)
    c[:, 2, :] = np.where(i[:, None] <= i[None, :], -1.0 / 16.0, 0.0)
    c[:, 3, :] = (i[:, None] <= i[None, :]).astype(np.float32)
    c[:, 4, :] = (i[:, None] < i[None, :]).astype(np.float32)
    q = np.arange(256)
    for half in range(2):
        kpos = half * 128 + i
        m = np.where(kpos[:, None] <= q[None, :], 0.0, -BIG).astype(np.float32)
        c[:, 5 + 2 * half, :] = m[:, :128]
        c[:, 6 + 2 * half, :] = m[:, 128:]
    return c


def _rope_tables():
    half = 16
    inv = np.float32(500000.0) ** (-np.arange(half, dtype=np.float32) * np.float32(2.0) / np.float32(32))
    ang = np.arange(SEQ, dtype=np.float32)[:, None] * inv[None, :].astype(np.float32)
    cos = np.cos(ang).astype(np.float32).T
    sin = np.sin(ang).astype(np.float32).T
    return np.concatenate([cos, cos], 0), np.concatenate([-sin, sin], 0)


def prep_inputs(inputs):
    f = lambda k: np.asarray(inputs[k], np.float32)
    x = f("x")
    c = f("c")
    w_in = f("w_in")[0]
    fm = lambda v: np.ascontiguousarray(v.reshape(-1, 128).T)
    bc = lambda v: np.ascontiguousarray(np.broadcast_to(v[None, :], (128, v.shape[0])))
    o = [0]
    for sz in (1024, 1024, 1024, 512, 512, 1024, 1024, 16, 1024, 1024):
        o.append(o[-1] + sz)
    mq, mk, mv, gq, gk, gv, gr, gl, ga, gb = [w_in[:, o[i]:o[i + 1]] for i in range(10)]
    shared = {
        "w_ada": f("w_ada")[0],
        "b_ada_bc": bc(f("b_ada")[0]),
        "nw": np.concatenate([fm(f("norm1_w")[0]), fm(f("norm2_w")[0])], 1),
        "fnw_bc": bc(f("final_norm_w")),
        "wA": np.ascontiguousarray(np.concatenate([mk, mv, gk, gv, gl], 1)),
        "wO": np.ascontiguousarray(np.concatenate([mq, gq, gr], 1)),
        "wG": np.ascontiguousarray(np.concatenate([ga, gb], 1)),
        "w_oa": f("w_o_moba")[0], "w_ob": f("w_o_gla")[0], "w_out": f("w_out")[0],
        "nbm": fm(f("b_merge")[0]),
        "gnw": fm(f("gla_norm_w")[0]),
        "w_r": f("w_router")[0], "b_r_bc": bc(f("b_router")[0]),
        "w2": f("w_exp_out")[0], "b2": f("b_exp_out")[0],
        "consts": _consts(),
        "ecap": bc(np.arange(NE, dtype=np.float32) * CAP),
    }
    gw = np.zeros((32, 512), np.float32)
    gw[0:16] = f("gla_gate_w")[0]
    gw[16] = f("gla_gate_b")[0]
    shared["gw_aug"] = gw
    w1 = f("w_exp_in")[0]
    shared["w1d"] = np.ascontiguousarray(np.concatenate([w1[:, :, 0::2], w1[:, :, 1::2]], 2))
    b1 = f("b_exp_in")[0]
    b1d = np.concatenate([b1[:, 0::2], b1[:, 1::2]], 1)
    shared["b1T"] = np.ascontiguousarray(b1d.reshape(NE, 16, 128).transpose(2, 0, 1).reshape(128, NE * 16))
    es = np.zeros((32, 32, 128), np.float32)
    for j in range(32):
        es[j, j, :] = 1.0
    shared["esel_in"] = es.reshape(32, 32 * 128)
    cosT, sinT = _rope_tables()
    per_core = []
    for core in range(NCORE):
        b, r = core // 4, core % 4
        nnull = (24 - 8 * r) * 256
        nreal = 2048 * (r + 1)
        xv = np.zeros((D, SEQ), np.float32)
        xv[:, nnull:] = x[b, :nreal, :].T
        rc_ = np.zeros((32, SEQ), np.float32)
        rs_ = np.zeros((32, SEQ), np.float32)
        rc_[:, nnull:] = cosT[:, :nreal]
        rs_[:, nnull:] = sinT[:, :nreal]
        vf = np.zeros((SEQ,), np.float32)
        vf[nnull:] = 1.0
        gm = np.full((8, 32), NEGINF, np.float32)
        gvv = np.zeros((8, 32), np.float32)
        for l in range(8):
            gm[l, 24 - 8 * r:24 + l] = 0.0
            gvv[l, 24 - 8 * r:24 + l] = 1.0
        d = dict(shared)
        d.update({
            "xTv": xv,
            "xtok": np.ascontiguousarray(x[b, 2048 * r:2048 * (r + 1), :]),
            "cT": fm(c[b]),
            "ropec": rc_, "ropes": rs_,
            "vflag": np.ascontiguousarray(vf.reshape(64, 128).T),
            "gmask": bc(gm.reshape(-1)), "gvalid": bc(gvv.reshape(-1)),
        })
        per_core.append(d)
    return per_core


_NC_CACHE = {}


def kernel(**inputs):
    in_maps = prep_inputs(inputs)
    if "nc" not in _NC_CACHE:
        _NC_CACHE["nc"] = build_nc()[0]
    nc = _NC_CACHE["nc"]
    res = run_bass_kernel_spmd(nc, in_maps, core_ids=list(range(NCORE)))
    outp = np.zeros((2, SEQ, D), np.float32)
    for core in range(NCORE):
        b, r = core // 4, core % 4
        outp[b, 2048 * r:2048 * (r + 1), :] = res.results[core]["out"]
    return outp
```

```python
import numpy as np
from contextlib import ExitStack
import concourse.bass as bass
import concourse.mybir as mybir
from concourse.bass_utils import run_bass_kernel_spmd

F32 = mybir.dt.float32
BF16 = mybir.dt.bfloat16
I32 = mybir.dt.int32
AF = mybir.ActivationFunctionType
ALU = mybir.AluOpType
AX = mybir.AxisListType

D = 1024
SEQ = 8192
NCORE = 8
OWN = 2048
GS = 256
TPG = 2
NG = 32
OWNG0 = 24
NVB = 32
NE = 32
CAP = 1024
BIG = 30000.0
NEGINF = -1.0e30
EPS = 1e-5
STAGE = 99
DEBUG = False


class Prog:
    def __init__(self, nc, same_engine_sync=True):
        self.nc = nc
        self.engs = {"pe": nc.tensor, "act": nc.scalar, "dve": nc.vector,
                     "pool": nc.gpsimd, "sp": nc.sync}
        self.sem = {k: nc.alloc_semaphore("prog_" + k) for k in self.engs}
        self.cnt = {k: 0 for k in self.engs}
        self.seen = {k: {} for k in self.engs}
        self.bufs = {}
        self.pending = {k: ([], []) for k in self.engs}
        self.dsem = {}
        self.dcnt = {}
        self.same = same_engine_sync
        self.nwait = 0
        self.nins = 0
        self.free_sems = {"sw": [], "hw": []}
        self.dkind = {}
        self.nalloc = 0

    def _deps(self, reads, writes):
        deps = []
        for k in reads:
            b = self.bufs.get(k)
            if b is not None and b[0] is not None:
                deps.append(b[0])
        for k in writes:
            b = self.bufs.get(k)
            if b is not None:
                if b[0] is not None:
                    deps.append(b[0])
                deps.extend(b[1])
        return deps

    def _wait(self, eng, deps, own_ok=True):
        need = {}
        for (s, v) in deps:
            if own_ok and s is self.sem[eng]:
                if eng == "pe" or not self.same:
                    continue
            key = id(s)
            if self.seen[eng].get(key, 0) >= v:
                continue
            if key not in need or need[key][1] < v:
                need[key] = (s, v)
        for key, (s, v) in need.items():
            self.engs[eng].wait_ge(s, v)
            self.seen[eng][key] = v
            self.nwait += 1

    def _record(self, ev, reads, writes):
        for k in reads:
            b = self.bufs.get(k)
            if b is None:
                self.bufs[k] = [None, [ev]]
            else:
                b[1].append(ev)
                if len(b[1]) > 24:
                    b[1] = b[1][-24:] if False else b[1]
        for k in writes:
            self.bufs[k] = [ev, []]

    def op(self, eng, emit, reads=(), writes=(), inc=True):
        reads = list(reads)
        writes = list(writes)
        self._wait(eng, self._deps(reads, writes))
        ins = emit(self.engs[eng])
        self.nins += 1
        if not inc:
            self.pending[eng][0].extend(reads)
            self.pending[eng][1].extend(writes)
            return None
        self.cnt[eng] += 1
        ev = (self.sem[eng], self.cnt[eng])
        ins.then_inc(self.sem[eng], 1)
        pr, pw = self.pending[eng]
        self._record(ev, pr + reads, pw + writes)
        self.pending[eng] = ([], [])
        return ev

    def dma(self, queue, out, in_, reads=(), writes=(), sem=None, emit=None, **kw):
        reads = list(reads)
        writes = list(writes)
        if sem is None:
            sem = ("auto",) + tuple(writes) + tuple(reads)
        kind = "sw" if queue == "pool" else "hw"
        if sem in self.dsem:
            assert self.dkind[sem] == kind, (sem, kind)
        if sem not in self.dsem:
            self.dkind[sem] = kind
            if self.free_sems[kind]:
                self.dsem[sem], self.dcnt[sem] = self.free_sems[kind].pop()
            else:
                self.dsem[sem] = self.nc.alloc_semaphore("dma_%d" % self.nalloc)
                self.nalloc += 1
                self.dcnt[sem] = 0
        self._wait(queue, self._deps(reads, writes))
        if emit is not None:
            ins = emit(self.engs[queue])
        else:
            ins = self.engs[queue].dma_start(out=out, in_=in_, **kw)
        self.nins += 1
        self.dcnt[sem] += 16
        ins.then_inc(self.dsem[sem], 16)
        ev = (self.dsem[sem], self.dcnt[sem])
        self._record(ev, reads, writes)
        return ev

    def wait_all(self, eng, keys=None):
        deps = []
        for k, b in self.bufs.items():
            if keys is not None and k not in keys:
                continue
            if b[0] is not None:
                deps.append(b[0])
            deps.extend(b[1])
        self._wait(eng, deps, own_ok=False)

    def barrier(self):
        assert all(len(p[0]) == 0 and len(p[1]) == 0 for p in self.pending.values())
        for eng in self.engs:
            deps = [(self.sem[o], self.cnt[o]) for o in self.engs if o != eng and self.cnt[o] > 0]
            deps += [(self.dsem[k], self.dcnt[k]) for k in self.dsem if self.dcnt[k] > 0]
            self._wait(eng, deps, own_ok=False)
        self.bufs = {}
        for k in list(self.dsem):
            self.free_sems[self.dkind.pop(k)].append((self.dsem.pop(k), self.dcnt.pop(k)))


def build_nc():
    nc = bass.Bass("TRN2", target_bir_lowering=False)
    P = Prog(nc)
    dt_in = lambda n, s, d=F32: nc.dram_tensor(n, list(s), d, kind="ExternalInput").ap()
    dt_sc = lambda n, s, d: nc.dram_tensor(n, list(s), d).ap()

    xTv = dt_in("xTv", [D, SEQ])
    xtok = dt_in("xtok", [OWN, D])
    cT = dt_in("cT", [128, 8])
    w_ada = dt_in("w_ada", [D, 6 * D])
    b_ada_bc = dt_in("b_ada_bc", [128, 6 * D])
    nw = dt_in("nw", [128, 16])
    fnw_bc = dt_in("fnw_bc", [128, D])
    wA = dt_in("wA", [D, 3600])
    wO = dt_in("wO", [D, 2560])
    wG = dt_in("wG", [D, 2048])
    w_oa = dt_in("w_oa", [D, D])
    w_ob = dt_in("w_ob", [D, D])
    w_out = dt_in("w_out", [D, D])
    nbm = dt_in("nbm", [128, 16])
    ropec = dt_in("ropec", [32, SEQ])
    ropes = dt_in("ropes", [32, SEQ])
    vflag = dt_in("vflag", [128, 64])
    gmask = dt_in("gmask", [128, 8 * 32])
    gvalid = dt_in("gvalid", [128, 8 * 32])
    gw_aug = dt_in("gw_aug", [32, 512])
    gnw = dt_in("gnw", [128, 2])
    w_r = dt_in("w_r", [D, NE])
    b_r_bc = dt_in("b_r_bc", [128, NE])
    w1d = dt_in("w1d", [NE, D, 2048]) if STAGE >= 4 else None
    w2 = dt_in("w2", [NE, D, D]) if STAGE >= 4 else None
    b1T = dt_in("b1T", [128, NE * 16])
    b2 = dt_in("b2", [NE, D])
    consts = dt_in("consts", [128, 10, 128])
    esel_in = dt_in("esel_in", [32, 32 * 128])
    ecap = dt_in("ecap", [128, NE])
    out = nc.dram_tensor("out", [OWN, D], F32, kind="ExternalOutput").ap()

    KT_d = dt_sc("KT_d", [8, 128, SEQ], BF16)
    V_d = dt_sc("V_d", [SEQ, D], BF16)
    QT_d = dt_sc("QT_d", [8, 128, OWN], BF16)
    OB_d = dt_sc("OB_d", [D, OWN], BF16)
    HT_d = dt_sc("HT_d", [D, OWN], BF16)
    X1_d = dt_sc("X1_d", [OWN, D], F32)
    XG_d = dt_sc("XG_d", [NE * CAP, D], BF16)
    OUT_d = dt_sc("OUT_d", [NE * CAP, D], F32)

    dbg = {}
    if DEBUG:
        dbg["mod"] = nc.dram_tensor("dbg_mod", [128, 48], F32, kind="ExternalOutput").ap()
        dbg["kT"] = nc.dram_tensor("dbg_kT", [8, 128, SEQ], BF16, kind="ExternalOutput").ap()
        dbg["qT"] = nc.dram_tensor("dbg_qT", [8, 128, OWN], BF16, kind="ExternalOutput").ap()
        dbg["ob"] = nc.dram_tensor("dbg_ob", [D, OWN], BF16, kind="ExternalOutput").ap()
        dbg["oa"] = nc.dram_tensor("dbg_oa", [128, 8, OWN], BF16, kind="ExternalOutput").ap()
        dbg["x1"] = nc.dram_tensor("dbg_x1", [OWN, D], F32, kind="ExternalOutput").ap()
        dbg["lg"] = nc.dram_tensor("dbg_lg", [128, 16, 32], F32, kind="ExternalOutput").ap()

    es_all = ExitStack()
    with es_all:
        sb = lambda es, n, s, d: es.enter_context(nc.sbuf_tensor(n, list(s), d))
        ps = lambda es, n, s, d: es.enter_context(nc.psum_tensor(n, list(s), d))
        cst = sb(es_all, "cst", [128, 10, 128], F32)
        ident_f = cst[:, 0, :]
        ident_b = sb(es_all, "ident_b", [128, 128], BF16)
        ones_rms = sb(es_all, "ones_rms", [128, 128], BF16)
        ones_256 = sb(es_all, "ones_256", [128, 128], BF16)
        ones_1 = sb(es_all, "ones_1", [128, 128], BF16)
        onesf = sb(es_all, "onesf", [128, 128], F32)
        modT = sb(es_all, "modT", [128, 48], F32)
        a1 = sb(es_all, "a1", [128, 8], F32)
        a2 = sb(es_all, "a2", [128, 8], F32)
        nw_sb = sb(es_all, "nw_sb", [128, 16], F32)
        gate1_bc = sb(es_all, "gate1_bc", [128, D], F32)
        gate2_bc = sb(es_all, "gate2_bc", [128, D], F32)
        kmb = sb(es_all, "kmb", [128, 8, 32], BF16)
        bc_reg = nc.gpsimd.to_reg(NE * CAP - 1)
        dest_i = sb(es_all, "dest_i", [128, 64], I32)
        w4 = sb(es_all, "w4", [128, 16, 4], F32)
        RWT = sb(es_all, "RWT", [32, 16, 128], F32)
        s1 = modT[:, 0:8]
        g1 = modT[:, 16:24]
        s2 = modT[:, 24:32]

        P.dma("sp", cst[:], consts, writes=["cst"])
        P.dma("sp", nw_sb[:], nw, writes=["nw"])
        P.dma("pool", ident_b[:], consts[:, 0, :], writes=["ident_b"])
        P.op("dve", lambda e: e.memset(ones_rms[:], 1.0 / 1024.0), writes=["ones_rms"])
        P.op("dve", lambda e: e.memset(ones_256[:], 1.0 / 256.0), writes=["ones_256"])
        P.op("dve", lambda e: e.memset(ones_1[:], 1.0), writes=["ones_1"])
        P.op("dve", lambda e: e.memset(onesf[:], 1.0), writes=["onesf"])

        with ExitStack() as es:
            c_sb = sb(es, "c_sb", [128, 8], F32)
            c_e = sb(es, "c_e", [128, 8], F32)
            cact_b = sb(es, "cact_b", [128, 8, 128], F32)
            wst = sb(es, "wst", [128, 2, 8, 512], F32)
            modb = sb(es, "modb", [128, 6 * D], F32)
            bab = sb(es, "bab", [128, 6 * D], F32)
            p_mod = ps(es, "p_mod", [128, 2, 512], F32)
            p_col = ps(es, "p_col", [128, 48], F32)
            P.dma("sp", c_sb[:], cT, writes=["c_sb"])
            P.dma("sp", bab[:], b_ada_bc, writes=["bab"])
            P.op("act", lambda e: e.activation(c_e[:], c_sb[:], AF.Exp, scale=-1.0), reads=["c_sb"], writes=["c_e"])
            P.op("dve", lambda e: e.tensor_scalar(c_e[:], c_e[:], 1.0, None, ALU.add), reads=["c_e"], writes=["c_e"])
            P.op("dve", lambda e: e.reciprocal(c_e[:], c_e[:]), reads=["c_e"], writes=["c_e"])
            P.op("dve", lambda e: e.tensor_tensor(c_e[:], c_e[:], c_sb[:], ALU.mult), reads=["c_e", "c_sb"], writes=["c_e"])
            for k in range(8):
                P.op("dve", lambda e: e.tensor_scalar(cact_b[:, k, :], onesf[:], c_e[:, k:k + 1], None, ALU.mult),
                     reads=["c_e", "onesf"], writes=[("cactb", k)])
            for j in range(12):
                s = j % 2
                P.dma("sp", wst[:, s], w_ada[:, j * 512:(j + 1) * 512].rearrange("(k p) n -> p k n", p=128),
                      writes=[("wst", s)], sem=("wst", s))
                for k in range(8):
                    P.op("pe", lambda e: e.matmul(p_mod[:, s, :], lhsT=cact_b[:, k, :], rhs=wst[:, s, k, :],
                                                  start=(k == 0), stop=(k == 7)),
                         reads=[("wst", s), ("cactb", k)], writes=[("p_mod", s)], inc=(k == 7))
                P.op("dve", lambda e: e.tensor_tensor(modb[:, j * 512:(j + 1) * 512], p_mod[:, s, :],
                                                      bab[:, j * 512:(j + 1) * 512], ALU.add),
                     reads=[("p_mod", s), "bab"], writes=[("modb", j)])
            for j in range(48):
                P.op("pe", lambda e: e.matmul(p_col[:, j:j + 1], lhsT=modb[0:1, j * 128:(j + 1) * 128],
                                              rhs=onesf[0:1, 0:1], start=True, stop=True),
                     reads=[("modb", j // 4), "onesf"], writes=["p_col"], inc=(j == 47))
            P.op("dve", lambda e: e.tensor_copy(modT[:], p_col[:]), reads=["p_col"], writes=["modT"])
            P.op("dve", lambda e: e.scalar_tensor_tensor(a1[:], modT[:, 8:16], 1.0, nw_sb[:, 0:8], ALU.add, ALU.mult),
                 reads=["modT", "nw"], writes=["a1"])
            P.op("dve", lambda e: e.scalar_tensor_tensor(a2[:], modT[:, 32:40], 1.0, nw_sb[:, 8:16], ALU.add, ALU.mult),
                 reads=["modT", "nw"], writes=["a2"])
            P.op("act", lambda e: e.copy(gate1_bc[:], modb[:, 2048:3072]), reads=[("modb", 4), ("modb", 5)], writes=["gate1_bc"])
            P.op("act", lambda e: e.copy(gate2_bc[:], modb[:, 5120:6144]), reads=[("modb", 10), ("modb", 11)], writes=["gate2_bc"])
            if DEBUG:
                P.dma("sp", dbg["mod"], modT[:], reads=["modT"])
            P.barrier()

        G_ = dict(locals())
        if STAGE >= 1:
            phase_a1(nc, P, G_)
        with ExitStack() as es_ab:
            oaT = sb(es_ab, "oaT", [128, 8, OWN], BF16)
            G_["oaT"] = oaT
            if STAGE >= 2:
                phase_a2(nc, P, G_)
            if STAGE >= 3:
                phase_b(nc, P, G_)
        if STAGE >= 4:
            phase_moe(nc, P, G_)
        P.wait_all("sp")
    return nc, P


def rmsnorm_fm(P, nc, xs_ap, sq_ap, p_ss, rstd_ap, ones_rms, keyx, tag, pkey):
    P.op("act", lambda e: e.activation(sq_ap, xs_ap, AF.Square), reads=list(keyx), writes=[tag + "sq"])
    for k in range(8):
        P.op("pe", lambda e: e.matmul(p_ss, lhsT=ones_rms[:], rhs=sq_ap[:, k, :], start=(k == 0), stop=(k == 7)),
             reads=[tag + "sq", "ones_rms"], writes=[pkey], inc=(k == 7))
    rsqrt_act(P, rstd_ap, p_ss, [pkey], tag + "rstd", 1.0)


def rsqrt_act(P, out_ap, in_ap, rkeys, wkey, mul):
    P.op("act", lambda e: e.activation(out_ap, in_ap, AF.Ln, bias=EPS, scale=mul), reads=list(rkeys), writes=[wkey])
    P.op("act", lambda e: e.activation(out_ap, out_ap, AF.Exp, scale=-0.5), reads=[wkey], writes=[wkey])


def phase_a1(nc, P, L):
    g = L
    sb, ps = g["sb"], g["ps"]
    xTv, wA, wO, ropec, ropes = g["xTv"], g["wA"], g["wO"], g["ropec"], g["ropes"]
    cst, ident_b = g["cst"], g["ident_b"]
    ones_rms, ones_256 = g["ones_rms"], g["ones_256"]
    a1, s1, kmb = g["a1"], g["s1"], g["kmb"]
    KT_d, V_d, QT_d, OB_d, HT_d = g["KT_d"], g["V_d"], g["QT_d"], g["OB_d"], g["HT_d"]
    dbg = g["dbg"]
    with ExitStack() as es:
        wA_b = sb(es, "wA_b", [128, 8, 3600], BF16)
        wO_b = sb(es, "wO_b", [128, 8, 2560], BF16)
        xs = sb(es, "xs", [128, 2, 8, GS], F32)
        sq = sb(es, "sq", [128, 8, GS], BF16)
        rstd = sb(es, "rstd", [128, GS], F32)
        hT = sb(es, "hT", [128, 8, GS], BF16)
        rc = sb(es, "rc", [32, 1, GS], F32)
        rs = sb(es, "rs", [32, 1, GS], F32)
        kf = sb(es, "kf", [128, 2, GS], F32)
        rt = sb(es, "rt", [32, 2, GS], F32)
        kb = sb(es, "kb", [128, 2, GS], BF16)
        kmean = sb(es, "kmean", [128, 8, 32], F32)
        gkf = sb(es, "gkf", [128, 4, GS], F32)
        gqf = sb(es, "gqf", [128, 4, GS], F32)
        sgr = sb(es, "sgr", [128, 8, GS], F32)
        sgt = sb(es, "sgt", [128, GS], F32)
        glow = sb(es, "glow", [32, GS], F32)
        gw_sb = sb(es, "gw_sb", [32, 512], F32)
        gnw_sb = sb(es, "gnw_sb", [128, 2], F32)
        vfl = sb(es, "vfl", [128, 64], F32)
        Ltok = sb(es, "Ltok", [128, TPG, 512], F32)
        lex = sb(es, "lex", [128, 512], F32)
        vst = sb(es, "vst", [128, TPG, 1024], BF16)
        gvst = sb(es, "gvst", [128, TPG, 1024], BF16)
        EbT = sb(es, "EbT", [128, 2, 128], F32)
        EnbT = sb(es, "EnbT", [128, 2, 128], F32)
        qtl = sb(es, "qtl", [128, 2, 128], BF16)
        ktl = sb(es, "ktl", [128, 2, 128], BF16)
        ktok = sb(es, "ktok", [128, 2, 128], BF16)
        atm = sb(es, "atm", [128, 2, 128], BF16)
        Sst = sb(es, "Sst", [128, 4, 256], F32)
        Ssc = sb(es, "Ssc", [128, 2, 256], F32)
        Sbf = sb(es, "Sbf", [128, 4, 256], BF16)
        osq = sb(es, "osq", [128, 2, 2, 128], BF16)
        orst = sb(es, "orst", [128, 2, 128], F32)
        otmp = sb(es, "otmp", [128, 2, 128], F32)
        obst = sb(es, "obst", [128, 8, GS], BF16)
        p_pj = ps(es, "p_pj", [128, 2, 512], F32)
        p_sx = ps(es, "p_sx", [128, 512], F32)
        p_ss = p_sx[:, 0:GS]
        p_xs = p_sx[0:32, 256:256 + GS]
        p_g = ps(es, "p_g", [128, 2, 4, 128], F32)
        p_km = ps(es, "p_km", [128, 512], F32)
        p_kvb = ps(es, "p_kvb", [128, 2, 256], F32)
        p_tr = ps(es, "p_tr", [128, 2, 128], BF16)
        permf = cst[:, 1, 0:32]
        triN = cst[:, 2, :]
        utm = cst[:, 3, :]

        for k in range(8):
            P.dma("pool", wA_b[:, k, :], wA[k * 128:(k + 1) * 128, :], writes=[("wA", k)], sem=("wA", k))
        for k in range(8):
            P.dma("pool", wO_b[:, k, :], wO[k * 128:(k + 1) * 128, :], writes=[("wO", k)], sem=("wO", k))
        P.dma("sp", gw_sb[:], g["gw_aug"], writes=["gw_sb"])
        P.dma("sp", gnw_sb[:], g["gnw"], writes=["gnw_sb"])
        P.dma("sp", vfl[:], g["vflag"], writes=["vfl"])
        P.op("pool", lambda e: e.memset(glow[:], 1.0), writes=["glow"])
        P.op("pool", lambda e: e.memset(Sst[:], 0.0), writes=[("S", h) for h in range(4)])
        P.op("pool", lambda e: e.memset(Sbf[:], 0.0), writes=[("Sbf", h) for h in range(4)])
        wAk = [("wA", k) for k in range(8)]
        wOk = [("wO", k) for k in range(8)]
        pj_n = [0]
        kf_n = [0]
        uniq = [0]

        def ukey(n):
            uniq[0] += 1
            return (n, uniq[0])

        def proj_fm(wt, col0, ncol, wkeys, evac):
            s_ = pj_n[0] % 2
            pj_n[0] += 1
            for k in range(8):
                P.op("pe", lambda e: e.matmul(p_pj[0:ncol, s_, 0:GS], lhsT=wt[:, k, col0:col0 + ncol], rhs=hT[:, k, :],
                                              start=(k == 0), stop=(k == 7)),
                     reads=wkeys + ["hT"], writes=[("p_pj", s_)], inc=(k == 7))
            flush_evac()
            pend[0] = lambda: evac(p_pj[0:ncol, s_, 0:GS], ("p_pj", s_))

        pend = [None]

        def flush_evac():
            if pend[0] is not None:
                f_ = pend[0]
                pend[0] = None
                f_()

        for gi in range(NG):
            own = gi >= OWNG0
            s = gi % 2
            t0 = gi * GS
            o0 = (gi - OWNG0) * GS
            xk = [("xs", s, k) for k in range(8)]
            P.dma("sp", xs[:, s], xTv[:, t0:t0 + GS].rearrange("(k p) n -> p k n", p=128), writes=xk, sem=("xs", s))
            P.dma("sp", rc[:, 0, :], ropec[:, t0:t0 + GS], writes=[("rc", 0)], sem=("rc", 0))
            P.dma("sp", rs[:, 0, :], ropes[:, t0:t0 + GS], writes=[("rs", 0)], sem=("rs", 0))
            rmsnorm_fm(P, nc, xs[:, s], sq[:], p_ss, rstd[:], ones_rms, xk, "a1", "p_sx")
            for k in range(8):
                P.op("dve", lambda e: e.scalar_tensor_tensor(xs[:, s, k, :], xs[:, s, k, :], a1[:, k:k + 1], rstd[:],
                                                             ALU.mult, ALU.mult),
                     reads=[("xs", s, k), "a1rstd", "a1"], writes=[("xs", s, k)])
                P.op("act", lambda e: e.activation(hT[:, k, :], xs[:, s, k, :], AF.Identity, bias=s1[:, k:k + 1], scale=1.0),
                     reads=[("xs", s, k), "modT"], writes=["hT"])
            if own:
                P.dma("sp", HT_d[:, o0:o0 + GS].rearrange("(k p) n -> p k n", p=128), hT[:], reads=["hT"], writes=[ukey("HT_d")],
                      sem="HT_o")

            def qk_evac(h, is_k):
                def ev(pt, pkey):
                    u = kf_n[0] % 2
                    kf_n[0] += 1
                    P.op("act", lambda e: e.copy(kf[:, u, :], pt), reads=[pkey], writes=[("kf", u)])
                    P.op("pe", lambda e: e.matmul(p_xs, lhsT=permf, rhs=kf[:, u, :], start=True, stop=True),
                         reads=[("kf", u), "cst"], writes=["p_sx"])
                    P.op("dve", lambda e: e.tensor_tensor(rt[:, 0, :], kf[0:32, u, :], rc[:, 0, :], ALU.mult),
                         reads=[("kf", u), ("rc", 0)], writes=["rt0"])
                    P.op("dve", lambda e: e.tensor_tensor(rt[:, 1, :], p_xs, rs[:, 0, :], ALU.mult),
                         reads=["p_sx", ("rs", 0)], writes=["rt1"])
                    P.op("dve", lambda e: e.tensor_tensor(kf[0:32, u, :], rt[:, 0, :], rt[:, 1, :], ALU.add),
                         reads=["rt0", "rt1", ("kf", u)], writes=[("kf", u)])
                    if is_k:
                        P.op("dve", lambda e: e.tensor_reduce(kmean[:, h, gi:gi + 1], kf[:, u, :], AX.X, ALU.add),
                             reads=[("kf", u)], writes=[("kmean", h)])
                    P.op("pool", lambda e: e.tensor_copy(kb[:, u, :], kf[:, u, :]), reads=[("kf", u)], writes=[("kb", u)])
                    if is_k:
                        P.dma("sp", KT_d[h, :, t0:t0 + GS], kb[:, u, :], reads=[("kb", u)], writes=[ukey("KT_d")], sem=("kbo", u))
                    else:
                        P.dma("sp", QT_d[h, :, o0:o0 + GS], kb[:, u, :], reads=[("kb", u)], writes=[ukey("QT_d")], sem=("kbo", u))
                return ev

            for h in range(8):
                proj_fm(wA_b, h * 128, 128, wAk, qk_evac(h, True))
            if own:
                for h in range(8):
                    proj_fm(wO_b, h * 128, 128, wOk, qk_evac(h, False))

            flush_evac()
            for t in range(TPG):
                for half in range(2):
                    sl = pj_n[0] % 2
                    pj_n[0] += 1
                    for k in range(8):
                        P.op("pe", lambda e: e.matmul(p_pj[:, sl, :], lhsT=hT[:, k, t * 128:(t + 1) * 128],
                                                      rhs=wA_b[:, k, 1024 + half * 512:1024 + (half + 1) * 512],
                                                      start=(k == 0), stop=(k == 7)),
                             reads=wAk + ["hT"], writes=[("p_pj", sl)], inc=(k == 7))
                    P.op("act", lambda e: e.copy(vst[:, t, half * 512:(half + 1) * 512], p_pj[:, sl, :]),
                         reads=[("p_pj", sl)], writes=["vst"])
                for half in range(2):
                    sl = pj_n[0] % 2
                    pj_n[0] += 1
                    for k in range(8):
                        P.op("pe", lambda e: e.matmul(p_pj[:, sl, :], lhsT=hT[:, k, t * 128:(t + 1) * 128],
                                                      rhs=wA_b[:, k, 2560 + half * 512:2560 + (half + 1) * 512],
                                                      start=(k == 0), stop=(k == 7)),
                             reads=wAk + ["hT"], writes=[("p_pj", sl)], inc=(k == 7))
                    P.op("dve", lambda e: e.tensor_scalar(gvst[:, t, half * 512:(half + 1) * 512], p_pj[:, sl, :],
                                                          vfl[:, gi * TPG + t:gi * TPG + t + 1], None, ALU.mult),
                         reads=[("p_pj", sl), "vfl"], writes=[("gvst", t)])
            P.dma("sp", V_d[t0:t0 + GS, :].rearrange("(t p) n -> p t n", p=128), vst[:], reads=["vst"], writes=[ukey("V_d")],
                  sem="V_o")

            for h in range(4):
                proj_fm(wA_b, 2048 + h * 128, 128, wAk,
                        lambda pt, pkey, h=h: P.op("act", lambda e: e.copy(gkf[:, h, :], pt), reads=[pkey], writes=[("gkf", h)]))
            proj_fm(wA_b, 3584, 16, wAk,
                    lambda pt, pkey: P.op("act", lambda e: e.copy(glow[0:16, :], pt), reads=[pkey], writes=["glow"]))
            if own:
                for h in range(4):
                    proj_fm(wO_b, 1024 + h * 128, 128, wOk,
                            lambda pt, pkey, h=h: P.op("act", lambda e: e.copy(gqf[:, h, :], pt), reads=[pkey], writes=[("gqf", h)]))
                for c in range(8):
                    def gr_ev(pt, pkey, c=c):
                        P.op("act", lambda e: e.activation(sgt[:], pt, AF.Exp, scale=-1.0), reads=[pkey], writes=["sgt"])
                        P.op("dve", lambda e: e.tensor_scalar(sgt[:], sgt[:], 1.0, None, ALU.add), reads=["sgt"], writes=["sgt"])
                        P.op("dve", lambda e: e.reciprocal(sgt[:], sgt[:]), reads=["sgt"], writes=["sgt"])
                        P.op("dve", lambda e: e.tensor_tensor(sgr[:, c, :], sgt[:], pt, ALU.mult), reads=["sgt", pkey], writes=[("sgr", c)])
                    proj_fm(wO_b, 1536 + c * 128, 128, wOk, gr_ev)
            flush_evac()
            for t in range(TPG):
                sl = pj_n[0] % 2
                pj_n[0] += 1
                P.op("pe", lambda e: e.matmul(p_pj[:, sl, :], lhsT=glow[:, t * 128:(t + 1) * 128], rhs=gw_sb[:],
                                              start=True, stop=True),
                     reads=["glow", "gw_sb"], writes=[("p_pj", sl)])
                P.op("act", lambda e: e.activation(lex[:], p_pj[:, sl, :], AF.Exp, scale=-1.0), reads=[("p_pj", sl)], writes=["lex"])
                P.op("act", lambda e: e.activation(Ltok[:, t, :], lex[:], AF.Ln, bias=1.0, scale=1.0), reads=["lex"], writes=[("Ltok", t)])

            def gla_chain(t, h):
                u = h % 2
                c0 = t * 128
                P.op("pe", lambda e: e.matmul(p_g[:, u, 0, :], lhsT=Ltok[:, t, h * 128:(h + 1) * 128], rhs=triN,
                                              start=True, stop=True),
                     reads=[("Ltok", t), "cst"], writes=[("p_g", u)])
                yield
                P.op("act", lambda e: e.activation(EbT[:, u, :], p_g[:, u, 0, :], AF.Exp), reads=[("p_g", u)], writes=[("EbT", u)])
                P.op("act", lambda e: e.activation(EnbT[:, u, :], p_g[:, u, 0, :], AF.Exp, scale=-1.0),
                     reads=[("p_g", u)], writes=[("EnbT", u)])
                yield
                P.op("dve", lambda e: e.tensor_tensor(ktl[:, u, :], gkf[:, h, c0:c0 + 128], EnbT[:, u, :], ALU.mult),
                     reads=[("gkf", h), ("EnbT", u)], writes=[("ktl", u)])
                yield
                P.op("pe", lambda e: e.transpose(p_tr[:, u, :], ktl[:, u, :], ident_b[:]),
                     reads=[("ktl", u), "ident_b"], writes=["p_tr"])
                yield
                P.op("act", lambda e: e.copy(ktok[:, u, :], p_tr[:, u, :]), reads=["p_tr"], writes=[("ktok", u)])
                P.op("pe", lambda e: e.matmul(p_kvb[:, u, :], lhsT=ktok[:, u, :], rhs=gvst[:, t, h * 256:(h + 1) * 256],
                                              start=True, stop=True),
                     reads=[("ktok", u), ("gvst", t)], writes=["p_kvb"])
                yield
                if own:
                    P.op("dve", lambda e: e.scalar_tensor_tensor(qtl[:, u, :], gqf[:, h, c0:c0 + 128], 128.0 ** -0.5,
                                                                 EbT[:, u, :], ALU.mult, ALU.mult),
                         reads=[("gqf", h), ("EbT", u)], writes=[("qtl", u)])
                    yield
                    P.op("pe", lambda e: e.matmul(p_g[:, u, 1, :], lhsT=ktl[:, u, :], rhs=qtl[:, u, :], start=True, stop=True),
                         reads=[("ktl", u), ("qtl", u)], writes=[("p_g", u)])
                    yield
                    P.op("dve", lambda e: e.tensor_tensor(atm[:, u, :], p_g[:, u, 1, :], utm, ALU.mult),
                         reads=[("p_g", u), "cst"], writes=[("atm", u)])
                    yield
                    for dv in range(2):
                        P.op("pe", lambda e: e.matmul(p_g[:, u, 2 + dv, :],
                                                      lhsT=gvst[:, t, h * 256 + dv * 128:h * 256 + (dv + 1) * 128],
                                                      rhs=atm[:, u, :], start=True, stop=False),
                             reads=[("gvst", t), ("atm", u)], writes=[("p_g", u)], inc=False)
                        yield
                        P.op("pe", lambda e: e.matmul(p_g[:, u, 2 + dv, :], lhsT=Sbf[:, h, dv * 128:(dv + 1) * 128],
                                                      rhs=qtl[:, u, :], start=False, stop=True),
                             reads=[("Sbf", h), ("qtl", u)], writes=[("p_g", u)])
                        yield
                        P.op("act", lambda e: e.activation(osq[:, u, dv, :], p_g[:, u, 2 + dv, :], AF.Square),
                             reads=[("p_g", u)], writes=[("osq", u, dv)])
                        yield
                    for dv in range(2):
                        P.op("pe", lambda e: e.matmul(p_km[:, 256 + u * 128:256 + (u + 1) * 128], lhsT=ones_256[:], rhs=osq[:, u, dv, :],
                                                      start=(dv == 0), stop=(dv == 1)),
                             reads=[("osq", u, dv), "ones_256"], writes=["p_km"], inc=(dv == 1))
                    rsqrt_act(P, orst[:, u, :], p_km[:, 256 + u * 128:256 + (u + 1) * 128], ["p_km"], ("orst", u), 1.0)
                    for dv in range(2):
                        P.op("dve", lambda e: e.scalar_tensor_tensor(otmp[:, u, :], p_g[:, u, 2 + dv, :], gnw_sb[:, dv:dv + 1],
                                                                     orst[:, u, :], ALU.mult, ALU.mult),
                             reads=[("p_g", u), ("orst", u), "gnw_sb"], writes=[("otmp", u)])
                        yield
                        P.op("dve", lambda e: e.tensor_tensor(obst[:, h * 2 + dv, c0:c0 + 128], otmp[:, u, :],
                                                              sgr[:, h * 2 + dv, c0:c0 + 128], ALU.mult),
                             reads=[("otmp", u), ("sgr", h * 2 + dv)], writes=["obst"])
                        yield
                P.op("pool", lambda e: e.tensor_scalar(Ssc[:, u, :], Sst[:, h, :], EbT[:, u, 127:128], None, ALU.mult),
                     reads=[("S", h), ("EbT", u)], writes=[("Ssc", u)])
                yield
                P.op("dve", lambda e: e.scalar_tensor_tensor(Sst[:, h, :], p_kvb[:, u, :], EbT[:, u, 127:128], Ssc[:, u, :],
                                                             ALU.mult, ALU.add),
                     reads=["p_kvb", ("EbT", u), ("Ssc", u)], writes=[("S", h)])
                yield
                P.op("act", lambda e: e.copy(Sbf[:, h, :], Sst[:, h, :]), reads=[("S", h)], writes=[("Sbf", h)])

            for t in range(TPG):
                for hp in (0, 2):
                    gens = [gla_chain(t, hp), gla_chain(t, hp + 1)]
                    while gens:
                        for g_ in list(gens):
                            try:
                                next(g_)
                            except StopIteration:
                                gens.remove(g_)
            if own:
                P.dma("sp", OB_d[:, o0:o0 + GS].rearrange("(k p) n -> p k n", p=128), obst[:], reads=["obst"], writes=[ukey("OB_d")],
                      sem="OB_o")
        P.op("dve", lambda e: e.tensor_copy(kmb[:], kmean[:]), reads=[("kmean", h) for h in range(8)], writes=["kmb"])
        if DEBUG:
            P.wait_all("sp")
            P.dma("sp", dbg["kT"], KT_d, reads=[])
            P.dma("sp", dbg["qT"], QT_d, reads=[])
            P.dma("sp", dbg["ob"], OB_d, reads=[])
        P.barrier()


def phase_a2(nc, P, L):
    g = L
    sb, ps = g["sb"], g["ps"]
    KT_d, V_d, QT_d = g["KT_d"], g["V_d"], g["QT_d"]
    cst, ident_b, ident_f, ones_1, oaT, kmb = g["cst"], g["ident_b"], g["ident_f"], g["ones_1"], g["oaT"], g["kmb"]
    scale = 128.0 ** -0.5
    with ExitStack() as es:
        KT = sb(es, "KT", [128, 2, SEQ], BF16)
        Vh = sb(es, "Vh", [128, 2, 64, 128], BF16)
        QT = sb(es, "QT", [128, 2, OWN], BF16)
        esel = sb(es, "esel", [32, 32, 128], BF16)
        cmask = sb(es, "cmask", [128, 2, 256], BF16)
        gm_sb = sb(es, "gm_sb", [128, 8, 32], F32)
        gv_sb = sb(es, "gv_sb", [128, 8, 32], F32)
        gt = sb(es, "gt", [128, 2, 32], F32)
        top8 = sb(es, "top8", [128, 2, 8], F32)
        mbT = sb(es, "mbT", [32, 2, 256], BF16)
        PT = sb(es, "PT", [128, 4, 256], BF16)
        rden = sb(es, "rden", [128, 256], F32)
        p_st = ps(es, "p_st", [128, 4, 512], F32)
        p_od = ps(es, "p_od", [128, 2, 512], F32)
        p_gm = ps(es, "p_gm", [128, 512], F32)
        p_gt = p_gm[:, 0:64].rearrange("p (a b) -> p a b", a=2)
        p_mb = p_gm[0:32, 256:512]
        P.dma("pool", esel[:].rearrange("p a b -> p (a b)"), g["esel_in"], writes=["esel"])
        P.dma("pool", cmask[:], cst_dram_causal(g), writes=["cmask"])
        P.dma("sp", gm_sb[:].rearrange("p a b -> p (a b)"), g["gmask"], writes=["gm_sb"])
        P.dma("sp", gv_sb[:].rearrange("p a b -> p (a b)"), g["gvalid"], writes=["gv_sb"])
        n_st = [0]
        n_blk = [0]
        for h in range(8):
            hs = h % 2
            P.dma("sp", KT[:, hs, :], KT_d[h], writes=[("KT", hs)], sem=("KT", hs))
            P.dma("sp", Vh[:, hs], V_d[:, h * 128:(h + 1) * 128].rearrange("(t p) d -> p t d", p=128),
                  writes=[("Vh", hs)], sem=("Vh", hs))
            P.dma("sp", QT[:, hs, :], QT_d[h], writes=[("QT", hs)], sem=("QT", hs))
            for l in range(8):
                bs = n_blk[0] % 2
                n_blk[0] += 1
                q0 = l * 256
                for qt in range(2):
                    P.op("pe", lambda e: e.matmul(p_gt[:, qt, :], lhsT=QT[:, hs, q0 + qt * 128:q0 + (qt + 1) * 128],
                                                  rhs=kmb[:, h, :], start=True, stop=True),
                         reads=[("QT", hs), "kmb"], writes=["p_gm"])
                    P.op("dve", lambda e: e.tensor_tensor(gt[:, qt, :], p_gt[:, qt, :], gm_sb[:, l, :], ALU.add),
                         reads=["p_gm", "gm_sb"], writes=[("gt", qt)])
                    P.op("dve", lambda e: e.max(top8[:, qt, :], gt[:, qt, :]), reads=[("gt", qt)], writes=[("top8", qt)])
                    P.op("dve", lambda e: e.tensor_scalar(gt[:, qt, :], gt[:, qt, :], top8[:, qt, 2:3], None, ALU.is_ge),
                         reads=[("gt", qt), ("top8", qt)], writes=[("gt", qt)])
                    P.op("dve", lambda e: e.tensor_tensor(gt[:, qt, :], gt[:, qt, :], gv_sb[:, l, :], ALU.mult),
                         reads=[("gt", qt), "gv_sb"], writes=[("gt", qt)])
                    P.op("dve", lambda e: e.tensor_scalar(gt[:, qt, :], gt[:, qt, :], BIG, -BIG, ALU.mult, ALU.add),
                         reads=[("gt", qt)], writes=[("gt", qt)])
                    P.op("pe", lambda e: e.transpose(p_mb[:, qt * 128:(qt + 1) * 128], gt[:, qt, :], ident_f),
                         reads=[("gt", qt), "cst"], writes=["p_gm"])
                P.op("dve", lambda e: e.tensor_copy(mbT[:, bs, :], p_mb), reads=["p_gm"], writes=[("mbT", bs)])
                nkv = 24 + l
                pairs = [(v, half) for v in range(nkv + 1) for half in range(2)]
                def front(i):
                    v, half = pairs[i]
                    st = n_st[0] % 4
                    n_st[0] += 1
                    k0 = v * 256 + half * 128
                    P.op("pe", lambda e: e.matmul(p_st[:, st, 0:256], lhsT=KT[:, hs, k0:k0 + 128], rhs=QT[:, hs, q0:q0 + 256],
                                                  start=True, stop=False),
                         reads=[("KT", hs), ("QT", hs)], writes=[("p_st", st)], inc=False)
                    if v < nkv:
                        P.op("pe", lambda e: e.matmul(p_st[:, st, 0:256], lhsT=esel[:, v, :], rhs=mbT[:, bs, :], start=False, stop=True),
                             reads=["esel", ("mbT", bs)], writes=[("p_st", st)])
                    else:
                        P.op("pe", lambda e: e.matmul(p_st[:, st, 0:256], lhsT=ident_b[:], rhs=cmask[:, half, :], start=False, stop=True),
                             reads=["ident_b", "cmask"], writes=[("p_st", st)])
                    P.op("act", lambda e: e.activation(PT[:, st, :], p_st[:, st, 0:256], AF.Exp, scale=scale),
                         reads=[("p_st", st)], writes=[("PT", st)])
                    return st

                def back(i, st):
                    v, half = pairs[i]
                    last = (i == len(pairs) - 1)
                    P.op("pe", lambda e: e.matmul(p_od[:, 0, 0:256], lhsT=Vh[:, hs, v * 2 + half, :], rhs=PT[:, st, :],
                                                  start=(i == 0), stop=last),
                         reads=[("Vh", hs), ("PT", st)], writes=["p_od"], inc=False)
                    P.op("pe", lambda e: e.matmul(p_od[:, 1, 0:256], lhsT=ones_1[:], rhs=PT[:, st, :],
                                                  start=(i == 0), stop=last),
                         reads=["ones_1", ("PT", st)], writes=["p_od"], inc=True)

                DEPTH = 2
                sts = {}
                for i in range(len(pairs) + DEPTH):
                    if i < len(pairs):
                        sts[i] = front(i)
                    if i - DEPTH >= 0:
                        back(i - DEPTH, sts.pop(i - DEPTH))
                P.op("dve", lambda e: e.reciprocal(rden[:], p_od[:, 1, 0:256]), reads=["p_od"], writes=["rden"])
                P.op("dve", lambda e: e.tensor_tensor(oaT[:, h, q0:q0 + 256], p_od[:, 0, 0:256], rden[:], ALU.mult),
                     reads=["p_od", "rden"], writes=[("oaT", h)])
        if DEBUG:
            P.dma("sp", g["dbg"]["oa"], oaT[:], reads=[("oaT", h) for h in range(8)])
        P.barrier()


def cst_dram_causal(g):
    return g["consts"][:, 5:9, :].rearrange("p (a b) c -> p a (b c)", a=2)


def phase_b(nc, P, L):
    g = L
    sb, ps = g["sb"], g["ps"]
    oaT, ones_rms, ones_1, ident_b, ident_f, cst = g["oaT"], g["ones_rms"], g["ones_1"], g["ident_b"], g["ident_f"], g["cst"]
    a2, s2, g1, gate1_bc = g["a2"], g["s2"], g["g1"], g["gate1_bc"]
    HT_d, OB_d, X1_d, XG_d = g["HT_d"], g["OB_d"], g["X1_d"], g["XG_d"]
    xTv, xtok = g["xTv"], g["xtok"]
    dest_i, w4, RWT = g["dest_i"], g["w4"], g["RWT"]
    with ExitStack() as es:
        wG_b = sb(es, "wG_b", [128, 8, 2048], BF16)
        woa_b = sb(es, "woa_b", [128, 8, D], BF16)
        wob_b = sb(es, "wob_b", [128, 8, D], BF16)
        wout_b = sb(es, "wout_b", [128, 8, D], BF16)
        nbm_sb = sb(es, "nbm_sb", [128, 16], F32)
        wr_sb = sb(es, "wr_sb", [128, 8, NE], F32)
        br_sb = sb(es, "br_sb", [128, NE], F32)
        ecap_sb = sb(es, "ecap_sb", [128, NE], F32)
        hTg = sb(es, "hTg", [128, 8, GS], BF16)
        obg = sb(es, "obg", [128, 8, GS], BF16)
        xo = sb(es, "xo", [128, 8, GS], F32)
        ge = sb(es, "ge", [128, GS], F32)
        mf = sb(es, "mf", [128, GS], F32)
        mt2 = sb(es, "mt2", [128, GS], F32)
        mT = sb(es, "mT", [128, 8, GS], BF16)
        sq = sb(es, "sqb", [128, 8, GS], BF16)
        rstd = sb(es, "rstdb", [128, GS], F32)
        h2f = sb(es, "h2f", [128, 8, GS], F32)
        h2b = sb(es, "h2b", [128, 8, GS], BF16)
        xt = sb(es, "xt", [128, D], F32)
        x1t = sb(es, "x1t", [128, D], F32)
        h2tok = sb(es, "h2tok", [128, D], BF16)
        lg = sb(es, "lg", [128, NE], F32)
        t8 = sb(es, "t8", [128, 8], F32)
        nmax = sb(es, "nmax", [128, 1], F32)
        den = sb(es, "den", [128, 1], F32)
        sel = sb(es, "sel", [128, NE], F32)
        selb = sb(es, "selb", [128, NE], BF16)
        carry = sb(es, "carry", [128, NE], F32)
        slot = sb(es, "slot", [128, NE], F32)
        oh = sb(es, "oh", [128, NE], F32)
        destf = sb(es, "destf", [128, 4], F32)
        RW = sb(es, "RW", [128, NE], F32)
        stri = sb(es, "stri", [128, 128], BF16)
        p_a = ps(es, "pb_a", [128, 2, 512], F32)
        p_b = ps(es, "pb_b", [128, 2, 512], F32)
        p_ss = ps(es, "pb_ss", [128, GS], F32)
        p_lg = ps(es, "pb_lg", [128, 2, NE], F32)
        p_tr = ps(es, "pb_tr", [128, 2, 512], BF16)
        p_rw = ps(es, "pb_rw", [32, 128], F32)
        for k in range(8):
            P.dma("pool", wG_b[:, k, :], g["wG"][k * 128:(k + 1) * 128, :], writes=[("wG", k)], sem=("wG", k))
            P.dma("pool", woa_b[:, k, :], g["w_oa"][k * 128:(k + 1) * 128, :], writes=[("woa", k)], sem=("woa", k))
            P.dma("pool", wob_b[:, k, :], g["w_ob"][k * 128:(k + 1) * 128, :], writes=[("wob", k)], sem=("wob", k))
            P.dma("pool", wout_b[:, k, :], g["w_out"][k * 128:(k + 1) * 128, :], writes=[("wout", k)], sem=("wout", k))
        P.dma("pool", stri[:], g["consts"][:, 4, :], writes=["stri"])
        P.dma("sp", nbm_sb[:], g["nbm"], writes=["nbm"])
        P.dma("sp", wr_sb[:], g["w_r"].rearrange("(k p) n -> p k n", p=128), writes=["wr"])
        P.dma("sp", br_sb[:], g["b_r_bc"], writes=["br"])
        P.dma("sp", ecap_sb[:], g["ecap"], writes=["ecap"])
        P.op("pool", lambda e: e.memset(carry[:], 0.0), writes=["carry"])
        P.op("dve", lambda e: e.tensor_scalar(nbm_sb[:], nbm_sb[:], -1.0, None, ALU.mult), reads=["nbm"], writes=["nbm"])
        kk = lambda n: [(n, k) for k in range(8)]
        for G in range(OWN // GS):
            o0 = G * GS
            P.dma("sp", hTg[:], HT_d[:, o0:o0 + GS].rearrange("(k p) n -> p k n", p=128), writes=["hTg"])
            P.dma("sp", obg[:], OB_d[:, o0:o0 + GS].rearrange("(k p) n -> p k n", p=128), writes=["obg"])
            P.dma("sp", xo[:], xTv[:, 6144 + o0:6144 + o0 + GS].rearrange("(k p) n -> p k n", p=128), writes=["xo"])
            for c in range(8):
                for br in range(2):
                    wy = woa_b if br == 0 else wob_b
                    wyk = kk("woa") if br == 0 else kk("wob")
                    for k in range(8):
                        P.op("pe", lambda e: e.matmul(p_a[:, br, 0:GS], lhsT=wG_b[:, k, br * 1024 + c * 128:br * 1024 + (c + 1) * 128],
                                                      rhs=hTg[:, k, :], start=(k == 0), stop=(k == 7)),
                             reads=kk("wG") + ["hTg"], writes=[("p_a", br)], inc=(k == 7))
                    for k in range(8):
                        rhs = oaT[:, k, o0:o0 + GS] if br == 0 else obg[:, k, :]
                        P.op("pe", lambda e: e.matmul(p_b[:, br, 0:GS], lhsT=wy[:, k, c * 128:(c + 1) * 128], rhs=rhs,
                                                      start=(k == 0), stop=(k == 7)),
                             reads=wyk + (["obg"] if br else []), writes=[("p_b", br)], inc=(k == 7))
                    P.op("act", lambda e: e.activation(ge[:], p_a[:, br, 0:GS], AF.Exp, bias=nbm_sb[:, br * 8 + c:br * 8 + c + 1], scale=-1.0),
                         reads=[("p_a", br), "nbm"], writes=["ge"])
                    P.op("dve", lambda e: e.tensor_scalar(ge[:], ge[:], 1.0, None, ALU.add), reads=["ge"], writes=["ge"])
                    P.op("dve", lambda e: e.reciprocal(ge[:], ge[:]), reads=["ge"], writes=["ge"])
                    if br == 0:
                        P.op("dve", lambda e: e.tensor_tensor(mf[:], ge[:], p_b[:, br, 0:GS], ALU.mult), reads=["ge", ("p_b", br)], writes=["mf"])
                    else:
                        P.op("dve", lambda e: e.tensor_tensor(mt2[:], ge[:], p_b[:, br, 0:GS], ALU.mult), reads=["ge", ("p_b", br)], writes=["mt2"])
                        P.op("dve", lambda e: e.tensor_tensor(mT[:, c, :], mf[:], mt2[:], ALU.add), reads=["mf", "mt2"], writes=["mT"])
            for c in range(8):
                for k in range(8):
                    P.op("pe", lambda e: e.matmul(p_a[:, c % 2, 0:GS], lhsT=wout_b[:, k, c * 128:(c + 1) * 128], rhs=mT[:, k, :],
                                                  start=(k == 0), stop=(k == 7)),
                         reads=kk("wout") + ["mT"], writes=[("p_a", c % 2)], inc=(k == 7))
                P.op("dve", lambda e: e.scalar_tensor_tensor(xo[:, c, :], p_a[:, c % 2, 0:GS], g1[:, c:c + 1], xo[:, c, :], ALU.mult, ALU.add),
                     reads=[("p_a", c % 2), "xo"], writes=["xo"])
            for t in range(TPG):
                T = G * TPG + t
                P.dma("sp", xt[:], xtok[T * 128:(T + 1) * 128, :], writes=["xt"])
                for half in range(2):
                    for k in range(8):
                        P.op("pe", lambda e: e.matmul(p_b[:, half, :], lhsT=mT[:, k, t * 128:(t + 1) * 128],
                                                      rhs=wout_b[:, k, half * 512:(half + 1) * 512], start=(k == 0), stop=(k == 7)),
                             reads=kk("wout") + ["mT"], writes=[("p_b", half)], inc=(k == 7))
                    P.op("dve", lambda e: e.tensor_tensor(x1t[:, half * 512:(half + 1) * 512], p_b[:, half, :],
                                                          gate1_bc[:, half * 512:(half + 1) * 512], ALU.mult),
                         reads=[("p_b", half)], writes=["x1t"])
                P.op("pool", lambda e: e.tensor_tensor(x1t[:], x1t[:], xt[:], ALU.add), reads=["x1t", "xt"], writes=["x1t"])
                P.dma("sp", X1_d[T * 128:(T + 1) * 128, :], x1t[:], reads=["x1t"], writes=[("X1_d", T)], sem="X1_o")
            rmsnorm_fm(P, nc, xo[:], sq[:], p_ss[:], rstd[:], ones_rms, ["xo"], "b", "pb_ss")
            for k in range(8):
                P.op("dve", lambda e: e.scalar_tensor_tensor(xo[:, k, :], xo[:, k, :], a2[:, k:k + 1], rstd[:], ALU.mult, ALU.mult),
                     reads=["xo", "brstd"], writes=["xo"])
                P.op("act", lambda e: e.activation(h2f[:, k, :], xo[:, k, :], AF.Identity, bias=s2[:, k:k + 1], scale=1.0),
                     reads=["xo"], writes=["h2f"])
            P.op("pool", lambda e: e.tensor_copy(h2b[:], h2f[:]), reads=["h2f"], writes=["h2b"])
            for t in range(TPG):
                T = G * TPG + t
                lq = t
                for k in range(8):
                    P.op("pe", lambda e: e.matmul(p_lg[:, lq, :], lhsT=h2f[:, k, t * 128:(t + 1) * 128], rhs=wr_sb[:, k, :],
                                                  start=(k == 0), stop=(k == 7)),
                         reads=["h2f", "wr"], writes=["p_lg"], inc=(k == 7))
                P.op("dve", lambda e: e.tensor_tensor(lg[:], p_lg[:, lq, :], br_sb[:], ALU.add), reads=["p_lg", "br"], writes=["lg"])
                if DEBUG:
                    P.dma("sp", g["dbg"]["lg"][:, T, :], lg[:], reads=["lg"], sem="dbg_lg")
                P.op("dve", lambda e: e.max(t8[:], lg[:]), reads=["lg"], writes=["t8"])
                P.op("dve", lambda e: e.tensor_scalar(nmax[:], t8[:, 0:1], -1.0, None, ALU.mult), reads=["t8"], writes=["nmax"])
                P.op("act", lambda e: e.activation(w4[:, T, :], t8[:, 0:4], AF.Exp, bias=nmax[:], scale=1.0),
                     reads=["t8", "nmax"], writes=[("w4", T)])
                P.op("dve", lambda e: e.tensor_reduce(den[:], w4[:, T, :], AX.X, ALU.add), reads=[("w4", T)], writes=["den"])
                P.op("dve", lambda e: e.reciprocal(den[:], den[:]), reads=["den"], writes=["den"])
                P.op("dve", lambda e: e.tensor_scalar(w4[:, T, :], w4[:, T, :], den[:], None, ALU.mult), reads=[("w4", T), "den"], writes=[("w4", T)])
                P.op("dve", lambda e: e.tensor_scalar(sel[:], lg[:], t8[:, 3:4], None, ALU.is_ge), reads=["lg", "t8"], writes=["sel"])
                P.op("dve", lambda e: e.tensor_copy(selb[:], sel[:]), reads=["sel"], writes=["selb"])
                P.op("pe", lambda e: e.matmul(p_lg[:, lq, :], lhsT=stri[:], rhs=selb[:], start=True, stop=True),
                     reads=["stri", "selb"], writes=["p_lg"])
                P.op("dve", lambda e: e.tensor_tensor(slot[:], p_lg[:, lq, :], carry[:], ALU.add), reads=["p_lg", "carry"], writes=["slot"])
                P.op("pe", lambda e: e.matmul(p_lg[:, lq, :], lhsT=ones_1[:], rhs=selb[:], start=True, stop=True),
                     reads=["selb"], writes=["p_lg"])
                P.op("dve", lambda e: e.tensor_tensor(carry[:], p_lg[:, lq, :], carry[:], ALU.add), reads=["p_lg", "carry"], writes=["carry"])
                P.op("dve", lambda e: e.tensor_tensor(slot[:], slot[:], ecap_sb[:], ALU.add), reads=["slot", "ecap"], writes=["slot"])
                P.op("pool", lambda e: e.memset(RW[:], 0.0), writes=["RW"])
                for k4 in range(4):
                    P.op("dve", lambda e: e.tensor_scalar(oh[:], lg[:], t8[:, k4:k4 + 1], None, ALU.is_equal), reads=["lg", "t8"], writes=["oh"])
                    P.op("dve", lambda e: e.scalar_tensor_tensor(RW[:], oh[:], w4[:, T, k4:k4 + 1], RW[:], ALU.mult, ALU.add),
                         reads=["oh", ("w4", T), "RW"], writes=["RW"])
                    P.op("dve", lambda e: e.tensor_tensor(oh[:], oh[:], slot[:], ALU.mult), reads=["oh", "slot"], writes=["oh"])
                    P.op("dve", lambda e: e.tensor_reduce(destf[:, k4:k4 + 1], oh[:], AX.X, ALU.add), reads=["oh"], writes=["destf"])
                P.op("dve", lambda e: e.tensor_copy(dest_i[:, T * 4:T * 4 + 4], destf[:]), reads=["destf"], writes=[("dest_i", T)])
                P.op("pe", lambda e: e.transpose(p_rw[:], RW[:], ident_f), reads=["RW"], writes=["p_rw"])
                P.op("act", lambda e: e.copy(RWT[:, T, :], p_rw[:]), reads=["p_rw"], writes=[("RWT", T)])
                for hf in range(2):
                    for k in range(4):
                        P.op("pe", lambda e: e.transpose(p_tr[:, hf, k * 128:(k + 1) * 128], h2b[:, hf * 4 + k, t * 128:(t + 1) * 128], ident_b[:]),
                             reads=["h2b"], writes=["p_tr"], inc=(k == 3))
                    P.op("act", lambda e: e.copy(h2tok[:, hf * 512:(hf + 1) * 512], p_tr[:, hf, :]), reads=["p_tr"], writes=["h2tok"])
                for k4 in range(4):
                    P.dma("pool", None, None, reads=["h2tok", ("dest_i", T)], writes=["XG_d"], sem=("xgs", k4),
                          emit=lambda e: e.indirect_dma_start(
                              out=XG_d[:, :], out_offset=bass.IndirectOffsetOnAxis(ap=dest_i[:, T * 4 + k4:T * 4 + k4 + 1], axis=0),
                              in_=h2tok[:, :], in_offset=None, bounds_check=g["bc_reg"], oob_is_err=False))
        if DEBUG:
            P.wait_all("sp")
            P.dma("sp", g["dbg"]["x1"], X1_d, reads=[])
        P.barrier()


def phase_moe(nc, P, L):
    g = L
    sb, ps = g["sb"], g["ps"]
    ident_b, gate2_bc = g["ident_b"], g["gate2_bc"]
    XG_d, OUT_d, X1_d = g["XG_d"], g["OUT_d"], g["X1_d"]
    w1d, w2, out = g["w1d"], g["w2"], g["out"]
    dest_i, w4, RWT = g["dest_i"], g["w4"], g["RWT"]
    NCH = CAP // 512
    with ExitStack() as es:
        wp = sb(es, "wp", [128, 12, 8, 512], BF16)
        b1 = sb(es, "b1", [128, NE, 16], F32)
        xg = sb(es, "xg", [128, 2, 4, D], BF16)
        xgT = sb(es, "xgT", [128, 2, 8, 512], BF16)
        actT = sb(es, "actT", [128, 2, 8, 512], BF16)
        gg = sb(es, "gg", [128, 2, 512], F32)
        ll = sb(es, "ll", [128, 2, 512], F32)
        ee = sb(es, "ee", [128, 2, 512], F32)
        orow = sb(es, "orow", [128, 4, D], F32)
        p_h = ps(es, "pm_h", [128, 4, 512], F32)
        p_o = ps(es, "pm_o", [128, 2, 512], F32)
        p_t = ps(es, "pm_t", [128, 2, 512], BF16)
        P.dma("sp", b1[:].rearrange("p a b -> p (a b)"), g["b1T"], writes=["b1"])

        def load_expert(e):
            base = (e % 2) * 6
            for p in range(4):
                for two in range(2):
                    src = w1d[e][:, two * 1024 + p * 256:two * 1024 + (p + 1) * 256]
                    P.dma("pool", wp[:, base + p, :, two * 256:(two + 1) * 256], src.rearrange("(k p) f -> p k f", p=128),
                          writes=[("wp", base + p, two)], sem=("wp", base + p, two))
            for hf in range(2):
                src = w2[e][:, hf * 512:(hf + 1) * 512]
                P.dma("pool", wp[:, base + 4 + hf], src.rearrange("(k p) n -> p k n", p=128),
                      writes=[("wp", base + 4 + hf, 0)], sem=("wp", base + 4 + hf, 0))

        load_expert(0)
        nch = [0]
        for e in range(NE):
            base = (e % 2) * 6
            if e + 1 < NE:
                load_expert(e + 1)
            for ch in range(NCH):
                u = nch[0] % 2
                nch[0] += 1
                r0 = e * CAP + ch * 512
                P.dma("sp", xg[:, u], XG_d[r0:r0 + 512, :].rearrange("(r p) d -> p r d", p=128),
                      writes=[("xg", u)], sem=("xg", u))
                for r in range(4):
                    for hf in range(2):
                        for k in range(4):
                            P.op("pe", lambda e_: e_.transpose(p_t[:, hf, k * 128:(k + 1) * 128],
                                                               xg[:, u, r, (hf * 4 + k) * 128:(hf * 4 + k + 1) * 128], ident_b[:]),
                                 reads=[("xg", u)], writes=["p_t"], inc=(k == 3))
                        P.op("act", lambda e_: e_.copy(xgT[:, u, hf * 4:(hf + 1) * 4, r * 128:(r + 1) * 128],
                                                       p_t[:, hf, :].rearrange("p (k c) -> p k c", k=4)),
                             reads=["p_t"], writes=[("xgT", u)])
                for j in range(8):
                    sl = base + j // 2
                    hs = j % 2
                    for part in range(2):
                        c0 = part * 256 + (j % 2) * 128
                        for k in range(8):
                            P.op("pe", lambda e_: e_.matmul(p_h[:, hs * 2 + part, :], lhsT=wp[:, sl, k, c0:c0 + 128], rhs=xgT[:, u, k, :],
                                                            start=(k == 0), stop=(k == 7)),
                                 reads=[("wp", sl, part), ("xgT", u)], writes=[("p_h", hs, part)], inc=(k == 7))
                    P.op("dve", lambda e_: e_.tensor_scalar(gg[:, hs, :], p_h[:, hs * 2, :], b1[:, e, j:j + 1], 7.0, ALU.add, ALU.min),
                         reads=[("p_h", hs, 0), "b1"], writes=[("gg", hs)])
                    P.op("dve", lambda e_: e_.tensor_scalar(ll[:, hs, :], p_h[:, hs * 2 + 1, :], b1[:, e, 8 + j:9 + j], 7.0, ALU.add, ALU.min),
                         reads=[("p_h", hs, 1), "b1"], writes=[("ll", hs)])
                    P.op("pool", lambda e_: e_.tensor_scalar(ll[:, hs, :], ll[:, hs, :], -7.0, 1.0, ALU.max, ALU.add),
                         reads=[("ll", hs)], writes=[("ll", hs)])
                    P.op("act", lambda e_: e_.activation(ee[:, hs, :], gg[:, hs, :], AF.Exp, scale=-1.702), reads=[("gg", hs)], writes=[("ee", hs)])
                    P.op("pool", lambda e_: e_.tensor_scalar(ee[:, hs, :], ee[:, hs, :], 1.0, None, ALU.add), reads=[("ee", hs)], writes=[("ee", hs)])
                    P.op("dve", lambda e_: e_.reciprocal(ee[:, hs, :], ee[:, hs, :]), reads=[("ee", hs)], writes=[("ee", hs)])
                    P.op("pool", lambda e_: e_.tensor_tensor(gg[:, hs, :], gg[:, hs, :], ll[:, hs, :], ALU.mult),
                         reads=[("gg", hs), ("ll", hs)], writes=[("gg", hs)])
                    P.op("dve", lambda e_: e_.tensor_tensor(actT[:, u, j, :], gg[:, hs, :], ee[:, hs, :], ALU.mult),
                         reads=[("gg", hs), ("ee", hs)], writes=[("actT", u)])
                for hf in range(2):
                    sl = base + 4 + hf
                    for r in range(4):
                        os_ = (hf * 4 + r) % 2
                        for k in range(8):
                            P.op("pe", lambda e_: e_.matmul(p_o[:, os_, :], lhsT=actT[:, u, k, r * 128:(r + 1) * 128], rhs=wp[:, sl, k, :],
                                                            start=(k == 0), stop=(k == 7)),
                                 reads=[("actT", u), ("wp", sl, 0)], writes=[("p_o", os_)], inc=(k == 7))
                        P.op("act", lambda e_: e_.copy(orow[:, r, hf * 512:(hf + 1) * 512], p_o[:, os_, :]),
                             reads=[("p_o", os_)], writes=["orow"])
                P.dma("sp", OUT_d[r0:r0 + 512, :].rearrange("(r p) d -> p r d", p=128), orow[:],
                      reads=["orow"], writes=[("OUT_d", e, ch)], sem="orow")
        P.barrier()
    with ExitStack() as es:
        yk = sb(es, "yk", [128, 2, 4, D], F32)
        acc = sb(es, "acc", [128, 2, D], F32)
        x1t = sb(es, "x1c", [128, 2, D], F32)
        fsq = sb(es, "fsq", [128, D], F32)
        ssq = sb(es, "ssq", [128, 2], F32)
        b2_sb = sb(es, "b2c", [32, D], F32)
        fnw = sb(es, "fnwc", [128, D], F32)
        p_bias = ps(es, "pc_b", [128, 2, 2, 512], F32)
        P.dma("sp", b2_sb[:], g["b2"], writes=["b2c"])
        P.dma("sp", fnw[:], g["fnw_bc"], writes=["fnwc"])
        for T in range(16):
            u = T % 2
            for k4 in range(4):
                P.dma("pool", None, None, reads=["OUT_d"], writes=[("yk", u, k4)], sem=("yk", u, k4),
                      emit=lambda e: e.indirect_dma_start(
                          out=yk[:, u, k4, :], out_offset=None, in_=OUT_d[:, :],
                          in_offset=bass.IndirectOffsetOnAxis(ap=dest_i[:, T * 4 + k4:T * 4 + k4 + 1], axis=0),
                          bounds_check=g["bc_reg"], oob_is_err=False))
            P.dma("sp", x1t[:, u, :], X1_d[T * 128:(T + 1) * 128, :], reads=["X1_d"], writes=[("x1c", u)], sem=("x1c", u))
            for half in range(2):
                P.op("pe", lambda e: e.matmul(p_bias[:, u, half, :], lhsT=RWT[:, T, :], rhs=b2_sb[:, half * 512:(half + 1) * 512],
                                              start=True, stop=True),
                     reads=["b2c"], writes=[("p_bias", u)], inc=(half == 1))
            P.op("dve", lambda e: e.tensor_scalar(acc[:, u, :], yk[:, u, 0, :], w4[:, T, 0:1], None, ALU.mult),
                 reads=[("yk", u, 0)], writes=[("acc", u)])
            for k4 in range(1, 4):
                P.op("dve", lambda e: e.scalar_tensor_tensor(acc[:, u, :], yk[:, u, k4, :], w4[:, T, k4:k4 + 1], acc[:, u, :], ALU.mult, ALU.add),
                     reads=[("yk", u, k4), ("acc", u)], writes=[("acc", u)])
            for half in range(2):
                P.op("dve", lambda e: e.tensor_tensor(acc[:, u, half * 512:(half + 1) * 512], acc[:, u, half * 512:(half + 1) * 512],
                                                      p_bias[:, u, half, :], ALU.add),
                     reads=[("acc", u), ("p_bias", u)], writes=[("acc", u)])
            P.op("pool", lambda e: e.tensor_tensor(acc[:, u, :], acc[:, u, :], gate2_bc[:], ALU.mult), reads=[("acc", u), "gate2_bc"], writes=[("acc", u)])
            P.op("pool", lambda e: e.tensor_tensor(acc[:, u, :], acc[:, u, :], x1t[:, u, :], ALU.add), reads=[("acc", u), ("x1c", u)], writes=[("acc", u)])
            P.op("pool", lambda e: e.memset(ssq[:, u:u + 1], 0.0), writes=[("ssq", u)])
            P.op("act", lambda e: e.activation(fsq[:], acc[:, u, :], AF.Square, accum_out=ssq[:, u:u + 1]), reads=[("acc", u), ("ssq", u)], writes=["fsq", ("ssq", u)])
            rsqrt_act(P, ssq[:, u:u + 1], ssq[:, u:u + 1], [("ssq", u)], ("ssq", u), 1.0 / D)
            P.op("dve", lambda e: e.scalar_tensor_tensor(acc[:, u, :], acc[:, u, :], ssq[:, u:u + 1], fnw[:], ALU.mult, ALU.mult),
                 reads=[("acc", u), ("ssq", u), "fnwc"], writes=[("acc", u)])
            P.dma("sp", out[T * 128:(T + 1) * 128, :], acc[:, u, :], reads=[("acc", u)], writes=[("out", T)], sem=("out", u))


def _consts():
    c = np.zeros((128, 10, 128), np.float32)
    c[:, 0, :] = np.eye(128, dtype=np.float32)
    perm = np.zeros((128, 128), np.float32)
    for m in range(32):
        perm[(m + 16) % 32, m] = 1.0
    c[:, 1, :] = perm
    i = np.arange(128)
    c[:, 2, :] = np.where(i[:, None] <= i[None, :], -1.0 / 16.0, 0.0)
    c[:, 3, :] = (i[:, None] <= i[None, :]).astype(np.float32)
    c[:, 4, :] = (i[:, None] < i[None, :]).astype(np.float32)
    q = np.arange(256)
    for half in range(2):
        kpos = half * 128 + i
        m = np.where(kpos[:, None] <= q[None, :], 0.0, -BIG).astype(np.float32)
        c[:, 5 + 2 * half, :] = m[:, :128]
        c[:, 6 + 2 * half, :] = m[:, 128:]
    return c


def _rope_tables():
    half = 16
    inv = np.float32(500000.0) ** (-np.arange(half, dtype=np.float32) * np.float32(2.0) / np.float32(32))
    ang = np.arange(SEQ, dtype=np.float32)[:, None] * inv[None, :].astype(np.float32)
    cos = np.cos(ang).astype(np.float32).T
    sin = np.sin(ang).astype(np.float32).T
    return np.concatenate([cos, cos], 0), np.concatenate([-sin, sin], 0)


def prep_inputs(inputs):
    f = lambda k: np.asarray(inputs[k], np.float32)
    x = f("x")
    c = f("c")
    w_in = f("w_in")[0]
    fm = lambda v: np.ascontiguousarray(v.reshape(-1, 128).T)
    bc = lambda v: np.ascontiguousarray(np.broadcast_to(v[None, :], (128, v.shape[0])))
    o = [0]
    for sz in (1024, 1024, 1024, 512, 512, 1024, 1024, 16, 1024, 1024):
        o.append(o[-1] + sz)
    mq, mk, mv, gq, gk, gv, gr, gl, ga, gb = [w_in[:, o[i]:o[i + 1]] for i in range(10)]
    shared = {
        "w_ada": f("w_ada")[0],
        "b_ada_bc": bc(f("b_ada")[0]),
        "nw": np.concatenate([fm(f("norm1_w")[0]), fm(f("norm2_w")[0])], 1),
        "fnw_bc": bc(f("final_norm_w")),
        "wA": np.ascontiguousarray(np.concatenate([mk, mv, gk, gv, gl], 1)),
        "wO": np.ascontiguousarray(np.concatenate([mq, gq, gr], 1)),
        "wG": np.ascontiguousarray(np.concatenate([ga, gb], 1)),
        "w_oa": f("w_o_moba")[0], "w_ob": f("w_o_gla")[0], "w_out": f("w_out")[0],
        "nbm": fm(f("b_merge")[0]),
        "gnw": fm(f("gla_norm_w")[0]),
        "w_r": f("w_router")[0], "b_r_bc": bc(f("b_router")[0]),
        "w2": f("w_exp_out")[0], "b2": f("b_exp_out")[0],
        "consts": _consts(),
        "ecap": bc(np.arange(NE, dtype=np.float32) * CAP),
    }
    gw = np.zeros((32, 512), np.float32)
    gw[0:16] = f("gla_gate_w")[0]
    gw[16] = f("gla_gate_b")[0]
    shared["gw_aug"] = gw
    w1 = f("w_exp_in")[0]
    shared["w1d"] = np.ascontiguousarray(np.concatenate([w1[:, :, 0::2], w1[:, :, 1::2]], 2))
    b1 = f("b_exp_in")[0]
    b1d = np.concatenate([b1[:, 0::2], b1[:, 1::2]], 1)
    shared["b1T"] = np.ascontiguousarray(b1d.reshape(NE, 16, 128).transpose(2, 0, 1).reshape(128, NE * 16))
    es = np.zeros((32, 32, 128), np.float32)
    for j in range(32):
        es[j, j, :] = 1.0
    shared["esel_in"] = es.reshape(32, 32 * 128)
    cosT, sinT = _rope_tables()
    per_core = []
    for core in range(NCORE):
        b, r = core // 4, core % 4
        nnull = (24 - 8 * r) * 256
        nreal = 2048 * (r + 1)
        xv = np.zeros((D, SEQ), np.float32)
        xv[:, nnull:] = x[b, :nreal, :].T
        rc_ = np.zeros((32, SEQ), np.float32)
        rs_ = np.zeros((32, SEQ), np.float32)
        rc_[:, nnull:] = cosT[:, :nreal]
        rs_[:, nnull:] = sinT[:, :nreal]
        vf = np.zeros((SEQ,), np.float32)
        vf[nnull:] = 1.0
        gm = np.full((8, 32), NEGINF, np.float32)
        gvv = np.zeros((8, 32), np.float32)
        for l in range(8):
            gm[l, 24 - 8 * r:24 + l] = 0.0
            gvv[l, 24 - 8 * r:24 + l] = 1.0
        d = dict(shared)
        d.update({
            "xTv": xv,
            "xtok": np.ascontiguousarray(x[b, 2048 * r:2048 * (r + 1), :]),
            "cT": fm(c[b]),
            "ropec": rc_, "ropes": rs_,
            "vflag": np.ascontiguousarray(vf.reshape(64, 128).T),
            "gmask": bc(gm.reshape(-1)), "gvalid": bc(gvv.reshape(-1)),
        })
        per_core.append(d)
    return per_core


_NC_CACHE = {}


def kernel(**inputs):
    in_maps = prep_inputs(inputs)
    if "nc" not in _NC_CACHE:
        _NC_CACHE["nc"] = build_nc()[0]
    nc = _NC_CACHE["nc"]
    res = run_bass_kernel_spmd(nc, in_maps, core_ids=list(range(NCORE)))
    outp = np.zeros((2, SEQ, D), np.float32)
    for core in range(NCORE):
        b, r = core // 4, core % 4
        outp[b, 2048 * r:2048 * (r + 1), :] = res.results[core]["out"]
    return outp
```

```python
import numpy as np
from contextlib import ExitStack
import concourse.bass as bass
import concourse.mybir as mybir
from concourse.bass_utils import run_bass_kernel_spmd

F32 = mybir.dt.float32
BF16 = mybir.dt.bfloat16
I32 = mybir.dt.int32
AF = mybir.ActivationFunctionType
ALU = mybir.AluOpType
AX = mybir.AxisListType

D = 1024
SEQ = 8192
NCORE = 8
OWN = 2048
GS = 256
TPG = 2
NG = 32
OWNG0 = 24
NVB = 32
NE = 32
CAP = 1024
BIG = 30000.0
NEGINF = -1.0e30
EPS = 1e-5
STAGE = 99
DEBUG = False


class Prog:
    def __init__(self, nc, same_engine_sync=True):
        self.nc = nc
        self.engs = {"pe": nc.tensor, "act": nc.scalar, "dve": nc.vector,
                     "pool": nc.gpsimd, "sp": nc.sync}
        self.sem = {k: nc.alloc_semaphore("prog_" + k) for k in self.engs}
        self.cnt = {k: 0 for k in self.engs}
        self.seen = {k: {} for k in self.engs}
        self.bufs = {}
        self.pending = {k: ([], []) for k in self.engs}
        self.dsem = {}
        self.dcnt = {}
        self.same = same_engine_sync
        self.nwait = 0
        self.nins = 0
        self.free_sems = {"sw": [], "hw": []}
        self.dkind = {}
        self.nalloc = 0

    def _deps(self, reads, writes):
        deps = []
        for k in reads:
            b = self.bufs.get(k)
            if b is not None and b[0] is not None:
                deps.append(b[0])
        for k in writes:
            b = self.bufs.get(k)
            if b is not None:
                if b[0] is not None:
                    deps.append(b[0])
                deps.extend(b[1])
        return deps

    def _wait(self, eng, deps, own_ok=True):
        need = {}
        for (s, v) in deps:
            if own_ok and s is self.sem[eng]:
                if eng == "pe" or not self.same:
                    continue
            key = id(s)
            if self.seen[eng].get(key, 0) >= v:
                continue
            if key not in need or need[key][1] < v:
                need[key] = (s, v)
        for key, (s, v) in need.items():
            self.engs[eng].wait_ge(s, v)
            self.seen[eng][key] = v
            self.nwait += 1

    def _record(self, ev, reads, writes):
        for k in reads:
            b = self.bufs.get(k)
            if b is None:
                self.bufs[k] = [None, [ev]]
            else:
                b[1].append(ev)
                if len(b[1]) > 24:
                    b[1] = b[1][-24:] if False else b[1]
        for k in writes:
            self.bufs[k] = [ev, []]

    def op(self, eng, emit, reads=(), writes=(), inc=True):
        reads = list(reads)
        writes = list(writes)
        self._wait(eng, self._deps(reads, writes))
        ins = emit(self.engs[eng])
        self.nins += 1
        if not inc:
            self.pending[eng][0].extend(reads)
            self.pending[eng][1].extend(writes)
            return None
        self.cnt[eng] += 1
        ev = (self.sem[eng], self.cnt[eng])
        ins.then_inc(self.sem[eng], 1)
        pr, pw = self.pending[eng]
        self._record(ev, pr + reads, pw + writes)
        self.pending[eng] = ([], [])
        return ev

    def dma(self, queue, out, in_, reads=(), writes=(), sem=None, emit=None, **kw):
        reads = list(reads)
        writes = list(writes)
        if sem is None:
            sem = ("auto",) + tuple(writes) + tuple(reads)
        kind = "sw" if queue == "pool" else "hw"
        if sem in self.dsem:
            assert self.dkind[sem] == kind, (sem, kind)
        if sem not in self.dsem:
            self.dkind[sem] = kind
            if self.free_sems[kind]:
                self.dsem[sem], self.dcnt[sem] = self.free_sems[kind].pop()
            else:
                self.dsem[sem] = self.nc.alloc_semaphore("dma_%d" % self.nalloc)
                self.nalloc += 1
                self.dcnt[sem] = 0
        self._wait(queue, self._deps(reads, writes))
        if emit is not None:
            ins = emit(self.engs[queue])
        else:
            ins = self.engs[queue].dma_start(out=out, in_=in_, **kw)
        self.nins += 1
        self.dcnt[sem] += 16
        ins.then_inc(self.dsem[sem], 16)
        ev = (self.dsem[sem], self.dcnt[sem])
        self._record(ev, reads, writes)
        return ev

    def wait_all(self, eng, keys=None):
        deps = []
        for k, b in self.bufs.items():
            if keys is not None and k not in keys:
                continue
            if b[0] is not None:
                deps.append(b[0])
            deps.extend(b[1])
        self._wait(eng, deps, own_ok=False)

    def barrier(self):
        assert all(len(p[0]) == 0 and len(p[1]) == 0 for p in self.pending.values())
        for eng in self.engs:
            deps = [(self.sem[o], self.cnt[o]) for o in self.engs if o != eng and self.cnt[o] > 0]
            deps += [(self.dsem[k], self.dcnt[k]) for k in self.dsem if self.dcnt[k] > 0]
            self._wait(eng, deps, own_ok=False)
        self.bufs = {}
        for k in list(self.dsem):
            self.free_sems[self.dkind.pop(k)].append((self.dsem.pop(k), self.dcnt.pop(k)))


def build_nc():
    nc = bass.Bass("TRN2", target_bir_lowering=False)
    P = Prog(nc)
    dt_in = lambda n, s, d=F32: nc.dram_tensor(n, list(s), d, kind="ExternalInput").ap()
    dt_sc = lambda n, s, d: nc.dram_tensor(n, list(s), d).ap()

    xTv = dt_in("xTv", [D, SEQ])
    xtok = dt_in("xtok", [OWN, D])
    cT = dt_in("cT", [128, 8])
    w_ada = dt_in("w_ada", [D, 6 * D])
    b_ada_bc = dt_in("b_ada_bc", [128, 6 * D])
    nw = dt_in("nw", [128, 16])
    fnw_bc = dt_in("fnw_bc", [128, D])
    wA = dt_in("wA", [D, 3600])
    wO = dt_in("wO", [D, 2560])
    wG = dt_in("wG", [D, 2048])
    w_oa = dt_in("w_oa", [D, D])
    w_ob = dt_in("w_ob", [D, D])
    w_out = dt_in("w_out", [D, D])
    nbm = dt_in("nbm", [128, 16])
    ropec = dt_in("ropec", [32, SEQ])
    ropes = dt_in("ropes", [32, SEQ])
    vflag = dt_in("vflag", [128, 64])
    gmask = dt_in("gmask", [128, 8 * 32])
    gvalid = dt_in("gvalid", [128, 8 * 32])
    gw_aug = dt_in("gw_aug", [32, 512])
    gnw = dt_in("gnw", [128, 2])
    w_r = dt_in("w_r", [D, NE])
    b_r_bc = dt_in("b_r_bc", [128, NE])
    w1d = dt_in("w1d", [NE, D, 2048]) if STAGE >= 4 else None
    w2 = dt_in("w2", [NE, D, D]) if STAGE >= 4 else None
    b1T = dt_in("b1T", [128, NE * 16])
    b2 = dt_in("b2", [NE, D])
    consts = dt_in("consts", [128, 10, 128])
    esel_in = dt_in("esel_in", [32, 32 * 128])
    ecap = dt_in("ecap", [128, NE])
    out = nc.dram_tensor("out", [OWN, D], F32, kind="ExternalOutput").ap()

    KT_d = dt_sc("KT_d", [8, 128, SEQ], BF16)
    V_d = dt_sc("V_d", [SEQ, D], BF16)
    QT_d = dt_sc("QT_d", [8, 128, OWN], BF16)
    OB_d = dt_sc("OB_d", [D, OWN], BF16)
    HT_d = dt_sc("HT_d", [D, OWN], BF16)
    X1_d = dt_sc("X1_d", [OWN, D], F32)
    XG_d = dt_sc("XG_d", [NE * CAP, D], BF16)
    OUT_d = dt_sc("OUT_d", [NE * CAP, D], F32)

    dbg = {}
    if DEBUG:
        dbg["mod"] = nc.dram_tensor("dbg_mod", [128, 48], F32, kind="ExternalOutput").ap()
        dbg["kT"] = nc.dram_tensor("dbg_kT", [8, 128, SEQ], BF16, kind="ExternalOutput").ap()
        dbg["qT"] = nc.dram_tensor("dbg_qT", [8, 128, OWN], BF16, kind="ExternalOutput").ap()
        dbg["ob"] = nc.dram_tensor("dbg_ob", [D, OWN], BF16, kind="ExternalOutput").ap()
        dbg["oa"] = nc.dram_tensor("dbg_oa", [128, 8, OWN], BF16, kind="ExternalOutput").ap()
        dbg["x1"] = nc.dram_tensor("dbg_x1", [OWN, D], F32, kind="ExternalOutput").ap()
        dbg["lg"] = nc.dram_tensor("dbg_lg", [128, 16, 32], F32, kind="ExternalOutput").ap()

    es_all = ExitStack()
    with es_all:
        sb = lambda es, n, s, d: es.enter_context(nc.sbuf_tensor(n, list(s), d))
        ps = lambda es, n, s, d: es.enter_context(nc.psum_tensor(n, list(s), d))
        cst = sb(es_all, "cst", [128, 10, 128], F32)
        ident_f = cst[:, 0, :]
        ident_b = sb(es_all, "ident_b", [128, 128], BF16)
        ones_rms = sb(es_all, "ones_rms", [128, 128], BF16)
        ones_256 = sb(es_all, "ones_256", [128, 128], BF16)
        ones_1 = sb(es_all, "ones_1", [128, 128], BF16)
        onesf = sb(es_all, "onesf", [128, 128], F32)
        modT = sb(es_all, "modT", [128, 48], F32)
        a1 = sb(es_all, "a1", [128, 8], F32)
        a2 = sb(es_all, "a2", [128, 8], F32)
        nw_sb = sb(es_all, "nw_sb", [128, 16], F32)
        gate1_bc = sb(es_all, "gate1_bc", [128, D], F32)
        gate2_bc = sb(es_all, "gate2_bc", [128, D], F32)
        kmb = sb(es_all, "kmb", [128, 8, 32], BF16)
        bc_reg = nc.gpsimd.to_reg(NE * CAP - 1)
        dest_i = sb(es_all, "dest_i", [128, 64], I32)
        w4 = sb(es_all, "w4", [128, 16, 4], F32)
        RWT = sb(es_all, "RWT", [32, 16, 128], F32)
        s1 = modT[:, 0:8]
        g1 = modT[:, 16:24]
        s2 = modT[:, 24:32]

        P.dma("sp", cst[:], consts, writes=["cst"])
        P.dma("sp", nw_sb[:], nw, writes=["nw"])
        P.dma("pool", ident_b[:], consts[:, 0, :], writes=["ident_b"])
        P.op("dve", lambda e: e.memset(ones_rms[:], 1.0 / 1024.0), writes=["ones_rms"])
        P.op("dve", lambda e: e.memset(ones_256[:], 1.0 / 256.0), writes=["ones_256"])
        P.op("dve", lambda e: e.memset(ones_1[:], 1.0), writes=["ones_1"])
        P.op("dve", lambda e: e.memset(onesf[:], 1.0), writes=["onesf"])

        with ExitStack() as es:
            c_sb = sb(es, "c_sb", [128, 8], F32)
            c_e = sb(es, "c_e", [128, 8], F32)
            cact_b = sb(es, "cact_b", [128, 8, 128], F32)
            wst = sb(es, "wst", [128, 2, 8, 512], F32)
            modb = sb(es, "modb", [128, 6 * D], F32)
            bab = sb(es, "bab", [128, 6 * D], F32)
            p_mod = ps(es, "p_mod", [128, 2, 512], F32)
            p_col = ps(es, "p_col", [128, 48], F32)
            P.dma("sp", c_sb[:], cT, writes=["c_sb"])
            P.dma("sp", bab[:], b_ada_bc, writes=["bab"])
            P.op("act", lambda e: e.activation(c_e[:], c_sb[:], AF.Exp, scale=-1.0), reads=["c_sb"], writes=["c_e"])
            P.op("dve", lambda e: e.tensor_scalar(c_e[:], c_e[:], 1.0, None, ALU.add), reads=["c_e"], writes=["c_e"])
            P.op("dve", lambda e: e.reciprocal(c_e[:], c_e[:]), reads=["c_e"], writes=["c_e"])
            P.op("dve", lambda e: e.tensor_tensor(c_e[:], c_e[:], c_sb[:], ALU.mult), reads=["c_e", "c_sb"], writes=["c_e"])
            for k in range(8):
                P.op("dve", lambda e: e.tensor_scalar(cact_b[:, k, :], onesf[:], c_e[:, k:k + 1], None, ALU.mult),
                     reads=["c_e", "onesf"], writes=[("cactb", k)])
            for j in range(12):
                s = j % 2
                P.dma("sp", wst[:, s], w_ada[:, j * 512:(j + 1) * 512].rearrange("(k p) n -> p k n", p=128),
                      writes=[("wst", s)], sem=("wst", s))
                for k in range(8):
                    P.op("pe", lambda e: e.matmul(p_mod[:, s, :], lhsT=cact_b[:, k, :], rhs=wst[:, s, k, :],
                                                  start=(k == 0), stop=(k == 7)),
                         reads=[("wst", s), ("cactb", k)], writes=[("p_mod", s)], inc=(k == 7))
                P.op("dve", lambda e: e.tensor_tensor(modb[:, j * 512:(j + 1) * 512], p_mod[:, s, :],
                                                      bab[:, j * 512:(j + 1) * 512], ALU.add),
                     reads=[("p_mod", s), "bab"], writes=[("modb", j)])
            for j in range(48):
                P.op("pe", lambda e: e.matmul(p_col[:, j:j + 1], lhsT=modb[0:1, j * 128:(j + 1) * 128],
                                              rhs=onesf[0:1, 0:1], start=True, stop=True),
                     reads=[("modb", j // 4), "onesf"], writes=["p_col"], inc=(j == 47))
            P.op("dve", lambda e: e.tensor_copy(modT[:], p_col[:]), reads=["p_col"], writes=["modT"])
            P.op("dve", lambda e: e.scalar_tensor_tensor(a1[:], modT[:, 8:16], 1.0, nw_sb[:, 0:8], ALU.add, ALU.mult),
                 reads=["modT", "nw"], writes=["a1"])
            P.op("dve", lambda e: e.scalar_tensor_tensor(a2[:], modT[:, 32:40], 1.0, nw_sb[:, 8:16], ALU.add, ALU.mult),
                 reads=["modT", "nw"], writes=["a2"])
            P.op("act", lambda e: e.copy(gate1_bc[:], modb[:, 2048:3072]), reads=[("modb", 4), ("modb", 5)], writes=["gate1_bc"])
            P.op("act", lambda e: e.copy(gate2_bc[:], modb[:, 5120:6144]), reads=[("modb", 10), ("modb", 11)], writes=["gate2_bc"])
            if DEBUG:
                P.dma("sp", dbg["mod"], modT[:], reads=["modT"])
            P.barrier()

        G_ = dict(locals())
        if STAGE >= 1:
            phase_a1(nc, P, G_)
        with ExitStack() as es_ab:
            oaT = sb(es_ab, "oaT", [128, 8, OWN], BF16)
            G_["oaT"] = oaT
            if STAGE >= 2:
                phase_a2(nc, P, G_)
            if STAGE >= 3:
                phase_b(nc, P, G_)
        if STAGE >= 4:
            phase_moe(nc, P, G_)
        P.wait_all("sp")
    return nc, P


def rmsnorm_fm(P, nc, xs_ap, sq_ap, p_ss, rstd_ap, ones_rms, keyx, tag, pkey):
    P.op("act", lambda e: e.activation(sq_ap, xs_ap, AF.Square), reads=list(keyx), writes=[tag + "sq"])
    for k in range(8):
        P.op("pe", lambda e: e.matmul(p_ss, lhsT=ones_rms[:], rhs=sq_ap[:, k, :], start=(k == 0), stop=(k == 7)),
             reads=[tag + "sq", "ones_rms"], writes=[pkey], inc=(k == 7))
    rsqrt_act(P, rstd_ap, p_ss, [pkey], tag + "rstd", 1.0)


def rsqrt_act(P, out_ap, in_ap, rkeys, wkey, mul):
    P.op("act", lambda e: e.activation(out_ap, in_ap, AF.Ln, bias=EPS, scale=mul), reads=list(rkeys), writes=[wkey])
    P.op("act", lambda e: e.activation(out_ap, out_ap, AF.Exp, scale=-0.5), reads=[wkey], writes=[wkey])


def phase_a1(nc, P, L):
    g = L
    sb, ps = g["sb"], g["ps"]
    xTv, wA, wO, ropec, ropes = g["xTv"], g["wA"], g["wO"], g["ropec"], g["ropes"]
    cst, ident_b = g["cst"], g["ident_b"]
    ones_rms, ones_256 = g["ones_rms"], g["ones_256"]
    a1, s1, kmb = g["a1"], g["s1"], g["kmb"]
    KT_d, V_d, QT_d, OB_d, HT_d = g["KT_d"], g["V_d"], g["QT_d"], g["OB_d"], g["HT_d"]
    dbg = g["dbg"]
    with ExitStack() as es:
        wA_b = sb(es, "wA_b", [128, 8, 3600], BF16)
        wO_b = sb(es, "wO_b", [128, 8, 2560], BF16)
        xs = sb(es, "xs", [128, 2, 8, GS], F32)
        sq = sb(es, "sq", [128, 8, GS], BF16)
        rstd = sb(es, "rstd", [128, GS], F32)
        hT = sb(es, "hT", [128, 8, GS], BF16)
        rc = sb(es, "rc", [32, 1, GS], F32)
        rs = sb(es, "rs", [32, 1, GS], F32)
        kf = sb(es, "kf", [128, 2, GS], F32)
        rt = sb(es, "rt", [32, 2, GS], F32)
        kb = sb(es, "kb", [128, 2, GS], BF16)
        kmean = sb(es, "kmean", [128, 8, 32], F32)
        gkf = sb(es, "gkf", [128, 4, GS], F32)
        gqf = sb(es, "gqf", [128, 4, GS], F32)
        sgr = sb(es, "sgr", [128, 8, GS], F32)
        sgt = sb(es, "sgt", [128, GS], F32)
        glow = sb(es, "glow", [32, GS], F32)
        gw_sb = sb(es, "gw_sb", [32, 512], F32)
        gnw_sb = sb(es, "gnw_sb", [128, 2], F32)
        vfl = sb(es, "vfl", [128, 64], F32)
        Ltok = sb(es, "Ltok", [128, TPG, 512], F32)
        lex = sb(es, "lex", [128, 512], F32)
        vst = sb(es, "vst", [128, TPG, 1024], BF16)
        gvst = sb(es, "gvst", [128, TPG, 1024], BF16)
        EbT = sb(es, "EbT", [128, 2, 128], F32)
        EnbT = sb(es, "EnbT", [128, 2, 128], F32)
        qtl = sb(es, "qtl", [128, 2, 128], BF16)
        ktl = sb(es, "ktl", [128, 2, 128], BF16)
        ktok = sb(es, "ktok", [128, 2, 128], BF16)
        atm = sb(es, "atm", [128, 2, 128], BF16)
        Sst = sb(es, "Sst", [128, 4, 256], F32)
        Ssc = sb(es, "Ssc", [128, 2, 256], F32)
        Sbf = sb(es, "Sbf", [128, 4, 256], BF16)
        osq = sb(es, "osq", [128, 2, 2, 128], BF16)
        orst = sb(es, "orst", [128, 2, 128], F32)
        otmp = sb(es, "otmp", [128, 2, 128], F32)
        obst = sb(es, "obst", [128, 8, GS], BF16)
        p_pj = ps(es, "p_pj", [128, 2, 512], F32)
        p_sx = ps(es, "p_sx", [128, 512], F32)
        p_ss = p_sx[:, 0:GS]
        p_xs = p_sx[0:32, 256:256 + GS]
        p_g = ps(es, "p_g", [128, 2, 4, 128], F32)
        p_km = ps(es, "p_km", [128, 512], F32)
        p_kvb = ps(es, "p_kvb", [128, 2, 256], F32)
        p_tr = ps(es, "p_tr", [128, 2, 128], BF16)
        permf = cst[:, 1, 0:32]
        triN = cst[:, 2, :]
        utm = cst[:, 3, :]

        for k in range(8):
            P.dma("pool", wA_b[:, k, :], wA[k * 128:(k + 1) * 128, :], writes=[("wA", k)], sem=("wA", k))
        for k in range(8):
            P.dma("pool", wO_b[:, k, :], wO[k * 128:(k + 1) * 128, :], writes=[("wO", k)], sem=("wO", k))
        P.dma("sp", gw_sb[:], g["gw_aug"], writes=["gw_sb"])
        P.dma("sp", gnw_sb[:], g["gnw"], writes=["gnw_sb"])
        P.dma("sp", vfl[:], g["vflag"], writes=["vfl"])
        P.op("pool", lambda e: e.memset(glow[:], 1.0), writes=["glow"])
        P.op("pool", lambda e: e.memset(Sst[:], 0.0), writes=[("S", h) for h in range(4)])
        P.op("pool", lambda e: e.memset(Sbf[:], 0.0), writes=[("Sbf", h) for h in range(4)])
        wAk = [("wA", k) for k in range(8)]
        wOk = [("wO", k) for k in range(8)]
        pj_n = [0]
        kf_n = [0]
        uniq = [0]

        def ukey(n):
            uniq[0] += 1
            return (n, uniq[0])

        def proj_fm(wt, col0, ncol, wkeys, evac):
            s_ = pj_n[0] % 2
            pj_n[0] += 1
            for k in range(8):
                P.op("pe", lambda e: e.matmul(p_pj[0:ncol, s_, 0:GS], lhsT=wt[:, k, col0:col0 + ncol], rhs=hT[:, k, :],
                                              start=(k == 0), stop=(k == 7)),
                     reads=wkeys + ["hT"], writes=[("p_pj", s_)], inc=(k == 7))
            flush_evac()
            pend[0] = lambda: evac(p_pj[0:ncol, s_, 0:GS], ("p_pj", s_))

        pend = [None]

        def flush_evac():
            if pend[0] is not None:
                f_ = pend[0]
                pend[0] = None
                f_()

        for gi in range(NG):
            own = gi >= OWNG0
            s = gi % 2
            t0 = gi * GS
            o0 = (gi - OWNG0) * GS
            xk = [("xs", s, k) for k in range(8)]
            P.dma("sp", xs[:, s], xTv[:, t0:t0 + GS].rearrange("(k p) n -> p k n", p=128), writes=xk, sem=("xs", s))
            P.dma("sp", rc[:, 0, :], ropec[:, t0:t0 + GS], writes=[("rc", 0)], sem=("rc", 0))
            P.dma("sp", rs[:, 0, :], ropes[:, t0:t0 + GS], writes=[("rs", 0)], sem=("rs", 0))
            rmsnorm_fm(P, nc, xs[:, s], sq[:], p_ss, rstd[:], ones_rms, xk, "a1", "p_sx")
            for k in range(8):
                P.op("dve", lambda e: e.scalar_tensor_tensor(xs[:, s, k, :], xs[:, s, k, :], a1[:, k:k + 1], rstd[:],
                                                             ALU.mult, ALU.mult),
                     reads=[("xs", s, k), "a1rstd", "a1"], writes=[("xs", s, k)])
                P.op("act", lambda e: e.activation(hT[:, k, :], xs[:, s, k, :], AF.Identity, bias=s1[:, k:k + 1], scale=1.0),
                     reads=[("xs", s, k), "modT"], writes=["hT"])
            if own:
                P.dma("sp", HT_d[:, o0:o0 + GS].rearrange("(k p) n -> p k n", p=128), hT[:], reads=["hT"], writes=[ukey("HT_d")],
                      sem="HT_o")

            def qk_evac(h, is_k):
                def ev(pt, pkey):
                    u = kf_n[0] % 2
                    kf_n[0] += 1
                    P.op("act", lambda e: e.copy(kf[:, u, :], pt), reads=[pkey], writes=[("kf", u)])
                    P.op("pe", lambda e: e.matmul(p_xs, lhsT=permf, rhs=kf[:, u, :], start=True, stop=True),
                         reads=[("kf", u), "cst"], writes=["p_sx"])
                    P.op("dve", lambda e: e.tensor_tensor(rt[:, 0, :], kf[0:32, u, :], rc[:, 0, :], ALU.mult),
                         reads=[("kf", u), ("rc", 0)], writes=["rt0"])
                    P.op("dve", lambda e: e.tensor_tensor(rt[:, 1, :], p_xs, rs[:, 0, :], ALU.mult),
                         reads=["p_sx", ("rs", 0)], writes=["rt1"])
                    P.op("dve", lambda e: e.tensor_tensor(kf[0:32, u, :], rt[:, 0, :], rt[:, 1, :], ALU.add),
                         reads=["rt0", "rt1", ("kf", u)], writes=[("kf", u)])
                    if is_k:
                        P.op("dve", lambda e: e.tensor_reduce(kmean[:, h, gi:gi + 1], kf[:, u, :], AX.X, ALU.add),
                             reads=[("kf", u)], writes=[("kmean", h)])
                    P.op("pool", lambda e: e.tensor_copy(kb[:, u, :], kf[:, u, :]), reads=[("kf", u)], writes=[("kb", u)])
                    if is_k:
                        P.dma("sp", KT_d[h, :, t0:t0 + GS], kb[:, u, :], reads=[("kb", u)], writes=[ukey("KT_d")], sem=("kbo", u))
                    else:
                        P.dma("sp", QT_d[h, :, o0:o0 + GS], kb[:, u, :], reads=[("kb", u)], writes=[ukey("QT_d")], sem=("kbo", u))
                return ev

            for h in range(8):
                proj_fm(wA_b, h * 128, 128, wAk, qk_evac(h, True))
            if own:
                for h in range(8):
                    proj_fm(wO_b, h * 128, 128, wOk, qk_evac(h, False))

            flush_evac()
            for t in range(TPG):
                for half in range(2):
                    sl = pj_n[0] % 2
                    pj_n[0] += 1
                    for k in range(8):
                        P.op("pe", lambda e: e.matmul(p_pj[:, sl, :], lhsT=hT[:, k, t * 128:(t + 1) * 128],
                                                      rhs=wA_b[:, k, 1024 + half * 512:1024 + (half + 1) * 512],
                                                      start=(k == 0), stop=(k == 7)),
                             reads=wAk + ["hT"], writes=[("p_pj", sl)], inc=(k == 7))
                    P.op("act", lambda e: e.copy(vst[:, t, half * 512:(half + 1) * 512], p_pj[:, sl, :]),
                         reads=[("p_pj", sl)], writes=["vst"])
                for half in range(2):
                    sl = pj_n[0] % 2
                    pj_n[0] += 1
                    for k in range(8):
                        P.op("pe", lambda e: e.matmul(p_pj[:, sl, :], lhsT=hT[:, k, t * 128:(t + 1) * 128],
                                                      rhs=wA_b[:, k, 2560 + half * 512:2560 + (half + 1) * 512],
                                                      start=(k == 0), stop=(k == 7)),
                             reads=wAk + ["hT"], writes=[("p_pj", sl)], inc=(k == 7))
                    P.op("dve", lambda e: e.tensor_scalar(gvst[:, t, half * 512:(half + 1) * 512], p_pj[:, sl, :],
                                                          vfl[:, gi * TPG + t:gi * TPG + t + 1], None, ALU.mult),
                         reads=[("p_pj", sl), "vfl"], writes=[("gvst", t)])
            P.dma("sp", V_d[t0:t0 + GS, :].rearrange("(t p) n -> p t n", p=128), vst[:], reads=["vst"], writes=[ukey("V_d")],
                  sem="V_o")

            for h in range(4):
                proj_fm(wA_b, 2048 + h * 128, 128, wAk,
                        lambda pt, pkey, h=h: P.op("act", lambda e: e.copy(gkf[:, h, :], pt), reads=[pkey], writes=[("gkf", h)]))
            proj_fm(wA_b, 3584, 16, wAk,
                    lambda pt, pkey: P.op("act", lambda e: e.copy(glow[0:16, :], pt), reads=[pkey], writes=["glow"]))
            if own:
                for h in range(4):
                    proj_fm(wO_b, 1024 + h * 128, 128, wOk,
                            lambda pt, pkey, h=h: P.op("act", lambda e: e.copy(gqf[:, h, :], pt), reads=[pkey], writes=[("gqf", h)]))
                for c in range(8):
                    def gr_ev(pt, pkey, c=c):
                        P.op("act", lambda e: e.activation(sgt[:], pt, AF.Exp, scale=-1.0), reads=[pkey], writes=["sgt"])
                        P.op("dve", lambda e: e.tensor_scalar(sgt[:], sgt[:], 1.0, None, ALU.add), reads=["sgt"], writes=["sgt"])
                        P.op("dve", lambda e: e.reciprocal(sgt[:], sgt[:]), reads=["sgt"], writes=["sgt"])
                        P.op("dve", lambda e: e.tensor_tensor(sgr[:, c, :], sgt[:], pt, ALU.mult), reads=["sgt", pkey], writes=[("sgr", c)])
                    proj_fm(wO_b, 1536 + c * 128, 128, wOk, gr_ev)
            flush_evac()
            for t in range(TPG):
                sl = pj_n[0] % 2
                pj_n[0] += 1
                P.op("pe", lambda e: e.matmul(p_pj[:, sl, :], lhsT=glow[:, t * 128:(t + 1) * 128], rhs=gw_sb[:],
                                              start=True, stop=True),
                     reads=["glow", "gw_sb"], writes=[("p_pj", sl)])
                P.op("act", lambda e: e.activation(lex[:], p_pj[:, sl, :], AF.Exp, scale=-1.0), reads=[("p_pj", sl)], writes=["lex"])
                P.op("act", lambda e: e.activation(Ltok[:, t, :], lex[:], AF.Ln, bias=1.0, scale=1.0), reads=["lex"], writes=[("Ltok", t)])

            def gla_chain(t, h):
                u = h % 2
                c0 = t * 128
                P.op("pe", lambda e: e.matmul(p_g[:, u, 0, :], lhsT=Ltok[:, t, h * 128:(h + 1) * 128], rhs=triN,
                                              start=True, stop=True),
                     reads=[("Ltok", t), "cst"], writes=[("p_g", u)])
                yield
                P.op("act", lambda e: e.activation(EbT[:, u, :], p_g[:, u, 0, :], AF.Exp), reads=[("p_g", u)], writes=[("EbT", u)])
                P.op("act", lambda e: e.activation(EnbT[:, u, :], p_g[:, u, 0, :], AF.Exp, scale=-1.0),
                     reads=[("p_g", u)], writes=[("EnbT", u)])
                yield
                P.op("dve", lambda e: e.tensor_tensor(ktl[:, u, :], gkf[:, h, c0:c0 + 128], EnbT[:, u, :], ALU.mult),
                     reads=[("gkf", h), ("EnbT", u)], writes=[("ktl", u)])
                yield
                P.op("pe", lambda e: e.transpose(p_tr[:, u, :], ktl[:, u, :], ident_b[:]),
                     reads=[("ktl", u), "ident_b"], writes=["p_tr"])
                yield
                P.op("act", lambda e: e.copy(ktok[:, u, :], p_tr[:, u, :]), reads=["p_tr"], writes=[("ktok", u)])
                P.op("pe", lambda e: e.matmul(p_kvb[:, u, :], lhsT=ktok[:, u, :], rhs=gvst[:, t, h * 256:(h + 1) * 256],
                                              start=True, stop=True),
                     reads=[("ktok", u), ("gvst", t)], writes=["p_kvb"])
                yield
                if own:
                    P.op("dve", lambda e: e.scalar_tensor_tensor(qtl[:, u, :], gqf[:, h, c0:c0 + 128], 128.0 ** -0.5,
                                                                 EbT[:, u, :], ALU.mult, ALU.mult),
                         reads=[("gqf", h), ("EbT", u)], writes=[("qtl", u)])
                    yield
                    P.op("pe", lambda e: e.matmul(p_g[:, u, 1, :], lhsT=ktl[:, u, :], rhs=qtl[:, u, :], start=True, stop=True),
                         reads=[("ktl", u), ("qtl", u)], writes=[("p_g", u)])
                    yield
                    P.op("dve", lambda e: e.tensor_tensor(atm[:, u, :], p_g[:, u, 1, :], utm, ALU.mult),
                         reads=[("p_g", u), "cst"], writes=[("atm", u)])
                    yield
                    for dv in range(2):
                        P.op("pe", lambda e: e.matmul(p_g[:, u, 2 + dv, :],
                                                      lhsT=gvst[:, t, h * 256 + dv * 128:h * 256 + (dv + 1) * 128],
                                                      rhs=atm[:, u, :], start=True, stop=False),
                             reads=[("gvst", t), ("atm", u)], writes=[("p_g", u)], inc=False)
                        yield
                        P.op("pe", lambda e: e.matmul(p_g[:, u, 2 + dv, :], lhsT=Sbf[:, h, dv * 128:(dv + 1) * 128],
                                                      rhs=qtl[:, u, :], start=False, stop=True),
                             reads=[("Sbf", h), ("qtl", u)], writes=[("p_g", u)])
                        yield
                        P.op("act", lambda e: e.activation(osq[:, u, dv, :], p_g[:, u, 2 + dv, :], AF.Square),
                             reads=[("p_g", u)], writes=[("osq", u, dv)])
                        yield
                    for dv in range(2):
                        P.op("pe", lambda e: e.matmul(p_km[:, 256 + u * 128:256 + (u + 1) * 128], lhsT=ones_256[:], rhs=osq[:, u, dv, :],
                                                      start=(dv == 0), stop=(dv == 1)),
                             reads=[("osq", u, dv), "ones_256"], writes=["p_km"], inc=(dv == 1))
                    rsqrt_act(P, orst[:, u, :], p_km[:, 256 + u * 128:256 + (u + 1) * 128], ["p_km"], ("orst", u), 1.0)
                    for dv in range(2):
                        P.op("dve", lambda e: e.scalar_tensor_tensor(otmp[:, u, :], p_g[:, u, 2 + dv, :], gnw_sb[:, dv:dv + 1],
                                                                     orst[:, u, :], ALU.mult, ALU.mult),
                             reads=[("p_g", u), ("orst", u), "gnw_sb"], writes=[("otmp", u)])
                        yield
                        P.op("dve", lambda e: e.tensor_tensor(obst[:, h * 2 + dv, c0:c0 + 128], otmp[:, u, :],
                                                              sgr[:, h * 2 + dv, c0:c0 + 128], ALU.mult),
                             reads=[("otmp", u), ("sgr", h * 2 + dv)], writes=["obst"])
                        yield
                P.op("pool", lambda e: e.tensor_scalar(Ssc[:, u, :], Sst[:, h, :], EbT[:, u, 127:128], None, ALU.mult),
                     reads=[("S", h), ("EbT", u)], writes=[("Ssc", u)])
                yield
                P.op("dve", lambda e: e.scalar_tensor_tensor(Sst[:, h, :], p_kvb[:, u, :], EbT[:, u, 127:128], Ssc[:, u, :],
                                                             ALU.mult, ALU.add),
                     reads=["p_kvb", ("EbT", u), ("Ssc", u)], writes=[("S", h)])
                yield
                P.op("act", lambda e: e.copy(Sbf[:, h, :], Sst[:, h, :]), reads=[("S", h)], writes=[("Sbf", h)])

            for t in range(TPG):
                for hp in (0, 2):
                    gens = [gla_chain(t, hp), gla_chain(t, hp + 1)]
                    while gens:
                        for g_ in list(gens):
                            try:
                                next(g_)
                            except StopIteration:
                                gens.remove(g_)
            if own:
                P.dma("sp", OB_d[:, o0:o0 + GS].rearrange("(k p) n -> p k n", p=128), obst[:], reads=["obst"], writes=[ukey("OB_d")],
                      sem="OB_o")
        P.op("dve", lambda e: e.tensor_copy(kmb[:], kmean[:]), reads=[("kmean", h) for h in range(8)], writes=["kmb"])
        if DEBUG:
            P.wait_all("sp")
            P.dma("sp", dbg["kT"], KT_d, reads=[])
            P.dma("sp", dbg["qT"], QT_d, reads=[])
            P.dma("sp", dbg["ob"], OB_d, reads=[])
        P.barrier()


def phase_a2(nc, P, L):
    g = L
    sb, ps = g["sb"], g["ps"]
    KT_d, V_d, QT_d = g["KT_d"], g["V_d"], g["QT_d"]
    cst, ident_b, ident_f, ones_1, oaT, kmb = g["cst"], g["ident_b"], g["ident_f"], g["ones_1"], g["oaT"], g["kmb"]
    scale = 128.0 ** -0.5
    with ExitStack() as es:
        KT = sb(es, "KT", [128, 2, SEQ], BF16)
        Vh = sb(es, "Vh", [128, 2, 64, 128], BF16)
        QT = sb(es, "QT", [128, 2, OWN], BF16)
        esel = sb(es, "esel", [32, 32, 128], BF16)
        cmask = sb(es, "cmask", [128, 2, 256], BF16)
        gm_sb = sb(es, "gm_sb", [128, 8, 32], F32)
        gv_sb = sb(es, "gv_sb", [128, 8, 32], F32)
        gt = sb(es, "gt", [128, 2, 32], F32)
        top8 = sb(es, "top8", [128, 2, 8], F32)
        mbT = sb(es, "mbT", [32, 2, 256], BF16)
        PT = sb(es, "PT", [128, 4, 256], BF16)
        rden = sb(es, "rden", [128, 256], F32)
        p_st = ps(es, "p_st", [128, 4, 512], F32)
        p_od = ps(es, "p_od", [128, 2, 512], F32)
        p_gm = ps(es, "p_gm", [128, 512], F32)
        p_gt = p_gm[:, 0:64].rearrange("p (a b) -> p a b", a=2)
        p_mb = p_gm[0:32, 256:512]
        P.dma("pool", esel[:].rearrange("p a b -> p (a b)"), g["esel_in"], writes=["esel"])
        P.dma("pool", cmask[:], cst_dram_causal(g), writes=["cmask"])
        P.dma("sp", gm_sb[:].rearrange("p a b -> p (a b)"), g["gmask"], writes=["gm_sb"])
        P.dma("sp", gv_sb[:].rearrange("p a b -> p (a b)"), g["gvalid"], writes=["gv_sb"])
        n_st = [0]
        n_blk = [0]
        for h in range(8):
            hs = h % 2
            P.dma("sp", KT[:, hs, :], KT_d[h], writes=[("KT", hs)], sem=("KT", hs))
            P.dma("sp", Vh[:, hs], V_d[:, h * 128:(h + 1) * 128].rearrange("(t p) d -> p t d", p=128),
                  writes=[("Vh", hs)], sem=("Vh", hs))
            P.dma("sp", QT[:, hs, :], QT_d[h], writes=[("QT", hs)], sem=("QT", hs))
            for l in range(8):
                bs = n_blk[0] % 2
                n_blk[0] += 1
                q0 = l * 256
                for qt in range(2):
                    P.op("pe", lambda e: e.matmul(p_gt[:, qt, :], lhsT=QT[:, hs, q0 + qt * 128:q0 + (qt + 1) * 128],
                                                  rhs=kmb[:, h, :], start=True, stop=True),
                         reads=[("QT", hs), "kmb"], writes=["p_gm"])
                    P.op("dve", lambda e: e.tensor_tensor(gt[:, qt, :], p_gt[:, qt, :], gm_sb[:, l, :], ALU.add),
                         reads=["p_gm", "gm_sb"], writes=[("gt", qt)])
                    P.op("dve", lambda e: e.max(top8[:, qt, :], gt[:, qt, :]), reads=[("gt", qt)], writes=[("top8", qt)])
                    P.op("dve", lambda e: e.tensor_scalar(gt[:, qt, :], gt[:, qt, :], top8[:, qt, 2:3], None, ALU.is_ge),
                         reads=[("gt", qt), ("top8", qt)], writes=[("gt", qt)])
                    P.op("dve", lambda e: e.tensor_tensor(gt[:, qt, :], gt[:, qt, :], gv_sb[:, l, :], ALU.mult),
                         reads=[("gt", qt), "gv_sb"], writes=[("gt", qt)])
                    P.op("dve", lambda e: e.tensor_scalar(gt[:, qt, :], gt[:, qt, :], BIG, -BIG, ALU.mult, ALU.add),
                         reads=[("gt", qt)], writes=[("gt", qt)])
                    P.op("pe", lambda e: e.transpose(p_mb[:, qt * 128:(qt + 1) * 128], gt[:, qt, :], ident_f),
                         reads=[("gt", qt), "cst"], writes=["p_gm"])
                P.op("dve", lambda e: e.tensor_copy(mbT[:, bs, :], p_mb), reads=["p_gm"], writes=[("mbT", bs)])
                nkv = 24 + l
                pairs = [(v, half) for v in range(nkv + 1) for half in range(2)]
                def front(i):
                    v, half = pairs[i]
                    st = n_st[0] % 4
                    n_st[0] += 1
                    k0 = v * 256 + half * 128
                    P.op("pe", lambda e: e.matmul(p_st[:, st, 0:256], lhsT=KT[:, hs, k0:k0 + 128], rhs=QT[:, hs, q0:q0 + 256],
                                                  start=True, stop=False),
                         reads=[("KT", hs), ("QT", hs)], writes=[("p_st", st)], inc=False)
                    if v < nkv:
                        P.op("pe", lambda e: e.matmul(p_st[:, st, 0:256], lhsT=esel[:, v, :], rhs=mbT[:, bs, :], start=False, stop=True),
                             reads=["esel", ("mbT", bs)], writes=[("p_st", st)])
                    else:
                        P.op("pe", lambda e: e.matmul(p_st[:, st, 0:256], lhsT=ident_b[:], rhs=cmask[:, half, :], start=False, stop=True),
                             reads=["ident_b", "cmask"], writes=[("p_st", st)])
                    P.op("act", lambda e: e.activation(PT[:, st, :], p_st[:, st, 0:256], AF.Exp, scale=scale),
                         reads=[("p_st", st)], writes=[("PT", st)])
                    return st

                def back(i, st):
                    v, half = pairs[i]
                    last = (i == len(pairs) - 1)
                    P.op("pe", lambda e: e.matmul(p_od[:, 0, 0:256], lhsT=Vh[:, hs, v * 2 + half, :], rhs=PT[:, st, :],
                                                  start=(i == 0), stop=last),
                         reads=[("Vh", hs), ("PT", st)], writes=["p_od"], inc=False)
                    P.op("pe", lambda e: e.matmul(p_od[:, 1, 0:256], lhsT=ones_1[:], rhs=PT[:, st, :],
                                                  start=(i == 0), stop=last),
                         reads=["ones_1", ("PT", st)], writes=["p_od"], inc=True)

                DEPTH = 3
                sts = {}
                for i in range(len(pairs) + DEPTH):
                    if i < len(pairs):
                        sts[i] = front(i)
                    if i - DEPTH >= 0:
                        back(i - DEPTH, sts.pop(i - DEPTH))
                P.op("dve", lambda e: e.reciprocal(rden[:], p_od[:, 1, 0:256]), reads=["p_od"], writes=["rden"])
                P.op("dve", lambda e: e.tensor_tensor(oaT[:, h, q0:q0 + 256], p_od[:, 0, 0:256], rden[:], ALU.mult),
                     reads=["p_od", "rden"], writes=[("oaT", h)])
        if DEBUG:
            P.dma("sp", g["dbg"]["oa"], oaT[:], reads=[("oaT", h) for h in range(8)])
        P.barrier()


def cst_dram_causal(g):
    return g["consts"][:, 5:9, :].rearrange("p (a b) c -> p a (b c)", a=2)


def phase_b(nc, P, L):
    g = L
    sb, ps = g["sb"], g["ps"]
    oaT, ones_rms, ones_1, ident_b, ident_f, cst = g["oaT"], g["ones_rms"], g["ones_1"], g["ident_b"], g["ident_f"], g["cst"]
    a2, s2, g1, gate1_bc = g["a2"], g["s2"], g["g1"], g["gate1_bc"]
    HT_d, OB_d, X1_d, XG_d = g["HT_d"], g["OB_d"], g["X1_d"], g["XG_d"]
    xTv, xtok = g["xTv"], g["xtok"]
    dest_i, w4, RWT = g["dest_i"], g["w4"], g["RWT"]
    with ExitStack() as es:
        wG_b = sb(es, "wG_b", [128, 8, 2048], BF16)
        woa_b = sb(es, "woa_b", [128, 8, D], BF16)
        wob_b = sb(es, "wob_b", [128, 8, D], BF16)
        wout_b = sb(es, "wout_b", [128, 8, D], BF16)
        nbm_sb = sb(es, "nbm_sb", [128, 16], F32)
        wr_sb = sb(es, "wr_sb", [128, 8, NE], F32)
        br_sb = sb(es, "br_sb", [128, NE], F32)
        ecap_sb = sb(es, "ecap_sb", [128, NE], F32)
        hTg = sb(es, "hTg", [128, 8, GS], BF16)
        obg = sb(es, "obg", [128, 8, GS], BF16)
        xo = sb(es, "xo", [128, 8, GS], F32)
        ge = sb(es, "ge", [128, GS], F32)
        mf = sb(es, "mf", [128, GS], F32)
        mt2 = sb(es, "mt2", [128, GS], F32)
        mT = sb(es, "mT", [128, 8, GS], BF16)
        sq = sb(es, "sqb", [128, 8, GS], BF16)
        rstd = sb(es, "rstdb", [128, GS], F32)
        h2f = sb(es, "h2f", [128, 8, GS], F32)
        h2b = sb(es, "h2b", [128, 8, GS], BF16)
        xt = sb(es, "xt", [128, D], F32)
        x1t = sb(es, "x1t", [128, D], F32)
        h2tok = sb(es, "h2tok", [128, D], BF16)
        lg = sb(es, "lg", [128, NE], F32)
        t8 = sb(es, "t8", [128, 8], F32)
        nmax = sb(es, "nmax", [128, 1], F32)
        den = sb(es, "den", [128, 1], F32)
        sel = sb(es, "sel", [128, NE], F32)
        selb = sb(es, "selb", [128, NE], BF16)
        carry = sb(es, "carry", [128, NE], F32)
        slot = sb(es, "slot", [128, NE], F32)
        oh = sb(es, "oh", [128, NE], F32)
        destf = sb(es, "destf", [128, 4], F32)
        RW = sb(es, "RW", [128, NE], F32)
        stri = sb(es, "stri", [128, 128], BF16)
        p_a = ps(es, "pb_a", [128, 2, 512], F32)
        p_b = ps(es, "pb_b", [128, 2, 512], F32)
        p_ss = ps(es, "pb_ss", [128, GS], F32)
        p_lg = ps(es, "pb_lg", [128, 2, NE], F32)
        p_tr = ps(es, "pb_tr", [128, 2, 512], BF16)
        p_rw = ps(es, "pb_rw", [32, 128], F32)
        for k in range(8):
            P.dma("pool", wG_b[:, k, :], g["wG"][k * 128:(k + 1) * 128, :], writes=[("wG", k)], sem=("wG", k))
            P.dma("pool", woa_b[:, k, :], g["w_oa"][k * 128:(k + 1) * 128, :], writes=[("woa", k)], sem=("woa", k))
            P.dma("pool", wob_b[:, k, :], g["w_ob"][k * 128:(k + 1) * 128, :], writes=[("wob", k)], sem=("wob", k))
            P.dma("pool", wout_b[:, k, :], g["w_out"][k * 128:(k + 1) * 128, :], writes=[("wout", k)], sem=("wout", k))
        P.dma("pool", stri[:], g["consts"][:, 4, :], writes=["stri"])
        P.dma("sp", nbm_sb[:], g["nbm"], writes=["nbm"])
        P.dma("sp", wr_sb[:], g["w_r"].rearrange("(k p) n -> p k n", p=128), writes=["wr"])
        P.dma("sp", br_sb[:], g["b_r_bc"], writes=["br"])
        P.dma("sp", ecap_sb[:], g["ecap"], writes=["ecap"])
        P.op("pool", lambda e: e.memset(carry[:], 0.0), writes=["carry"])
        P.op("dve", lambda e: e.tensor_scalar(nbm_sb[:], nbm_sb[:], -1.0, None, ALU.mult), reads=["nbm"], writes=["nbm"])
        kk = lambda n: [(n, k) for k in range(8)]
        for G in range(OWN // GS):
            o0 = G * GS
            P.dma("sp", hTg[:], HT_d[:, o0:o0 + GS].rearrange("(k p) n -> p k n", p=128), writes=["hTg"])
            P.dma("sp", obg[:], OB_d[:, o0:o0 + GS].rearrange("(k p) n -> p k n", p=128), writes=["obg"])
            P.dma("sp", xo[:], xTv[:, 6144 + o0:6144 + o0 + GS].rearrange("(k p) n -> p k n", p=128), writes=["xo"])
            for c in range(8):
                for br in range(2):
                    wy = woa_b if br == 0 else wob_b
                    wyk = kk("woa") if br == 0 else kk("wob")
                    for k in range(8):
                        P.op("pe", lambda e: e.matmul(p_a[:, br, 0:GS], lhsT=wG_b[:, k, br * 1024 + c * 128:br * 1024 + (c + 1) * 128],
                                                      rhs=hTg[:, k, :], start=(k == 0), stop=(k == 7)),
                             reads=kk("wG") + ["hTg"], writes=[("p_a", br)], inc=(k == 7))
                    for k in range(8):
                        rhs = oaT[:, k, o0:o0 + GS] if br == 0 else obg[:, k, :]
                        P.op("pe", lambda e: e.matmul(p_b[:, br, 0:GS], lhsT=wy[:, k, c * 128:(c + 1) * 128], rhs=rhs,
                                                      start=(k == 0), stop=(k == 7)),
                             reads=wyk + (["obg"] if br else []), writes=[("p_b", br)], inc=(k == 7))
                    P.op("act", lambda e: e.activation(ge[:], p_a[:, br, 0:GS], AF.Exp, bias=nbm_sb[:, br * 8 + c:br * 8 + c + 1], scale=-1.0),
                         reads=[("p_a", br), "nbm"], writes=["ge"])
                    P.op("dve", lambda e: e.tensor_scalar(ge[:], ge[:], 1.0, None, ALU.add), reads=["ge"], writes=["ge"])
                    P.op("dve", lambda e: e.reciprocal(ge[:], ge[:]), reads=["ge"], writes=["ge"])
                    if br == 0:
                        P.op("dve", lambda e: e.tensor_tensor(mf[:], ge[:], p_b[:, br, 0:GS], ALU.mult), reads=["ge", ("p_b", br)], writes=["mf"])
                    else:
                        P.op("dve", lambda e: e.tensor_tensor(mt2[:], ge[:], p_b[:, br, 0:GS], ALU.mult), reads=["ge", ("p_b", br)], writes=["mt2"])
                        P.op("dve", lambda e: e.tensor_tensor(mT[:, c, :], mf[:], mt2[:], ALU.add), reads=["mf", "mt2"], writes=["mT"])
            for c in range(8):
                for k in range(8):
                    P.op("pe", lambda e: e.matmul(p_a[:, c % 2, 0:GS], lhsT=wout_b[:, k, c * 128:(c + 1) * 128], rhs=mT[:, k, :],
                                                  start=(k == 0), stop=(k == 7)),
                         reads=kk("wout") + ["mT"], writes=[("p_a", c % 2)], inc=(k == 7))
                P.op("dve", lambda e: e.scalar_tensor_tensor(xo[:, c, :], p_a[:, c % 2, 0:GS], g1[:, c:c + 1], xo[:, c, :], ALU.mult, ALU.add),
                     reads=[("p_a", c % 2), "xo"], writes=["xo"])
            for t in range(TPG):
                T = G * TPG + t
                P.dma("sp", xt[:], xtok[T * 128:(T + 1) * 128, :], writes=["xt"])
                for half in range(2):
                    for k in range(8):
                        P.op("pe", lambda e: e.matmul(p_b[:, half, :], lhsT=mT[:, k, t * 128:(t + 1) * 128],
                                                      rhs=wout_b[:, k, half * 512:(half + 1) * 512], start=(k == 0), stop=(k == 7)),
                             reads=kk("wout") + ["mT"], writes=[("p_b", half)], inc=(k == 7))
                    P.op("dve", lambda e: e.tensor_tensor(x1t[:, half * 512:(half + 1) * 512], p_b[:, half, :],
                                                          gate1_bc[:, half * 512:(half + 1) * 512], ALU.mult),
                         reads=[("p_b", half)], writes=["x1t"])
                P.op("pool", lambda e: e.tensor_tensor(x1t[:], x1t[:], xt[:], ALU.add), reads=["x1t", "xt"], writes=["x1t"])
                P.dma("sp", X1_d[T * 128:(T + 1) * 128, :], x1t[:], reads=["x1t"], writes=[("X1_d", T)], sem="X1_o")
            rmsnorm_fm(P, nc, xo[:], sq[:], p_ss[:], rstd[:], ones_rms, ["xo"], "b", "pb_ss")
            for k in range(8):
                P.op("dve", lambda e: e.scalar_tensor_tensor(xo[:, k, :], xo[:, k, :], a2[:, k:k + 1], rstd[:], ALU.mult, ALU.mult),
                     reads=["xo", "brstd"], writes=["xo"])
                P.op("act", lambda e: e.activation(h2f[:, k, :], xo[:, k, :], AF.Identity, bias=s2[:, k:k + 1], scale=1.0),
                     reads=["xo"], writes=["h2f"])
            P.op("pool", lambda e: e.tensor_copy(h2b[:], h2f[:]), reads=["h2f"], writes=["h2b"])
            for t in range(TPG):
                T = G * TPG + t
                lq = t
                for k in range(8):
                    P.op("pe", lambda e: e.matmul(p_lg[:, lq, :], lhsT=h2f[:, k, t * 128:(t + 1) * 128], rhs=wr_sb[:, k, :],
                                                  start=(k == 0), stop=(k == 7)),
                         reads=["h2f", "wr"], writes=["p_lg"], inc=(k == 7))
                P.op("dve", lambda e: e.tensor_tensor(lg[:], p_lg[:, lq, :], br_sb[:], ALU.add), reads=["p_lg", "br"], writes=["lg"])
                if DEBUG:
                    P.dma("sp", g["dbg"]["lg"][:, T, :], lg[:], reads=["lg"], sem="dbg_lg")
                P.op("dve", lambda e: e.max(t8[:], lg[:]), reads=["lg"], writes=["t8"])
                P.op("dve", lambda e: e.tensor_scalar(nmax[:], t8[:, 0:1], -1.0, None, ALU.mult), reads=["t8"], writes=["nmax"])
                P.op("act", lambda e: e.activation(w4[:, T, :], t8[:, 0:4], AF.Exp, bias=nmax[:], scale=1.0),
                     reads=["t8", "nmax"], writes=[("w4", T)])
                P.op("dve", lambda e: e.tensor_reduce(den[:], w4[:, T, :], AX.X, ALU.add), reads=[("w4", T)], writes=["den"])
                P.op("dve", lambda e: e.reciprocal(den[:], den[:]), reads=["den"], writes=["den"])
                P.op("dve", lambda e: e.tensor_scalar(w4[:, T, :], w4[:, T, :], den[:], None, ALU.mult), reads=[("w4", T), "den"], writes=[("w4", T)])
                P.op("dve", lambda e: e.tensor_scalar(sel[:], lg[:], t8[:, 3:4], None, ALU.is_ge), reads=["lg", "t8"], writes=["sel"])
                P.op("dve", lambda e: e.tensor_copy(selb[:], sel[:]), reads=["sel"], writes=["selb"])
                P.op("pe", lambda e: e.matmul(p_lg[:, lq, :], lhsT=stri[:], rhs=selb[:], start=True, stop=True),
                     reads=["stri", "selb"], writes=["p_lg"])
                P.op("dve", lambda e: e.tensor_tensor(slot[:], p_lg[:, lq, :], carry[:], ALU.add), reads=["p_lg", "carry"], writes=["slot"])
                P.op("pe", lambda e: e.matmul(p_lg[:, lq, :], lhsT=ones_1[:], rhs=selb[:], start=True, stop=True),
                     reads=["selb"], writes=["p_lg"])
                P.op("dve", lambda e: e.tensor_tensor(carry[:], p_lg[:, lq, :], carry[:], ALU.add), reads=["p_lg", "carry"], writes=["carry"])
                P.op("dve", lambda e: e.tensor_tensor(slot[:], slot[:], ecap_sb[:], ALU.add), reads=["slot", "ecap"], writes=["slot"])
                P.op("pool", lambda e: e.memset(RW[:], 0.0), writes=["RW"])
                for k4 in range(4):
                    P.op("dve", lambda e: e.tensor_scalar(oh[:], lg[:], t8[:, k4:k4 + 1], None, ALU.is_equal), reads=["lg", "t8"], writes=["oh"])
                    P.op("dve", lambda e: e.scalar_tensor_tensor(RW[:], oh[:], w4[:, T, k4:k4 + 1], RW[:], ALU.mult, ALU.add),
                         reads=["oh", ("w4", T), "RW"], writes=["RW"])
                    P.op("dve", lambda e: e.tensor_tensor(oh[:], oh[:], slot[:], ALU.mult), reads=["oh", "slot"], writes=["oh"])
                    P.op("dve", lambda e: e.tensor_reduce(destf[:, k4:k4 + 1], oh[:], AX.X, ALU.add), reads=["oh"], writes=["destf"])
                P.op("dve", lambda e: e.tensor_copy(dest_i[:, T * 4:T * 4 + 4], destf[:]), reads=["destf"], writes=[("dest_i", T)])
                P.op("pe", lambda e: e.transpose(p_rw[:], RW[:], ident_f), reads=["RW"], writes=["p_rw"])
                P.op("act", lambda e: e.copy(RWT[:, T, :], p_rw[:]), reads=["p_rw"], writes=[("RWT", T)])
                for hf in range(2):
                    for k in range(4):
                        P.op("pe", lambda e: e.transpose(p_tr[:, hf, k * 128:(k + 1) * 128], h2b[:, hf * 4 + k, t * 128:(t + 1) * 128], ident_b[:]),
                             reads=["h2b"], writes=["p_tr"], inc=(k == 3))
                    P.op("act", lambda e: e.copy(h2tok[:, hf * 512:(hf + 1) * 512], p_tr[:, hf, :]), reads=["p_tr"], writes=["h2tok"])
                for k4 in range(4):
                    P.dma("pool", None, None, reads=["h2tok", ("dest_i", T)], writes=["XG_d"], sem=("xgs", k4),
                          emit=lambda e: e.indirect_dma_start(
                              out=XG_d[:, :], out_offset=bass.IndirectOffsetOnAxis(ap=dest_i[:, T * 4 + k4:T * 4 + k4 + 1], axis=0),
                              in_=h2tok[:, :], in_offset=None, bounds_check=g["bc_reg"], oob_is_err=False))
        if DEBUG:
            P.wait_all("sp")
            P.dma("sp", g["dbg"]["x1"], X1_d, reads=[])
        P.barrier()


def phase_moe(nc, P, L):
    g = L
    sb, ps = g["sb"], g["ps"]
    ident_b, gate2_bc = g["ident_b"], g["gate2_bc"]
    XG_d, OUT_d, X1_d = g["XG_d"], g["OUT_d"], g["X1_d"]
    w1d, w2, out = g["w1d"], g["w2"], g["out"]
    dest_i, w4, RWT = g["dest_i"], g["w4"], g["RWT"]
    NCH = CAP // 512
    with ExitStack() as es:
        wp = sb(es, "wp", [128, 12, 8, 512], BF16)
        b1 = sb(es, "b1", [128, NE, 16], F32)
        xg = sb(es, "xg", [128, 2, 4, D], BF16)
        xgT = sb(es, "xgT", [128, 2, 8, 512], BF16)
        actT = sb(es, "actT", [128, 2, 8, 512], BF16)
        gg = sb(es, "gg", [128, 2, 512], F32)
        ll = sb(es, "ll", [128, 2, 512], F32)
        ee = sb(es, "ee", [128, 2, 512], F32)
        orow = sb(es, "orow", [128, 4, D], F32)
        p_h = ps(es, "pm_h", [128, 4, 512], F32)
        p_o = ps(es, "pm_o", [128, 2, 512], F32)
        p_t = ps(es, "pm_t", [128, 2, 512], BF16)
        P.dma("sp", b1[:].rearrange("p a b -> p (a b)"), g["b1T"], writes=["b1"])

        def load_expert(e):
            base = (e % 2) * 6
            for p in range(4):
                for two in range(2):
                    src = w1d[e][:, two * 1024 + p * 256:two * 1024 + (p + 1) * 256]
                    P.dma("pool", wp[:, base + p, :, two * 256:(two + 1) * 256], src.rearrange("(k p) f -> p k f", p=128),
                          writes=[("wp", base + p, two)], sem=("wp", base + p, two))
            for hf in range(2):
                src = w2[e][:, hf * 512:(hf + 1) * 512]
                P.dma("pool", wp[:, base + 4 + hf], src.rearrange("(k p) n -> p k n", p=128),
                      writes=[("wp", base + 4 + hf, 0)], sem=("wp", base + 4 + hf, 0))

        load_expert(0)
        nch = [0]
        for e in range(NE):
            base = (e % 2) * 6
            if e + 1 < NE:
                load_expert(e + 1)
            for ch in range(NCH):
                u = nch[0] % 2
                nch[0] += 1
                r0 = e * CAP + ch * 512
                P.dma("sp", xg[:, u], XG_d[r0:r0 + 512, :].rearrange("(r p) d -> p r d", p=128),
                      writes=[("xg", u)], sem=("xg", u))
                for r in range(4):
                    for hf in range(2):
                        for k in range(4):
                            P.op("pe", lambda e_: e_.transpose(p_t[:, hf, k * 128:(k + 1) * 128],
                                                               xg[:, u, r, (hf * 4 + k) * 128:(hf * 4 + k + 1) * 128], ident_b[:]),
                                 reads=[("xg", u)], writes=["p_t"], inc=(k == 3))
                        P.op("act", lambda e_: e_.copy(xgT[:, u, hf * 4:(hf + 1) * 4, r * 128:(r + 1) * 128],
                                                       p_t[:, hf, :].rearrange("p (k c) -> p k c", k=4)),
                             reads=["p_t"], writes=[("xgT", u)])
                for j in range(8):
                    sl = base + j // 2
                    hs = j % 2
                    for part in range(2):
                        c0 = part * 256 + (j % 2) * 128
                        for k in range(8):
                            P.op("pe", lambda e_: e_.matmul(p_h[:, hs * 2 + part, :], lhsT=wp[:, sl, k, c0:c0 + 128], rhs=xgT[:, u, k, :],
                                                            start=(k == 0), stop=(k == 7)),
                                 reads=[("wp", sl, part), ("xgT", u)], writes=[("p_h", hs, part)], inc=(k == 7))
                    P.op("dve", lambda e_: e_.tensor_scalar(gg[:, hs, :], p_h[:, hs * 2, :], b1[:, e, j:j + 1], 7.0, ALU.add, ALU.min),
                         reads=[("p_h", hs, 0), "b1"], writes=[("gg", hs)])
                    P.op("dve", lambda e_: e_.tensor_scalar(ll[:, hs, :], p_h[:, hs * 2 + 1, :], b1[:, e, 8 + j:9 + j], 7.0, ALU.add, ALU.min),
                         reads=[("p_h", hs, 1), "b1"], writes=[("ll", hs)])
                    P.op("pool", lambda e_: e_.tensor_scalar(ll[:, hs, :], ll[:, hs, :], -7.0, 1.0, ALU.max, ALU.add),
                         reads=[("ll", hs)], writes=[("ll", hs)])
                    P.op("act", lambda e_: e_.activation(ee[:, hs, :], gg[:, hs, :], AF.Exp, scale=-1.702), reads=[("gg", hs)], writes=[("ee", hs)])
                    P.op("pool", lambda e_: e_.tensor_scalar(ee[:, hs, :], ee[:, hs, :], 1.0, None, ALU.add), reads=[("ee", hs)], writes=[("ee", hs)])
                    P.op("dve", lambda e_: e_.reciprocal(ee[:, hs, :], ee[:, hs, :]), reads=[("ee", hs)], writes=[("ee", hs)])
                    P.op("pool", lambda e_: e_.tensor_tensor(gg[:, hs, :], gg[:, hs, :], ll[:, hs, :], ALU.mult),
                         reads=[("gg", hs), ("ll", hs)], writes=[("gg", hs)])
                    P.op("dve", lambda e_: e_.tensor_tensor(actT[:, u, j, :], gg[:, hs, :], ee[:, hs, :], ALU.mult),
                         reads=[("gg", hs), ("ee", hs)], writes=[("actT", u)])
                for hf in range(2):
                    sl = base + 4 + hf
                    for r in range(4):
                        os_ = (hf * 4 + r) % 2
                        for k in range(8):
                            P.op("pe", lambda e_: e_.matmul(p_o[:, os_, :], lhsT=actT[:, u, k, r * 128:(r + 1) * 128], rhs=wp[:, sl, k, :],
                                                            start=(k == 0), stop=(k == 7)),
                                 reads=[("actT", u), ("wp", sl, 0)], writes=[("p_o", os_)], inc=(k == 7))
                        P.op("act", lambda e_: e_.copy(orow[:, r, hf * 512:(hf + 1) * 512], p_o[:, os_, :]),
                             reads=[("p_o", os_)], writes=["orow"])
                P.dma("sp", OUT_d[r0:r0 + 512, :].rearrange("(r p) d -> p r d", p=128), orow[:],
                      reads=["orow"], writes=[("OUT_d", e, ch)], sem="orow")
        P.barrier()
    with ExitStack() as es:
        yk = sb(es, "yk", [128, 2, 4, D], F32)
        acc = sb(es, "acc", [128, 2, D], F32)
        x1t = sb(es, "x1c", [128, 2, D], F32)
        fsq = sb(es, "fsq", [128, D], F32)
        ssq = sb(es, "ssq", [128, 2], F32)
        b2_sb = sb(es, "b2c", [32, D], F32)
        fnw = sb(es, "fnwc", [128, D], F32)
        p_bias = ps(es, "pc_b", [128, 2, 2, 512], F32)
        P.dma("sp", b2_sb[:], g["b2"], writes=["b2c"])
        P.dma("sp", fnw[:], g["fnw_bc"], writes=["fnwc"])
        for T in range(16):
            u = T % 2
            for k4 in range(4):
                P.dma("pool", None, None, reads=["OUT_d"], writes=[("yk", u, k4)], sem=("yk", u, k4),
                      emit=lambda e: e.indirect_dma_start(
                          out=yk[:, u, k4, :], out_offset=None, in_=OUT_d[:, :],
                          in_offset=bass.IndirectOffsetOnAxis(ap=dest_i[:, T * 4 + k4:T * 4 + k4 + 1], axis=0),
                          bounds_check=g["bc_reg"], oob_is_err=False))
            P.dma("sp", x1t[:, u, :], X1_d[T * 128:(T + 1) * 128, :], reads=["X1_d"], writes=[("x1c", u)], sem=("x1c", u))
            for half in range(2):
                P.op("pe", lambda e: e.matmul(p_bias[:, u, half, :], lhsT=RWT[:, T, :], rhs=b2_sb[:, half * 512:(half + 1) * 512],
                                              start=True, stop=True),
                     reads=["b2c"], writes=[("p_bias", u)], inc=(half == 1))
            P.op("dve", lambda e: e.tensor_scalar(acc[:, u, :], yk[:, u, 0, :], w4[:, T, 0:1], None, ALU.mult),
                 reads=[("yk", u, 0)], writes=[("acc", u)])
            for k4 in range(1, 4):
                P.op("dve", lambda e: e.scalar_tensor_tensor(acc[:, u, :], yk[:, u, k4, :], w4[:, T, k4:k4 + 1], acc[:, u, :], ALU.mult, ALU.add),
                     reads=[("yk", u, k4), ("acc", u)], writes=[("acc", u)])
            for half in range(2):
                P.op("dve", lambda e: e.tensor_tensor(acc[:, u, half * 512:(half + 1) * 512], acc[:, u, half * 512:(half + 1) * 512],
                                                      p_bias[:, u, half, :], ALU.add),
                     reads=[("acc", u), ("p_bias", u)], writes=[("acc", u)])
            P.op("pool", lambda e: e.tensor_tensor(acc[:, u, :], acc[:, u, :], gate2_bc[:], ALU.mult), reads=[("acc", u), "gate2_bc"], writes=[("acc", u)])
            P.op("pool", lambda e: e.tensor_tensor(acc[:, u, :], acc[:, u, :], x1t[:, u, :], ALU.add), reads=[("acc", u), ("x1c", u)], writes=[("acc", u)])
            P.op("pool", lambda e: e.memset(ssq[:, u:u + 1], 0.0), writes=[("ssq", u)])
            P.op("act", lambda e: e.activation(fsq[:], acc[:, u, :], AF.Square, accum_out=ssq[:, u:u + 1]), reads=[("acc", u), ("ssq", u)], writes=["fsq", ("ssq", u)])
            rsqrt_act(P, ssq[:, u:u + 1], ssq[:, u:u + 1], [("ssq", u)], ("ssq", u), 1.0 / D)
            P.op("dve", lambda e: e.scalar_tensor_tensor(acc[:, u, :], acc[:, u, :], ssq[:, u:u + 1], fnw[:], ALU.mult, ALU.mult),
                 reads=[("acc", u), ("ssq", u), "fnwc"], writes=[("acc", u)])
            P.dma("sp", out[T * 128:(T + 1) * 128, :], acc[:, u, :], reads=[("acc", u)], writes=[("out", T)], sem=("out", u))


def _consts():
    c = np.zeros((128, 10, 128), np.float32)
    c[:, 0, :] = np.eye(128, dtype=np.float32)
    perm = np.zeros((128, 128), np.float32)
    for m in range(32):
        perm[(m + 16) % 32, m] = 1.0
    c[:, 1, :] = perm
    i = np.arange(128)
    c[:, 2, :] = np.where(i[:, None] <= i[None, :], -1.0 / 16.0, 0.0)
    c[:, 3, :] = (i[:, None] <= i[None, :]).astype(np.float32)
    c[:, 4, :] = (i[:, None] < i[None, :]).astype(np.float32)
    q = np.arange(256)
    for half in range(2):
        kpos = half * 128 + i
        m = np.where(kpos[:, None] <= q[None, :], 0.0, -BIG).astype(np.float32)
        c[:, 5 + 2 * half, :] = m[:, :128]
        c[:, 6 + 2 * half, :] = m[:, 128:]
    return c


def _rope_tables():
    half = 16
    inv = np.float32(500000.0) ** (-np.arange(half, dtype=np.float32) * np.float32(2.0) / np.float32(32))
    ang = np.arange(SEQ, dtype=np.float32)[:, None] * inv[None, :].astype(np.float32)
    cos = np.cos(ang).astype(np.float32).T
    sin = np.sin(ang).astype(np.float32).T
    return np.concatenate([cos, cos], 0), np.concatenate([-sin, sin], 0)


def prep_inputs(inputs):
    f = lambda k: np.asarray(inputs[k], np.float32)
    x = f("x")
    c = f("c")
    w_in = f("w_in")[0]
    fm = lambda v: np.ascontiguousarray(v.reshape(-1, 128).T)
    bc = lambda v: np.ascontiguousarray(np.broadcast_to(v[None, :], (128, v.shape[0])))
    o = [0]
    for sz in (1024, 1024, 1024, 512, 512, 1024, 1024, 16, 1024, 1024):
        o.append(o[-1] + sz)
    mq, mk, mv, gq, gk, gv, gr, gl, ga, gb = [w_in[:, o[i]:o[i + 1]] for i in range(10)]
    shared = {
        "w_ada": f("w_ada")[0],
        "b_ada_bc": bc(f("b_ada")[0]),
        "nw": np.concatenate([fm(f("norm1_w")[0]), fm(f("norm2_w")[0])], 1),
        "fnw_bc": bc(f("final_norm_w")),
        "wA": np.ascontiguousarray(np.concatenate([mk, mv, gk, gv, gl], 1)),
        "wO": np.ascontiguousarray(np.concatenate([mq, gq, gr], 1)),
        "wG": np.ascontiguousarray(np.concatenate([ga, gb], 1)),
        "w_oa": f("w_o_moba")[0], "w_ob": f("w_o_gla")[0], "w_out": f("w_out")[0],
        "nbm": fm(f("b_merge")[0]),
        "gnw": fm(f("gla_norm_w")[0]),
        "w_r": f("w_router")[0], "b_r_bc": bc(f("b_router")[0]),
        "w2": f("w_exp_out")[0], "b2": f("b_exp_out")[0],
        "consts": _consts(),
        "ecap": bc(np.arange(NE, dtype=np.float32) * CAP),
    }
    gw = np.zeros((32, 512), np.float32)
    gw[0:16] = f("gla_gate_w")[0]
    gw[16] = f("gla_gate_b")[0]
    shared["gw_aug"] = gw
    w1 = f("w_exp_in")[0]
    shared["w1d"] = np.ascontiguousarray(np.concatenate([w1[:, :, 0::2], w1[:, :, 1::2]], 2))
    b1 = f("b_exp_in")[0]
    b1d = np.concatenate([b1[:, 0::2], b1[:, 1::2]], 1)
    shared["b1T"] = np.ascontiguousarray(b1d.reshape(NE, 16, 128).transpose(2, 0, 1).reshape(128, NE * 16))
    es = np.zeros((32, 32, 128), np.float32)
    for j in range(32):
        es[j, j, :] = 1.0
    shared["esel_in"] = es.reshape(32, 32 * 128)
    cosT, sinT = _rope_tables()
    per_core = []
    for core in range(NCORE):
        b, r = core // 4, core % 4
        nnull = (24 - 8 * r) * 256
        nreal = 2048 * (r + 1)
        xv = np.zeros((D, SEQ), np.float32)
        xv[:, nnull:] = x[b, :nreal, :].T
        rc_ = np.zeros((32, SEQ), np.float32)
        rs_ = np.zeros((32, SEQ), np.float32)
        rc_[:, nnull:] = cosT[:, :nreal]
        rs_[:, nnull:] = sinT[:, :nreal]
        vf = np.zeros((SEQ,), np.float32)
        vf[nnull:] = 1.0
        gm = np.full((8, 32), NEGINF, np.float32)
        gvv = np.zeros((8, 32), np.float32)
        for l in range(8):
            gm[l, 24 - 8 * r:24 + l] = 0.0
            gvv[l, 24 - 8 * r:24 + l] = 1.0
        d = dict(shared)
        d.update({
            "xTv": xv,
            "xtok": np.ascontiguousarray(x[b, 2048 * r:2048 * (r + 1), :]),
            "cT": fm(c[b]),
            "ropec": rc_, "ropes": rs_,
            "vflag": np.ascontiguousarray(vf.reshape(64, 128).T),
            "gmask": bc(gm.reshape(-1)), "gvalid": bc(gvv.reshape(-1)),
        })
        per_core.append(d)
    return per_core


_NC_CACHE = {}


def kernel(**inputs):
    in_maps = prep_inputs(inputs)
    if "nc" not in _NC_CACHE:
        _NC_CACHE["nc"] = build_nc()[0]
    nc = _NC_CACHE["nc"]
    res = run_bass_kernel_spmd(nc, in_maps, core_ids=list(range(NCORE)))
    outp = np.zeros((2, SEQ, D), np.float32)
    for core in range(NCORE):
        b, r = core // 4, core % 4
        outp[b, 2048 * r:2048 * (r + 1), :] = res.results[core]["out"]
    return outp
```

```python
import numpy as np
from contextlib import ExitStack
import concourse.bass as bass
import concourse.mybir as mybir
from concourse.bass_utils import run_bass_kernel_spmd

F32 = mybir.dt.float32
BF16 = mybir.dt.bfloat16
I32 = mybir.dt.int32
AF = mybir.ActivationFunctionType
ALU = mybir.AluOpType
AX = mybir.AxisListType

D = 1024
SEQ = 8192
NCORE = 8
OWN = 2048
GS = 256
TPG = 2
NG = 32
OWNG0 = 24
NVB = 32
NE = 32
CAP = 1024
BIG = 30000.0
NEGINF = -1.0e30
EPS = 1e-5
STAGE = 99
DEBUG = False


class Prog:
    def __init__(self, nc, same_engine_sync=True):
        self.nc = nc
        self.engs = {"pe": nc.tensor, "act": nc.scalar, "dve": nc.vector,
                     "pool": nc.gpsimd, "sp": nc.sync}
        self.sem = {k: nc.alloc_semaphore("prog_" + k) for k in self.engs}
        self.cnt = {k: 0 for k in self.engs}
        self.seen = {k: {} for k in self.engs}
        self.bufs = {}
        self.pending = {k: ([], []) for k in self.engs}
        self.dsem = {}
        self.dcnt = {}
        self.same = same_engine_sync
        self.nwait = 0
        self.nins = 0
        self.free_sems = {"sw": [], "hw": []}
        self.dkind = {}
        self.nalloc = 0

    def _deps(self, reads, writes):
        deps = []
        for k in reads:
            b = self.bufs.get(k)
            if b is not None and b[0] is not None:
                deps.append(b[0])
        for k in writes:
            b = self.bufs.get(k)
            if b is not None:
                if b[0] is not None:
                    deps.append(b[0])
                deps.extend(b[1])
        return deps

    def _wait(self, eng, deps, own_ok=True):
        need = {}
        for (s, v) in deps:
            if own_ok and s is self.sem[eng]:
                if eng == "pe" or not self.same:
                    continue
            key = id(s)
            if self.seen[eng].get(key, 0) >= v:
                continue
            if key not in need or need[key][1] < v:
                need[key] = (s, v)
        for key, (s, v) in need.items():
            self.engs[eng].wait_ge(s, v)
            self.seen[eng][key] = v
            self.nwait += 1

    def _record(self, ev, reads, writes):
        for k in reads:
            b = self.bufs.get(k)
            if b is None:
                self.bufs[k] = [None, [ev]]
            else:
                b[1].append(ev)
                if len(b[1]) > 24:
                    b[1] = b[1][-24:] if False else b[1]
        for k in writes:
            self.bufs[k] = [ev, []]

    def op(self, eng, emit, reads=(), writes=(), inc=True):
        reads = list(reads)
        writes = list(writes)
        self._wait(eng, self._deps(reads, writes))
        ins = emit(self.engs[eng])
        self.nins += 1
        if not inc:
            self.pending[eng][0].extend(reads)
            self.pending[eng][1].extend(writes)
            return None
        self.cnt[eng] += 1
        ev = (self.sem[eng], self.cnt[eng])
        ins.then_inc(self.sem[eng], 1)
        pr, pw = self.pending[eng]
        self._record(ev, pr + reads, pw + writes)
        self.pending[eng] = ([], [])
        return ev

    def dma(self, queue, out, in_, reads=(), writes=(), sem=None, emit=None, **kw):
        reads = list(reads)
        writes = list(writes)
        if sem is None:
            sem = ("auto",) + tuple(writes) + tuple(reads)
        kind = "sw" if queue == "pool" else "hw"
        if sem in self.dsem:
            assert self.dkind[sem] == kind, (sem, kind)
        if sem not in self.dsem:
            self.dkind[sem] = kind
            if self.free_sems[kind]:
                self.dsem[sem], self.dcnt[sem] = self.free_sems[kind].pop()
            else:
                self.dsem[sem] = self.nc.alloc_semaphore("dma_%d" % self.nalloc)
                self.nalloc += 1
                self.dcnt[sem] = 0
        self._wait(queue, self._deps(reads, writes))
        if emit is not None:
            ins = emit(self.engs[queue])
        else:
            ins = self.engs[queue].dma_start(out=out, in_=in_, **kw)
        self.nins += 1
        self.dcnt[sem] += 16
        ins.then_inc(self.dsem[sem], 16)
        ev = (self.dsem[sem], self.dcnt[sem])
        self._record(ev, reads, writes)
        return ev

    def wait_all(self, eng, keys=None):
        deps = []
        for k, b in self.bufs.items():
            if keys is not None and k not in keys:
                continue
            if b[0] is not None:
                deps.append(b[0])
            deps.extend(b[1])
        self._wait(eng, deps, own_ok=False)

    def barrier(self):
        assert all(len(p[0]) == 0 and len(p[1]) == 0 for p in self.pending.values())
        for eng in self.engs:
            deps = [(self.sem[o], self.cnt[o]) for o in self.engs if o != eng and self.cnt[o] > 0]
            deps += [(self.dsem[k], self.dcnt[k]) for k in self.dsem if self.dcnt[k] > 0]
            self._wait(eng, deps, own_ok=False)
        self.bufs = {}
        for k in list(self.dsem):
            self.free_sems[self.dkind.pop(k)].append((self.dsem.pop(k), self.dcnt.pop(k)))


def build_nc():
    nc = bass.Bass("TRN2", target_bir_lowering=False)
    P = Prog(nc)
    dt_in = lambda n, s, d=F32: nc.dram_tensor(n, list(s), d, kind="ExternalInput").ap()
    dt_sc = lambda n, s, d: nc.dram_tensor(n, list(s), d).ap()

    xTv = dt_in("xTv", [D, SEQ])
    xtok = dt_in("xtok", [OWN, D])
    cT = dt_in("cT", [128, 8])
    w_ada = dt_in("w_ada", [D, 6 * D])
    b_ada_bc = dt_in("b_ada_bc", [128, 6 * D])
    nw = dt_in("nw", [128, 16])
    fnw_bc = dt_in("fnw_bc", [128, D])
    wA = dt_in("wA", [D, 3600])
    wO = dt_in("wO", [D, 2560])
    wG = dt_in("wG", [D, 2048])
    w_oa = dt_in("w_oa", [D, D])
    w_ob = dt_in("w_ob", [D, D])
    w_out = dt_in("w_out", [D, D])
    nbm = dt_in("nbm", [128, 16])
    ropec = dt_in("ropec", [32, SEQ])
    ropes = dt_in("ropes", [32, SEQ])
    vflag = dt_in("vflag", [128, 64])
    gmask = dt_in("gmask", [128, 8 * 32])
    gvalid = dt_in("gvalid", [128, 8 * 32])
    gw_aug = dt_in("gw_aug", [32, 512])
    gnw = dt_in("gnw", [128, 2])
    w_r = dt_in("w_r", [D, NE])
    b_r_bc = dt_in("b_r_bc", [128, NE])
    w1d = dt_in("w1d", [NE, D, 2048]) if STAGE >= 4 else None
    w2 = dt_in("w2", [NE, D, D]) if STAGE >= 4 else None
    b1T = dt_in("b1T", [128, NE * 16])
    b2 = dt_in("b2", [NE, D])
    consts = dt_in("consts", [128, 10, 128])
    esel_in = dt_in("esel_in", [32, 32 * 128])
    ecap = dt_in("ecap", [128, NE])
    out = nc.dram_tensor("out", [OWN, D], F32, kind="ExternalOutput").ap()

    KT_d = dt_sc("KT_d", [8, 128, SEQ], BF16)
    V_d = dt_sc("V_d", [SEQ, D], BF16)
    QT_d = dt_sc("QT_d", [8, 128, OWN], BF16)
    OB_d = dt_sc("OB_d", [D, OWN], BF16)
    HT_d = dt_sc("HT_d", [D, OWN], BF16)
    X1_d = dt_sc("X1_d", [OWN, D], F32)
    XG_d = dt_sc("XG_d", [NE * CAP, D], BF16)
    OUT_d = dt_sc("OUT_d", [NE * CAP, D], F32)

    dbg = {}
    if DEBUG:
        dbg["mod"] = nc.dram_tensor("dbg_mod", [128, 48], F32, kind="ExternalOutput").ap()
        dbg["kT"] = nc.dram_tensor("dbg_kT", [8, 128, SEQ], BF16, kind="ExternalOutput").ap()
        dbg["qT"] = nc.dram_tensor("dbg_qT", [8, 128, OWN], BF16, kind="ExternalOutput").ap()
        dbg["ob"] = nc.dram_tensor("dbg_ob", [D, OWN], BF16, kind="ExternalOutput").ap()
        dbg["oa"] = nc.dram_tensor("dbg_oa", [128, 8, OWN], BF16, kind="ExternalOutput").ap()
        dbg["x1"] = nc.dram_tensor("dbg_x1", [OWN, D], F32, kind="ExternalOutput").ap()
        dbg["lg"] = nc.dram_tensor("dbg_lg", [128, 16, 32], F32, kind="ExternalOutput").ap()

    es_all = ExitStack()
    with es_all:
        sb = lambda es, n, s, d: es.enter_context(nc.sbuf_tensor(n, list(s), d))
        ps = lambda es, n, s, d: es.enter_context(nc.psum_tensor(n, list(s), d))
        cst = sb(es_all, "cst", [128, 10, 128], F32)
        ident_f = cst[:, 0, :]
        ident_b = sb(es_all, "ident_b", [128, 128], BF16)
        ones_rms = sb(es_all, "ones_rms", [128, 128], BF16)
        ones_256 = sb(es_all, "ones_256", [128, 128], BF16)
        ones_1 = sb(es_all, "ones_1", [128, 128], BF16)
        onesf = sb(es_all, "onesf", [128, 128], F32)
        modT = sb(es_all, "modT", [128, 48], F32)
        a1 = sb(es_all, "a1", [128, 8], F32)
        a2 = sb(es_all, "a2", [128, 8], F32)
        nw_sb = sb(es_all, "nw_sb", [128, 16], F32)
        gate1_bc = sb(es_all, "gate1_bc", [128, D], F32)
        gate2_bc = sb(es_all, "gate2_bc", [128, D], F32)
        kmb = sb(es_all, "kmb", [128, 8, 32], BF16)
        bc_reg = nc.gpsimd.to_reg(NE * CAP - 1)
        dest_i = sb(es_all, "dest_i", [128, 64], I32)
        w4 = sb(es_all, "w4", [128, 16, 4], F32)
        RWT = sb(es_all, "RWT", [32, 16, 128], F32)
        s1 = modT[:, 0:8]
        g1 = modT[:, 16:24]
        s2 = modT[:, 24:32]

        P.dma("sp", cst[:], consts, writes=["cst"])
        P.dma("sp", nw_sb[:], nw, writes=["nw"])
        P.dma("pool", ident_b[:], consts[:, 0, :], writes=["ident_b"])
        P.op("dve", lambda e: e.memset(ones_rms[:], 1.0 / 1024.0), writes=["ones_rms"])
        P.op("dve", lambda e: e.memset(ones_256[:], 1.0 / 256.0), writes=["ones_256"])
        P.op("dve", lambda e: e.memset(ones_1[:], 1.0), writes=["ones_1"])
        P.op("dve", lambda e: e.memset(onesf[:], 1.0), writes=["onesf"])

        with ExitStack() as es:
            c_sb = sb(es, "c_sb", [128, 8], F32)
            c_e = sb(es, "c_e", [128, 8], F32)
            cact_b = sb(es, "cact_b", [128, 8, 128], F32)
            wst = sb(es, "wst", [128, 2, 8, 512], F32)
            modb = sb(es, "modb", [128, 6 * D], F32)
            bab = sb(es, "bab", [128, 6 * D], F32)
            p_mod = ps(es, "p_mod", [128, 2, 512], F32)
            p_col = ps(es, "p_col", [128, 48], F32)
            P.dma("sp", c_sb[:], cT, writes=["c_sb"])
            P.dma("sp", bab[:], b_ada_bc, writes=["bab"])
            P.op("act", lambda e: e.activation(c_e[:], c_sb[:], AF.Exp, scale=-1.0), reads=["c_sb"], writes=["c_e"])
            P.op("dve", lambda e: e.tensor_scalar(c_e[:], c_e[:], 1.0, None, ALU.add), reads=["c_e"], writes=["c_e"])
            P.op("dve", lambda e: e.reciprocal(c_e[:], c_e[:]), reads=["c_e"], writes=["c_e"])
            P.op("dve", lambda e: e.tensor_tensor(c_e[:], c_e[:], c_sb[:], ALU.mult), reads=["c_e", "c_sb"], writes=["c_e"])
            for k in range(8):
                P.op("dve", lambda e: e.tensor_scalar(cact_b[:, k, :], onesf[:], c_e[:, k:k + 1], None, ALU.mult),
                     reads=["c_e", "onesf"], writes=[("cactb", k)])
            for j in range(12):
                s = j % 2
                P.dma("sp", wst[:, s], w_ada[:, j * 512:(j + 1) * 512].rearrange("(k p) n -> p k n", p=128),
                      writes=[("wst", s)], sem=("wst", s))
                for k in range(8):
                    P.op("pe", lambda e: e.matmul(p_mod[:, s, :], lhsT=cact_b[:, k, :], rhs=wst[:, s, k, :],
                                                  start=(k == 0), stop=(k == 7)),
                         reads=[("wst", s), ("cactb", k)], writes=[("p_mod", s)], inc=(k == 7))
                P.op("dve", lambda e: e.tensor_tensor(modb[:, j * 512:(j + 1) * 512], p_mod[:, s, :],
                                                      bab[:, j * 512:(j + 1) * 512], ALU.add),
                     reads=[("p_mod", s), "bab"], writes=[("modb", j)])
            for j in range(48):
                P.op("pe", lambda e: e.matmul(p_col[:, j:j + 1], lhsT=modb[0:1, j * 128:(j + 1) * 128],
                                              rhs=onesf[0:1, 0:1], start=True, stop=True),
                     reads=[("modb", j // 4), "onesf"], writes=["p_col"], inc=(j == 47))
            P.op("dve", lambda e: e.tensor_copy(modT[:], p_col[:]), reads=["p_col"], writes=["modT"])
            P.op("dve", lambda e: e.scalar_tensor_tensor(a1[:], modT[:, 8:16], 1.0, nw_sb[:, 0:8], ALU.add, ALU.mult),
                 reads=["modT", "nw"], writes=["a1"])
            P.op("dve", lambda e: e.scalar_tensor_tensor(a2[:], modT[:, 32:40], 1.0, nw_sb[:, 8:16], ALU.add, ALU.mult),
                 reads=["modT", "nw"], writes=["a2"])
            P.op("act", lambda e: e.copy(gate1_bc[:], modb[:, 2048:3072]), reads=[("modb", 4), ("modb", 5)], writes=["gate1_bc"])
            P.op("act", lambda e: e.copy(gate2_bc[:], modb[:, 5120:6144]), reads=[("modb", 10), ("modb", 11)], writes=["gate2_bc"])
            if DEBUG:
                P.dma("sp", dbg["mod"], modT[:], reads=["modT"])
            P.barrier()

        G_ = dict(locals())
        if STAGE >= 1:
            phase_a1(nc, P, G_)
        with ExitStack() as es_ab:
            oaT = sb(es_ab, "oaT", [128, 8, OWN], BF16)
            G_["oaT"] = oaT
            if STAGE >= 2:
                phase_a2(nc, P, G_)
            if STAGE >= 3:
                phase_b(nc, P, G_)
        if STAGE >= 4:
            phase_moe(nc, P, G_)
        P.wait_all("sp")
    return nc, P


def rmsnorm_fm(P, nc, xs_ap, sq_ap, p_ss, rstd_ap, ones_rms, keyx, tag, pkey):
    P.op("act", lambda e: e.activation(sq_ap, xs_ap, AF.Square), reads=list(keyx), writes=[tag + "sq"])
    for k in range(8):
        P.op("pe", lambda e: e.matmul(p_ss, lhsT=ones_rms[:], rhs=sq_ap[:, k, :], start=(k == 0), stop=(k == 7)),
             reads=[tag + "sq", "ones_rms"], writes=[pkey], inc=(k == 7))
    rsqrt_act(P, rstd_ap, p_ss, [pkey], tag + "rstd", 1.0)


def rsqrt_act(P, out_ap, in_ap, rkeys, wkey, mul):
    P.op("act", lambda e: e.activation(out_ap, in_ap, AF.Ln, bias=EPS, scale=mul), reads=list(rkeys), writes=[wkey])
    P.op("act", lambda e: e.activation(out_ap, out_ap, AF.Exp, scale=-0.5), reads=[wkey], writes=[wkey])


def phase_a1(nc, P, L):
    g = L
    sb, ps = g["sb"], g["ps"]
    xTv, wA, wO, ropec, ropes = g["xTv"], g["wA"], g["wO"], g["ropec"], g["ropes"]
    cst, ident_b = g["cst"], g["ident_b"]
    ones_rms, ones_256 = g["ones_rms"], g["ones_256"]
    a1, s1, kmb = g["a1"], g["s1"], g["kmb"]
    KT_d, V_d, QT_d, OB_d, HT_d = g["KT_d"], g["V_d"], g["QT_d"], g["OB_d"], g["HT_d"]
    dbg = g["dbg"]
    with ExitStack() as es:
        wA_b = sb(es, "wA_b", [128, 8, 3600], BF16)
        wO_b = sb(es, "wO_b", [128, 8, 2560], BF16)
        xs = sb(es, "xs", [128, 2, 8, GS], F32)
        sq = sb(es, "sq", [128, 8, GS], BF16)
        rstd = sb(es, "rstd", [128, GS], F32)
        hT = sb(es, "hT", [128, 8, GS], BF16)
        rc = sb(es, "rc", [32, 1, GS], F32)
        rs = sb(es, "rs", [32, 1, GS], F32)
        kf = sb(es, "kf", [128, 2, GS], F32)
        rt = sb(es, "rt", [32, 2, GS], F32)
        kb = sb(es, "kb", [128, 2, GS], BF16)
        kmean = sb(es, "kmean", [128, 8, 32], F32)
        gkf = sb(es, "gkf", [128, 4, GS], F32)
        gqf = sb(es, "gqf", [128, 4, GS], F32)
        sgr = sb(es, "sgr", [128, 8, GS], F32)
        sgt = sb(es, "sgt", [128, GS], F32)
        glow = sb(es, "glow", [32, GS], F32)
        gw_sb = sb(es, "gw_sb", [32, 512], F32)
        gnw_sb = sb(es, "gnw_sb", [128, 2], F32)
        vfl = sb(es, "vfl", [128, 64], F32)
        Ltok = sb(es, "Ltok", [128, TPG, 512], F32)
        lex = sb(es, "lex", [128, 512], F32)
        vst = sb(es, "vst", [128, TPG, 1024], BF16)
        gvst = sb(es, "gvst", [128, TPG, 1024], BF16)
        EbT = sb(es, "EbT", [128, 2, 128], F32)
        EnbT = sb(es, "EnbT", [128, 2, 128], F32)
        qtl = sb(es, "qtl", [128, 2, 128], BF16)
        ktl = sb(es, "ktl", [128, 2, 128], BF16)
        ktok = sb(es, "ktok", [128, 2, 128], BF16)
        atm = sb(es, "atm", [128, 2, 128], BF16)
        Sst = sb(es, "Sst", [128, 4, 256], F32)
        Ssc = sb(es, "Ssc", [128, 2, 256], F32)
        Sbf = sb(es, "Sbf", [128, 4, 256], BF16)
        osq = sb(es, "osq", [128, 2, 2, 128], BF16)
        orst = sb(es, "orst", [128, 2, 128], F32)
        otmp = sb(es, "otmp", [128, 2, 128], F32)
        obst = sb(es, "obst", [128, 8, GS], BF16)
        p_pj = ps(es, "p_pj", [128, 2, 512], F32)
        p_sx = ps(es, "p_sx", [128, 512], F32)
        p_ss = p_sx[:, 0:GS]
        p_xs = p_sx[0:32, 256:256 + GS]
        p_g = ps(es, "p_g", [128, 2, 4, 128], F32)
        p_km = ps(es, "p_km", [128, 512], F32)
        p_kvb = ps(es, "p_kvb", [128, 2, 256], F32)
        p_tr = ps(es, "p_tr", [128, 2, 128], BF16)
        permf = cst[:, 1, 0:32]
        triN = cst[:, 2, :]
        utm = cst[:, 3, :]

        for k in range(8):
            P.dma("pool", wA_b[:, k, :], wA[k * 128:(k + 1) * 128, :], writes=[("wA", k)], sem=("wA", k))
        for k in range(8):
            P.dma("pool", wO_b[:, k, :], wO[k * 128:(k + 1) * 128, :], writes=[("wO", k)], sem=("wO", k))
        P.dma("sp", gw_sb[:], g["gw_aug"], writes=["gw_sb"])
        P.dma("sp", gnw_sb[:], g["gnw"], writes=["gnw_sb"])
        P.dma("sp", vfl[:], g["vflag"], writes=["vfl"])
        P.op("pool", lambda e: e.memset(glow[:], 1.0), writes=["glow"])
        P.op("pool", lambda e: e.memset(Sst[:], 0.0), writes=[("S", h) for h in range(4)])
        P.op("pool", lambda e: e.memset(Sbf[:], 0.0), writes=[("Sbf", h) for h in range(4)])
        wAk = [("wA", k) for k in range(8)]
        wOk = [("wO", k) for k in range(8)]
        pj_n = [0]
        kf_n = [0]
        uniq = [0]

        def ukey(n):
            uniq[0] += 1
            return (n, uniq[0])

        def proj_fm(wt, col0, ncol, wkeys, evac):
            s_ = pj_n[0] % 2
            pj_n[0] += 1
            for k in range(8):
                P.op("pe", lambda e: e.matmul(p_pj[0:ncol, s_, 0:GS], lhsT=wt[:, k, col0:col0 + ncol], rhs=hT[:, k, :],
                                              start=(k == 0), stop=(k == 7)),
                     reads=wkeys + ["hT"], writes=[("p_pj", s_)], inc=(k == 7))
            flush_evac()
            pend[0] = lambda: evac(p_pj[0:ncol, s_, 0:GS], ("p_pj", s_))

        pend = [None]

        def flush_evac():
            if pend[0] is not None:
                f_ = pend[0]
                pend[0] = None
                f_()

        for gi in range(NG):
            own = gi >= OWNG0
            s = gi % 2
            t0 = gi * GS
            o0 = (gi - OWNG0) * GS
            xk = [("xs", s, k) for k in range(8)]
            P.dma("sp", xs[:, s], xTv[:, t0:t0 + GS].rearrange("(k p) n -> p k n", p=128), writes=xk, sem=("xs", s))
            P.dma("sp", rc[:, 0, :], ropec[:, t0:t0 + GS], writes=[("rc", 0)], sem=("rc", 0))
            P.dma("sp", rs[:, 0, :], ropes[:, t0:t0 + GS], writes=[("rs", 0)], sem=("rs", 0))
            rmsnorm_fm(P, nc, xs[:, s], sq[:], p_ss, rstd[:], ones_rms, xk, "a1", "p_sx")
            for k in range(8):
                P.op("dve", lambda e: e.scalar_tensor_tensor(xs[:, s, k, :], xs[:, s, k, :], a1[:, k:k + 1], rstd[:],
                                                             ALU.mult, ALU.mult),
                     reads=[("xs", s, k), "a1rstd", "a1"], writes=[("xs", s, k)])
                P.op("act", lambda e: e.activation(hT[:, k, :], xs[:, s, k, :], AF.Identity, bias=s1[:, k:k + 1], scale=1.0),
                     reads=[("xs", s, k), "modT"], writes=["hT"])
            if own:
                P.dma("sp", HT_d[:, o0:o0 + GS].rearrange("(k p) n -> p k n", p=128), hT[:], reads=["hT"], writes=[ukey("HT_d")],
                      sem="HT_o")

            def qk_evac(h, is_k):
                def ev(pt, pkey):
                    u = kf_n[0] % 2
                    kf_n[0] += 1
                    P.op("act", lambda e: e.copy(kf[:, u, :], pt), reads=[pkey], writes=[("kf", u)])
                    P.op("pe", lambda e: e.matmul(p_xs, lhsT=permf, rhs=kf[:, u, :], start=True, stop=True),
                         reads=[("kf", u), "cst"], writes=["p_sx"])
                    P.op("dve", lambda e: e.tensor_tensor(rt[:, 0, :], kf[0:32, u, :], rc[:, 0, :], ALU.mult),
                         reads=[("kf", u), ("rc", 0)], writes=["rt0"])
                    P.op("dve", lambda e: e.tensor_tensor(rt[:, 1, :], p_xs, rs[:, 0, :], ALU.mult),
                         reads=["p_sx", ("rs", 0)], writes=["rt1"])
                    P.op("dve", lambda e: e.tensor_tensor(kf[0:32, u, :], rt[:, 0, :], rt[:, 1, :], ALU.add),
                         reads=["rt0", "rt1", ("kf", u)], writes=[("kf", u)])
                    if is_k:
                        P.op("dve", lambda e: e.tensor_reduce(kmean[:, h, gi:gi + 1], kf[:, u, :], AX.X, ALU.add),
                             reads=[("kf", u)], writes=[("kmean", h)])
                    P.op("pool", lambda e: e.tensor_copy(kb[:, u, :], kf[:, u, :]), reads=[("kf", u)], writes=[("kb", u)])
                    if is_k:
                        P.dma("sp", KT_d[h, :, t0:t0 + GS], kb[:, u, :], reads=[("kb", u)], writes=[ukey("KT_d")], sem=("kbo", u))
                    else:
                        P.dma("sp", QT_d[h, :, o0:o0 + GS], kb[:, u, :], reads=[("kb", u)], writes=[ukey("QT_d")], sem=("kbo", u))
                return ev

            for h in range(8):
                proj_fm(wA_b, h * 128, 128, wAk, qk_evac(h, True))
            if own:
                for h in range(8):
                    proj_fm(wO_b, h * 128, 128, wOk, qk_evac(h, False))

            flush_evac()
            for t in range(TPG):
                for half in range(2):
                    sl = pj_n[0] % 2
                    pj_n[0] += 1
                    for k in range(8):
                        P.op("pe", lambda e: e.matmul(p_pj[:, sl, :], lhsT=hT[:, k, t * 128:(t + 1) * 128],
                                                      rhs=wA_b[:, k, 1024 + half * 512:1024 + (half + 1) * 512],
                                                      start=(k == 0), stop=(k == 7)),
                             reads=wAk + ["hT"], writes=[("p_pj", sl)], inc=(k == 7))
                    P.op("act", lambda e: e.copy(vst[:, t, half * 512:(half + 1) * 512], p_pj[:, sl, :]),
                         reads=[("p_pj", sl)], writes=["vst"])
                for half in range(2):
                    sl = pj_n[0] % 2
                    pj_n[0] += 1
                    for k in range(8):
                        P.op("pe", lambda e: e.matmul(p_pj[:, sl, :], lhsT=hT[:, k, t * 128:(t + 1) * 128],
                                                      rhs=wA_b[:, k, 2560 + half * 512:2560 + (half + 1) * 512],
                                                      start=(k == 0), stop=(k == 7)),
                             reads=wAk + ["hT"], writes=[("p_pj", sl)], inc=(k == 7))
                    P.op("dve", lambda e: e.tensor_scalar(gvst[:, t, half * 512:(half + 1) * 512], p_pj[:, sl, :],
                                                          vfl[:, gi * TPG + t:gi * TPG + t + 1], None, ALU.mult),
                         reads=[("p_pj", sl), "vfl"], writes=[("gvst", t)])
            P.dma("sp", V_d[t0:t0 + GS, :].rearrange("(t p) n -> p t n", p=128), vst[:], reads=["vst"], writes=[ukey("V_d")],
                  sem="V_o")

            for h in range(4):
                proj_fm(wA_b, 2048 + h * 128, 128, wAk,
                        lambda pt, pkey, h=h: P.op("act", lambda e: e.copy(gkf[:, h, :], pt), reads=[pkey], writes=[("gkf", h)]))
            proj_fm(wA_b, 3584, 16, wAk,
                    lambda pt, pkey: P.op("act", lambda e: e.copy(glow[0:16, :], pt), reads=[pkey], writes=["glow"]))
            if own:
                for h in range(4):
                    proj_fm(wO_b, 1024 + h * 128, 128, wOk,
                            lambda pt, pkey, h=h: P.op("act", lambda e: e.copy(gqf[:, h, :], pt), reads=[pkey], writes=[("gqf", h)]))
                for c in range(8):
                    def gr_ev(pt, pkey, c=c):
                        P.op("act", lambda e: e.activation(sgt[:], pt, AF.Exp, scale=-1.0), reads=[pkey], writes=["sgt"])
                        P.op("dve", lambda e: e.tensor_scalar(sgt[:], sgt[:], 1.0, None, ALU.add), reads=["sgt"], writes=["sgt"])
                        P.op("dve", lambda e: e.reciprocal(sgt[:], sgt[:]), reads=["sgt"], writes=["sgt"])
                        P.op("dve", lambda e: e.tensor_tensor(sgr[:, c, :], sgt[:], pt, ALU.mult), reads=["sgt", pkey], writes=[("sgr", c)])
                    proj_fm(wO_b, 1536 + c * 128, 128, wOk, gr_ev)
            flush_evac()
            for t in range(TPG):
                sl = pj_n[0] % 2
                pj_n[0] += 1
                P.op("pe", lambda e: e.matmul(p_pj[:, sl, :], lhsT=glow[:, t * 128:(t + 1) * 128], rhs=gw_sb[:],
                                              start=True, stop=True),
                     reads=["glow", "gw_sb"], writes=[("p_pj", sl)])
                P.op("act", lambda e: e.activation(lex[:], p_pj[:, sl, :], AF.Exp, scale=-1.0), reads=[("p_pj", sl)], writes=["lex"])
                P.op("act", lambda e: e.activation(Ltok[:, t, :], lex[:], AF.Ln, bias=1.0, scale=1.0), reads=["lex"], writes=[("Ltok", t)])

            def gla_chain(t, h):
                u = h % 2
                c0 = t * 128
                P.op("pe", lambda e: e.matmul(p_g[:, u, 0, :], lhsT=Ltok[:, t, h * 128:(h + 1) * 128], rhs=triN,
                                              start=True, stop=True),
                     reads=[("Ltok", t), "cst"], writes=[("p_g", u)])
                yield
                P.op("act", lambda e: e.activation(EbT[:, u, :], p_g[:, u, 0, :], AF.Exp), reads=[("p_g", u)], writes=[("EbT", u)])
                P.op("act", lambda e: e.activation(EnbT[:, u, :], p_g[:, u, 0, :], AF.Exp, scale=-1.0),
                     reads=[("p_g", u)], writes=[("EnbT", u)])
                yield
                P.op("dve", lambda e: e.tensor_tensor(ktl[:, u, :], gkf[:, h, c0:c0 + 128], EnbT[:, u, :], ALU.mult),
                     reads=[("gkf", h), ("EnbT", u)], writes=[("ktl", u)])
                yield
                P.op("pe", lambda e: e.transpose(p_tr[:, u, :], ktl[:, u, :], ident_b[:]),
                     reads=[("ktl", u), "ident_b"], writes=["p_tr"])
                yield
                P.op("act", lambda e: e.copy(ktok[:, u, :], p_tr[:, u, :]), reads=["p_tr"], writes=[("ktok", u)])
                P.op("pe", lambda e: e.matmul(p_kvb[:, u, :], lhsT=ktok[:, u, :], rhs=gvst[:, t, h * 256:(h + 1) * 256],
                                              start=True, stop=True),
                     reads=[("ktok", u), ("gvst", t)], writes=["p_kvb"])
                yield
                if own:
                    P.op("dve", lambda e: e.scalar_tensor_tensor(qtl[:, u, :], gqf[:, h, c0:c0 + 128], 128.0 ** -0.5,
                                                                 EbT[:, u, :], ALU.mult, ALU.mult),
                         reads=[("gqf", h), ("EbT", u)], writes=[("qtl", u)])
                    yield
                    P.op("pe", lambda e: e.matmul(p_g[:, u, 1, :], lhsT=ktl[:, u, :], rhs=qtl[:, u, :], start=True, stop=True),
                         reads=[("ktl", u), ("qtl", u)], writes=[("p_g", u)])
                    yield
                    P.op("dve", lambda e: e.tensor_tensor(atm[:, u, :], p_g[:, u, 1, :], utm, ALU.mult),
                         reads=[("p_g", u), "cst"], writes=[("atm", u)])
                    yield
                    for dv in range(2):
                        P.op("pe", lambda e: e.matmul(p_g[:, u, 2 + dv, :],
                                                      lhsT=gvst[:, t, h * 256 + dv * 128:h * 256 + (dv + 1) * 128],
                                                      rhs=atm[:, u, :], start=True, stop=False),
                             reads=[("gvst", t), ("atm", u)], writes=[("p_g", u)], inc=False)
                        yield
                        P.op("pe", lambda e: e.matmul(p_g[:, u, 2 + dv, :], lhsT=Sbf[:, h, dv * 128:(dv + 1) * 128],
                                                      rhs=qtl[:, u, :], start=False, stop=True),
                             reads=[("Sbf", h), ("qtl", u)], writes=[("p_g", u)])
                        yield
                        P.op("act", lambda e: e.activation(osq[:, u, dv, :], p_g[:, u, 2 + dv, :], AF.Square),
                             reads=[("p_g", u)], writes=[("osq", u, dv)])
                        yield
                    for dv in range(2):
                        P.op("pe", lambda e: e.matmul(p_km[:, 256 + u * 128:256 + (u + 1) * 128], lhsT=ones_256[:], rhs=osq[:, u, dv, :],
                                                      start=(dv == 0), stop=(dv == 1)),
                             reads=[("osq", u, dv), "ones_256"], writes=["p_km"], inc=(dv == 1))
                    rsqrt_act(P, orst[:, u, :], p_km[:, 256 + u * 128:256 + (u + 1) * 128], ["p_km"], ("orst", u), 1.0)
                    for dv in range(2):
                        P.op("dve", lambda e: e.scalar_tensor_tensor(otmp[:, u, :], p_g[:, u, 2 + dv, :], gnw_sb[:, dv:dv + 1],
                                                                     orst[:, u, :], ALU.mult, ALU.mult),
                             reads=[("p_g", u), ("orst", u), "gnw_sb"], writes=[("otmp", u)])
                        yield
                        P.op("dve", lambda e: e.tensor_tensor(obst[:, h * 2 + dv, c0:c0 + 128], otmp[:, u, :],
                                                              sgr[:, h * 2 + dv, c0:c0 + 128], ALU.mult),
                             reads=[("otmp", u), ("sgr", h * 2 + dv)], writes=["obst"])
                        yield
                P.op("pool", lambda e: e.tensor_scalar(Ssc[:, u, :], Sst[:, h, :], EbT[:, u, 127:128], None, ALU.mult),
                     reads=[("S", h), ("EbT", u)], writes=[("Ssc", u)])
                yield
                P.op("dve", lambda e: e.scalar_tensor_tensor(Sst[:, h, :], p_kvb[:, u, :], EbT[:, u, 127:128], Ssc[:, u, :],
                                                             ALU.mult, ALU.add),
                     reads=["p_kvb", ("EbT", u), ("Ssc", u)], writes=[("S", h)])
                yield
                P.op("act", lambda e: e.copy(Sbf[:, h, :], Sst[:, h, :]), reads=[("S", h)], writes=[("Sbf", h)])

            for t in range(TPG):
                for hp in (0, 2):
                    gens = [gla_chain(t, hp), gla_chain(t, hp + 1)]
                    while gens:
                        for g_ in list(gens):
                            try:
                                next(g_)
                            except StopIteration:
                                gens.remove(g_)
            if own:
                P.dma("sp", OB_d[:, o0:o0 + GS].rearrange("(k p) n -> p k n", p=128), obst[:], reads=["obst"], writes=[ukey("OB_d")],
                      sem="OB_o")
        P.op("dve", lambda e: e.tensor_copy(kmb[:], kmean[:]), reads=[("kmean", h) for h in range(8)], writes=["kmb"])
        if DEBUG:
            P.wait_all("sp")
            P.dma("sp", dbg["kT"], KT_d, reads=[])
            P.dma("sp", dbg["qT"], QT_d, reads=[])
            P.dma("sp", dbg["ob"], OB_d, reads=[])
        P.barrier()


def phase_a2(nc, P, L):
    g = L
    sb, ps = g["sb"], g["ps"]
    KT_d, V_d, QT_d = g["KT_d"], g["V_d"], g["QT_d"]
    cst, ident_b, ident_f, ones_1, oaT, kmb = g["cst"], g["ident_b"], g["ident_f"], g["ones_1"], g["oaT"], g["kmb"]
    scale = 128.0 ** -0.5
    with ExitStack() as es:
        KT = sb(es, "KT", [128, 2, SEQ], BF16)
        Vh = sb(es, "Vh", [128, 2, 64, 128], BF16)
        QT = sb(es, "QT", [128, 2, OWN], BF16)
        esel = sb(es, "esel", [32, 32, 128], BF16)
        cmask = sb(es, "cmask", [128, 2, 256], BF16)
        gm_sb = sb(es, "gm_sb", [128, 8, 32], F32)
        gv_sb = sb(es, "gv_sb", [128, 8, 32], F32)
        gt = sb(es, "gt", [128, 2, 32], F32)
        top8 = sb(es, "top8", [128, 2, 8], F32)
        mbT = sb(es, "mbT", [32, 2, 256], BF16)
        PT = sb(es, "PT", [128, 4, 256], BF16)
        rden = sb(es, "rden", [128, 256], F32)
        p_st = ps(es, "p_st", [128, 4, 512], F32)
        p_od = ps(es, "p_od", [128, 2, 512], F32)
        p_gm = ps(es, "p_gm", [128, 512], F32)
        p_gt = p_gm[:, 0:64].rearrange("p (a b) -> p a b", a=2)
        p_mb = p_gm[0:32, 256:512]
        P.dma("pool", esel[:].rearrange("p a b -> p (a b)"), g["esel_in"], writes=["esel"])
        P.dma("pool", cmask[:], cst_dram_causal(g), writes=["cmask"])
        P.dma("sp", gm_sb[:].rearrange("p a b -> p (a b)"), g["gmask"], writes=["gm_sb"])
        P.dma("sp", gv_sb[:].rearrange("p a b -> p (a b)"), g["gvalid"], writes=["gv_sb"])
        n_st = [0]
        n_blk = [0]
        for h in range(8):
            hs = h % 2
            P.dma("sp", KT[:, hs, :], KT_d[h], writes=[("KT", hs)], sem=("KT", hs))
            P.dma("sp", Vh[:, hs], V_d[:, h * 128:(h + 1) * 128].rearrange("(t p) d -> p t d", p=128),
                  writes=[("Vh", hs)], sem=("Vh", hs))
            P.dma("sp", QT[:, hs, :], QT_d[h], writes=[("QT", hs)], sem=("QT", hs))
            for l in range(8):
                bs = n_blk[0] % 2
                n_blk[0] += 1
                q0 = l * 256
                for qt in range(2):
                    P.op("pe", lambda e: e.matmul(p_gt[:, qt, :], lhsT=QT[:, hs, q0 + qt * 128:q0 + (qt + 1) * 128],
                                                  rhs=kmb[:, h, :], start=True, stop=True),
                         reads=[("QT", hs), "kmb"], writes=["p_gm"])
                    P.op("dve", lambda e: e.tensor_tensor(gt[:, qt, :], p_gt[:, qt, :], gm_sb[:, l, :], ALU.add),
                         reads=["p_gm", "gm_sb"], writes=[("gt", qt)])
                    P.op("dve", lambda e: e.max(top8[:, qt, :], gt[:, qt, :]), reads=[("gt", qt)], writes=[("top8", qt)])
                    P.op("dve", lambda e: e.tensor_scalar(gt[:, qt, :], gt[:, qt, :], top8[:, qt, 2:3], None, ALU.is_ge),
                         reads=[("gt", qt), ("top8", qt)], writes=[("gt", qt)])
                    P.op("dve", lambda e: e.tensor_tensor(gt[:, qt, :], gt[:, qt, :], gv_sb[:, l, :], ALU.mult),
                         reads=[("gt", qt), "gv_sb"], writes=[("gt", qt)])
                    P.op("dve", lambda e: e.tensor_scalar(gt[:, qt, :], gt[:, qt, :], BIG, -BIG, ALU.mult, ALU.add),
                         reads=[("gt", qt)], writes=[("gt", qt)])
                    P.op("pe", lambda e: e.transpose(p_mb[:, qt * 128:(qt + 1) * 128], gt[:, qt, :], ident_f),
                         reads=[("gt", qt), "cst"], writes=["p_gm"])
                P.op("dve", lambda e: e.tensor_copy(mbT[:, bs, :], p_mb), reads=["p_gm"], writes=[("mbT", bs)])
                nkv = 24 + l
                pairs = [(v, half) for v in range(nkv + 1) for half in range(2)]
                def front(i):
                    v, half = pairs[i]
                    st = n_st[0] % 4
                    n_st[0] += 1
                    k0 = v * 256 + half * 128
                    P.op("pe", lambda e: e.matmul(p_st[:, st, 0:256], lhsT=KT[:, hs, k0:k0 + 128], rhs=QT[:, hs, q0:q0 + 256],
                                                  start=True, stop=False),
                         reads=[("KT", hs), ("QT", hs)], writes=[("p_st", st)], inc=False)
                    if v < nkv:
                        P.op("pe", lambda e: e.matmul(p_st[:, st, 0:256], lhsT=esel[:, v, :], rhs=mbT[:, bs, :], start=False, stop=True),
                             reads=["esel", ("mbT", bs)], writes=[("p_st", st)])
                    else:
                        P.op("pe", lambda e: e.matmul(p_st[:, st, 0:256], lhsT=ident_b[:], rhs=cmask[:, half, :], start=False, stop=True),
                             reads=["ident_b", "cmask"], writes=[("p_st", st)])
                    P.op("act", lambda e: e.activation(PT[:, st, :], p_st[:, st, 0:256], AF.Exp, scale=scale),
                         reads=[("p_st", st)], writes=[("PT", st)])
                    return st

                def back(i, st):
                    v, half = pairs[i]
                    last = (i == len(pairs) - 1)
                    P.op("pe", lambda e: e.matmul(p_od[:, 0, 0:256], lhsT=Vh[:, hs, v * 2 + half, :], rhs=PT[:, st, :],
                                                  start=(i == 0), stop=last),
                         reads=[("Vh", hs), ("PT", st)], writes=["p_od"], inc=False)
                    P.op("pe", lambda e: e.matmul(p_od[:, 1, 0:256], lhsT=ones_1[:], rhs=PT[:, st, :],
                                                  start=(i == 0), stop=last),
                         reads=["ones_1", ("PT", st)], writes=["p_od"], inc=True)

                DEPTH = 3
                sts = {}
                for i in range(len(pairs) + DEPTH):
                    if i < len(pairs):
                        sts[i] = front(i)
                    if i - DEPTH >= 0:
                        back(i - DEPTH, sts.pop(i - DEPTH))
                P.op("dve", lambda e: e.reciprocal(rden[:], p_od[:, 1, 0:256]), reads=["p_od"], writes=["rden"])
                P.op("dve", lambda e: e.tensor_tensor(oaT[:, h, q0:q0 + 256], p_od[:, 0, 0:256], rden[:], ALU.mult),
                     reads=["p_od", "rden"], writes=[("oaT", h)])
        if DEBUG:
            P.dma("sp", g["dbg"]["oa"], oaT[:], reads=[("oaT", h) for h in range(8)])
        P.barrier()


def cst_dram_causal(g):
    return g["consts"][:, 5:9, :].rearrange("p (a b) c -> p a (b c)", a=2)


def phase_b(nc, P, L):
    g = L
    sb, ps = g["sb"], g["ps"]
    oaT, ones_rms, ones_1, ident_b, ident_f, cst = g["oaT"], g["ones_rms"], g["ones_1"], g["ident_b"], g["ident_f"], g["cst"]
    a2, s2, g1, gate1_bc = g["a2"], g["s2"], g["g1"], g["gate1_bc"]
    HT_d, OB_d, X1_d, XG_d = g["HT_d"], g["OB_d"], g["X1_d"], g["XG_d"]
    xTv, xtok = g["xTv"], g["xtok"]
    dest_i, w4, RWT = g["dest_i"], g["w4"], g["RWT"]
    with ExitStack() as es:
        wG_b = sb(es, "wG_b", [128, 8, 2048], BF16)
        woa_b = sb(es, "woa_b", [128, 8, D], BF16)
        wob_b = sb(es, "wob_b", [128, 8, D], BF16)
        wout_b = sb(es, "wout_b", [128, 8, D], BF16)
        nbm_sb = sb(es, "nbm_sb", [128, 16], F32)
        wr_sb = sb(es, "wr_sb", [128, 8, NE], F32)
        br_sb = sb(es, "br_sb", [128, NE], F32)
        ecap_sb = sb(es, "ecap_sb", [128, NE], F32)
        hTg = sb(es, "hTg", [128, 8, GS], BF16)
        obg = sb(es, "obg", [128, 8, GS], BF16)
        xo = sb(es, "xo", [128, 8, GS], F32)
        ge = sb(es, "ge", [128, GS], F32)
        mf = sb(es, "mf", [128, GS], F32)
        mt2 = sb(es, "mt2", [128, GS], F32)
        mT = sb(es, "mT", [128, 8, GS], BF16)
        sq = sb(es, "sqb", [128, 8, GS], BF16)
        rstd = sb(es, "rstdb", [128, GS], F32)
        h2f = sb(es, "h2f", [128, 8, GS], F32)
        h2b = sb(es, "h2b", [128, 8, GS], BF16)
        xt = sb(es, "xt", [128, D], F32)
        x1t = sb(es, "x1t", [128, D], F32)
        h2tok = sb(es, "h2tok", [128, D], BF16)
        lg = sb(es, "lg", [128, NE], F32)
        t8 = sb(es, "t8", [128, 8], F32)
        nmax = sb(es, "nmax", [128, 1], F32)
        den = sb(es, "den", [128, 1], F32)
        sel = sb(es, "sel", [128, NE], F32)
        selb = sb(es, "selb", [128, NE], BF16)
        carry = sb(es, "carry", [128, NE], F32)
        slot = sb(es, "slot", [128, NE], F32)
        oh = sb(es, "oh", [128, NE], F32)
        destf = sb(es, "destf", [128, 4], F32)
        RW = sb(es, "RW", [128, NE], F32)
        stri = sb(es, "stri", [128, 128], BF16)
        p_a = ps(es, "pb_a", [128, 2, 512], F32)
        p_b = ps(es, "pb_b", [128, 2, 512], F32)
        p_ss = ps(es, "pb_ss", [128, GS], F32)
        p_lg = ps(es, "pb_lg", [128, 2, NE], F32)
        p_tr = ps(es, "pb_tr", [128, 2, 512], BF16)
        p_rw = ps(es, "pb_rw", [32, 128], F32)
        for k in range(8):
            P.dma("pool", wG_b[:, k, :], g["wG"][k * 128:(k + 1) * 128, :], writes=[("wG", k)], sem=("wG", k))
            P.dma("pool", woa_b[:, k, :], g["w_oa"][k * 128:(k + 1) * 128, :], writes=[("woa", k)], sem=("woa", k))
            P.dma("pool", wob_b[:, k, :], g["w_ob"][k * 128:(k + 1) * 128, :], writes=[("wob", k)], sem=("wob", k))
            P.dma("pool", wout_b[:, k, :], g["w_out"][k * 128:(k + 1) * 128, :], writes=[("wout", k)], sem=("wout", k))
        P.dma("pool", stri[:], g["consts"][:, 4, :], writes=["stri"])
        P.dma("sp", nbm_sb[:], g["nbm"], writes=["nbm"])
        P.dma("sp", wr_sb[:], g["w_r"].rearrange("(k p) n -> p k n", p=128), writes=["wr"])
        P.dma("sp", br_sb[:], g["b_r_bc"], writes=["br"])
        P.dma("sp", ecap_sb[:], g["ecap"], writes=["ecap"])
        P.op("pool", lambda e: e.memset(carry[:], 0.0), writes=["carry"])
        P.op("dve", lambda e: e.tensor_scalar(nbm_sb[:], nbm_sb[:], -1.0, None, ALU.mult), reads=["nbm"], writes=["nbm"])
        kk = lambda n: [(n, k) for k in range(8)]
        for G in range(OWN // GS):
            o0 = G * GS
            P.dma("sp", hTg[:], HT_d[:, o0:o0 + GS].rearrange("(k p) n -> p k n", p=128), writes=["hTg"])
            P.dma("sp", obg[:], OB_d[:, o0:o0 + GS].rearrange("(k p) n -> p k n", p=128), writes=["obg"])
            P.dma("sp", xo[:], xTv[:, 6144 + o0:6144 + o0 + GS].rearrange("(k p) n -> p k n", p=128), writes=["xo"])
            for c in range(8):
                for br in range(2):
                    wy = woa_b if br == 0 else wob_b
                    wyk = kk("woa") if br == 0 else kk("wob")
                    for k in range(8):
                        P.op("pe", lambda e: e.matmul(p_a[:, br, 0:GS], lhsT=wG_b[:, k, br * 1024 + c * 128:br * 1024 + (c + 1) * 128],
                                                      rhs=hTg[:, k, :], start=(k == 0), stop=(k == 7)),
                             reads=kk("wG") + ["hTg"], writes=[("p_a", br)], inc=(k == 7))
                    for k in range(8):
                        rhs = oaT[:, k, o0:o0 + GS] if br == 0 else obg[:, k, :]
                        P.op("pe", lambda e: e.matmul(p_b[:, br, 0:GS], lhsT=wy[:, k, c * 128:(c + 1) * 128], rhs=rhs,
                                                      start=(k == 0), stop=(k == 7)),
                             reads=wyk + (["obg"] if br else []), writes=[("p_b", br)], inc=(k == 7))
                    P.op("act", lambda e: e.activation(ge[:], p_a[:, br, 0:GS], AF.Exp, bias=nbm_sb[:, br * 8 + c:br * 8 + c + 1], scale=-1.0),
                         reads=[("p_a", br), "nbm"], writes=["ge"])
                    P.op("dve", lambda e: e.tensor_scalar(ge[:], ge[:], 1.0, None, ALU.add), reads=["ge"], writes=["ge"])
                    P.op("dve", lambda e: e.reciprocal(ge[:], ge[:]), reads=["ge"], writes=["ge"])
                    if br == 0:
                        P.op("dve", lambda e: e.tensor_tensor(mf[:], ge[:], p_b[:, br, 0:GS], ALU.mult), reads=["ge", ("p_b", br)], writes=["mf"])
                    else:
                        P.op("dve", lambda e: e.tensor_tensor(mt2[:], ge[:], p_b[:, br, 0:GS], ALU.mult), reads=["ge", ("p_b", br)], writes=["mt2"])
                        P.op("dve", lambda e: e.tensor_tensor(mT[:, c, :], mf[:], mt2[:], ALU.add), reads=["mf", "mt2"], writes=["mT"])
            for c in range(8):
                for k in range(8):
                    P.op("pe", lambda e: e.matmul(p_a[:, c % 2, 0:GS], lhsT=wout_b[:, k, c * 128:(c + 1) * 128], rhs=mT[:, k, :],
                                                  start=(k == 0), stop=(k == 7)),
                         reads=kk("wout") + ["mT"], writes=[("p_a", c % 2)], inc=(k == 7))
                P.op("dve", lambda e: e.scalar_tensor_tensor(xo[:, c, :], p_a[:, c % 2, 0:GS], g1[:, c:c + 1], xo[:, c, :], ALU.mult, ALU.add),
                     reads=[("p_a", c % 2), "xo"], writes=["xo"])
            for t in range(TPG):
                T = G * TPG + t
                P.dma("sp", xt[:], xtok[T * 128:(T + 1) * 128, :], writes=["xt"])
                for half in range(2):
                    for k in range(8):
                        P.op("pe", lambda e: e.matmul(p_b[:, half, :], lhsT=mT[:, k, t * 128:(t + 1) * 128],
                                                      rhs=wout_b[:, k, half * 512:(half + 1) * 512], start=(k == 0), stop=(k == 7)),
                             reads=kk("wout") + ["mT"], writes=[("p_b", half)], inc=(k == 7))
                    P.op("dve", lambda e: e.tensor_tensor(x1t[:, half * 512:(half + 1) * 512], p_b[:, half, :],
                                                          gate1_bc[:, half * 512:(half + 1) * 512], ALU.mult),
                         reads=[("p_b", half)], writes=["x1t"])
                P.op("pool", lambda e: e.tensor_tensor(x1t[:], x1t[:], xt[:], ALU.add), reads=["x1t", "xt"], writes=["x1t"])
                P.dma("sp", X1_d[T * 128:(T + 1) * 128, :], x1t[:], reads=["x1t"], writes=[("X1_d", T)], sem="X1_o")
            rmsnorm_fm(P, nc, xo[:], sq[:], p_ss[:], rstd[:], ones_rms, ["xo"], "b", "pb_ss")
            for k in range(8):
                P.op("dve", lambda e: e.scalar_tensor_tensor(xo[:, k, :], xo[:, k, :], a2[:, k:k + 1], rstd[:], ALU.mult, ALU.mult),
                     reads=["xo", "brstd"], writes=["xo"])
                P.op("act", lambda e: e.activation(h2f[:, k, :], xo[:, k, :], AF.Identity, bias=s2[:, k:k + 1], scale=1.0),
                     reads=["xo"], writes=["h2f"])
            P.op("pool", lambda e: e.tensor_copy(h2b[:], h2f[:]), reads=["h2f"], writes=["h2b"])
            for t in range(TPG):
                T = G * TPG + t
                lq = t
                for k in range(8):
                    P.op("pe", lambda e: e.matmul(p_lg[:, lq, :], lhsT=h2f[:, k, t * 128:(t + 1) * 128], rhs=wr_sb[:, k, :],
                                                  start=(k == 0), stop=(k == 7)),
                         reads=["h2f", "wr"], writes=["p_lg"], inc=(k == 7))
                P.op("dve", lambda e: e.tensor_tensor(lg[:], p_lg[:, lq, :], br_sb[:], ALU.add), reads=["p_lg", "br"], writes=["lg"])
                if DEBUG:
                    P.dma("sp", g["dbg"]["lg"][:, T, :], lg[:], reads=["lg"], sem="dbg_lg")
                P.op("dve", lambda e: e.max(t8[:], lg[:]), reads=["lg"], writes=["t8"])
                P.op("dve", lambda e: e.tensor_scalar(nmax[:], t8[:, 0:1], -1.0, None, ALU.mult), reads=["t8"], writes=["nmax"])
                P.op("act", lambda e: e.activation(w4[:, T, :], t8[:, 0:4], AF.Exp, bias=nmax[:], scale=1.0),
                     reads=["t8", "nmax"], writes=[("w4", T)])
                P.op("dve", lambda e: e.tensor_reduce(den[:], w4[:, T, :], AX.X, ALU.add), reads=[("w4", T)], writes=["den"])
                P.op("dve", lambda e: e.reciprocal(den[:], den[:]), reads=["den"], writes=["den"])
                P.op("dve", lambda e: e.tensor_scalar(w4[:, T, :], w4[:, T, :], den[:], None, ALU.mult), reads=[("w4", T), "den"], writes=[("w4", T)])
                P.op("dve", lambda e: e.tensor_scalar(sel[:], lg[:], t8[:, 3:4], None, ALU.is_ge), reads=["lg", "t8"], writes=["sel"])
                P.op("dve", lambda e: e.tensor_copy(selb[:], sel[:]), reads=["sel"], writes=["selb"])
                P.op("pe", lambda e: e.matmul(p_lg[:, lq, :], lhsT=stri[:], rhs=selb[:], start=True, stop=True),
                     reads=["stri", "selb"], writes=["p_lg"])
                P.op("dve", lambda e: e.tensor_tensor(slot[:], p_lg[:, lq, :], carry[:], ALU.add), reads=["p_lg", "carry"], writes=["slot"])
                P.op("pe", lambda e: e.matmul(p_lg[:, lq, :], lhsT=ones_1[:], rhs=selb[:], start=True, stop=True),
                     reads=["selb"], writes=["p_lg"])
                P.op("dve", lambda e: e.tensor_tensor(carry[:], p_lg[:, lq, :], carry[:], ALU.add), reads=["p_lg", "carry"], writes=["carry"])
                P.op("dve", lambda e: e.tensor_tensor(slot[:], slot[:], ecap_sb[:], ALU.add), reads=["slot", "ecap"], writes=["slot"])
                P.op("pool", lambda e: e.memset(RW[:], 0.0), writes=["RW"])
                for k4 in range(4):
                    P.op("dve", lambda e: e.tensor_scalar(oh[:], lg[:], t8[:, k4:k4 + 1], None, ALU.is_equal), reads=["lg", "t8"], writes=["oh"])
                    P.op("dve", lambda e: e.scalar_tensor_tensor(RW[:], oh[:], w4[:, T, k4:k4 + 1], RW[:], ALU.mult, ALU.add),
                         reads=["oh", ("w4", T), "RW"], writes=["RW"])
                    P.op("dve", lambda e: e.tensor_tensor(oh[:], oh[:], slot[:], ALU.mult), reads=["oh", "slot"], writes=["oh"])
                    P.op("dve", lambda e: e.tensor_reduce(destf[:, k4:k4 + 1], oh[:], AX.X, ALU.add), reads=["oh"], writes=["destf"])
                P.op("dve", lambda e: e.tensor_copy(dest_i[:, T * 4:T * 4 + 4], destf[:]), reads=["destf"], writes=[("dest_i", T)])
                P.op("pe", lambda e: e.transpose(p_rw[:], RW[:], ident_f), reads=["RW"], writes=["p_rw"])
                P.op("act", lambda e: e.copy(RWT[:, T, :], p_rw[:]), reads=["p_rw"], writes=[("RWT", T)])
                for hf in range(2):
                    for k in range(4):
                        P.op("pe", lambda e: e.transpose(p_tr[:, hf, k * 128:(k + 1) * 128], h2b[:, hf * 4 + k, t * 128:(t + 1) * 128], ident_b[:]),
                             reads=["h2b"], writes=["p_tr"], inc=(k == 3))
                    P.op("act", lambda e: e.copy(h2tok[:, hf * 512:(hf + 1) * 512], p_tr[:, hf, :]), reads=["p_tr"], writes=["h2tok"])
                for k4 in range(4):
                    P.dma("pool", None, None, reads=["h2tok", ("dest_i", T)], writes=["XG_d"], sem=("xgs", k4),
                          emit=lambda e: e.indirect_dma_start(
                              out=XG_d[:, :], out_offset=bass.IndirectOffsetOnAxis(ap=dest_i[:, T * 4 + k4:T * 4 + k4 + 1], axis=0),
                              in_=h2tok[:, :], in_offset=None, bounds_check=g["bc_reg"], oob_is_err=False))
        if DEBUG:
            P.wait_all("sp")
            P.dma("sp", g["dbg"]["x1"], X1_d, reads=[])
        P.barrier()


def phase_moe(nc, P, L):
    g = L
    sb, ps = g["sb"], g["ps"]
    ident_b, gate2_bc = g["ident_b"], g["gate2_bc"]
    XG_d, OUT_d, X1_d = g["XG_d"], g["OUT_d"], g["X1_d"]
    w1d, w2, out = g["w1d"], g["w2"], g["out"]
    dest_i, w4, RWT = g["dest_i"], g["w4"], g["RWT"]
    NCH = CAP // 512
    with ExitStack() as es:
        wp = sb(es, "wp", [128, 12, 8, 512], BF16)
        b1 = sb(es, "b1", [128, NE, 16], F32)
        xg = sb(es, "xg", [128, 2, 4, D], BF16)
        xgT = sb(es, "xgT", [128, 2, 8, 512], BF16)
        actT = sb(es, "actT", [128, 2, 8, 512], BF16)
        gg = sb(es, "gg", [128, 2, 512], F32)
        ll = sb(es, "ll", [128, 2, 512], F32)
        ee = sb(es, "ee", [128, 2, 512], F32)
        orow = sb(es, "orow", [128, 4, D], F32)
        p_h = ps(es, "pm_h", [128, 4, 512], F32)
        p_o = ps(es, "pm_o", [128, 2, 512], F32)
        p_t = ps(es, "pm_t", [128, 2, 512], BF16)
        P.dma("sp", b1[:].rearrange("p a b -> p (a b)"), g["b1T"], writes=["b1"])

        def load_expert(e):
            base = (e % 2) * 6
            for p in range(4):
                for two in range(2):
                    src = w1d[e][:, two * 1024 + p * 256:two * 1024 + (p + 1) * 256]
                    P.dma("pool", wp[:, base + p, :, two * 256:(two + 1) * 256], src.rearrange("(k p) f -> p k f", p=128),
                          writes=[("wp", base + p, two)], sem=("wp", base + p, two))
            for hf in range(2):
                src = w2[e][:, hf * 512:(hf + 1) * 512]
                P.dma("pool", wp[:, base + 4 + hf], src.rearrange("(k p) n -> p k n", p=128),
                      writes=[("wp", base + 4 + hf, 0)], sem=("wp", base + 4 + hf, 0))

        load_expert(0)
        nch = [0]
        for e in range(NE):
            base = (e % 2) * 6
            if e + 1 < NE:
                load_expert(e + 1)
            for ch in range(NCH):
                u = nch[0] % 2
                nch[0] += 1
                r0 = e * CAP + ch * 512
                P.dma("sp", xg[:, u], XG_d[r0:r0 + 512, :].rearrange("(r p) d -> p r d", p=128),
                      writes=[("xg", u)], sem=("xg", u))
                for r in range(4):
                    for hf in range(2):
                        for k in range(4):
                            P.op("pe", lambda e_: e_.transpose(p_t[:, hf, k * 128:(k + 1) * 128],
                                                               xg[:, u, r, (hf * 4 + k) * 128:(hf * 4 + k + 1) * 128], ident_b[:]),
                                 reads=[("xg", u)], writes=["p_t"], inc=(k == 3))
                        P.op("act", lambda e_: e_.copy(xgT[:, u, hf * 4:(hf + 1) * 4, r * 128:(r + 1) * 128],
                                                       p_t[:, hf, :].rearrange("p (k c) -> p k c", k=4)),
                             reads=["p_t"], writes=[("xgT", u)])
                for j in range(8):
                    sl = base + j // 2
                    hs = j % 2
                    for part in range(2):
                        c0 = part * 256 + (j % 2) * 128
                        for k in range(8):
                            P.op("pe", lambda e_: e_.matmul(p_h[:, hs * 2 + part, :], lhsT=wp[:, sl, k, c0:c0 + 128], rhs=xgT[:, u, k, :],
                                                            start=(k == 0), stop=(k == 7)),
                                 reads=[("wp", sl, part), ("xgT", u)], writes=[("p_h", hs, part)], inc=(k == 7))
                    P.op("dve", lambda e_: e_.tensor_scalar(gg[:, hs, :], p_h[:, hs * 2, :], b1[:, e, j:j + 1], 7.0, ALU.add, ALU.min),
                         reads=[("p_h", hs, 0), "b1"], writes=[("gg", hs)])
                    P.op("dve", lambda e_: e_.tensor_scalar(ll[:, hs, :], p_h[:, hs * 2 + 1, :], b1[:, e, 8 + j:9 + j], 7.0, ALU.add, ALU.min),
                         reads=[("p_h", hs, 1), "b1"], writes=[("ll", hs)])
                    P.op("dve", lambda e_: e_.tensor_scalar(ll[:, hs, :], ll[:, hs, :], -7.0, 1.0, ALU.max, ALU.add),
                         reads=[("ll", hs)], writes=[("ll", hs)])
                    P.op("act", lambda e_: e_.activation(ee[:, hs, :], gg[:, hs, :], AF.Exp, scale=-1.702), reads=[("gg", hs)], writes=[("ee", hs)])
                    P.op("act", lambda e_: e_.activation(ee[:, hs, :], ee[:, hs, :], AF.Identity, bias=1.0, scale=1.0), reads=[("ee", hs)], writes=[("ee", hs)])
                    P.op("dve", lambda e_: e_.reciprocal(ee[:, hs, :], ee[:, hs, :]), reads=[("ee", hs)], writes=[("ee", hs)])
                    P.op("dve", lambda e_: e_.tensor_tensor(gg[:, hs, :], gg[:, hs, :], ll[:, hs, :], ALU.mult),
                         reads=[("gg", hs), ("ll", hs)], writes=[("gg", hs)])
                    P.op("dve", lambda e_: e_.tensor_tensor(actT[:, u, j, :], gg[:, hs, :], ee[:, hs, :], ALU.mult),
                         reads=[("gg", hs), ("ee", hs)], writes=[("actT", u)])
                for hf in range(2):
                    sl = base + 4 + hf
                    for r in range(4):
                        os_ = (hf * 4 + r) % 2
                        for k in range(8):
                            P.op("pe", lambda e_: e_.matmul(p_o[:, os_, :], lhsT=actT[:, u, k, r * 128:(r + 1) * 128], rhs=wp[:, sl, k, :],
                                                            start=(k == 0), stop=(k == 7)),
                                 reads=[("actT", u), ("wp", sl, 0)], writes=[("p_o", os_)], inc=(k == 7))
                        P.op("act", lambda e_: e_.copy(orow[:, r, hf * 512:(hf + 1) * 512], p_o[:, os_, :]),
                             reads=[("p_o", os_)], writes=["orow"])
                P.dma("sp", OUT_d[r0:r0 + 512, :].rearrange("(r p) d -> p r d", p=128), orow[:],
                      reads=["orow"], writes=[("OUT_d", e, ch)], sem="orow")
        P.barrier()
    with ExitStack() as es:
        yk = sb(es, "yk", [128, 2, 4, D], F32)
        acc = sb(es, "acc", [128, 2, D], F32)
        x1t = sb(es, "x1c", [128, 2, D], F32)
        fsq = sb(es, "fsq", [128, D], F32)
        ssq = sb(es, "ssq", [128, 2], F32)
        b2_sb = sb(es, "b2c", [32, D], F32)
        fnw = sb(es, "fnwc", [128, D], F32)
        p_bias = ps(es, "pc_b", [128, 2, 2, 512], F32)
        P.dma("sp", b2_sb[:], g["b2"], writes=["b2c"])
        P.dma("sp", fnw[:], g["fnw_bc"], writes=["fnwc"])
        for T in range(16):
            u = T % 2
            for k4 in range(4):
                P.dma("pool", None, None, reads=["OUT_d"], writes=[("yk", u, k4)], sem=("yk", u, k4),
                      emit=lambda e: e.indirect_dma_start(
                          out=yk[:, u, k4, :], out_offset=None, in_=OUT_d[:, :],
                          in_offset=bass.IndirectOffsetOnAxis(ap=dest_i[:, T * 4 + k4:T * 4 + k4 + 1], axis=0),
                          bounds_check=g["bc_reg"], oob_is_err=False))
            P.dma("sp", x1t[:, u, :], X1_d[T * 128:(T + 1) * 128, :], reads=["X1_d"], writes=[("x1c", u)], sem=("x1c", u))
            for half in range(2):
                P.op("pe", lambda e: e.matmul(p_bias[:, u, half, :], lhsT=RWT[:, T, :], rhs=b2_sb[:, half * 512:(half + 1) * 512],
                                              start=True, stop=True),
                     reads=["b2c"], writes=[("p_bias", u)], inc=(half == 1))
            P.op("dve", lambda e: e.tensor_scalar(acc[:, u, :], yk[:, u, 0, :], w4[:, T, 0:1], None, ALU.mult),
                 reads=[("yk", u, 0)], writes=[("acc", u)])
            for k4 in range(1, 4):
                P.op("dve", lambda e: e.scalar_tensor_tensor(acc[:, u, :], yk[:, u, k4, :], w4[:, T, k4:k4 + 1], acc[:, u, :], ALU.mult, ALU.add),
                     reads=[("yk", u, k4), ("acc", u)], writes=[("acc", u)])
            for half in range(2):
                P.op("dve", lambda e: e.tensor_tensor(acc[:, u, half * 512:(half + 1) * 512], acc[:, u, half * 512:(half + 1) * 512],
                                                      p_bias[:, u, half, :], ALU.add),
                     reads=[("acc", u), ("p_bias", u)], writes=[("acc", u)])
            P.op("pool", lambda e: e.tensor_tensor(acc[:, u, :], acc[:, u, :], gate2_bc[:], ALU.mult), reads=[("acc", u), "gate2_bc"], writes=[("acc", u)])
            P.op("pool", lambda e: e.tensor_tensor(acc[:, u, :], acc[:, u, :], x1t[:, u, :], ALU.add), reads=[("acc", u), ("x1c", u)], writes=[("acc", u)])
            P.op("pool", lambda e: e.memset(ssq[:, u:u + 1], 0.0), writes=[("ssq", u)])
            P.op("act", lambda e: e.activation(fsq[:], acc[:, u, :], AF.Square, accum_out=ssq[:, u:u + 1]), reads=[("acc", u), ("ssq", u)], writes=["fsq", ("ssq", u)])
            rsqrt_act(P, ssq[:, u:u + 1], ssq[:, u:u + 1], [("ssq", u)], ("ssq", u), 1.0 / D)
            P.op("dve", lambda e: e.scalar_tensor_tensor(acc[:, u, :], acc[:, u, :], ssq[:, u:u + 1], fnw[:], ALU.mult, ALU.mult),
                 reads=[("acc", u), ("ssq", u), "fnwc"], writes=[("acc", u)])
            P.dma("sp", out[T * 128:(T + 1) * 128, :], acc[:, u, :], reads=[("acc", u)], writes=[("out", T)], sem=("out", u))


def _consts():
    c = np.zeros((128, 10, 128), np.float32)
    c[:, 0, :] = np.eye(128, dtype=np.float32)
    perm = np.zeros((128, 128), np.float32)
    for m in range(32):
        perm[(m + 16) % 32, m] = 1.0
    c[:, 1, :] = perm
    i = np.arange(128)
    c[:, 2, :] = np.where(i[:, None] <= i[None, :], -1.0 / 16.0, 0.0)
    c[:, 3, :] = (i[:, None] <= i[None, :]).astype(np.float32)
    c[:, 4, :] = (i[:, None] < i[None, :]).astype(np.float32)
    q = np.arange(256)
    for half in range(2):
        kpos = half * 128 + i
        m = np.where(kpos[:, None] <= q[None, :], 0.0, -BIG).astype(np.float32)
        c[:, 5 + 2 * half, :] = m[:, :128]
        c[:, 6 + 2 * half, :] = m[:, 128:]
    return c


def _rope_tables():
    half = 16
    inv = np.float32(500000.0) ** (-np.arange(half, dtype=np.float32) * np.float32(2.0) / np.float32(32))
    ang = np.arange(SEQ, dtype=np.float32)[:, None] * inv[None, :].astype(np.float32)
    cos = np.cos(ang).astype(np.float32).T
    sin = np.sin(ang).astype(np.float32).T
    return np.concatenate([cos, cos], 0), np.concatenate([-sin, sin], 0)


def prep_inputs(inputs):
    f = lambda k: np.asarray(inputs[k], np.float32)
    x = f("x")
    c = f("c")
    w_in = f("w_in")[0]
    fm = lambda v: np.ascontiguousarray(v.reshape(-1, 128).T)
    bc = lambda v: np.ascontiguousarray(np.broadcast_to(v[None, :], (128, v.shape[0])))
    o = [0]
    for sz in (1024, 1024, 1024, 512, 512, 1024, 1024, 16, 1024, 1024):
        o.append(o[-1] + sz)
    mq, mk, mv, gq, gk, gv, gr, gl, ga, gb = [w_in[:, o[i]:o[i + 1]] for i in range(10)]
    shared = {
        "w_ada": f("w_ada")[0],
        "b_ada_bc": bc(f("b_ada")[0]),
        "nw": np.concatenate([fm(f("norm1_w")[0]), fm(f("norm2_w")[0])], 1),
        "fnw_bc": bc(f("final_norm_w")),
        "wA": np.ascontiguousarray(np.concatenate([mk, mv, gk, gv, gl], 1)),
        "wO": np.ascontiguousarray(np.concatenate([mq, gq, gr], 1)),
        "wG": np.ascontiguousarray(np.concatenate([ga, gb], 1)),
        "w_oa": f("w_o_moba")[0], "w_ob": f("w_o_gla")[0], "w_out": f("w_out")[0],
        "nbm": fm(f("b_merge")[0]),
        "gnw": fm(f("gla_norm_w")[0]),
        "w_r": f("w_router")[0], "b_r_bc": bc(f("b_router")[0]),
        "w2": f("w_exp_out")[0], "b2": f("b_exp_out")[0],
        "consts": _consts(),
        "ecap": bc(np.arange(NE, dtype=np.float32) * CAP),
    }
    gw = np.zeros((32, 512), np.float32)
    gw[0:16] = f("gla_gate_w")[0]
    gw[16] = f("gla_gate_b")[0]
    shared["gw_aug"] = gw
    w1 = f("w_exp_in")[0]
    shared["w1d"] = np.ascontiguousarray(np.concatenate([w1[:, :, 0::2], w1[:, :, 1::2]], 2))
    b1 = f("b_exp_in")[0]
    b1d = np.concatenate([b1[:, 0::2], b1[:, 1::2]], 1)
    shared["b1T"] = np.ascontiguousarray(b1d.reshape(NE, 16, 128).transpose(2, 0, 1).reshape(128, NE * 16))
    es = np.zeros((32, 32, 128), np.float32)
    for j in range(32):
        es[j, j, :] = 1.0
    shared["esel_in"] = es.reshape(32, 32 * 128)
    cosT, sinT = _rope_tables()
    per_core = []
    for core in range(NCORE):
        b, r = core // 4, core % 4
        nnull = (24 - 8 * r) * 256
        nreal = 2048 * (r + 1)
        xv = np.zeros((D, SEQ), np.float32)
        xv[:, nnull:] = x[b, :nreal, :].T
        rc_ = np.zeros((32, SEQ), np.float32)
        rs_ = np.zeros((32, SEQ), np.float32)
        rc_[:, nnull:] = cosT[:, :nreal]
        rs_[:, nnull:] = sinT[:, :nreal]
        vf = np.zeros((SEQ,), np.float32)
        vf[nnull:] = 1.0
        gm = np.full((8, 32), NEGINF, np.float32)
        gvv = np.zeros((8, 32), np.float32)
        for l in range(8):
            gm[l, 24 - 8 * r:24 + l] = 0.0
            gvv[l, 24 - 8 * r:24 + l] = 1.0
        d = dict(shared)
        d.update({
            "xTv": xv,
            "xtok": np.ascontiguousarray(x[b, 2048 * r:2048 * (r + 1), :]),
            "cT": fm(c[b]),
            "ropec": rc_, "ropes": rs_,
            "vflag": np.ascontiguousarray(vf.reshape(64, 128).T),
            "gmask": bc(gm.reshape(-1)), "gvalid": bc(gvv.reshape(-1)),
        })
        per_core.append(d)
    return per_core


_NC_CACHE = {}


def kernel(**inputs):
    in_maps = prep_inputs(inputs)
    if "nc" not in _NC_CACHE:
        _NC_CACHE["nc"] = build_nc()[0]
    nc = _NC_CACHE["nc"]
    res = run_bass_kernel_spmd(nc, in_maps, core_ids=list(range(NCORE)))
    outp = np.zeros((2, SEQ, D), np.float32)
    for core in range(NCORE):
        b, r = core // 4, core % 4
        outp[b, 2048 * r:2048 * (r + 1), :] = res.results[core]["out"]
    return outp
```

```python
import numpy as np
from contextlib import ExitStack
import concourse.bass as bass
import concourse.mybir as mybir
from concourse.bass_utils import run_bass_kernel_spmd

F32 = mybir.dt.float32
BF16 = mybir.dt.bfloat16
I32 = mybir.dt.int32
AF = mybir.ActivationFunctionType
ALU = mybir.AluOpType
AX = mybir.AxisListType

D = 1024
SEQ = 8192
NCORE = 8
OWN = 2048
GS = 256
TPG = 2
NG = 32
OWNG0 = 24
NVB = 32
NE = 32
CAP = 1024
BIG = 30000.0
NEGINF = -1.0e30
EPS = 1e-5
STAGE = 99
DEBUG = False


class Prog:
    def __init__(self, nc, same_engine_sync=True):
        self.nc = nc
        self.engs = {"pe": nc.tensor, "act": nc.scalar, "dve": nc.vector,
                     "pool": nc.gpsimd, "sp": nc.sync}
        self.sem = {k: nc.alloc_semaphore("prog_" + k) for k in self.engs}
        self.cnt = {k: 0 for k in self.engs}
        self.seen = {k: {} for k in self.engs}
        self.bufs = {}
        self.pending = {k: ([], []) for k in self.engs}
        self.dsem = {}
        self.dcnt = {}
        self.same = same_engine_sync
        self.nwait = 0
        self.nins = 0
        self.free_sems = {"sw": [], "hw": []}
        self.dkind = {}
        self.nalloc = 0

    def _deps(self, reads, writes):
        deps = []
        for k in reads:
            b = self.bufs.get(k)
            if b is not None and b[0] is not None:
                deps.append(b[0])
        for k in writes:
            b = self.bufs.get(k)
            if b is not None:
                if b[0] is not None:
                    deps.append(b[0])
                deps.extend(b[1])
        return deps

    def _wait(self, eng, deps, own_ok=True):
        need = {}
        for (s, v) in deps:
            if own_ok and s is self.sem[eng]:
                if eng == "pe" or not self.same:
                    continue
            key = id(s)
            if self.seen[eng].get(key, 0) >= v:
                continue
            if key not in need or need[key][1] < v:
                need[key] = (s, v)
        for key, (s, v) in need.items():
            self.engs[eng].wait_ge(s, v)
            self.seen[eng][key] = v
            self.nwait += 1

    def _record(self, ev, reads, writes):
        for k in reads:
            b = self.bufs.get(k)
            if b is None:
                self.bufs[k] = [None, [ev]]
            else:
                b[1].append(ev)
                if len(b[1]) > 24:
                    b[1] = b[1][-24:] if False else b[1]
        for k in writes:
            self.bufs[k] = [ev, []]

    def op(self, eng, emit, reads=(), writes=(), inc=True):
        reads = list(reads)
        writes = list(writes)
        self._wait(eng, self._deps(reads, writes))
        ins = emit(self.engs[eng])
        self.nins += 1
        if not inc:
            self.pending[eng][0].extend(reads)
            self.pending[eng][1].extend(writes)
            return None
        self.cnt[eng] += 1
        ev = (self.sem[eng], self.cnt[eng])
        ins.then_inc(self.sem[eng], 1)
        pr, pw = self.pending[eng]
        self._record(ev, pr + reads, pw + writes)
        self.pending[eng] = ([], [])
        return ev

    def dma(self, queue, out, in_, reads=(), writes=(), sem=None, emit=None, **kw):
        reads = list(reads)
        writes = list(writes)
        if sem is None:
            sem = ("auto",) + tuple(writes) + tuple(reads)
        kind = "sw" if queue == "pool" else "hw"
        if sem in self.dsem:
            assert self.dkind[sem] == kind, (sem, kind)
        if sem not in self.dsem:
            self.dkind[sem] = kind
            if self.free_sems[kind]:
                self.dsem[sem], self.dcnt[sem] = self.free_sems[kind].pop()
            else:
                self.dsem[sem] = self.nc.alloc_semaphore("dma_%d" % self.nalloc)
                self.nalloc += 1
                self.dcnt[sem] = 0
        self._wait(queue, self._deps(reads, writes))
        if emit is not None:
            ins = emit(self.engs[queue])
        else:
            ins = self.engs[queue].dma_start(out=out, in_=in_, **kw)
        self.nins += 1
        self.dcnt[sem] += 16
        ins.then_inc(self.dsem[sem], 16)
        ev = (self.dsem[sem], self.dcnt[sem])
        self._record(ev, reads, writes)
        return ev

    def wait_all(self, eng, keys=None):
        deps = []
        for k, b in self.bufs.items():
            if keys is not None and k not in keys:
                continue
            if b[0] is not None:
                deps.append(b[0])
            deps.extend(b[1])
        self._wait(eng, deps, own_ok=False)

    def barrier(self):
        assert all(len(p[0]) == 0 and len(p[1]) == 0 for p in self.pending.values())
        for eng in self.engs:
            deps = [(self.sem[o], self.cnt[o]) for o in self.engs if o != eng and self.cnt[o] > 0]
            deps += [(self.dsem[k], self.dcnt[k]) for k in self.dsem if self.dcnt[k] > 0]
            self._wait(eng, deps, own_ok=False)
        self.bufs = {}
        for k in list(self.dsem):
            self.free_sems[self.dkind.pop(k)].append((self.dsem.pop(k), self.dcnt.pop(k)))


def build_nc():
    nc = bass.Bass("TRN2", target_bir_lowering=False)
    P = Prog(nc)
    dt_in = lambda n, s, d=F32: nc.dram_tensor(n, list(s), d, kind="ExternalInput").ap()
    dt_sc = lambda n, s, d: nc.dram_tensor(n, list(s), d).ap()

    xTv = dt_in("xTv", [D, SEQ])
    xtok = dt_in("xtok", [OWN, D])
    cT = dt_in("cT", [128, 8])
    w_ada = dt_in("w_ada", [D, 6 * D])
    b_ada_bc = dt_in("b_ada_bc", [128, 6 * D])
    nw = dt_in("nw", [128, 16])
    fnw_bc = dt_in("fnw_bc", [128, D])
    wA = dt_in("wA", [D, 3600])
    wO = dt_in("wO", [D, 2560])
    wG = dt_in("wG", [D, 2048])
    w_oa = dt_in("w_oa", [D, D])
    w_ob = dt_in("w_ob", [D, D])
    w_out = dt_in("w_out", [D, D])
    nbm = dt_in("nbm", [128, 16])
    ropec = dt_in("ropec", [32, SEQ])
    ropes = dt_in("ropes", [32, SEQ])
    vflag = dt_in("vflag", [128, 64])
    gmask = dt_in("gmask", [128, 8 * 32])
    gvalid = dt_in("gvalid", [128, 8 * 32])
    gw_aug = dt_in("gw_aug", [32, 512])
    gnw = dt_in("gnw", [128, 2])
    w_r = dt_in("w_r", [D, NE])
    b_r_bc = dt_in("b_r_bc", [128, NE])
    w1d = dt_in("w1d", [NE, D, 2048]) if STAGE >= 4 else None
    w2 = dt_in("w2", [NE, D, D]) if STAGE >= 4 else None
    b1T = dt_in("b1T", [128, NE * 16])
    b2 = dt_in("b2", [NE, D])
    consts = dt_in("consts", [128, 10, 128])
    esel_in = dt_in("esel_in", [32, 32 * 128])
    ecap = dt_in("ecap", [128, NE])
    out = nc.dram_tensor("out", [OWN, D], F32, kind="ExternalOutput").ap()

    KT_d = dt_sc("KT_d", [8, 128, SEQ], BF16)
    V_d = dt_sc("V_d", [SEQ, D], BF16)
    QT_d = dt_sc("QT_d", [8, 128, OWN], BF16)
    OB_d = dt_sc("OB_d", [D, OWN], BF16)
    HT_d = dt_sc("HT_d", [D, OWN], BF16)
    X1_d = dt_sc("X1_d", [OWN, D], F32)
    XG_d = dt_sc("XG_d", [NE * CAP, D], BF16)
    OUT_d = dt_sc("OUT_d", [NE * CAP, D], F32)

    dbg = {}
    if DEBUG:
        dbg["mod"] = nc.dram_tensor("dbg_mod", [128, 48], F32, kind="ExternalOutput").ap()
        dbg["kT"] = nc.dram_tensor("dbg_kT", [8, 128, SEQ], BF16, kind="ExternalOutput").ap()
        dbg["qT"] = nc.dram_tensor("dbg_qT", [8, 128, OWN], BF16, kind="ExternalOutput").ap()
        dbg["ob"] = nc.dram_tensor("dbg_ob", [D, OWN], BF16, kind="ExternalOutput").ap()
        dbg["oa"] = nc.dram_tensor("dbg_oa", [128, 8, OWN], BF16, kind="ExternalOutput").ap()
        dbg["x1"] = nc.dram_tensor("dbg_x1", [OWN, D], F32, kind="ExternalOutput").ap()
        dbg["lg"] = nc.dram_tensor("dbg_lg", [128, 16, 32], F32, kind="ExternalOutput").ap()

    es_all = ExitStack()
    with es_all:
        sb = lambda es, n, s, d: es.enter_context(nc.sbuf_tensor(n, list(s), d))
        ps = lambda es, n, s, d: es.enter_context(nc.psum_tensor(n, list(s), d))
        cst = sb(es_all, "cst", [128, 10, 128], F32)
        ident_f = cst[:, 0, :]
        ident_b = sb(es_all, "ident_b", [128, 128], BF16)
        ones_rms = sb(es_all, "ones_rms", [128, 128], BF16)
        ones_256 = sb(es_all, "ones_256", [128, 128], BF16)
        ones_1 = sb(es_all, "ones_1", [128, 128], BF16)
        onesf = sb(es_all, "onesf", [128, 128], F32)
        modT = sb(es_all, "modT", [128, 48], F32)
        a1 = sb(es_all, "a1", [128, 8], F32)
        a2 = sb(es_all, "a2", [128, 8], F32)
        nw_sb = sb(es_all, "nw_sb", [128, 16], F32)
        gate1_bc = sb(es_all, "gate1_bc", [128, D], F32)
        gate2_bc = sb(es_all, "gate2_bc", [128, D], F32)
        kmb = sb(es_all, "kmb", [128, 8, 32], BF16)
        bc_reg = nc.gpsimd.to_reg(NE * CAP - 1)
        dest_i = sb(es_all, "dest_i", [128, 64], I32)
        w4 = sb(es_all, "w4", [128, 16, 4], F32)
        RWT = sb(es_all, "RWT", [32, 16, 128], F32)
        s1 = modT[:, 0:8]
        g1 = modT[:, 16:24]
        s2 = modT[:, 24:32]

        P.dma("sp", cst[:], consts, writes=["cst"])
        P.dma("sp", nw_sb[:], nw, writes=["nw"])
        P.dma("pool", ident_b[:], consts[:, 0, :], writes=["ident_b"])
        P.op("dve", lambda e: e.memset(ones_rms[:], 1.0 / 1024.0), writes=["ones_rms"])
        P.op("dve", lambda e: e.memset(ones_256[:], 1.0 / 256.0), writes=["ones_256"])
        P.op("dve", lambda e: e.memset(ones_1[:], 1.0), writes=["ones_1"])
        P.op("dve", lambda e: e.memset(onesf[:], 1.0), writes=["onesf"])

        with ExitStack() as es:
            c_sb = sb(es, "c_sb", [128, 8], F32)
            c_e = sb(es, "c_e", [128, 8], F32)
            cact_b = sb(es, "cact_b", [128, 8, 128], F32)
            wst = sb(es, "wst", [128, 2, 8, 512], F32)
            modb = sb(es, "modb", [128, 6 * D], F32)
            bab = sb(es, "bab", [128, 6 * D], F32)
            p_mod = ps(es, "p_mod", [128, 2, 512], F32)
            p_col = ps(es, "p_col", [128, 48], F32)
            P.dma("sp", c_sb[:], cT, writes=["c_sb"])
            P.dma("sp", bab[:], b_ada_bc, writes=["bab"])
            P.op("act", lambda e: e.activation(c_e[:], c_sb[:], AF.Exp, scale=-1.0), reads=["c_sb"], writes=["c_e"])
            P.op("dve", lambda e: e.tensor_scalar(c_e[:], c_e[:], 1.0, None, ALU.add), reads=["c_e"], writes=["c_e"])
            P.op("dve", lambda e: e.reciprocal(c_e[:], c_e[:]), reads=["c_e"], writes=["c_e"])
            P.op("dve", lambda e: e.tensor_tensor(c_e[:], c_e[:], c_sb[:], ALU.mult), reads=["c_e", "c_sb"], writes=["c_e"])
            for k in range(8):
                P.op("dve", lambda e: e.tensor_scalar(cact_b[:, k, :], onesf[:], c_e[:, k:k + 1], None, ALU.mult),
                     reads=["c_e", "onesf"], writes=[("cactb", k)])
            for j in range(12):
                s = j % 2
                P.dma("sp", wst[:, s], w_ada[:, j * 512:(j + 1) * 512].rearrange("(k p) n -> p k n", p=128),
                      writes=[("wst", s)], sem=("wst", s))
                for k in range(8):
                    P.op("pe", lambda e: e.matmul(p_mod[:, s, :], lhsT=cact_b[:, k, :], rhs=wst[:, s, k, :],
                                                  start=(k == 0), stop=(k == 7)),
                         reads=[("wst", s), ("cactb", k)], writes=[("p_mod", s)], inc=(k == 7))
                P.op("dve", lambda e: e.tensor_tensor(modb[:, j * 512:(j + 1) * 512], p_mod[:, s, :],
                                                      bab[:, j * 512:(j + 1) * 512], ALU.add),
                     reads=[("p_mod", s), "bab"], writes=[("modb", j)])
            for j in range(48):
                P.op("pe", lambda e: e.matmul(p_col[:, j:j + 1], lhsT=modb[0:1, j * 128:(j + 1) * 128],
                                              rhs=onesf[0:1, 0:1], start=True, stop=True),
                     reads=[("modb", j // 4), "onesf"], writes=["p_col"], inc=(j == 47))
            P.op("dve", lambda e: e.tensor_copy(modT[:], p_col[:]), reads=["p_col"], writes=["modT"])
            P.op("dve", lambda e: e.scalar_tensor_tensor(a1[:], modT[:, 8:16], 1.0, nw_sb[:, 0:8], ALU.add, ALU.mult),
                 reads=["modT", "nw"], writes=["a1"])
            P.op("dve", lambda e: e.scalar_tensor_tensor(a2[:], modT[:, 32:40], 1.0, nw_sb[:, 8:16], ALU.add, ALU.mult),
                 reads=["modT", "nw"], writes=["a2"])
            P.op("act", lambda e: e.copy(gate1_bc[:], modb[:, 2048:3072]), reads=[("modb", 4), ("modb", 5)], writes=["gate1_bc"])
            P.op("act", lambda e: e.copy(gate2_bc[:], modb[:, 5120:6144]), reads=[("modb", 10), ("modb", 11)], writes=["gate2_bc"])
            if DEBUG:
                P.dma("sp", dbg["mod"], modT[:], reads=["modT"])
            P.barrier()

        G_ = dict(locals())
        if STAGE >= 1:
            phase_a1(nc, P, G_)
        with ExitStack() as es_ab:
            oaT = sb(es_ab, "oaT", [128, 8, OWN], BF16)
            G_["oaT"] = oaT
            if STAGE >= 2:
                phase_a2(nc, P, G_)
            if STAGE >= 3:
                phase_b(nc, P, G_)
        if STAGE >= 4:
            phase_moe(nc, P, G_)
        P.wait_all("sp")
    return nc, P


def rmsnorm_fm(P, nc, xs_ap, sq_ap, p_ss, rstd_ap, ones_rms, keyx, tag, pkey):
    P.op("act", lambda e: e.activation(sq_ap, xs_ap, AF.Square), reads=list(keyx), writes=[tag + "sq"])
    for k in range(8):
        P.op("pe", lambda e: e.matmul(p_ss, lhsT=ones_rms[:], rhs=sq_ap[:, k, :], start=(k == 0), stop=(k == 7)),
             reads=[tag + "sq", "ones_rms"], writes=[pkey], inc=(k == 7))
    rsqrt_act(P, rstd_ap, p_ss, [pkey], tag + "rstd", 1.0)


def rsqrt_act(P, out_ap, in_ap, rkeys, wkey, mul):
    P.op("act", lambda e: e.activation(out_ap, in_ap, AF.Ln, bias=EPS, scale=mul), reads=list(rkeys), writes=[wkey])
    P.op("act", lambda e: e.activation(out_ap, out_ap, AF.Exp, scale=-0.5), reads=[wkey], writes=[wkey])


def phase_a1(nc, P, L):
    g = L
    sb, ps = g["sb"], g["ps"]
    xTv, wA, wO, ropec, ropes = g["xTv"], g["wA"], g["wO"], g["ropec"], g["ropes"]
    cst, ident_b = g["cst"], g["ident_b"]
    ones_rms, ones_256 = g["ones_rms"], g["ones_256"]
    a1, s1, kmb = g["a1"], g["s1"], g["kmb"]
    KT_d, V_d, QT_d, OB_d, HT_d = g["KT_d"], g["V_d"], g["QT_d"], g["OB_d"], g["HT_d"]
    dbg = g["dbg"]
    with ExitStack() as es:
        wA_b = sb(es, "wA_b", [128, 8, 3600], BF16)
        wO_b = sb(es, "wO_b", [128, 8, 2560], BF16)
        xs = sb(es, "xs", [128, 2, 8, GS], F32)
        sq = sb(es, "sq", [128, 8, GS], BF16)
        rstd = sb(es, "rstd", [128, GS], F32)
        hT = sb(es, "hT", [128, 8, GS], BF16)
        rc = sb(es, "rc", [32, 1, GS], F32)
        rs = sb(es, "rs", [32, 1, GS], F32)
        kf = sb(es, "kf", [128, 2, GS], F32)
        rt = sb(es, "rt", [32, 2, GS], F32)
        kb = sb(es, "kb", [128, 2, GS], BF16)
        kmean = sb(es, "kmean", [128, 8, 32], F32)
        gkf = sb(es, "gkf", [128, 4, GS], F32)
        gqf = sb(es, "gqf", [128, 4, GS], F32)
        sgr = sb(es, "sgr", [128, 8, GS], F32)
        sgt = sb(es, "sgt", [128, GS], F32)
        glow = sb(es, "glow", [32, GS], F32)
        gw_sb = sb(es, "gw_sb", [32, 512], F32)
        gnw_sb = sb(es, "gnw_sb", [128, 2], F32)
        vfl = sb(es, "vfl", [128, 64], F32)
        Ltok = sb(es, "Ltok", [128, TPG, 512], F32)
        lex = sb(es, "lex", [128, 512], F32)
        vst = sb(es, "vst", [128, TPG, 1024], BF16)
        gvst = sb(es, "gvst", [128, TPG, 1024], BF16)
        EbT = sb(es, "EbT", [128, 2, 128], F32)
        EnbT = sb(es, "EnbT", [128, 2, 128], F32)
        qtl = sb(es, "qtl", [128, 2, 128], BF16)
        ktl = sb(es, "ktl", [128, 2, 128], BF16)
        ktok = sb(es, "ktok", [128, 2, 128], BF16)
        atm = sb(es, "atm", [128, 2, 128], BF16)
        Sst = sb(es, "Sst", [128, 4, 256], F32)
        Ssc = sb(es, "Ssc", [128, 2, 256], F32)
        Sbf = sb(es, "Sbf", [128, 4, 256], BF16)
        osq = sb(es, "osq", [128, 2, 2, 128], BF16)
        orst = sb(es, "orst", [128, 2, 128], F32)
        otmp = sb(es, "otmp", [128, 2, 128], F32)
        obst = sb(es, "obst", [128, 8, GS], BF16)
        p_pj = ps(es, "p_pj", [128, 2, 512], F32)
        p_sx = ps(es, "p_sx", [128, 512], F32)
        p_ss = p_sx[:, 0:GS]
        p_xs = p_sx[0:32, 256:256 + GS]
        p_g = ps(es, "p_g", [128, 2, 4, 128], F32)
        p_km = ps(es, "p_km", [128, 512], F32)
        p_kvb = ps(es, "p_kvb", [128, 2, 256], F32)
        p_tr = ps(es, "p_tr", [128, 2, 128], BF16)
        permf = cst[:, 1, 0:32]
        triN = cst[:, 2, :]
        utm = cst[:, 3, :]

        for k in range(8):
            P.dma("pool", wA_b[:, k, :], wA[k * 128:(k + 1) * 128, :], writes=[("wA", k)], sem=("wA", k))
        for k in range(8):
            P.dma("pool", wO_b[:, k, :], wO[k * 128:(k + 1) * 128, :], writes=[("wO", k)], sem=("wO", k))
        P.dma("sp", gw_sb[:], g["gw_aug"], writes=["gw_sb"])
        P.dma("sp", gnw_sb[:], g["gnw"], writes=["gnw_sb"])
        P.dma("sp", vfl[:], g["vflag"], writes=["vfl"])
        P.op("pool", lambda e: e.memset(glow[:], 1.0), writes=["glow"])
        P.op("pool", lambda e: e.memset(Sst[:], 0.0), writes=[("S", h) for h in range(4)])
        P.op("pool", lambda e: e.memset(Sbf[:], 0.0), writes=[("Sbf", h) for h in range(4)])
        wAk = [("wA", k) for k in range(8)]
        wOk = [("wO", k) for k in range(8)]
        pj_n = [0]
        kf_n = [0]
        uniq = [0]

        def ukey(n):
            uniq[0] += 1
            return (n, uniq[0])

        def proj_fm(wt, col0, ncol, wkeys, evac):
            s_ = pj_n[0] % 2
            pj_n[0] += 1
            for k in range(8):
                P.op("pe", lambda e: e.matmul(p_pj[0:ncol, s_, 0:GS], lhsT=wt[:, k, col0:col0 + ncol], rhs=hT[:, k, :],
                                              start=(k == 0), stop=(k == 7)),
                     reads=wkeys + ["hT"], writes=[("p_pj", s_)], inc=(k == 7))
            flush_evac()
            pend[0] = lambda: evac(p_pj[0:ncol, s_, 0:GS], ("p_pj", s_))

        pend = [None]

        def flush_evac():
            if pend[0] is not None:
                f_ = pend[0]
                pend[0] = None
                f_()

        for gi in range(NG):
            own = gi >= OWNG0
            s = gi % 2
            t0 = gi * GS
            o0 = (gi - OWNG0) * GS
            xk = [("xs", s, k) for k in range(8)]
            P.dma("sp", xs[:, s], xTv[:, t0:t0 + GS].rearrange("(k p) n -> p k n", p=128), writes=xk, sem=("xs", s))
            P.dma("sp", rc[:, 0, :], ropec[:, t0:t0 + GS], writes=[("rc", 0)], sem=("rc", 0))
            P.dma("sp", rs[:, 0, :], ropes[:, t0:t0 + GS], writes=[("rs", 0)], sem=("rs", 0))
            rmsnorm_fm(P, nc, xs[:, s], sq[:], p_ss, rstd[:], ones_rms, xk, "a1", "p_sx")
            for k in range(8):
                P.op("dve", lambda e: e.scalar_tensor_tensor(xs[:, s, k, :], xs[:, s, k, :], a1[:, k:k + 1], rstd[:],
                                                             ALU.mult, ALU.mult),
                     reads=[("xs", s, k), "a1rstd", "a1"], writes=[("xs", s, k)])
                P.op("act", lambda e: e.activation(hT[:, k, :], xs[:, s, k, :], AF.Identity, bias=s1[:, k:k + 1], scale=1.0),
                     reads=[("xs", s, k), "modT"], writes=["hT"])
            if own:
                P.dma("sp", HT_d[:, o0:o0 + GS].rearrange("(k p) n -> p k n", p=128), hT[:], reads=["hT"], writes=[ukey("HT_d")],
                      sem="HT_o")

            def qk_evac(h, is_k):
                def ev(pt, pkey):
                    u = kf_n[0] % 2
                    kf_n[0] += 1
                    P.op("act", lambda e: e.copy(kf[:, u, :], pt), reads=[pkey], writes=[("kf", u)])
                    P.op("pe", lambda e: e.matmul(p_xs, lhsT=permf, rhs=kf[:, u, :], start=True, stop=True),
                         reads=[("kf", u), "cst"], writes=["p_sx"])
                    P.op("dve", lambda e: e.tensor_tensor(rt[:, 0, :], kf[0:32, u, :], rc[:, 0, :], ALU.mult),
                         reads=[("kf", u), ("rc", 0)], writes=["rt0"])
                    P.op("dve", lambda e: e.tensor_tensor(rt[:, 1, :], p_xs, rs[:, 0, :], ALU.mult),
                         reads=["p_sx", ("rs", 0)], writes=["rt1"])
                    P.op("dve", lambda e: e.tensor_tensor(kf[0:32, u, :], rt[:, 0, :], rt[:, 1, :], ALU.add),
                         reads=["rt0", "rt1", ("kf", u)], writes=[("kf", u)])
                    if is_k:
                        P.op("dve", lambda e: e.tensor_reduce(kmean[:, h, gi:gi + 1], kf[:, u, :], AX.X, ALU.add),
                             reads=[("kf", u)], writes=[("kmean", h)])
                    P.op("pool", lambda e: e.tensor_copy(kb[:, u, :], kf[:, u, :]), reads=[("kf", u)], writes=[("kb", u)])
                    if is_k:
                        P.dma("sp", KT_d[h, :, t0:t0 + GS], kb[:, u, :], reads=[("kb", u)], writes=[ukey("KT_d")], sem=("kbo", u))
                    else:
                        P.dma("sp", QT_d[h, :, o0:o0 + GS], kb[:, u, :], reads=[("kb", u)], writes=[ukey("QT_d")], sem=("kbo", u))
                return ev

            for h in range(8):
                proj_fm(wA_b, h * 128, 128, wAk, qk_evac(h, True))
            if own:
                for h in range(8):
                    proj_fm(wO_b, h * 128, 128, wOk, qk_evac(h, False))

            flush_evac()
            for t in range(TPG):
                for half in range(2):
                    sl = pj_n[0] % 2
                    pj_n[0] += 1
                    for k in range(8):
                        P.op("pe", lambda e: e.matmul(p_pj[:, sl, :], lhsT=hT[:, k, t * 128:(t + 1) * 128],
                                                      rhs=wA_b[:, k, 1024 + half * 512:1024 + (half + 1) * 512],
                                                      start=(k == 0), stop=(k == 7)),
                             reads=wAk + ["hT"], writes=[("p_pj", sl)], inc=(k == 7))
                    P.op("act", lambda e: e.copy(vst[:, t, half * 512:(half + 1) * 512], p_pj[:, sl, :]),
                         reads=[("p_pj", sl)], writes=["vst"])
                for half in range(2):
                    sl = pj_n[0] % 2
                    pj_n[0] += 1
                    for k in range(8):
                        P.op("pe", lambda e: e.matmul(p_pj[:, sl, :], lhsT=hT[:, k, t * 128:(t + 1) * 128],
                                                      rhs=wA_b[:, k, 2560 + half * 512:2560 + (half + 1) * 512],
                                                      start=(k == 0), stop=(k == 7)),
                             reads=wAk + ["hT"], writes=[("p_pj", sl)], inc=(k == 7))
                    P.op("dve", lambda e: e.tensor_scalar(gvst[:, t, half * 512:(half + 1) * 512], p_pj[:, sl, :],
                                                          vfl[:, gi * TPG + t:gi * TPG + t + 1], None, ALU.mult),
                         reads=[("p_pj", sl), "vfl"], writes=[("gvst", t)])
            P.dma("sp", V_d[t0:t0 + GS, :].rearrange("(t p) n -> p t n", p=128), vst[:], reads=["vst"], writes=[ukey("V_d")],
                  sem="V_o")

            for h in range(4):
                proj_fm(wA_b, 2048 + h * 128, 128, wAk,
                        lambda pt, pkey, h=h: P.op("act", lambda e: e.copy(gkf[:, h, :], pt), reads=[pkey], writes=[("gkf", h)]))
            proj_fm(wA_b, 3584, 16, wAk,
                    lambda pt, pkey: P.op("act", lambda e: e.copy(glow[0:16, :], pt), reads=[pkey], writes=["glow"]))
            if own:
                for h in range(4):
                    proj_fm(wO_b, 1024 + h * 128, 128, wOk,
                            lambda pt, pkey, h=h: P.op("act", lambda e: e.copy(gqf[:, h, :], pt), reads=[pkey], writes=[("gqf", h)]))
                for c in range(8):
                    def gr_ev(pt, pkey, c=c):
                        P.op("act", lambda e: e.activation(sgt[:], pt, AF.Exp, scale=-1.0), reads=[pkey], writes=["sgt"])
                        P.op("dve", lambda e: e.tensor_scalar(sgt[:], sgt[:], 1.0, None, ALU.add), reads=["sgt"], writes=["sgt"])
                        P.op("dve", lambda e: e.reciprocal(sgt[:], sgt[:]), reads=["sgt"], writes=["sgt"])
                        P.op("dve", lambda e: e.tensor_tensor(sgr[:, c, :], sgt[:], pt, ALU.mult), reads=["sgt", pkey], writes=[("sgr", c)])
                    proj_fm(wO_b, 1536 + c * 128, 128, wOk, gr_ev)
            flush_evac()
            for t in range(TPG):
                sl = pj_n[0] % 2
                pj_n[0] += 1
                P.op("pe", lambda e: e.matmul(p_pj[:, sl, :], lhsT=glow[:, t * 128:(t + 1) * 128], rhs=gw_sb[:],
                                              start=True, stop=True),
                     reads=["glow", "gw_sb"], writes=[("p_pj", sl)])
                P.op("act", lambda e: e.activation(lex[:], p_pj[:, sl, :], AF.Exp, scale=-1.0), reads=[("p_pj", sl)], writes=["lex"])
                P.op("act", lambda e: e.activation(Ltok[:, t, :], lex[:], AF.Ln, bias=1.0, scale=1.0), reads=["lex"], writes=[("Ltok", t)])

            def gla_chain(t, h):
                u = h % 2
                c0 = t * 128
                P.op("pe", lambda e: e.matmul(p_g[:, u, 0, :], lhsT=Ltok[:, t, h * 128:(h + 1) * 128], rhs=triN,
                                              start=True, stop=True),
                     reads=[("Ltok", t), "cst"], writes=[("p_g", u)])
                yield
                P.op("act", lambda e: e.activation(EbT[:, u, :], p_g[:, u, 0, :], AF.Exp), reads=[("p_g", u)], writes=[("EbT", u)])
                P.op("act", lambda e: e.activation(EnbT[:, u, :], p_g[:, u, 0, :], AF.Exp, scale=-1.0),
                     reads=[("p_g", u)], writes=[("EnbT", u)])
                yield
                P.op("dve", lambda e: e.tensor_tensor(ktl[:, u, :], gkf[:, h, c0:c0 + 128], EnbT[:, u, :], ALU.mult),
                     reads=[("gkf", h), ("EnbT", u)], writes=[("ktl", u)])
                yield
                P.op("pe", lambda e: e.transpose(p_tr[:, u, :], ktl[:, u, :], ident_b[:]),
                     reads=[("ktl", u), "ident_b"], writes=["p_tr"])
                yield
                P.op("act", lambda e: e.copy(ktok[:, u, :], p_tr[:, u, :]), reads=["p_tr"], writes=[("ktok", u)])
                P.op("pe", lambda e: e.matmul(p_kvb[:, u, :], lhsT=ktok[:, u, :], rhs=gvst[:, t, h * 256:(h + 1) * 256],
                                              start=True, stop=True),
                     reads=[("ktok", u), ("gvst", t)], writes=["p_kvb"])
                yield
                if own:
                    P.op("dve", lambda e: e.scalar_tensor_tensor(qtl[:, u, :], gqf[:, h, c0:c0 + 128], 128.0 ** -0.5,
                                                                 EbT[:, u, :], ALU.mult, ALU.mult),
                         reads=[("gqf", h), ("EbT", u)], writes=[("qtl", u)])
                    yield
                    P.op("pe", lambda e: e.matmul(p_g[:, u, 1, :], lhsT=ktl[:, u, :], rhs=qtl[:, u, :], start=True, stop=True),
                         reads=[("ktl", u), ("qtl", u)], writes=[("p_g", u)])
                    yield
                    P.op("dve", lambda e: e.tensor_tensor(atm[:, u, :], p_g[:, u, 1, :], utm, ALU.mult),
                         reads=[("p_g", u), "cst"], writes=[("atm", u)])
                    yield
                    for dv in range(2):
                        P.op("pe", lambda e: e.matmul(p_g[:, u, 2 + dv, :],
                                                      lhsT=gvst[:, t, h * 256 + dv * 128:h * 256 + (dv + 1) * 128],
                                                      rhs=atm[:, u, :], start=True, stop=False),
                             reads=[("gvst", t), ("atm", u)], writes=[("p_g", u)], inc=False)
                        yield
                        P.op("pe", lambda e: e.matmul(p_g[:, u, 2 + dv, :], lhsT=Sbf[:, h, dv * 128:(dv + 1) * 128],
                                                      rhs=qtl[:, u, :], start=False, stop=True),
                             reads=[("Sbf", h), ("qtl", u)], writes=[("p_g", u)])
                        yield
                        P.op("act", lambda e: e.activation(osq[:, u, dv, :], p_g[:, u, 2 + dv, :], AF.Square),
                             reads=[("p_g", u)], writes=[("osq", u, dv)])
                        yield
                    for dv in range(2):
                        P.op("pe", lambda e: e.matmul(p_km[:, 256 + u * 128:256 + (u + 1) * 128], lhsT=ones_256[:], rhs=osq[:, u, dv, :],
                                                      start=(dv == 0), stop=(dv == 1)),
                             reads=[("osq", u, dv), "ones_256"], writes=["p_km"], inc=(dv == 1))
                    rsqrt_act(P, orst[:, u, :], p_km[:, 256 + u * 128:256 + (u + 1) * 128], ["p_km"], ("orst", u), 1.0)
                    for dv in range(2):
                        P.op("dve", lambda e: e.scalar_tensor_tensor(otmp[:, u, :], p_g[:, u, 2 + dv, :], gnw_sb[:, dv:dv + 1],
                                                                     orst[:, u, :], ALU.mult, ALU.mult),
                             reads=[("p_g", u), ("orst", u), "gnw_sb"], writes=[("otmp", u)])
                        yield
                        P.op("dve", lambda e: e.tensor_tensor(obst[:, h * 2 + dv, c0:c0 + 128], otmp[:, u, :],
                                                              sgr[:, h * 2 + dv, c0:c0 + 128], ALU.mult),
                             reads=[("otmp", u), ("sgr", h * 2 + dv)], writes=["obst"])
                        yield
                P.op("pool", lambda e: e.tensor_scalar(Ssc[:, u, :], Sst[:, h, :], EbT[:, u, 127:128], None, ALU.mult),
                     reads=[("S", h), ("EbT", u)], writes=[("Ssc", u)])
                yield
                P.op("dve", lambda e: e.scalar_tensor_tensor(Sst[:, h, :], p_kvb[:, u, :], EbT[:, u, 127:128], Ssc[:, u, :],
                                                             ALU.mult, ALU.add),
                     reads=["p_kvb", ("EbT", u), ("Ssc", u)], writes=[("S", h)])
                yield
                P.op("act", lambda e: e.copy(Sbf[:, h, :], Sst[:, h, :]), reads=[("S", h)], writes=[("Sbf", h)])

            for t in range(TPG):
                for hp in (0, 2):
                    gens = [gla_chain(t, hp), gla_chain(t, hp + 1)]
                    while gens:
                        for g_ in list(gens):
                            try:
                                next(g_)
                            except StopIteration:
                                gens.remove(g_)
            if own:
                P.dma("sp", OB_d[:, o0:o0 + GS].rearrange("(k p) n -> p k n", p=128), obst[:], reads=["obst"], writes=[ukey("OB_d")],
                      sem="OB_o")
        P.op("dve", lambda e: e.tensor_copy(kmb[:], kmean[:]), reads=[("kmean", h) for h in range(8)], writes=["kmb"])
        if DEBUG:
            P.wait_all("sp")
            P.dma("sp", dbg["kT"], KT_d, reads=[])
            P.dma("sp", dbg["qT"], QT_d, reads=[])
            P.dma("sp", dbg["ob"], OB_d, reads=[])
        P.barrier()


def phase_a2(nc, P, L):
    g = L
    sb, ps = g["sb"], g["ps"]
    KT_d, V_d, QT_d = g["KT_d"], g["V_d"], g["QT_d"]
    cst, ident_b, ident_f, ones_1, oaT, kmb = g["cst"], g["ident_b"], g["ident_f"], g["ones_1"], g["oaT"], g["kmb"]
    scale = 128.0 ** -0.5
    with ExitStack() as es:
        KT = sb(es, "KT", [128, 2, SEQ], BF16)
        Vh = sb(es, "Vh", [128, 2, 64, 128], BF16)
        QT = sb(es, "QT", [128, 2, OWN], BF16)
        esel = sb(es, "esel", [32, 32, 128], BF16)
        cmask = sb(es, "cmask", [128, 2, 256], BF16)
        gm_sb = sb(es, "gm_sb", [128, 8, 32], F32)
        gv_sb = sb(es, "gv_sb", [128, 8, 32], F32)
        gt = sb(es, "gt", [128, 2, 32], F32)
        top8 = sb(es, "top8", [128, 2, 8], F32)
        mbT = sb(es, "mbT", [32, 2, 256], BF16)
        PT = sb(es, "PT", [128, 4, 256], BF16)
        rden = sb(es, "rden", [128, 256], F32)
        p_st = ps(es, "p_st", [128, 4, 512], F32)
        p_od = ps(es, "p_od", [128, 2, 512], F32)
        p_gm = ps(es, "p_gm", [128, 512], F32)
        p_gt = p_gm[:, 0:64].rearrange("p (a b) -> p a b", a=2)
        p_mb = p_gm[0:32, 256:512]
        P.dma("pool", esel[:].rearrange("p a b -> p (a b)"), g["esel_in"], writes=["esel"])
        P.dma("pool", cmask[:], cst_dram_causal(g), writes=["cmask"])
        P.dma("sp", gm_sb[:].rearrange("p a b -> p (a b)"), g["gmask"], writes=["gm_sb"])
        P.dma("sp", gv_sb[:].rearrange("p a b -> p (a b)"), g["gvalid"], writes=["gv_sb"])
        n_st = [0]
        n_blk = [0]
        for h in range(8):
            hs = h % 2
            P.dma("sp", KT[:, hs, :], KT_d[h], writes=[("KT", hs)], sem=("KT", hs))
            P.dma("sp", Vh[:, hs], V_d[:, h * 128:(h + 1) * 128].rearrange("(t p) d -> p t d", p=128),
                  writes=[("Vh", hs)], sem=("Vh", hs))
            P.dma("sp", QT[:, hs, :], QT_d[h], writes=[("QT", hs)], sem=("QT", hs))
            for l in range(8):
                bs = n_blk[0] % 2
                n_blk[0] += 1
                q0 = l * 256
                for qt in range(2):
                    P.op("pe", lambda e: e.matmul(p_gt[:, qt, :], lhsT=QT[:, hs, q0 + qt * 128:q0 + (qt + 1) * 128],
                                                  rhs=kmb[:, h, :], start=True, stop=True),
                         reads=[("QT", hs), "kmb"], writes=["p_gm"])
                    P.op("dve", lambda e: e.tensor_tensor(gt[:, qt, :], p_gt[:, qt, :], gm_sb[:, l, :], ALU.add),
                         reads=["p_gm", "gm_sb"], writes=[("gt", qt)])
                    P.op("dve", lambda e: e.max(top8[:, qt, :], gt[:, qt, :]), reads=[("gt", qt)], writes=[("top8", qt)])
                    P.op("dve", lambda e: e.tensor_scalar(gt[:, qt, :], gt[:, qt, :], top8[:, qt, 2:3], None, ALU.is_ge),
                         reads=[("gt", qt), ("top8", qt)], writes=[("gt", qt)])
                    P.op("dve", lambda e: e.tensor_tensor(gt[:, qt, :], gt[:, qt, :], gv_sb[:, l, :], ALU.mult),
                         reads=[("gt", qt), "gv_sb"], writes=[("gt", qt)])
                    P.op("dve", lambda e: e.tensor_scalar(gt[:, qt, :], gt[:, qt, :], BIG, -BIG, ALU.mult, ALU.add),
                         reads=[("gt", qt)], writes=[("gt", qt)])
                    P.op("pe", lambda e: e.transpose(p_mb[:, qt * 128:(qt + 1) * 128], gt[:, qt, :], ident_f),
                         reads=[("gt", qt), "cst"], writes=["p_gm"])
                P.op("dve", lambda e: e.tensor_copy(mbT[:, bs, :], p_mb), reads=["p_gm"], writes=[("mbT", bs)])
                nkv = 24 + l
                pairs = [(v, half) for v in range(nkv + 1) for half in range(2)]
                def front(i):
                    v, half = pairs[i]
                    st = n_st[0] % 4
                    n_st[0] += 1
                    k0 = v * 256 + half * 128
                    P.op("pe", lambda e: e.matmul(p_st[:, st, 0:256], lhsT=KT[:, hs, k0:k0 + 128], rhs=QT[:, hs, q0:q0 + 256],
                                                  start=True, stop=False),
                         reads=[("KT", hs), ("QT", hs)], writes=[("p_st", st)], inc=False)
                    if v < nkv:
                        P.op("pe", lambda e: e.matmul(p_st[:, st, 0:256], lhsT=esel[:, v, :], rhs=mbT[:, bs, :], start=False, stop=True),
                             reads=["esel", ("mbT", bs)], writes=[("p_st", st)])
                    else:
                        P.op("pe", lambda e: e.matmul(p_st[:, st, 0:256], lhsT=ident_b[:], rhs=cmask[:, half, :], start=False, stop=True),
                             reads=["ident_b", "cmask"], writes=[("p_st", st)])
                    P.op("act", lambda e: e.activation(PT[:, st, :], p_st[:, st, 0:256], AF.Exp, scale=scale),
                         reads=[("p_st", st)], writes=[("PT", st)])
                    return st

                def back(i, st):
                    v, half = pairs[i]
                    last = (i == len(pairs) - 1)
                    P.op("pe", lambda e: e.matmul(p_od[:, 0, 0:256], lhsT=Vh[:, hs, v * 2 + half, :], rhs=PT[:, st, :],
                                                  start=(i == 0), stop=last),
                         reads=[("Vh", hs), ("PT", st)], writes=["p_od"], inc=False)
                    P.op("pe", lambda e: e.matmul(p_od[:, 1, 0:256], lhsT=ones_1[:], rhs=PT[:, st, :],
                                                  start=(i == 0), stop=last),
                         reads=["ones_1", ("PT", st)], writes=["p_od"], inc=True)

                DEPTH = 3
                sts = {}
                for i in range(len(pairs) + DEPTH):
                    if i < len(pairs):
                        sts[i] = front(i)
                    if i - DEPTH >= 0:
                        back(i - DEPTH, sts.pop(i - DEPTH))
                P.op("dve", lambda e: e.reciprocal(rden[:], p_od[:, 1, 0:256]), reads=["p_od"], writes=["rden"])
                P.op("dve", lambda e: e.tensor_tensor(oaT[:, h, q0:q0 + 256], p_od[:, 0, 0:256], rden[:], ALU.mult),
                     reads=["p_od", "rden"], writes=[("oaT", h)])
        if DEBUG:
            P.dma("sp", g["dbg"]["oa"], oaT[:], reads=[("oaT", h) for h in range(8)])
        P.barrier()


def cst_dram_causal(g):
    return g["consts"][:, 5:9, :].rearrange("p (a b) c -> p a (b c)", a=2)


def phase_b(nc, P, L):
    g = L
    sb, ps = g["sb"], g["ps"]
    oaT, ones_rms, ones_1, ident_b, ident_f, cst = g["oaT"], g["ones_rms"], g["ones_1"], g["ident_b"], g["ident_f"], g["cst"]
    a2, s2, g1, gate1_bc = g["a2"], g["s2"], g["g1"], g["gate1_bc"]
    HT_d, OB_d, X1_d, XG_d = g["HT_d"], g["OB_d"], g["X1_d"], g["XG_d"]
    xTv, xtok = g["xTv"], g["xtok"]
    dest_i, w4, RWT = g["dest_i"], g["w4"], g["RWT"]
    with ExitStack() as es:
        wG_b = sb(es, "wG_b", [128, 8, 2048], BF16)
        woa_b = sb(es, "woa_b", [128, 8, D], BF16)
        wob_b = sb(es, "wob_b", [128, 8, D], BF16)
        wout_b = sb(es, "wout_b", [128, 8, D], BF16)
        nbm_sb = sb(es, "nbm_sb", [128, 16], F32)
        wr_sb = sb(es, "wr_sb", [128, 8, NE], F32)
        br_sb = sb(es, "br_sb", [128, NE], F32)
        ecap_sb = sb(es, "ecap_sb", [128, NE], F32)
        hTg = sb(es, "hTg", [128, 8, GS], BF16)
        obg = sb(es, "obg", [128, 8, GS], BF16)
        xo = sb(es, "xo", [128, 8, GS], F32)
        ge = sb(es, "ge", [128, GS], F32)
        mf = sb(es, "mf", [128, GS], F32)
        mt2 = sb(es, "mt2", [128, GS], F32)
        mT = sb(es, "mT", [128, 8, GS], BF16)
        sq = sb(es, "sqb", [128, 8, GS], BF16)
        rstd = sb(es, "rstdb", [128, GS], F32)
        h2f = sb(es, "h2f", [128, 8, GS], F32)
        h2b = sb(es, "h2b", [128, 8, GS], BF16)
        xt = sb(es, "xt", [128, D], F32)
        x1t = sb(es, "x1t", [128, D], F32)
        h2tok = sb(es, "h2tok", [128, D], BF16)
        lg = sb(es, "lg", [128, NE], F32)
        t8 = sb(es, "t8", [128, 8], F32)
        nmax = sb(es, "nmax", [128, 1], F32)
        den = sb(es, "den", [128, 1], F32)
        sel = sb(es, "sel", [128, NE], F32)
        selb = sb(es, "selb", [128, NE], BF16)
        carry = sb(es, "carry", [128, NE], F32)
        slot = sb(es, "slot", [128, NE], F32)
        oh = sb(es, "oh", [128, NE], F32)
        destf = sb(es, "destf", [128, 4], F32)
        RW = sb(es, "RW", [128, NE], F32)
        stri = sb(es, "stri", [128, 128], BF16)
        p_a = ps(es, "pb_a", [128, 2, 512], F32)
        p_b = ps(es, "pb_b", [128, 2, 512], F32)
        p_ss = ps(es, "pb_ss", [128, GS], F32)
        p_lg = ps(es, "pb_lg", [128, 2, NE], F32)
        p_tr = ps(es, "pb_tr", [128, 2, 512], BF16)
        p_rw = ps(es, "pb_rw", [32, 128], F32)
        for k in range(8):
            P.dma("pool", wG_b[:, k, :], g["wG"][k * 128:(k + 1) * 128, :], writes=[("wG", k)], sem=("wG", k))
            P.dma("pool", woa_b[:, k, :], g["w_oa"][k * 128:(k + 1) * 128, :], writes=[("woa", k)], sem=("woa", k))
            P.dma("pool", wob_b[:, k, :], g["w_ob"][k * 128:(k + 1) * 128, :], writes=[("wob", k)], sem=("wob", k))
            P.dma("pool", wout_b[:, k, :], g["w_out"][k * 128:(k + 1) * 128, :], writes=[("wout", k)], sem=("wout", k))
        P.dma("pool", stri[:], g["consts"][:, 4, :], writes=["stri"])
        P.dma("sp", nbm_sb[:], g["nbm"], writes=["nbm"])
        P.dma("sp", wr_sb[:], g["w_r"].rearrange("(k p) n -> p k n", p=128), writes=["wr"])
        P.dma("sp", br_sb[:], g["b_r_bc"], writes=["br"])
        P.dma("sp", ecap_sb[:], g["ecap"], writes=["ecap"])
        P.op("pool", lambda e: e.memset(carry[:], 0.0), writes=["carry"])
        P.op("dve", lambda e: e.tensor_scalar(nbm_sb[:], nbm_sb[:], -1.0, None, ALU.mult), reads=["nbm"], writes=["nbm"])
        kk = lambda n: [(n, k) for k in range(8)]
        for G in range(OWN // GS):
            o0 = G * GS
            P.dma("sp", hTg[:], HT_d[:, o0:o0 + GS].rearrange("(k p) n -> p k n", p=128), writes=["hTg"])
            P.dma("sp", obg[:], OB_d[:, o0:o0 + GS].rearrange("(k p) n -> p k n", p=128), writes=["obg"])
            P.dma("sp", xo[:], xTv[:, 6144 + o0:6144 + o0 + GS].rearrange("(k p) n -> p k n", p=128), writes=["xo"])
            for c in range(8):
                for br in range(2):
                    wy = woa_b if br == 0 else wob_b
                    wyk = kk("woa") if br == 0 else kk("wob")
                    for k in range(8):
                        P.op("pe", lambda e: e.matmul(p_a[:, br, 0:GS], lhsT=wG_b[:, k, br * 1024 + c * 128:br * 1024 + (c + 1) * 128],
                                                      rhs=hTg[:, k, :], start=(k == 0), stop=(k == 7)),
                             reads=kk("wG") + ["hTg"], writes=[("p_a", br)], inc=(k == 7))
                    for k in range(8):
                        rhs = oaT[:, k, o0:o0 + GS] if br == 0 else obg[:, k, :]
                        P.op("pe", lambda e: e.matmul(p_b[:, br, 0:GS], lhsT=wy[:, k, c * 128:(c + 1) * 128], rhs=rhs,
                                                      start=(k == 0), stop=(k == 7)),
                             reads=wyk + (["obg"] if br else []), writes=[("p_b", br)], inc=(k == 7))
                    P.op("act", lambda e: e.activation(ge[:], p_a[:, br, 0:GS], AF.Exp, bias=nbm_sb[:, br * 8 + c:br * 8 + c + 1], scale=-1.0),
                         reads=[("p_a", br), "nbm"], writes=["ge"])
                    P.op("dve", lambda e: e.tensor_scalar(ge[:], ge[:], 1.0, None, ALU.add), reads=["ge"], writes=["ge"])
                    P.op("dve", lambda e: e.reciprocal(ge[:], ge[:]), reads=["ge"], writes=["ge"])
                    if br == 0:
                        P.op("dve", lambda e: e.tensor_tensor(mf[:], ge[:], p_b[:, br, 0:GS], ALU.mult), reads=["ge", ("p_b", br)], writes=["mf"])
                    else:
                        P.op("dve", lambda e: e.tensor_tensor(mt2[:], ge[:], p_b[:, br, 0:GS], ALU.mult), reads=["ge", ("p_b", br)], writes=["mt2"])
                        P.op("dve", lambda e: e.tensor_tensor(mT[:, c, :], mf[:], mt2[:], ALU.add), reads=["mf", "mt2"], writes=["mT"])
            for c in range(8):
                for k in range(8):
                    P.op("pe", lambda e: e.matmul(p_a[:, c % 2, 0:GS], lhsT=wout_b[:, k, c * 128:(c + 1) * 128], rhs=mT[:, k, :],
                                                  start=(k == 0), stop=(k == 7)),
                         reads=kk("wout") + ["mT"], writes=[("p_a", c % 2)], inc=(k == 7))
                P.op("dve", lambda e: e.scalar_tensor_tensor(xo[:, c, :], p_a[:, c % 2, 0:GS], g1[:, c:c + 1], xo[:, c, :], ALU.mult, ALU.add),
                     reads=[("p_a", c % 2), "xo"], writes=["xo"])
            for t in range(TPG):
                T = G * TPG + t
                P.dma("sp", xt[:], xtok[T * 128:(T + 1) * 128, :], writes=["xt"])
                for half in range(2):
                    for k in range(8):
                        P.op("pe", lambda e: e.matmul(p_b[:, half, :], lhsT=mT[:, k, t * 128:(t + 1) * 128],
                                                      rhs=wout_b[:, k, half * 512:(half + 1) * 512], start=(k == 0), stop=(k == 7)),
                             reads=kk("wout") + ["mT"], writes=[("p_b", half)], inc=(k == 7))
                    P.op("dve", lambda e: e.tensor_tensor(x1t[:, half * 512:(half + 1) * 512], p_b[:, half, :],
                                                          gate1_bc[:, half * 512:(half + 1) * 512], ALU.mult),
                         reads=[("p_b", half)], writes=["x1t"])
                P.op("dve", lambda e: e.tensor_tensor(x1t[:], x1t[:], xt[:], ALU.add), reads=["x1t", "xt"], writes=["x1t"])
                P.dma("sp", X1_d[T * 128:(T + 1) * 128, :], x1t[:], reads=["x1t"], writes=[("X1_d", T)], sem="X1_o")
            rmsnorm_fm(P, nc, xo[:], sq[:], p_ss[:], rstd[:], ones_rms, ["xo"], "b", "pb_ss")
            for k in range(8):
                P.op("dve", lambda e: e.scalar_tensor_tensor(xo[:, k, :], xo[:, k, :], a2[:, k:k + 1], rstd[:], ALU.mult, ALU.mult),
                     reads=["xo", "brstd"], writes=["xo"])
                P.op("act", lambda e: e.activation(h2f[:, k, :], xo[:, k, :], AF.Identity, bias=s2[:, k:k + 1], scale=1.0),
                     reads=["xo"], writes=["h2f"])
            P.op("dve", lambda e: e.tensor_copy(h2b[:], h2f[:]), reads=["h2f"], writes=["h2b"])
            for t in range(TPG):
                T = G * TPG + t
                lq = t
                for k in range(8):
                    P.op("pe", lambda e: e.matmul(p_lg[:, lq, :], lhsT=h2f[:, k, t * 128:(t + 1) * 128], rhs=wr_sb[:, k, :],
                                                  start=(k == 0), stop=(k == 7)),
                         reads=["h2f", "wr"], writes=["p_lg"], inc=(k == 7))
                P.op("dve", lambda e: e.tensor_tensor(lg[:], p_lg[:, lq, :], br_sb[:], ALU.add), reads=["p_lg", "br"], writes=["lg"])
                if DEBUG:
                    P.dma("sp", g["dbg"]["lg"][:, T, :], lg[:], reads=["lg"], sem="dbg_lg")
                P.op("dve", lambda e: e.max(t8[:], lg[:]), reads=["lg"], writes=["t8"])
                P.op("dve", lambda e: e.tensor_scalar(nmax[:], t8[:, 0:1], -1.0, None, ALU.mult), reads=["t8"], writes=["nmax"])
                P.op("act", lambda e: e.activation(w4[:, T, :], t8[:, 0:4], AF.Exp, bias=nmax[:], scale=1.0),
                     reads=["t8", "nmax"], writes=[("w4", T)])
                P.op("dve", lambda e: e.tensor_reduce(den[:], w4[:, T, :], AX.X, ALU.add), reads=[("w4", T)], writes=["den"])
                P.op("dve", lambda e: e.reciprocal(den[:], den[:]), reads=["den"], writes=["den"])
                P.op("dve", lambda e: e.tensor_scalar(w4[:, T, :], w4[:, T, :], den[:], None, ALU.mult), reads=[("w4", T), "den"], writes=[("w4", T)])
                P.op("dve", lambda e: e.tensor_scalar(sel[:], lg[:], t8[:, 3:4], None, ALU.is_ge), reads=["lg", "t8"], writes=["sel"])
                P.op("dve", lambda e: e.tensor_copy(selb[:], sel[:]), reads=["sel"], writes=["selb"])
                P.op("pe", lambda e: e.matmul(p_lg[:, lq, :], lhsT=stri[:], rhs=selb[:], start=True, stop=True),
                     reads=["stri", "selb"], writes=["p_lg"])
                P.op("dve", lambda e: e.tensor_tensor(slot[:], p_lg[:, lq, :], carry[:], ALU.add), reads=["p_lg", "carry"], writes=["slot"])
                P.op("pe", lambda e: e.matmul(p_lg[:, lq, :], lhsT=ones_1[:], rhs=selb[:], start=True, stop=True),
                     reads=["selb"], writes=["p_lg"])
                P.op("dve", lambda e: e.tensor_tensor(carry[:], p_lg[:, lq, :], carry[:], ALU.add), reads=["p_lg", "carry"], writes=["carry"])
                P.op("dve", lambda e: e.tensor_tensor(slot[:], slot[:], ecap_sb[:], ALU.add), reads=["slot", "ecap"], writes=["slot"])
                P.op("dve", lambda e: e.memset(RW[:], 0.0), writes=["RW"])
                for k4 in range(4):
                    P.op("dve", lambda e: e.tensor_scalar(oh[:], lg[:], t8[:, k4:k4 + 1], None, ALU.is_equal), reads=["lg", "t8"], writes=["oh"])
                    P.op("dve", lambda e: e.scalar_tensor_tensor(RW[:], oh[:], w4[:, T, k4:k4 + 1], RW[:], ALU.mult, ALU.add),
                         reads=["oh", ("w4", T), "RW"], writes=["RW"])
                    P.op("dve", lambda e: e.tensor_tensor(oh[:], oh[:], slot[:], ALU.mult), reads=["oh", "slot"], writes=["oh"])
                    P.op("dve", lambda e: e.tensor_reduce(destf[:, k4:k4 + 1], oh[:], AX.X, ALU.add), reads=["oh"], writes=["destf"])
                P.op("dve", lambda e: e.tensor_copy(dest_i[:, T * 4:T * 4 + 4], destf[:]), reads=["destf"], writes=[("dest_i", T)])
                P.op("pe", lambda e: e.transpose(p_rw[:], RW[:], ident_f), reads=["RW"], writes=["p_rw"])
                P.op("act", lambda e: e.copy(RWT[:, T, :], p_rw[:]), reads=["p_rw"], writes=[("RWT", T)])
                for hf in range(2):
                    for k in range(4):
                        P.op("pe", lambda e: e.transpose(p_tr[:, hf, k * 128:(k + 1) * 128], h2b[:, hf * 4 + k, t * 128:(t + 1) * 128], ident_b[:]),
                             reads=["h2b"], writes=["p_tr"], inc=(k == 3))
                    P.op("act", lambda e: e.copy(h2tok[:, hf * 512:(hf + 1) * 512], p_tr[:, hf, :]), reads=["p_tr"], writes=["h2tok"])
                for k4 in range(4):
                    P.dma("pool", None, None, reads=["h2tok", ("dest_i", T)], writes=["XG_d"], sem=("xgs", k4),
                          emit=lambda e: e.indirect_dma_start(
                              out=XG_d[:, :], out_offset=bass.IndirectOffsetOnAxis(ap=dest_i[:, T * 4 + k4:T * 4 + k4 + 1], axis=0),
                              in_=h2tok[:, :], in_offset=None, bounds_check=g["bc_reg"], oob_is_err=False))
        if DEBUG:
            P.wait_all("sp")
            P.dma("sp", g["dbg"]["x1"], X1_d, reads=[])
        P.barrier()


def phase_moe(nc, P, L):
    g = L
    sb, ps = g["sb"], g["ps"]
    ident_b, gate2_bc = g["ident_b"], g["gate2_bc"]
    XG_d, OUT_d, X1_d = g["XG_d"], g["OUT_d"], g["X1_d"]
    w1d, w2, out = g["w1d"], g["w2"], g["out"]
    dest_i, w4, RWT = g["dest_i"], g["w4"], g["RWT"]
    NCH = CAP // 512
    with ExitStack() as es:
        wp = sb(es, "wp", [128, 12, 8, 512], BF16)
        b1 = sb(es, "b1", [128, NE, 16], F32)
        xg = sb(es, "xg", [128, 2, 4, D], BF16)
        xgT = sb(es, "xgT", [128, 2, 8, 512], BF16)
        actT = sb(es, "actT", [128, 2, 8, 512], BF16)
        gg = sb(es, "gg", [128, 2, 512], F32)
        ll = sb(es, "ll", [128, 2, 512], F32)
        ee = sb(es, "ee", [128, 2, 512], F32)
        orow = sb(es, "orow", [128, 4, D], F32)
        p_h = ps(es, "pm_h", [128, 4, 512], F32)
        p_o = ps(es, "pm_o", [128, 2, 512], F32)
        p_t = ps(es, "pm_t", [128, 2, 512], BF16)
        P.dma("sp", b1[:].rearrange("p a b -> p (a b)"), g["b1T"], writes=["b1"])

        def load_expert(e):
            base = (e % 2) * 6
            for p in range(4):
                for two in range(2):
                    src = w1d[e][:, two * 1024 + p * 256:two * 1024 + (p + 1) * 256]
                    P.dma("pool", wp[:, base + p, :, two * 256:(two + 1) * 256], src.rearrange("(k p) f -> p k f", p=128),
                          writes=[("wp", base + p, two)], sem=("wp", base + p, two))
            for hf in range(2):
                src = w2[e][:, hf * 512:(hf + 1) * 512]
                P.dma("pool", wp[:, base + 4 + hf], src.rearrange("(k p) n -> p k n", p=128),
                      writes=[("wp", base + 4 + hf, 0)], sem=("wp", base + 4 + hf, 0))

        load_expert(0)
        nch = [0]
        for e in range(NE):
            base = (e % 2) * 6
            if e + 1 < NE:
                load_expert(e + 1)
            for ch in range(NCH):
                u = nch[0] % 2
                nch[0] += 1
                r0 = e * CAP + ch * 512
                P.dma("sp", xg[:, u], XG_d[r0:r0 + 512, :].rearrange("(r p) d -> p r d", p=128),
                      writes=[("xg", u)], sem=("xg", u))
                for r in range(4):
                    for hf in range(2):
                        for k in range(4):
                            P.op("pe", lambda e_: e_.transpose(p_t[:, hf, k * 128:(k + 1) * 128],
                                                               xg[:, u, r, (hf * 4 + k) * 128:(hf * 4 + k + 1) * 128], ident_b[:]),
                                 reads=[("xg", u)], writes=["p_t"], inc=(k == 3))
                        P.op("act", lambda e_: e_.copy(xgT[:, u, hf * 4:(hf + 1) * 4, r * 128:(r + 1) * 128],
                                                       p_t[:, hf, :].rearrange("p (k c) -> p k c", k=4)),
                             reads=["p_t"], writes=[("xgT", u)])
                for j in range(8):
                    sl = base + j // 2
                    hs = j % 2
                    for part in range(2):
                        c0 = part * 256 + (j % 2) * 128
                        for k in range(8):
                            P.op("pe", lambda e_: e_.matmul(p_h[:, hs * 2 + part, :], lhsT=wp[:, sl, k, c0:c0 + 128], rhs=xgT[:, u, k, :],
                                                            start=(k == 0), stop=(k == 7)),
                                 reads=[("wp", sl, part), ("xgT", u)], writes=[("p_h", hs, part)], inc=(k == 7))
                    P.op("dve", lambda e_: e_.tensor_scalar(gg[:, hs, :], p_h[:, hs * 2, :], b1[:, e, j:j + 1], 7.0, ALU.add, ALU.min),
                         reads=[("p_h", hs, 0), "b1"], writes=[("gg", hs)])
                    P.op("dve", lambda e_: e_.tensor_scalar(ll[:, hs, :], p_h[:, hs * 2 + 1, :], b1[:, e, 8 + j:9 + j], 7.0, ALU.add, ALU.min),
                         reads=[("p_h", hs, 1), "b1"], writes=[("ll", hs)])
                    P.op("dve", lambda e_: e_.tensor_scalar(ll[:, hs, :], ll[:, hs, :], -7.0, 1.0, ALU.max, ALU.add),
                         reads=[("ll", hs)], writes=[("ll", hs)])
                    P.op("act", lambda e_: e_.activation(ee[:, hs, :], gg[:, hs, :], AF.Exp, scale=-1.702), reads=[("gg", hs)], writes=[("ee", hs)])
                    P.op("act", lambda e_: e_.activation(ee[:, hs, :], ee[:, hs, :], AF.Identity, bias=1.0, scale=1.0), reads=[("ee", hs)], writes=[("ee", hs)])
                    P.op("dve", lambda e_: e_.reciprocal(ee[:, hs, :], ee[:, hs, :]), reads=[("ee", hs)], writes=[("ee", hs)])
                    P.op("dve", lambda e_: e_.tensor_tensor(gg[:, hs, :], gg[:, hs, :], ll[:, hs, :], ALU.mult),
                         reads=[("gg", hs), ("ll", hs)], writes=[("gg", hs)])
                    P.op("dve", lambda e_: e_.tensor_tensor(actT[:, u, j, :], gg[:, hs, :], ee[:, hs, :], ALU.mult),
                         reads=[("gg", hs), ("ee", hs)], writes=[("actT", u)])
                for hf in range(2):
                    sl = base + 4 + hf
                    for r in range(4):
                        os_ = (hf * 4 + r) % 2
                        for k in range(8):
                            P.op("pe", lambda e_: e_.matmul(p_o[:, os_, :], lhsT=actT[:, u, k, r * 128:(r + 1) * 128], rhs=wp[:, sl, k, :],
                                                            start=(k == 0), stop=(k == 7)),
                                 reads=[("actT", u), ("wp", sl, 0)], writes=[("p_o", os_)], inc=(k == 7))
                        P.op("act", lambda e_: e_.copy(orow[:, r, hf * 512:(hf + 1) * 512], p_o[:, os_, :]),
                             reads=[("p_o", os_)], writes=["orow"])
                P.dma("sp", OUT_d[r0:r0 + 512, :].rearrange("(r p) d -> p r d", p=128), orow[:],
                      reads=["orow"], writes=[("OUT_d", e, ch)], sem="orow")
        P.barrier()
    with ExitStack() as es:
        yk = sb(es, "yk", [128, 2, 4, D], F32)
        acc = sb(es, "acc", [128, 2, D], F32)
        x1t = sb(es, "x1c", [128, 2, D], F32)
        fsq = sb(es, "fsq", [128, D], F32)
        ssq = sb(es, "ssq", [128, 2], F32)
        b2_sb = sb(es, "b2c", [32, D], F32)
        fnw = sb(es, "fnwc", [128, D], F32)
        p_bias = ps(es, "pc_b", [128, 2, 2, 512], F32)
        P.dma("sp", b2_sb[:], g["b2"], writes=["b2c"])
        P.dma("sp", fnw[:], g["fnw_bc"], writes=["fnwc"])
        for T in range(16):
            u = T % 2
            for k4 in range(4):
                P.dma("pool", None, None, reads=["OUT_d"], writes=[("yk", u, k4)], sem=("yk", u, k4),
                      emit=lambda e: e.indirect_dma_start(
                          out=yk[:, u, k4, :], out_offset=None, in_=OUT_d[:, :],
                          in_offset=bass.IndirectOffsetOnAxis(ap=dest_i[:, T * 4 + k4:T * 4 + k4 + 1], axis=0),
                          bounds_check=g["bc_reg"], oob_is_err=False))
            P.dma("sp", x1t[:, u, :], X1_d[T * 128:(T + 1) * 128, :], reads=["X1_d"], writes=[("x1c", u)], sem=("x1c", u))
            for half in range(2):
                P.op("pe", lambda e: e.matmul(p_bias[:, u, half, :], lhsT=RWT[:, T, :], rhs=b2_sb[:, half * 512:(half + 1) * 512],
                                              start=True, stop=True),
                     reads=["b2c"], writes=[("p_bias", u)], inc=(half == 1))
            P.op("dve", lambda e: e.tensor_scalar(acc[:, u, :], yk[:, u, 0, :], w4[:, T, 0:1], None, ALU.mult),
                 reads=[("yk", u, 0)], writes=[("acc", u)])
            for k4 in range(1, 4):
                P.op("dve", lambda e: e.scalar_tensor_tensor(acc[:, u, :], yk[:, u, k4, :], w4[:, T, k4:k4 + 1], acc[:, u, :], ALU.mult, ALU.add),
                     reads=[("yk", u, k4), ("acc", u)], writes=[("acc", u)])
            for half in range(2):
                P.op("dve", lambda e: e.tensor_tensor(acc[:, u, half * 512:(half + 1) * 512], acc[:, u, half * 512:(half + 1) * 512],
                                                      p_bias[:, u, half, :], ALU.add),
                     reads=[("acc", u), ("p_bias", u)], writes=[("acc", u)])
            P.op("dve", lambda e: e.tensor_tensor(acc[:, u, :], acc[:, u, :], gate2_bc[:], ALU.mult), reads=[("acc", u), "gate2_bc"], writes=[("acc", u)])
            P.op("dve", lambda e: e.tensor_tensor(acc[:, u, :], acc[:, u, :], x1t[:, u, :], ALU.add), reads=[("acc", u), ("x1c", u)], writes=[("acc", u)])
            P.op("dve", lambda e: e.memset(ssq[:, u:u + 1], 0.0), writes=[("ssq", u)])
            P.op("act", lambda e: e.activation(fsq[:], acc[:, u, :], AF.Square, accum_out=ssq[:, u:u + 1]), reads=[("acc", u), ("ssq", u)], writes=["fsq", ("ssq", u)])
            rsqrt_act(P, ssq[:, u:u + 1], ssq[:, u:u + 1], [("ssq", u)], ("ssq", u), 1.0 / D)
            P.op("dve", lambda e: e.scalar_tensor_tensor(acc[:, u, :], acc[:, u, :], ssq[:, u:u + 1], fnw[:], ALU.mult, ALU.mult),
                 reads=[("acc", u), ("ssq", u), "fnwc"], writes=[("acc", u)])
            P.dma("sp", out[T * 128:(T + 1) * 128, :], acc[:, u, :], reads=[("acc", u)], writes=[("out", T)], sem=("out", u))


def _consts():
    c = np.zeros((128, 10, 128), np.float32)
    c[:, 0, :] = np.eye(128, dtype=np.float32)
    perm = np.zeros((128, 128), np.float32)
    for m in range(32):
        perm[(m + 16) % 32, m] = 1.0
    c[:, 1, :] = perm
    i = np.arange(128)
    c[:, 2, :] = np.where(i[:, None] <= i[None, :], -1.0 / 16.0, 0.0)
    c[:, 3, :] = (i[:, None] <= i[None, :]).astype(np.float32)
    c[:, 4, :] = (i[:, None] < i[None, :]).astype(np.float32)
    q = np.arange(256)
    for half in range(2):
        kpos = half * 128 + i
        m = np.where(kpos[:, None] <= q[None, :], 0.0, -BIG).astype(np.float32)
        c[:, 5 + 2 * half, :] = m[:, :128]
        c[:, 6 + 2 * half, :] = m[:, 128:]
    return c


def _rope_tables():
    half = 16
    inv = np.float32(500000.0) ** (-np.arange(half, dtype=np.float32) * np.float32(2.0) / np.float32(32))
    ang = np.arange(SEQ, dtype=np.float32)[:, None] * inv[None, :].astype(np.float32)
    cos = np.cos(ang).astype(np.float32).T
    sin = np.sin(ang).astype(np.float32).T
    return np.concatenate([cos, cos], 0), np.concatenate([-sin, sin], 0)


def prep_inputs(inputs):
    f = lambda k: np.asarray(inputs[k], np.float32)
    x = f("x")
    c = f("c")
    w_in = f("w_in")[0]
    fm = lambda v: np.ascontiguousarray(v.reshape(-1, 128).T)
    bc = lambda v: np.ascontiguousarray(np.broadcast_to(v[None, :], (128, v.shape[0])))
    o = [0]
    for sz in (1024, 1024, 1024, 512, 512, 1024, 1024, 16, 1024, 1024):
        o.append(o[-1] + sz)
    mq, mk, mv, gq, gk, gv, gr, gl, ga, gb = [w_in[:, o[i]:o[i + 1]] for i in range(10)]
    shared = {
        "w_ada": f("w_ada")[0],
        "b_ada_bc": bc(f("b_ada")[0]),
        "nw": np.concatenate([fm(f("norm1_w")[0]), fm(f("norm2_w")[0])], 1),
        "fnw_bc": bc(f("final_norm_w")),
        "wA": np.ascontiguousarray(np.concatenate([mk, mv, gk, gv, gl], 1)),
        "wO": np.ascontiguousarray(np.concatenate([mq, gq, gr], 1)),
        "wG": np.ascontiguousarray(np.concatenate([ga, gb], 1)),
        "w_oa": f("w_o_moba")[0], "w_ob": f("w_o_gla")[0], "w_out": f("w_out")[0],
        "nbm": fm(f("b_merge")[0]),
        "gnw": fm(f("gla_norm_w")[0]),
        "w_r": f("w_router")[0], "b_r_bc": bc(f("b_router")[0]),
        "w2": f("w_exp_out")[0], "b2": f("b_exp_out")[0],
        "consts": _consts(),
        "ecap": bc(np.arange(NE, dtype=np.float32) * CAP),
    }
    gw = np.zeros((32, 512), np.float32)
    gw[0:16] = f("gla_gate_w")[0]
    gw[16] = f("gla_gate_b")[0]
    shared["gw_aug"] = gw
    w1 = f("w_exp_in")[0]
    shared["w1d"] = np.ascontiguousarray(np.concatenate([w1[:, :, 0::2], w1[:, :, 1::2]], 2))
    b1 = f("b_exp_in")[0]
    b1d = np.concatenate([b1[:, 0::2], b1[:, 1::2]], 1)
    shared["b1T"] = np.ascontiguousarray(b1d.reshape(NE, 16, 128).transpose(2, 0, 1).reshape(128, NE * 16))
    es = np.zeros((32, 32, 128), np.float32)
    for j in range(32):
        es[j, j, :] = 1.0
    shared["esel_in"] = es.reshape(32, 32 * 128)
    cosT, sinT = _rope_tables()
    per_core = []
    for core in range(NCORE):
        b, r = core // 4, core % 4
        nnull = (24 - 8 * r) * 256
        nreal = 2048 * (r + 1)
        xv = np.zeros((D, SEQ), np.float32)
        xv[:, nnull:] = x[b, :nreal, :].T
        rc_ = np.zeros((32, SEQ), np.float32)
        rs_ = np.zeros((32, SEQ), np.float32)
        rc_[:, nnull:] = cosT[:, :nreal]
        rs_[:, nnull:] = sinT[:, :nreal]
        vf = np.zeros((SEQ,), np.float32)
        vf[nnull:] = 1.0
        gm = np.full((8, 32), NEGINF, np.float32)
        gvv = np.zeros((8, 32), np.float32)
        for l in range(8):
            gm[l, 24 - 8 * r:24 + l] = 0.0
            gvv[l, 24 - 8 * r:24 + l] = 1.0
        d = dict(shared)
        d.update({
            "xTv": xv,
            "xtok": np.ascontiguousarray(x[b, 2048 * r:2048 * (r + 1), :]),
            "cT": fm(c[b]),
            "ropec": rc_, "ropes": rs_,
            "vflag": np.ascontiguousarray(vf.reshape(64, 128).T),
            "gmask": bc(gm.reshape(-1)), "gvalid": bc(gvv.reshape(-1)),
        })
        per_core.append(d)
    return per_core


_NC_CACHE = {}


def kernel(**inputs):
    in_maps = prep_inputs(inputs)
    if "nc" not in _NC_CACHE:
        _NC_CACHE["nc"] = build_nc()[0]
    nc = _NC_CACHE["nc"]
    res = run_bass_kernel_spmd(nc, in_maps, core_ids=list(range(NCORE)))
    outp = np.zeros((2, SEQ, D), np.float32)
    for core in range(NCORE):
        b, r = core // 4, core % 4
        outp[b, 2048 * r:2048 * (r + 1), :] = res.results[core]["out"]
    return outp
```
